# Optimizing a Trainium2 kernel written in Bass

```python
import math
import jax, jax.numpy as jnp
from jax import lax
import numpy as np

D_MODEL = 1024
BATCH = 16
SEQ = 4096
DEPTH = 2

N_MEM = 256
D_MIX = D_MODEL
GROUP_WIDTH = D_MIX // 4
HEAD_DIM = 64
N_HEADS = GROUP_WIDTH // HEAD_DIM
RET_QK_DIM = HEAD_DIM // 2
RET_CHUNK = 128
RW_DECAY_LORA = 64
RW_A_LORA = 64
RW_GATE_LORA = 128
RW_DECAY_SCALE = math.exp(-0.5)
RW_GN_EPS = 64e-5
SSM_STATE = 128
SSM_GROUPS = 2
SSM_CONV = 4
SSM_CHUNK = 128
SSM_XBC = GROUP_WIDTH + 2 * SSM_GROUPS * SSM_STATE
DSA_KV_DIM = HEAD_DIM
IDX_HEADS = 4
IDX_DIM = 32
DSA_TOPK_MAX = 256
DSA_QBLOCK = 128
X_HEADS = 4
X_HEAD_DIM = D_MODEL // X_HEADS
D_FF = 2816
FFN_CONV = 3
ROPE_THETA = 10000.0
NORM_EPS = 1e-6

RET_SPLITS = (N_HEADS * RET_QK_DIM, N_HEADS * RET_QK_DIM, GROUP_WIDTH, GROUP_WIDTH)
RW_SPLITS = (GROUP_WIDTH, GROUP_WIDTH, GROUP_WIDTH, RW_DECAY_LORA, RW_A_LORA, RW_GATE_LORA)
SSM_SPLITS = (GROUP_WIDTH, SSM_XBC, N_HEADS)
DSA_SPLITS = (GROUP_WIDTH, DSA_KV_DIM, DSA_KV_DIM, IDX_HEADS * IDX_DIM, IDX_DIM, IDX_HEADS)
GROUP_COLS = (sum(RET_SPLITS), sum(RW_SPLITS), sum(SSM_SPLITS), sum(DSA_SPLITS))
N_IN = sum(GROUP_COLS)

kernel_name = "hybrid_headgroup_ret_rwkv7_ssd_dsa_block"


def split_cols(t, sizes):
    idx = [int(i) for i in np.cumsum(sizes)[:-1]]
    return jnp.split(t, idx, axis=-1)


def rms_norm(x, g, eps=NORM_EPS):
    xf = x.astype(jnp.float32)
    y = xf * lax.rsqrt(jnp.mean(xf * xf, axis=-1, keepdims=True) + eps)
    return (y * g.astype(jnp.float32)).astype(x.dtype)


def head_rms(y, eps=NORM_EPS):
    yf = y.astype(jnp.float32)
    return (yf * lax.rsqrt(jnp.mean(yf * yf, axis=-1, keepdims=True) + eps)).astype(y.dtype)


def head_layer_norm(y, eps):
    yf = y.astype(jnp.float32)
    mu = jnp.mean(yf, axis=-1, keepdims=True)
    var = jnp.mean(jnp.square(yf - mu), axis=-1, keepdims=True)
    return ((yf - mu) * lax.rsqrt(var + eps)).astype(y.dtype)


def rope(x, pos):
    half = x.shape[-1] // 2
    inv = ROPE_THETA ** (-jnp.arange(half, dtype=jnp.float32) / half)
    ang = pos.astype(jnp.float32)[:, None] * inv[None, :]
    cos = jnp.cos(ang)[:, None, :]
    sin = jnp.sin(ang)[:, None, :]
    xf = x.astype(jnp.float32)
    x1, x2 = xf[..., :half], xf[..., half:]
    return jnp.concatenate([x1 * cos - x2 * sin, x2 * cos + x1 * sin], axis=-1).astype(x.dtype)


def causal_dwconv(x, w, b):
    width, ch = w.shape
    y = lax.conv_general_dilated(x, w[:, None, :].astype(x.dtype), window_strides=(1,),
                                 padding=[(width - 1, 0)],
                                 dimension_numbers=('NWC', 'WIO', 'NWC'),
                                 feature_group_count=ch)
    return y + b.astype(x.dtype)


def token_shift_mix(f, mu):
    prev = jnp.pad(f, ((0, 0), (1, 0), (0, 0)))[:, :-1]
    return f + (prev - f) * mu


def chunked_decay_recurrence(q, k, v, log_a, chunk):
    out_dtype = v.dtype
    b, s, h, n = q.shape
    p = v.shape[-1]
    nc = s // chunk
    qc = q.astype(jnp.float32).reshape(b, nc, chunk, h, n)
    kc = k.astype(jnp.float32).reshape(b, nc, chunk, h, n)
    vc = v.astype(jnp.float32).reshape(b, nc, chunk, h, p)
    cum = jnp.cumsum(log_a.astype(jnp.float32).reshape(b, nc, chunk, h), axis=2)
    causal = jnp.tril(jnp.ones((chunk, chunk), dtype=bool))[None, None, :, :, None]
    seg = cum[:, :, :, None, :] - cum[:, :, None, :, :]
    decay = jnp.exp(jnp.where(causal, seg, -jnp.inf))
    scores = jnp.einsum('bctHn,bcsHn->bctsH', qc, kc) * decay
    y_intra = jnp.einsum('bctsH,bcsHp->bctHp', scores, vc)
    decay_to_end = jnp.exp(cum[:, :, -1:, :] - cum)
    chunk_state = jnp.einsum('bcsHn,bcsH,bcsHp->bcHnp', kc, decay_to_end, vc)
    chunk_decay = jnp.exp(cum[:, :, -1, :])

    def step(state, inp):
        cs, cd = inp
        return state * cd[:, :, None, None] + cs, state

    init = jnp.zeros((b, h, n, p), jnp.float32)
    _, prev_states = lax.scan(step, init, (jnp.moveaxis(chunk_state, 1, 0), jnp.moveaxis(chunk_decay, 1, 0)))
    prev_states = jnp.moveaxis(prev_states, 0, 1)
    y_inter = jnp.einsum('bctHn,bcHnp,bctH->bctHp', qc, prev_states, jnp.exp(cum))
    return (y_intra + y_inter).reshape(b, s, h, p).astype(out_dtype)


def retention_group(q, k, v, g, pos):
    b, s, _ = q.shape
    q = rope(q.reshape(b, s, N_HEADS, RET_QK_DIM), pos)
    k = rope(k.reshape(b, s, N_HEADS, RET_QK_DIM), pos) * (RET_QK_DIM ** -0.5)
    v = v.reshape(b, s, N_HEADS, HEAD_DIM)
    log_gamma = jnp.log(1.0 - jnp.power(2.0, -5.0 - jnp.arange(N_HEADS, dtype=jnp.float32)))
    la = jnp.broadcast_to(log_gamma, (b, s, N_HEADS))
    o = head_rms(chunked_decay_recurrence(q, k, v, la, RET_CHUNK))
    return o.reshape(b, s, GROUP_WIDTH) * jax.nn.silu(g)


def rwkv7_scan(r, w, k, v, kk, a):
    seq = [jnp.moveaxis(t.astype(jnp.float32), 1, 0) for t in (r, w, k, v, kk, a)]
    b, _, h, n = r.shape

    def step(state, inp):
        r_t, w_t, k_t, v_t, kk_t, a_t = inp
        sa = jnp.einsum('bhvk,bhk->bhv', state, -kk_t)
        state = (state * w_t[:, :, None, :] + sa[..., None] * (kk_t * a_t)[:, :, None, :]
                 + v_t[..., None] * k_t[:, :, None, :])
        return state, jnp.einsum('bhvk,bhk->bhv', state, r_t)

    init = jnp.zeros((b, h, n, n), jnp.float32)
    _, y = lax.scan(step, init, tuple(seq))
    return jnp.moveaxis(y, 0, 1).astype(r.dtype)


def rwkv7_group(r, k, v, wl, al, gl, w0, w2, a0, a2, g2, k_k, k_a, r_k, ln_w, ln_b):
    b, s, _ = r.shape
    hd = (b, s, N_HEADS, HEAD_DIM)
    log_w = -RW_DECAY_SCALE * jax.nn.sigmoid((w0 + jnp.tanh(wl) @ w2).astype(jnp.float32))
    w = jnp.exp(log_w)
    a = jax.nn.sigmoid(a0 + al @ a2)
    g = jax.nn.sigmoid(gl) @ g2
    kk = (k * k_k).reshape(hd)
    kk = kk * lax.rsqrt(jnp.sum(kk * kk, axis=-1, keepdims=True) + 1e-12)
    k = k * (1.0 + (a - 1.0) * k_a)
    rh, kh, vh = r.reshape(hd), k.reshape(hd), v.reshape(hd)
    y = rwkv7_scan(rh, w.reshape(hd), kh, vh, kk, a.reshape(hd))
    y = head_layer_norm(y, RW_GN_EPS).reshape(b, s, GROUP_WIDTH) * ln_w + ln_b
    bonus = (jnp.sum(rh * kh * r_k, axis=-1, keepdims=True) * vh).reshape(b, s, GROUP_WIDTH)
    return (y + bonus) * g


def ssd_group(z, xbc, dt, conv_w, conv_b, dt_bias, a_log, d_skip, norm_w):
    b, s, _ = z.shape
    xbc = jax.nn.silu(causal_dwconv(xbc, conv_w, conv_b))
    xs, bm, cm = split_cols(xbc, (GROUP_WIDTH, SSM_GROUPS * SSM_STATE, SSM_GROUPS * SSM_STATE))
    xs = xs.reshape(b, s, N_HEADS, HEAD_DIM)
    rep = N_HEADS // SSM_GROUPS
    bm = jnp.repeat(bm.reshape(b, s, SSM_GROUPS, SSM_STATE), rep, axis=2)
    cm = jnp.repeat(cm.reshape(b, s, SSM_GROUPS, SSM_STATE), rep, axis=2)
    dt = jax.nn.softplus((dt + dt_bias).astype(jnp.float32))
    a = -jnp.exp(a_log.astype(jnp.float32))
    y = chunked_decay_recurrence(cm, bm, xs * dt[..., None].astype(xs.dtype), dt * a, SSM_CHUNK)
    y = y + xs * d_skip[:, None]
    y = y.reshape(b, s, GROUP_WIDTH) * jax.nn.silu(z)
    return rms_norm(y, norm_w)


def dsa_group(q, k, v, iq, ik, iw, idx_k_norm, pos):
    b, s, _ = q.shape
    q = rope(q.reshape(b, s, N_HEADS, HEAD_DIM), pos)
    k = rope(k[:, :, None, :], pos)[:, :, 0]
    iq = rope(iq.reshape(b, s, IDX_HEADS, IDX_DIM), pos)
    ik = rope(rms_norm(ik, idx_k_norm)[:, :, None, :], pos)[:, :, 0]
    iw = iw * (IDX_HEADS ** -0.5 * IDX_DIM ** -0.5)
    topk = min(DSA_TOPK_MAX, s // 4)
    nb = s // DSA_QBLOCK
    scale = HEAD_DIM ** -0.5

    def to_blocks(t):
        return jnp.moveaxis(t.reshape((b, nb, DSA_QBLOCK) + t.shape[2:]), 1, 0)

    def block(args):
        q_b, iq_b, iw_b, t_b = args
        rel = jax.nn.relu(jnp.einsum('bqhd,bsd->bqhs', iq_b, ik))
        score = jnp.einsum('bqhs,bqh->bqs', rel, iw_b).astype(jnp.float32)
        causal = pos[None, :] <= t_b[:, None]
        score = jnp.where(causal[None], score, -jnp.inf)
        _, sel = lax.top_k(score, topk)
        k_sel = jax.vmap(lambda kb, ib: kb[ib])(k, sel)
        v_sel = jax.vmap(lambda vb, ib: vb[ib])(v, sel)
        logits = jnp.einsum('bqhd,bqkd->bqhk', q_b, k_sel).astype(jnp.float32) * scale
        valid = (sel <= t_b[None, :, None])[:, :, None, :]
        logits = jnp.where(valid, logits, -jnp.inf)
        p = jax.nn.softmax(logits, axis=-1).astype(v_sel.dtype)
        return jnp.einsum('bqhk,bqkd->bqhd', p, v_sel)

    out = lax.map(block, (to_blocks(q), to_blocks(iq), to_blocks(iw), pos.reshape(nb, DSA_QBLOCK)))
    return jnp.moveaxis(out, 0, 1).reshape(b, s, GROUP_WIDTH)


def cross_attention(h, memn, wq, wk, wv, wo):
    b, s, _ = h.shape
    m = memn.shape[1]
    q = (h @ wq).reshape(b, s, X_HEADS, X_HEAD_DIM)
    k = (memn @ wk).reshape(b, m, X_HEADS, X_HEAD_DIM)
    v = (memn @ wv).reshape(b, m, X_HEADS, X_HEAD_DIM)
    logits = jnp.einsum('bshd,bmhd->bhsm', q, k).astype(jnp.float32) * (X_HEAD_DIM ** -0.5)
    p = jax.nn.softmax(logits, axis=-1).astype(v.dtype)
    o = jnp.einsum('bhsm,bmhd->bshd', p, v).reshape(b, s, D_MODEL)
    return o @ wo


def conv_glu(h, w_up, conv_w, conv_b, w_down):
    gate, val = jnp.split(h @ w_up, 2, axis=-1)
    gate = jax.nn.silu(causal_dwconv(gate, conv_w, conv_b))
    return (gate * val) @ w_down


def setup_inputs(seed: int = 0) -> dict:
    key = jax.random.key(seed)
    ks = iter(jax.random.split(key, 48))
    L = DEPTH
    f32 = jnp.float32

    def nrm(shape, scale):
        return jax.random.normal(next(ks), shape, f32) * scale

    def gain(shape):
        return 1.0 + nrm(shape, 0.05)

    dt0 = jnp.exp(jax.random.uniform(next(ks), (L, N_HEADS), f32, math.log(1e-3), math.log(1e-1)))
    return {
        "x": nrm((BATCH, SEQ, D_MODEL), 1.0),
        "mem": nrm((BATCH, N_MEM, D_MODEL), 1.0),
        "norm_mix": gain((L, D_MODEL)),
        "w_in": nrm((L, D_MODEL, N_IN), D_MODEL ** -0.5),
        "rwkv_mu": jax.random.uniform(next(ks), (L, GROUP_COLS[1]), f32, 0.0, 1.0),
        "rwkv_w0": nrm((L, GROUP_WIDTH), 1.0) - 1.0,
        "rwkv_w2": nrm((L, RW_DECAY_LORA, GROUP_WIDTH), 0.5 * RW_DECAY_LORA ** -0.5),
        "rwkv_a0": nrm((L, GROUP_WIDTH), 0.1),
        "rwkv_a2": nrm((L, RW_A_LORA, GROUP_WIDTH), RW_A_LORA ** -0.5),
        "rwkv_g2": nrm((L, RW_GATE_LORA, GROUP_WIDTH), RW_GATE_LORA ** -0.5),
        "rwkv_k_k": 0.85 + nrm((L, GROUP_WIDTH), 0.05),
        "rwkv_k_a": gain((L, GROUP_WIDTH)),
        "rwkv_r_k": nrm((L, N_HEADS, HEAD_DIM), 0.1),
        "rwkv_ln_w": gain((L, GROUP_WIDTH)),
        "rwkv_ln_b": nrm((L, GROUP_WIDTH), 0.02),
        "ssm_conv_w": nrm((L, SSM_CONV, SSM_XBC), SSM_CONV ** -0.5),
        "ssm_conv_b": nrm((L, SSM_XBC), 0.02),
        "ssm_dt_bias": dt0 + jnp.log(-jnp.expm1(-dt0)),
        "ssm_a_log": jnp.log(jax.random.uniform(next(ks), (L, N_HEADS), f32, 1.0, 16.0)),
        "ssm_d": gain((L, N_HEADS)),
        "ssm_norm": gain((L, GROUP_WIDTH)),
        "idx_k_norm": gain((L, IDX_DIM)),
        "w_out": nrm((L, D_MIX, D_MODEL), D_MIX ** -0.5),
        "norm_cross": gain((L, D_MODEL)),
        "norm_mem": gain((L, D_MODEL)),
        "wq_x": nrm((L, D_MODEL, D_MODEL), D_MODEL ** -0.5),
        "wk_x": nrm((L, D_MODEL, D_MODEL), D_MODEL ** -0.5),
        "wv_x": nrm((L, D_MODEL, D_MODEL), D_MODEL ** -0.5),
        "wo_x": nrm((L, D_MODEL, D_MODEL), D_MODEL ** -0.5),
        "norm_ffn": gain((L, D_MODEL)),
        "w_up": nrm((L, D_MODEL, 2 * D_FF), D_MODEL ** -0.5),
        "ffn_conv_w": nrm((L, FFN_CONV, D_FF), FFN_CONV ** -0.5),
        "ffn_conv_b": nrm((L, D_FF), 0.02),
        "w_down": nrm((L, D_FF, D_MODEL), D_FF ** -0.5),
        "norm_final": gain((D_MODEL,)),
    }


def reference(x, mem, norm_mix, w_in, rwkv_mu, rwkv_w0, rwkv_w2, rwkv_a0, rwkv_a2, rwkv_g2,
              rwkv_k_k, rwkv_k_a, rwkv_r_k, rwkv_ln_w, rwkv_ln_b, ssm_conv_w, ssm_conv_b,
              ssm_dt_bias, ssm_a_log, ssm_d, ssm_norm, idx_k_norm, w_out, norm_cross, norm_mem,
              wq_x, wk_x, wv_x, wo_x, norm_ffn, w_up, ffn_conv_w, ffn_conv_b, w_down, norm_final):
    s = x.shape[1]
    pos = jnp.arange(s, dtype=jnp.int32)
    h = x
    for l in range(DEPTH):
        hn = rms_norm(h, norm_mix[l])
        proj = hn @ w_in[l]
        ret_p, rw_p, ssm_p, dsa_p = split_cols(proj, GROUP_COLS)
        rq, rk, rv, rg = split_cols(ret_p, RET_SPLITS)
        o_ret = retention_group(rq, rk, rv, rg, pos)
        rw_p = token_shift_mix(rw_p, rwkv_mu[l])
        wr, wk, wv, wwl, wal, wgl = split_cols(rw_p, RW_SPLITS)
        o_rw = rwkv7_group(wr, wk, wv, wwl, wal, wgl, rwkv_w0[l], rwkv_w2[l], rwkv_a0[l], rwkv_a2[l],
                           rwkv_g2[l], rwkv_k_k[l], rwkv_k_a[l], rwkv_r_k[l], rwkv_ln_w[l], rwkv_ln_b[l])
        sz, sxbc, sdt = split_cols(ssm_p, SSM_SPLITS)
        o_ssm = ssd_group(sz, sxbc, sdt, ssm_conv_w[l], ssm_conv_b[l], ssm_dt_bias[l], ssm_a_log[l],
                          ssm_d[l], ssm_norm[l])
        dq, dk, dv, diq, dik, diw = split_cols(dsa_p, DSA_SPLITS)
        o_dsa = dsa_group(dq, dk, dv, diq, dik, diw, idx_k_norm[l], pos)
        h = h + jnp.concatenate([o_ret, o_rw, o_ssm, o_dsa], axis=-1) @ w_out[l]
        memn = rms_norm(mem, norm_mem[l])
        h = h + cross_attention(rms_norm(h, norm_cross[l]), memn, wq_x[l], wk_x[l], wv_x[l], wo_x[l])
        h = h + conv_glu(rms_norm(h, norm_ffn[l]), w_up[l], ffn_conv_w[l], ffn_conv_b[l], w_down[l])
    return rms_norm(h, norm_final)
```

```python
import math
import numpy as np
from contextlib import ExitStack
import ml_dtypes
import concourse.bass as bass
import concourse.mybir as mybir
from concourse.bass_utils import run_bass_kernel_spmd

F32 = mybir.dt.float32
BF16 = mybir.dt.bfloat16
U32 = mybir.dt.uint32
ALU = mybir.AluOpType
AF = mybir.ActivationFunctionType
AX = mybir.AxisListType

D = 1024
KC = 8
NMEM = 256
DFF = 2816
NFC = DFF // 128
N_IN = 3368
EPS = 1e-6
NEG_BIG = -3.0e38
NEG_THR = -1.0e38


class Buf:
    __slots__ = ("t", "name", "psum")

    def __init__(self, t, name, psum=False):
        self.t = t
        self.name = name
        self.psum = psum

    def __getitem__(self, k):
        return self.t[k]


class TK:
    LIM = 900
    DLIM = 55
    NSLOT = 8
    ENG = ("pe", "act", "dve", "pool", "sp")
    DQ = ("sp", "pool")

    def __init__(self, nc, es):
        self.nc = nc
        self.es = es
        self.eng = {"pe": nc.tensor, "act": nc.scalar, "dve": nc.vector, "pool": nc.gpsimd, "sp": nc.sync}
        self.nsem = 0
        self.csem = {e: [self._newsem(e) for _ in range(3)] for e in self.ENG}
        self.dsem = {(q, sl): [self._newsem(f"d{q}{sl}") for _ in range(3)] for q in self.DQ for sl in range(self.NSLOT)}
        self.epoch = 0
        self.ninstr = 0
        self.nbar = 0
        self.dnext = {q: 0 for q in self.DQ}
        self._reset_epoch()

    def _reset_epoch(self):
        self.cnt = {e: 0 for e in self.ENG}
        self.dcnt = {k: 0 for k in self.dsem}
        self.seen = {e: {} for e in self.ENG}
        self.lastw = {}
        self.readers = {}
        self.last_d = {}

    def _newsem(self, name):
        self.nsem += 1
        return self.es.enter_context(self.nc.semaphore(f"{name}_{self.nsem}"))

    def _need(self, e, tok):
        if tok is None or tok[2] != self.epoch:
            return
        if tok[0] == "c":
            _, e2, _, idx = tok
            key = e2 if e2 != e else ("self", e)
            if self.seen[e].get(key, -1) >= idx:
                return
            self.seen[e][key] = idx
            self.eng[e].wait_ge(self.csem[e2][self.epoch % 3], idx + 1)
        else:
            _, key, _, val = tok
            if self.seen[e].get(key, -1) >= val:
                return
            self.seen[e][key] = val
            self.eng[e].wait_ge(self.dsem[key][self.epoch % 3], val)

    def _deps(self, e, r, w, is_dma=False):
        def chk(tok):
            if tok is None:
                return
            if tok[0] == "c" and tok[1] == e and not is_dma and e == "pe":
                return
            self._need(e, tok)
        for b in r:
            chk(self.lastw.get(b))
            if getattr(b, "psum", False):
                rd = self.readers.get(b)
                if rd:
                    for t2 in rd.values():
                        if not (t2[0] == "c" and t2[1] == e):
                            chk(t2)
        for b in w:
            chk(self.lastw.get(b))
            rd = self.readers.get(b)
            if rd:
                for t2 in rd.values():
                    chk(t2)

    def _record(self, tok, r, w, rkey):
        for b in w:
            self.lastw[b] = tok
            self.readers[b] = {}
        for b in r:
            self.readers.setdefault(b, {})[rkey] = tok

    def op(self, e, fn, r=(), w=()):
        if self.cnt[e] >= self.LIM:
            self.barrier()
        self._deps(e, r, w)
        idx = self.cnt[e]
        ins = fn()
        ins.then_inc(self.csem[e][self.epoch % 3], 1)
        self.cnt[e] = idx + 1
        tok = ("c", e, self.epoch, idx)
        self._record(tok, r, w, e)
        self.ninstr += 1
        return ins

    def dma(self, q, out, in_, r=(), w=(), **kw):
        q = "sp"
        slot = self.dnext[q] % self.NSLOT
        k = (q, slot)
        if self.dcnt[k] >= self.DLIM:
            self.barrier()
        self.dnext[q] += 1
        self._deps(q, r, w, is_dma=True)
        self._need(q, self.last_d.get(k))
        self.dcnt[k] += 1
        val = 16 * self.dcnt[k]
        self.eng[q].dma_start(out=out, in_=in_, **kw).then_inc(self.dsem[k][self.epoch % 3], 16)
        tok = ("d", k, self.epoch, val)
        self.last_d[k] = tok
        self._record(tok, r, w, k)
        self.ninstr += 1
        return tok

    def barrier(self):
        bank = self.epoch % 3
        for e in self.ENG:
            if self.cnt[e] == 0:
                self.eng[e].sem_inc(self.csem[e][bank], 1)
                self.cnt[e] = 1
        toks = [("c", e, self.epoch, self.cnt[e] - 1) for e in self.ENG] + list(self.last_d.values())
        for e in self.ENG:
            for t in toks:
                self._need(e, t)
        self.epoch += 1
        self.nbar += 1
        nb = (self.epoch + 1) % 3
        for e in self.ENG:
            self.eng[e].sem_clear(self.csem[e][nb])
            if e in self.DQ:
                for sl in range(self.NSLOT):
                    self.eng[e].sem_clear(self.dsem[(e, sl)][nb])
        self._reset_epoch()

    def pe(self, fn, r=(), w=()):
        return self.op("pe", fn, r, w)

    def act(self, fn, r=(), w=()):
        return self.op("act", fn, r, w)

    def dve(self, fn, r=(), w=()):
        return self.op("dve", fn, r, w)

    def pool(self, fn, r=(), w=()):
        return self.op("pool", fn, r, w)


_UN = [0]


def uname(name):
    _UN[0] += 1
    return f"{name}_u{_UN[0]}"


class Rot:
    def __init__(self, nc, es, name, shape, dtype, n, psum=False):
        self.bufs = []
        for i in range(n):
            if psum:
                t = es.enter_context(nc.psum_tensor(uname(f"{name}{i}"), shape, dtype))
            else:
                t = es.enter_context(nc.sbuf_tensor(uname(f"{name}{i}"), shape, dtype))
            self.bufs.append(Buf(t, f"{name}{i}", psum=psum))
        self.i = 0

    def next(self):
        b = self.bufs[self.i % len(self.bufs)]
        self.i += 1
        return b


C_RQ, C_RK, C_RV, C_RG = 0, 128, 256, 512
C_WR, C_WK, C_WV, C_WWL, C_WAL, C_WGL = 768, 1024, 1280, 1536, 1600, 1664
C_SZ, C_SX, C_SB, C_SC, C_SDT = 1792, 2048, 2304, 2560, 2816
C_DQ, C_DK, C_DV, C_DIQ, C_DIK, C_DIW = 2820, 3076, 3140, 3204, 3332, 3364

PARAMS = [
    ("norm_mix", (2, 1024)), ("w_in", (2, 1024, N_IN)), ("rwkv_mu", (2, 1024)), ("rwkv_w0", (2, 256)),
    ("rwkv_w2", (2, 64, 256)), ("rwkv_a0", (2, 256)), ("rwkv_a2", (2, 64, 256)), ("rwkv_g2", (2, 128, 256)),
    ("rwkv_k_k", (2, 256)), ("rwkv_k_a", (2, 256)), ("rwkv_r_k", (2, 4, 64)), ("rwkv_ln_w", (2, 256)),
    ("rwkv_ln_b", (2, 256)), ("ssm_conv_w", (2, 4, 768)), ("ssm_conv_b", (2, 768)), ("ssm_dt_bias", (2, 4)),
    ("ssm_a_log", (2, 4)), ("ssm_d", (2, 4)), ("ssm_norm", (2, 256)), ("idx_k_norm", (2, 32)),
    ("w_out", (2, 1024, 1024)), ("norm_cross", (2, 1024)), ("norm_mem", (2, 1024)), ("wq_x", (2, 1024, 1024)),
    ("wk_x", (2, 1024, 1024)), ("wv_x", (2, 1024, 1024)), ("wo_x", (2, 1024, 1024)), ("norm_ffn", (2, 1024)),
    ("w_up", (2, 1024, 2 * DFF)), ("ffn_conv_w", (2, 3, DFF)), ("ffn_conv_b", (2, DFF)),
    ("w_down", (2, DFF, 1024)), ("norm_final", (1024,)),
]


def make_consts(S):
    c = {}
    c["c_ident"] = np.eye(128, dtype=np.float32)
    i = np.arange(128)
    c["c_U"] = (i[:, None] <= i[None, :]).astype(np.float32)
    c["c_negU"] = np.where(i[None, :] > i[:, None], np.float32(NEG_BIG), np.float32(0)).astype(np.float32)
    pos = np.arange(S, dtype=np.float32)

    def tabs(hd, rows):
        half = hd // 2
        inv = (np.float32(10000.0) ** (-np.arange(half, dtype=np.float32) / np.float32(half))).astype(np.float32)
        ang = (pos[:, None] * inv[None, :]).astype(np.float32)
        cos = np.cos(ang).astype(np.float32)
        sin = np.sin(ang).astype(np.float32)
        cf = np.concatenate([cos, cos], 1)
        sf = np.concatenate([-sin, sin], 1)
        rep = rows // hd
        return np.ascontiguousarray(np.tile(cf, (1, rep)).T), np.ascontiguousarray(np.tile(sf, (1, rep)).T)

    c["c_cos64T"], c["c_sin64T"] = tabs(64, 128)
    c["c_cos32T"], c["c_sin32T"] = tabs(32, 128)
    e2 = np.zeros((128, 64, 128), dtype=np.float32)
    for b in range(2):
        for s in range(64):
            e2[b * 64 + s, s, b * 64:(b + 1) * 64] = 1.0
    c["c_E2"] = e2.reshape(128, 64 * 128).astype(ml_dtypes.bfloat16)
    lg = np.log(1.0 - np.power(2.0, -5.0 - np.arange(4, dtype=np.float32))).astype(np.float32)
    c["c_retla"] = np.tile(lg[None, :], (128, 1)).astype(np.float32)
    return c


CONST_SPECS = lambda S: [("c_ident", (128, 128), F32), ("c_U", (128, 128), F32), ("c_negU", (128, 128), F32),
                         ("c_cos64T", (128, S), F32), ("c_sin64T", (128, S), F32), ("c_cos32T", (128, S), F32),
                         ("c_sin32T", (128, S), F32), ("c_E2", (128, 64 * 128), BF16), ("c_retla", (128, 4), F32)]


class Ctx:
    pass


def build(S, NB=2, L=2, stop_after=None, dbg=()):
    nc = bass.Bass("TRN2", target_bir_lowering=False)
    C = Ctx()
    C.nc, C.S, C.NB, C.L = nc, S, NB, L
    C.TS = 512
    C.NST = S // C.TS
    dr = {}
    dr["x"] = nc.dram_tensor("x", [NB, S, D], F32, kind="ExternalInput").ap()
    dr["mem"] = nc.dram_tensor("mem", [NB, NMEM, D], F32, kind="ExternalInput").ap()
    for name, shp in PARAMS:
        dr[name] = nc.dram_tensor(name, list(shp), F32, kind="ExternalInput").ap()
    for name, shp, dt in CONST_SPECS(S):
        dr[name] = nc.dram_tensor(name, list(shp), dt, kind="ExternalInput").ap()
    dr["y"] = nc.dram_tensor("y", [NB, S, D], F32, kind="ExternalOutput").ap()

    def scratch(name, shape, dt):
        kind = "ExternalOutput" if name in dbg else "Internal"
        dr[name] = nc.dram_tensor(name, list(shape), dt, kind=kind).ap()

    scratch("hT", [NB, KC, 128, S], F32)
    scratch("oT", [NB, KC, 128, S], BF16)
    scratch("r_qT", [NB, 128, S], BF16)
    scratch("r_kT", [NB, 128, S], BF16)
    scratch("r_kTok", [NB, S, 128], BF16)
    scratch("r_v", [NB, S, 256], BF16)
    scratch("r_sg", [NB, S, 256], F32)
    for nm in ("w_whi", "w_wlo", "w_nkk", "w_bb", "w_kp", "w_r"):
        scratch(nm, [NB, S, 256], BF16)
    scratch("w_vT", [NB, 256, S], F32)
    scratch("w_bonus", [NB, S, 256], F32)
    scratch("w_g", [NB, S, 256], F32)
    scratch("s_CT", [NB, 256, S], BF16)
    scratch("s_BT", [NB, 256, S], BF16)
    scratch("s_BTok", [NB, S, 256], BF16)
    scratch("s_xdt", [NB, S, 256], BF16)
    scratch("s_xs", [NB, S, 256], F32)
    scratch("s_sz", [NB, S, 256], F32)
    scratch("s_la", [NB, S, 4], F32)
    scratch("d_qT", [NB, 256, S], BF16)
    scratch("d_kT", [NB, 64, S], BF16)
    scratch("d_v", [NB, S, 65], BF16)
    scratch("d_iqT", [NB, 128, S], BF16)
    scratch("d_ikT", [NB, 32, S], BF16)
    scratch("d_iw", [NB, S, 4], F32)
    C.dr = dr

    import os
    with ExitStack() as es0:
        tk = TK(nc, es0)
        C.tk = tk
        phases = []
        phases.append(("p0", lambda: phase0(C)))
        for l in range(L):
            if os.environ.get("KONEP", "1") == "1":
                phases.append((f"P{l}", lambda l=l: phaseP(C, l)))
            else:
                for sec in ("ret", "ssd", "rw", "dsa"):
                    phases.append((f"P{l}{sec}" if sec != "dsa" else f"P{l}", lambda l=l, sec=sec: phaseP(C, l, only=sec)))
            phases.append((f"REC{l}", lambda l=l: phaseRec(C, l)))
            phases.append((f"RW{l}", lambda l=l: phaseRW(C, l)))
            phases.append((f"DSA{l}", lambda l=l: phaseDSA(C, l)))
            phases.append((f"F1{l}", lambda l=l: phaseF1(C, l)))
            phases.append((f"F2{l}", lambda l=l: phaseF2(C, l)))
        phases.append(("fin", lambda: phaseFinal(C)))
        import os
        skipph = os.environ.get("KPH", "").split(",")
        for name, fn in phases:
            if name in skipph:
                continue
            fn()
            tk.barrier()
            if stop_after == name:
                break
        C.ninstr = tk.ninstr
    return nc, C


def load_consts(C, es, names):
    nc, tk, dr = C.nc, C.tk, C.dr
    out = {}
    for nm in names:
        ap = dr[nm]
        t = Buf(es.enter_context(nc.sbuf_tensor(uname("k_" + nm), list(ap.shape), ap.dtype)), nm)
        tk.dma("sp", t[:], ap[:, :], w=[t])
        out[nm] = t
    return out


def phase0(C):
    nc, tk, dr, S, NB = C.nc, C.tk, C.dr, C.S, C.NB
    with ExitStack() as es:
        cs = load_consts(C, es, ["c_ident"])
        ident = cs["c_ident"]
        xin = Rot(nc, es, "p0x", [128, D], F32, 2)
        hout = Rot(nc, es, "p0h", [128, KC, 128], F32, 2)
        pps = Rot(nc, es, "p0ps", [128, 512], F32, 4, psum=True)
        for b in range(NB):
            for ti in range(S // 128):
                xt = xin.next()
                tk.dma("sp", xt[:], dr["x"][b, ti * 128:(ti + 1) * 128, :], w=[xt])
                ho = hout.next()
                for half in range(2):
                    pp = pps.next()
                    for j in range(4):
                        kc = half * 4 + j
                        tk.pe(lambda: nc.tensor.transpose(pp[:, j * 128:(j + 1) * 128], xt[:, kc * 128:(kc + 1) * 128], ident[:]),
                              r=[xt, ident], w=[pp])
                    dst = ho[:, half * 4:(half + 1) * 4, :]
                    src = pp[:].rearrange("p (a b) -> p a b", a=4)
                    if half == 0:
                        tk.act(lambda: nc.scalar.copy(dst, src), r=[pp], w=[(ho, half)])
                    else:
                        tk.dve(lambda: nc.vector.tensor_copy(dst, src), r=[pp], w=[(ho, half)])
                tk.dma("pool", dr["hT"][b, :, :, ti * 128:(ti + 1) * 128].rearrange("k p t -> p k t"), ho[:],
                       r=[(ho, 0), (ho, 1)])


def emit_norm(C, hT, n, hn_ap, hn_key, sq, ones_bf, pp, rstd):
    nc, tk = C.nc, C.tk
    tk.act(lambda: nc.scalar.activation(sq[:, :, :n], hT[:, :, :n], AF.Square), r=[hT], w=[sq])
    for kc in range(KC):
        tk.pe(lambda: nc.tensor.matmul(pp[:, :n], ones_bf[:], sq[:, kc, :n], start=(kc == 0), stop=(kc == KC - 1)),
              r=[sq, ones_bf], w=[pp])
    tk.act(lambda: nc.scalar.activation(rstd[:, :n], pp[:, :n], AF.Sqrt, scale=1.0 / D, bias=EPS), r=[pp], w=[rstd])
    tk.dve(lambda: nc.vector.reciprocal(rstd[:, :n], rstd[:, :n]), r=[rstd], w=[rstd])
    if hn_ap is not None:
        tk.dve(lambda: nc.vector.tensor_tensor(hn_ap, hT[:, :, :n], rstd[:, :n].unsqueeze(1).to_broadcast([128, KC, n]),
                                               op=ALU.mult), r=[hT, rstd], w=[hn_key])


def load_w_sec(C, stg, dstW, off, src2d, n, gT, cs=None, swap=0, rows=KC):
    nc, tk = C.nc, C.tk
    for c0 in range(0, n, 512):
        m = min(512, n - c0)
        st = stg.next()
        tk.dma("sp", st[:, :rows, :m], src2d[:, c0:c0 + m].rearrange("(kc p) n -> p kc n", p=128), w=[st])
        if gT is not None:
            tk.dve(lambda: nc.vector.tensor_tensor(st[:, :rows, :m], st[:, :rows, :m],
                                                   gT[:, :rows].unsqueeze(2).to_broadcast([128, rows, m]), op=ALU.mult),
                   r=[st, gT], w=[st])
        if cs is not None:
            csb, coff = cs
            tk.dve(lambda: nc.vector.tensor_tensor(st[:, :rows, :m], st[:, :rows, :m],
                                                   csb[:, coff + c0:coff + c0 + m].unsqueeze(1).to_broadcast([128, rows, m]),
                                                   op=ALU.mult), r=[st, csb], w=[st])
        if swap:
            sv = st[:, :rows, :m].rearrange("p k (x two d) -> p k x two d", two=2, d=swap)
            dv = dstW[:, :rows, off + c0:off + c0 + m].rearrange("p k (x two d) -> p k x two d", two=2, d=swap)
            tk.act(lambda: nc.scalar.copy(dv[:, :, :, 0, :], sv[:, :, :, 1, :]), r=[st], w=[dstW])
            tk.act(lambda: nc.scalar.copy(dv[:, :, :, 1, :], sv[:, :, :, 0, :]), r=[st], w=[dstW])
        else:
            tk.act(lambda: nc.scalar.copy(dstW[:, :rows, off + c0:off + c0 + m], st[:, :rows, :m]), r=[st], w=[dstW])


def sbt(C, es, name, shape, dt):
    return Buf(es.enter_context(C.nc.sbuf_tensor(uname(name), list(shape), dt)), name)


def bc_load(C, es, name, src1d, n):
    t = sbt(C, es, name, [128, n], F32)
    C.tk.dma("sp", t[:], src1d.partition_broadcast(128), w=[t])
    return t


P_SECS = [("rq", 128), ("rq_r", 128), ("rk", 128), ("rk_r", 128), ("rv", 256), ("rg", 256),
          ("wrkv_a", 768), ("wrkv_b", 768), ("wl_a", 64), ("wl_b", 64), ("al_a", 64), ("al_b", 64),
          ("gl_a", 128), ("gl_b", 128), ("sz", 256), ("sx", 768), ("sdt", 4),
          ("dq", 256), ("dq_r", 256), ("dk", 64), ("dk_r", 64), ("dv", 64), ("diq", 128), ("diq_r", 128),
          ("dik", 32), ("dik_r", 32), ("diw", 4)]


def phaseP(C, l, only=None):
    nc, tk, dr, S, NB, TS = C.nc, C.tk, C.dr, C.S, C.NB, C.TS
    OFF = {}
    o = 0
    for nm, n in P_SECS:
        OFF[nm] = o
        o += n
    NW = o
    with ExitStack() as es:
        cs = load_consts(C, es, ["c_ident"])
        identf = cs["c_ident"]
        identb = sbt(C, es, "identb", [128, 128], BF16)
        tk.dve(lambda: nc.vector.tensor_copy(identb[:], identf[:]), r=[identf], w=[identb])
        ones_bf = sbt(C, es, "ones_bf", [128, 128], BF16)
        tk.pool(lambda: nc.gpsimd.memset(ones_bf[:], 1.0), w=[ones_bf])
        ones_f = sbt(C, es, "ones_f", [32, 32], F32)
        tk.pool(lambda: nc.gpsimd.memset(ones_f[:], 1.0), w=[ones_f])
        W = sbt(C, es, "Wp", [128, KC, NW], BF16)
        gT = sbt(C, es, "gT", [128, KC], F32)
        tk.dma("sp", gT[:], dr["norm_mix"][l].rearrange("(k p) -> p k", p=128), w=[gT], allow_slow_non_contiguous=True)
        mu_bc = bc_load(C, es, "mu_bc", dr["rwkv_mu"][l], 1024)
        om_bc = sbt(C, es, "om_bc", [128, 1024], F32)
        tk.dve(lambda: nc.vector.tensor_scalar(om_bc[:], mu_bc[:], -1.0, 1.0, op0=ALU.mult, op1=ALU.add), r=[mu_bc], w=[om_bc])
        win = dr["w_in"][l]
        with ExitStack() as es2:
            stg = Rot(nc, es2, "stg", [128, KC, 512], F32, 2)
            LW = lambda nm, c0, n, **kw: load_w_sec(C, stg, W, OFF[nm], win[:, c0:c0 + n], n, gT, **kw)
            LW("rq", C_RQ, 128); LW("rq_r", C_RQ, 128, swap=16); LW("rk", C_RK, 128); LW("rk_r", C_RK, 128, swap=16)
            LW("rv", C_RV, 256); LW("rg", C_RG, 256)
            LW("wrkv_a", C_WR, 768, cs=(om_bc, 0)); LW("wrkv_b", C_WR, 768, cs=(mu_bc, 0))
            LW("wl_a", C_WWL, 64, cs=(om_bc, 768)); LW("wl_b", C_WWL, 64, cs=(mu_bc, 768))
            LW("al_a", C_WAL, 64, cs=(om_bc, 832)); LW("al_b", C_WAL, 64, cs=(mu_bc, 832))
            LW("gl_a", C_WGL, 128, cs=(om_bc, 896)); LW("gl_b", C_WGL, 128, cs=(mu_bc, 896))
            LW("sz", C_SZ, 256); LW("sx", C_SX, 768); LW("sdt", C_SDT, 4)
            LW("dq", C_DQ, 256); LW("dq_r", C_DQ, 256, swap=32); LW("dk", C_DK, 64); LW("dk_r", C_DK, 64, swap=32)
            LW("dv", C_DV, 64); LW("diq", C_DIQ, 128); LW("diq_r", C_DIQ, 128, swap=16)
            LW("dik", C_DIK, 32); LW("dik_r", C_DIK, 32, swap=16); LW("diw", C_DIW, 4)
            tk.barrier()
        w0a0 = sbt(C, es, "w0a0", [128, 512], F32)
        tk.dma("sp", w0a0[:, 0:256], dr["rwkv_w0"][l].partition_broadcast(128), w=[w0a0])
        tk.dma("sp", w0a0[:, 256:512], dr["rwkv_a0"][l].partition_broadcast(128), w=[w0a0])
        kk_bc = bc_load(C, es, "kk_bc", dr["rwkv_k_k"][l], 256)
        ka_bc = bc_load(C, es, "ka_bc", dr["rwkv_k_a"][l], 256)
        rk_bc = bc_load(C, es, "rk_bc", dr["rwkv_r_k"][l].rearrange("h d -> (h d)"), 256)
        dtb_bc = bc_load(C, es, "dtb_bc", dr["ssm_dt_bias"][l], 4)
        alog_bc = bc_load(C, es, "alog_bc", dr["ssm_a_log"][l], 4)
        a_bc = sbt(C, es, "a_bc", [128, 4], F32)
        tk.act(lambda: nc.scalar.activation(a_bc[:], alog_bc[:], AF.Exp), r=[alog_bc], w=[a_bc])
        tk.dve(lambda: nc.vector.tensor_scalar(a_bc[:], a_bc[:], -1.0, None, op0=ALU.mult), r=[a_bc], w=[a_bc])
        w2f = sbt(C, es, "w2f", [128, 768], F32)
        tk.dma("sp", w2f[:64, 0:256], dr["rwkv_w2"][l], w=[w2f])
        tk.dma("sp", w2f[:64, 256:512], dr["rwkv_a2"][l], w=[w2f])
        tk.dma("sp", w2f[:, 512:768], dr["rwkv_g2"][l], w=[w2f])
        w2b = sbt(C, es, "w2b", [128, 768], BF16)
        tk.dve(lambda: nc.vector.tensor_copy(w2b[:64, 0:512], w2f[:64, 0:512]), r=[w2f], w=[w2b])
        tk.dve(lambda: nc.vector.tensor_copy(w2b[:, 512:768], w2f[:, 512:768]), r=[w2f], w=[w2b])
        cwT = sbt(C, es, "cwT", [128, 6, 4], F32)
        for j in range(4):
            tk.dma("sp", cwT[:, :, j], dr["ssm_conv_w"][l, j].rearrange("(c p) -> p c", p=128), w=[cwT],
                   allow_slow_non_contiguous=True)
        cbT = sbt(C, es, "cbT", [128, 6], F32)
        tk.dma("sp", cbT[:], dr["ssm_conv_b"][l].rearrange("(c p) -> p c", p=128), w=[cbT], allow_slow_non_contiguous=True)
        nw = sbt(C, es, "nw", [32, 2], F32)
        ikn_ap = dr["idx_k_norm"][l]
        tk.dma("sp", nw[:, 0:1], ikn_ap.rearrange("(p o) -> p o", o=1), w=[nw], allow_slow_non_contiguous=True)
        tk.dma("sp", nw[0:16, 1:2], ikn_ap[16:32].rearrange("(p o) -> p o", o=1), w=[nw], allow_slow_non_contiguous=True)
        tk.dma("sp", nw[16:32, 1:2], ikn_ap[0:16].rearrange("(p o) -> p o", o=1), w=[nw], allow_slow_non_contiguous=True)

        hTt = sbt(C, es, "hTt", [128, KC, TS], F32)
        sq = sbt(C, es, "sq", [128, KC, TS], BF16)
        hns = [sbt(C, es, f"hn{i}", [128, KC, TS + 1], BF16) for i in range(2)]
        rstd = sbt(C, es, "rstd", [128, TS], F32)
        tabs = {nm: sbt(C, es, "t_" + nm, [128, TS], F32) for nm in ("c_cos64T", "c_sin64T", "c_cos32T", "c_sin32T")}
        pp = Rot(nc, es, "pp", [128, 512], F32, 6, psum=True)
        ptr = Rot(nc, es, "ptr", [128, 1024], BF16, 2, psum=True)
        tA = Rot(nc, es, "tA", [128, 512], F32, 3)
        tB = Rot(nc, es, "tB", [128, 512], F32, 3)
        ob = Rot(nc, es, "ob", [128, 512], BF16, 4)
        of = Rot(nc, es, "of", [128, 512], F32, 3)
        sbj = Rot(nc, es, "sbj", [128, 256], BF16, 12)
        sfj = Rot(nc, es, "sfj", [128, 256], F32, 10)
        sm = Rot(nc, es, "sm", [128, 16], F32, 12)
        cb = Rot(nc, es, "cb", [128, TS + 3], F32, 2)
        halo = sbt(C, es, "halo", [128, 6, 3], F32)
        vout = sbt(C, es, "vout", [128, 4, 65], BF16)
        tk.pool(lambda: nc.gpsimd.memset(vout[:], 1.0), w=[vout])
        twl = sbt(C, es, "twl", [64, TS], BF16)
        alb = sbt(C, es, "alb", [64, TS], BF16)
        sgl = sbt(C, es, "sgl", [128, TS], BF16)
        dtall = sbt(C, es, "dtall", [128, 4, 4], F32)
        xsT = [sbt(C, es, f"xsT{i}", [128, TS], F32) for i in range(2)]
        BTs = [sbt(C, es, f"BTs{i}", [128, TS], BF16) for i in range(2)]
        kTs = sbt(C, es, "kTs", [128, TS], BF16)

        def fm(ps_ap, hn, terms, n=TS):
            nt = len(terms) * KC
            i = 0
            for (off, M, shift) in terms:
                for kc in range(KC):
                    tk.pe(lambda: nc.tensor.matmul(ps_ap, W[:, kc, off:off + M], hn[:, kc, 1 - shift:1 - shift + n],
                                                   start=(i == 0), stop=(i == nt - 1)), r=[W, hn], w=[ps_ap.tensor_key])
                    i += 1

        for b in range(NB):
            tk.pool(lambda: nc.gpsimd.memset(halo[:], 0.0), w=[(halo, c_) for c_ in range(6)])
            for st in range(C.NST):
                t0 = st * TS
                hn = hns[st % 2]
                hprev = hns[(st + 1) % 2]
                tk.dma("sp", hTt[:], dr["hT"][b, :, :, t0:t0 + TS].rearrange("k p t -> p k t"), w=[hTt])
                for nm, t in tabs.items():
                    tk.dma("sp", t[:], dr[nm][:, t0:t0 + TS], w=[t])
                ppn = pp.next()
                emit_norm(C, hTt, TS, hn[:, :, 1:TS + 1], hn, sq, ones_bf, ppn, rstd)
                if st == 0:
                    tk.pool(lambda: nc.gpsimd.memset(hn[:, :, 0:1], 0.0), w=[hn])
                else:
                    tk.pool(lambda: nc.gpsimd.tensor_copy(hn[:, :, 0:1], hprev[:, :, TS:TS + 1]), r=[hprev], w=[hn])
                tsl = slice(t0, t0 + TS)
                cos64, sin64, cos32, sin32 = (tabs[k] for k in ("c_cos64T", "c_sin64T", "c_cos32T", "c_sin32T"))

                def FM(terms, M=128):
                    p = pp.next()
                    ap = p[:M, :]
                    nt = len(terms) * KC
                    i = 0
                    for (off, shift) in terms:
                        for kc in range(KC):
                            tk.pe(lambda: nc.tensor.matmul(ap, W[:, kc, off:off + M], hn[:, kc, 1 - shift:1 - shift + TS],
                                                           start=(i == 0), stop=(i == nt - 1)), r=[W, hn], w=[p])
                            i += 1
                    return p

                def TM(p, c0, j, terms, N):
                    nt = len(terms) * KC
                    i = 0
                    for (off, shift) in terms:
                        for kc in range(KC):
                            a = 1 - shift + j * 128
                            tk.pe(lambda: nc.tensor.matmul(p[:, c0:c0 + N], hn[:, kc, a:a + 128], W[:, kc, off:off + N],
                                                           start=(i == 0), stop=(i == nt - 1)), r=[W, hn], w=[p])
                            i += 1

                def rope_fm(pa, pb, cos, sin, M, scale=None, dt_out=BF16):
                    a_, b_ = tA.next(), tB.next()
                    if scale is None:
                        tk.dve(lambda: nc.vector.tensor_tensor(a_[:M], pa[:M, :], cos[:M], op=ALU.mult), r=[pa, cos], w=[a_])
                        tk.dve(lambda: nc.vector.tensor_tensor(b_[:M], pb[:M, :], sin[:M], op=ALU.mult), r=[pb, sin], w=[b_])
                    else:
                        tk.dve(lambda: nc.vector.scalar_tensor_tensor(a_[:M], pa[:M, :], scale, cos[:M], op0=ALU.mult, op1=ALU.mult),
                               r=[pa, cos], w=[a_])
                        tk.dve(lambda: nc.vector.scalar_tensor_tensor(b_[:M], pb[:M, :], scale, sin[:M], op0=ALU.mult, op1=ALU.mult),
                               r=[pb, sin], w=[b_])
                    o_ = ob.next()
                    tk.pool(lambda: nc.gpsimd.tensor_tensor(o_[:M], a_[:M], b_[:M], op=ALU.add), r=[a_, b_], w=[o_])
                    return o_

                import os
                SK = os.environ.get('KSKIP', '').split(',')
                if only is not None:
                    SK = [x for x in ('ret', 'ssd', 'rw', 'dsa') if x != only]
                def sec_ret():
                    pa = FM([(OFF["rq"], 0)]); pb = FM([(OFF["rq_r"], 0)])
                    o_ = rope_fm(pa, pb, cos32, sin32, 128)
                    tk.dma("pool", dr["r_qT"][b, :, tsl], o_[:], r=[o_])
                    pa = FM([(OFF["rk"], 0)]); pb = FM([(OFF["rk_r"], 0)])
                    o_ = rope_fm(pa, pb, cos32, sin32, 128, scale=32.0 ** -0.5)
                    tk.dma("pool", dr["r_kT"][b, :, tsl], o_[:], r=[o_])
                    pt = ptr.next()
                    for j in range(4):
                        tk.pe(lambda: nc.tensor.transpose(pt[:, j * 128:(j + 1) * 128], o_[:, j * 128:(j + 1) * 128], identb[:]),
                              r=[o_, identb], w=[pt])
                    o2 = ob.next()
                    tk.act(lambda: nc.scalar.copy(o2[:], pt[:, 0:512]), r=[pt], w=[o2])
                    tk.dma("pool", dr["r_kTok"][b, tsl, :].rearrange("(j p) n -> p j n", p=128),
                           o2[:].rearrange("p (j n) -> p j n", j=4), r=[o2])
                    for j in range(4):
                        p = pp.next()
                        TM(p, 0, j, [(OFF["rv"], 0)], 512)
                        vb = sbj.next()
                        tk.act(lambda: nc.scalar.copy(vb[:], p[:, 0:256]), r=[p], w=[vb])
                        jsl = slice(t0 + j * 128, t0 + (j + 1) * 128)
                        tk.dma("pool", dr["r_v"][b, jsl, :], vb[:], r=[vb])
                        sg = sfj.next()
                        tk.act(lambda: nc.scalar.activation(sg[:], p[:, 256:512], AF.Silu), r=[p], w=[sg])
                        tk.dma("pool", dr["r_sg"][b, jsl, :], sg[:], r=[sg])

                if 'ret' not in SK:
                    sec_ret()
                def sec_ssd():
                    for j in range(4):
                        jsl = slice(t0 + j * 128, t0 + (j + 1) * 128)
                        p = pp.next()
                        TM(p, 0, j, [(OFF["sz"], 0)], 256)
                        TM(p, 256, j, [(OFF["sdt"], 0)], 4)
                        sz = sfj.next()
                        tk.act(lambda: nc.scalar.activation(sz[:], p[:, 0:256], AF.Silu), r=[p], w=[sz])
                        tk.dma("pool", dr["s_sz"][b, jsl, :], sz[:], r=[sz])
                        s1 = sm.next()
                        tk.dve(lambda: nc.vector.tensor_tensor(s1[:, 0:4], p[:, 256:260], dtb_bc[:], op=ALU.add), r=[p, dtb_bc], w=[s1])
                        tk.act(lambda: nc.scalar.activation(s1[:, 0:4], s1[:, 0:4], AF.Exp), r=[s1], w=[s1])
                        tk.act(lambda: nc.scalar.activation(dtall[:, j, :], s1[:, 0:4], AF.Ln, bias=1.0), r=[s1], w=[(dtall, j)])
                        s2 = sm.next()
                        tk.dve(lambda: nc.vector.tensor_tensor(s2[:, 0:4], dtall[:, j, :], a_bc[:], op=ALU.mult),
                               r=[(dtall, j), a_bc], w=[s2])
                        tk.dma("pool", dr["s_la"][b, jsl, :], s2[:, 0:4], r=[s2])
                    for c in range(6):
                        p = FM([(OFF["sx"] + c * 128, 0)])
                        cbuf = cb.next()
                        tk.pool(lambda: nc.gpsimd.tensor_copy(cbuf[:, 0:3], halo[:, c, :]), r=[(halo, c)], w=[(cbuf, 0)])
                        tk.act(lambda: nc.scalar.copy(cbuf[:, 3:TS + 3], p[:, :]), r=[p], w=[(cbuf, 1)])
                        tk.pool(lambda: nc.gpsimd.tensor_copy(halo[:, c, :], cbuf[:, TS:TS + 3]), r=[(cbuf, 1)], w=[(halo, c)])
                        acc = tA.next()
                        tk.dve(lambda: nc.vector.tensor_scalar(acc[:], cbuf[:, 3:TS + 3], cwT[:, c, 3:4], cbT[:, c:c + 1],
                                                               op0=ALU.mult, op1=ALU.add), r=[(cbuf, 1), cwT, cbT], w=[acc])
                        for jj in (2, 1, 0):
                            tk.dve(lambda: nc.vector.scalar_tensor_tensor(acc[:], cbuf[:, jj:jj + TS], cwT[:, c, jj:jj + 1], acc[:],
                                                                          op0=ALU.mult, op1=ALU.add),
                                   r=[(cbuf, 0), (cbuf, 1), acc, cwT], w=[acc])
                        if c < 2:
                            tk.act(lambda: nc.scalar.activation(xsT[c][:], acc[:], AF.Silu), r=[acc], w=[xsT[c]])
                        else:
                            o_ = BTs[c - 2] if c < 4 else ob.next()
                            tk.act(lambda: nc.scalar.activation(o_[:], acc[:], AF.Silu), r=[acc], w=[o_])
                            dst = dr["s_BT"] if c < 4 else dr["s_CT"]
                            r0 = (c - 2) % 2 * 128
                            tk.dma("pool", dst[b, r0:r0 + 128, tsl], o_[:], r=[o_])
                    for j in range(4):
                        jsl = slice(t0 + j * 128, t0 + (j + 1) * 128)
                        p = pp.next()
                        for c2 in range(2):
                            tk.pe(lambda: nc.tensor.transpose(p[:, c2 * 128:(c2 + 1) * 128], xsT[c2][:, j * 128:(j + 1) * 128], identf[:]),
                                  r=[xsT[c2], identf], w=[p])
                        xs = sfj.next()
                        tk.act(lambda: nc.scalar.copy(xs[:], p[:, 0:256]), r=[p], w=[xs])
                        tk.dma("pool", dr["s_xs"][b, jsl, :], xs[:], r=[xs])
                        xd = sbj.next()
                        tk.dve(lambda: nc.vector.tensor_tensor(xd[:].rearrange("p (h d) -> p h d", h=4),
                                                               xs[:].rearrange("p (h d) -> p h d", h=4),
                                                               dtall[:, j, :].unsqueeze(2).to_broadcast([128, 4, 64]), op=ALU.mult),
                               r=[xs, (dtall, j)], w=[xd])
                        tk.dma("pool", dr["s_xdt"][b, jsl, :], xd[:], r=[xd])
                        pt = ptr.next()
                        for g2 in range(2):
                            tk.pe(lambda: nc.tensor.transpose(pt[:, g2 * 128:(g2 + 1) * 128], BTs[g2][:, j * 128:(j + 1) * 128], identb[:]),
                                  r=[BTs[g2], identb], w=[pt])
                        bt = sbj.next()
                        tk.act(lambda: nc.scalar.copy(bt[:], pt[:, 0:256]), r=[pt], w=[bt])
                        tk.dma("pool", dr["s_BTok"][b, jsl, :], bt[:], r=[bt])

                if 'ssd' not in SK:
                    sec_ssd()
                def sec_rw():
                    p = FM([(OFF["wl_a"], 0), (OFF["wl_b"], 1)], M=64)
                    tk.act(lambda: nc.scalar.activation(twl[:], p[:64, :], AF.Tanh), r=[p], w=[twl])
                    p = FM([(OFF["al_a"], 0), (OFF["al_b"], 1)], M=64)
                    tk.act(lambda: nc.scalar.copy(alb[:], p[:64, :]), r=[p], w=[alb])
                    p = FM([(OFF["gl_a"], 0), (OFF["gl_b"], 1)])
                    tk.act(lambda: nc.scalar.activation(sgl[:], p[:, :], AF.Sigmoid), r=[p], w=[sgl])
                    for c2 in range(2):
                        p = FM([(OFF["wrkv_a"] + 512 + c2 * 128, 0), (OFF["wrkv_b"] + 512 + c2 * 128, 1)])
                        o_ = of.next()
                        tk.act(lambda: nc.scalar.copy(o_[:], p[:, :]), r=[p], w=[o_])
                        tk.dma("pool", dr["w_vT"][b, c2 * 128:(c2 + 1) * 128, tsl], o_[:], r=[o_])
                    for j in range(4):
                        jsl = slice(t0 + j * 128, t0 + (j + 1) * 128)
                        js = slice(j * 128, (j + 1) * 128)
                        p1 = pp.next()
                        TM(p1, 0, j, [(OFF["wrkv_a"], 0), (OFF["wrkv_b"], 1)], 512)
                        p2 = pp.next()
                        TM(p2, 0, j, [(OFF["wrkv_a"] + 512, 0), (OFF["wrkv_b"] + 512, 1)], 256)
                        tk.pe(lambda: nc.tensor.matmul(p2[:, 256:512], sgl[:, js], w2b[:, 512:768], start=True, stop=True),
                              r=[sgl, w2b], w=[p2])
                        p3 = pp.next()
                        tk.pe(lambda: nc.tensor.matmul(p3[:, 0:256], twl[:, js], w2b[:64, 0:256], start=True, stop=True),
                              r=[twl, w2b], w=[p3])
                        tk.pe(lambda: nc.tensor.matmul(p3[:, 256:512], alb[:, js], w2b[:64, 256:512], start=True, stop=True),
                              r=[alb, w2b], w=[p3])
                        wa = tA.next()
                        tk.dve(lambda: nc.vector.tensor_tensor(wa[:], p3[:], w0a0[:], op=ALU.add), r=[p3, w0a0], w=[wa])
                        tk.act(lambda: nc.scalar.activation(wa[:], wa[:], AF.Sigmoid), r=[wa], w=[wa])
                        a_ = wa[:, 256:512]
                        wf = sfj.next()
                        tk.act(lambda: nc.scalar.activation(wf[:], wa[:, 0:256], AF.Exp, scale=-math.exp(-0.5)), r=[wa], w=[wf])
                        whi = sbj.next()
                        tk.pool(lambda: nc.gpsimd.tensor_copy(whi[:], wf[:]), r=[wf], w=[whi])
                        wlo = sbj.next()
                        tk.dve(lambda: nc.vector.tensor_tensor(wlo[:], wf[:], whi[:], op=ALU.subtract), r=[wf, whi], w=[wlo])
                        tk.dma("pool", dr["w_whi"][b, jsl, :], whi[:], r=[whi])
                        tk.dma("pool", dr["w_wlo"][b, jsl, :], wlo[:], r=[wlo])
                        kkf = sfj.next()
                        tk.dve(lambda: nc.vector.tensor_tensor(kkf[:], p1[:, 256:512], kk_bc[:], op=ALU.mult), r=[p1, kk_bc], w=[kkf])
                        sqk = sfj.next()
                        tk.pool(lambda: nc.gpsimd.tensor_tensor(sqk[:], kkf[:], kkf[:], op=ALU.mult), r=[kkf], w=[sqk])
                        s1 = sm.next()
                        tk.dve(lambda: nc.vector.tensor_reduce(s1[:, 0:4], sqk[:].rearrange("p (h d) -> p h d", h=4), axis=AX.X, op=ALU.add),
                               r=[sqk], w=[s1])
                        tk.act(lambda: nc.scalar.activation(s1[:, 0:4], s1[:, 0:4], AF.Sqrt, bias=1e-12), r=[s1], w=[s1])
                        tk.dve(lambda: nc.vector.reciprocal(s1[:, 0:4], s1[:, 0:4]), r=[s1], w=[s1])
                        kkn = sfj.next()
                        tk.dve(lambda: nc.vector.tensor_tensor(kkn[:].rearrange("p (h d) -> p h d", h=4),
                                                               kkf[:].rearrange("p (h d) -> p h d", h=4),
                                                               s1[:, 0:4].unsqueeze(2).to_broadcast([128, 4, 64]), op=ALU.mult),
                               r=[kkf, s1], w=[kkn])
                        nkk = sbj.next()
                        tk.pool(lambda: nc.gpsimd.tensor_scalar(nkk[:], kkn[:], -1.0, None, op0=ALU.mult), r=[kkn], w=[nkk])
                        tk.dma("pool", dr["w_nkk"][b, jsl, :], nkk[:], r=[nkk])
                        bb = sbj.next()
                        tk.pool(lambda: nc.gpsimd.tensor_tensor(bb[:], kkn[:], a_, op=ALU.mult), r=[kkn, wa], w=[bb])
                        tk.dma("pool", dr["w_bb"][b, jsl, :], bb[:], r=[bb])
                        t1 = sfj.next()
                        tk.dve(lambda: nc.vector.scalar_tensor_tensor(t1[:], a_, -1.0, ka_bc[:], op0=ALU.add, op1=ALU.mult),
                               r=[wa, ka_bc], w=[t1])
                        kp = sfj.next()
                        tk.dve(lambda: nc.vector.scalar_tensor_tensor(kp[:], t1[:], 1.0, p1[:, 256:512], op0=ALU.add, op1=ALU.mult),
                               r=[t1, p1], w=[kp])
                        kpb = sbj.next()
                        tk.pool(lambda: nc.gpsimd.tensor_copy(kpb[:], kp[:]), r=[kp], w=[kpb])
                        tk.dma("pool", dr["w_kp"][b, jsl, :], kpb[:], r=[kpb])
                        rb = sbj.next()
                        tk.act(lambda: nc.scalar.copy(rb[:], p1[:, 0:256]), r=[p1], w=[rb])
                        tk.dma("pool", dr["w_r"][b, jsl, :], rb[:], r=[rb])
                        t2 = sfj.next()
                        tk.dve(lambda: nc.vector.tensor_tensor(t2[:], p1[:, 0:256], kp[:], op=ALU.mult), r=[p1, kp], w=[t2])
                        tk.pool(lambda: nc.gpsimd.tensor_tensor(t2[:], t2[:], rk_bc[:], op=ALU.mult), r=[t2, rk_bc], w=[t2])
                        s2 = sm.next()
                        tk.dve(lambda: nc.vector.tensor_reduce(s2[:, 0:4], t2[:].rearrange("p (h d) -> p h d", h=4), axis=AX.X, op=ALU.add),
                               r=[t2], w=[s2])
                        bo = sfj.next()
                        tk.dve(lambda: nc.vector.tensor_tensor(bo[:].rearrange("p (h d) -> p h d", h=4),
                                                               p2[:, 0:256].rearrange("p (h d) -> p h d", h=4),
                                                               s2[:, 0:4].unsqueeze(2).to_broadcast([128, 4, 64]), op=ALU.mult),
                               r=[p2, s2], w=[bo])
                        tk.dma("pool", dr["w_bonus"][b, jsl, :], bo[:], r=[bo])
                        go = sfj.next()
                        tk.act(lambda: nc.scalar.copy(go[:], p2[:, 256:512]), r=[p2], w=[go])
                        tk.dma("pool", dr["w_g"][b, jsl, :], go[:], r=[go])

                if 'rw' not in SK:
                    sec_rw()
                def sec_dsa():
                    for c2 in range(2):
                        pa = FM([(OFF["dq"] + c2 * 128, 0)]); pb = FM([(OFF["dq_r"] + c2 * 128, 0)])
                        o_ = rope_fm(pa, pb, cos64, sin64, 128)
                        tk.dma("pool", dr["d_qT"][b, c2 * 128:(c2 + 1) * 128, tsl], o_[:], r=[o_])
                    pa = FM([(OFF["dk"], 0)], M=64); pb = FM([(OFF["dk_r"], 0)], M=64)
                    o_ = rope_fm(pa, pb, cos64, sin64, 64)
                    tk.dma("pool", dr["d_kT"][b, :, tsl], o_[:64], r=[o_])
                    pa = FM([(OFF["diq"], 0)]); pb = FM([(OFF["diq_r"], 0)])
                    o_ = rope_fm(pa, pb, cos32, sin32, 128)
                    tk.dma("pool", dr["d_iqT"][b, :, tsl], o_[:], r=[o_])
                    pa = FM([(OFF["dik"], 0)], M=32); pb = FM([(OFF["dik_r"], 0)], M=32)
                    sqi = tA.next()
                    tk.act(lambda: nc.scalar.activation(sqi[:32], pa[:32, :], AF.Square), r=[pa], w=[sqi])
                    p3 = pp.next()
                    tk.pe(lambda: nc.tensor.matmul(p3[:32, :], ones_f[:], sqi[:32], start=True, stop=True), r=[sqi, ones_f], w=[p3])
                    rs = tB.next()
                    tk.act(lambda: nc.scalar.activation(rs[:32], p3[:32, :], AF.Sqrt, scale=1.0 / 32, bias=EPS), r=[p3], w=[rs])
                    tk.dve(lambda: nc.vector.reciprocal(rs[:32], rs[:32]), r=[rs], w=[rs])
                    ia = tA.next(); ib = tB.next()
                    tk.dve(lambda: nc.vector.scalar_tensor_tensor(ia[:32], pa[:32, :], nw[:, 0:1], rs[:32], op0=ALU.mult, op1=ALU.mult),
                           r=[pa, nw, rs], w=[ia])
                    tk.dve(lambda: nc.vector.scalar_tensor_tensor(ib[:32], pb[:32, :], nw[:, 1:2], rs[:32], op0=ALU.mult, op1=ALU.mult),
                           r=[pb, nw, rs], w=[ib])
                    tk.dve(lambda: nc.vector.tensor_tensor(ia[:32], ia[:32], cos32[:32], op=ALU.mult), r=[ia, cos32], w=[ia])
                    tk.dve(lambda: nc.vector.tensor_tensor(ib[:32], ib[:32], sin32[:32], op=ALU.mult), r=[ib, sin32], w=[ib])
                    o_ = ob.next()
                    tk.pool(lambda: nc.gpsimd.tensor_tensor(o_[:32], ia[:32], ib[:32], op=ALU.add), r=[ia, ib], w=[o_])
                    tk.dma("pool", dr["d_ikT"][b, :, tsl], o_[:32], r=[o_])
                    p = pp.next()
                    for j in range(4):
                        TM(p, j * 64, j, [(OFF["dv"], 0)], 64)
                        TM(p, 256 + j * 4, j, [(OFF["diw"], 0)], 4)
                    tk.act(lambda: nc.scalar.copy(vout[:, :, 0:64], p[:, 0:256].rearrange("p (j d) -> p j d", j=4)), r=[p], w=[vout])
                    tk.dma("pool", dr["d_v"][b, tsl, :].rearrange("(j p) n -> p j n", p=128), vout[:], r=[vout])
                    s1 = sm.next()
                    tk.act(lambda: nc.scalar.mul(s1[:, 0:16], p[:, 256:272], (4.0 ** -0.5) * (32.0 ** -0.5)), r=[p], w=[s1])
                    tk.dma("pool", dr["d_iw"][b, tsl, :].rearrange("(j p) n -> p j n", p=128),
                           s1[:, 0:16].rearrange("p (j n) -> p j n", j=4), r=[s1])
                if 'dsa' not in SK:
                    sec_dsa()


def phaseRec(C, l):
    nc, tk, dr, S, NB = C.nc, C.tk, C.dr, C.S, C.NB
    NCH = S // 128
    with ExitStack() as es:
        cs = load_consts(C, es, ["c_ident", "c_U", "c_retla"])
        identf, U, retla = cs["c_ident"], cs["c_U"], cs["c_retla"]
        identb = sbt(C, es, "identb", [128, 128], BF16)
        tk.dve(lambda: nc.vector.tensor_copy(identb[:], identf[:]), r=[identf], w=[identb])
        ones_f = sbt(C, es, "ones_f", [128, 128], F32)
        tk.pool(lambda: nc.gpsimd.memset(ones_f[:], 1.0), w=[ones_f])
        dsk_bc = bc_load(C, es, "dsk_bc", dr["ssm_d"][l], 4)
        nrm_bc = bc_load(C, es, "nrm_bc", dr["ssm_norm"][l], 256)
        psm = Rot(nc, es, "psm", [128, 512], F32, 1, psum=True)
        pBc = Rot(nc, es, "pBc", [128, 512], F32, 1, psum=True)
        psc = Rot(nc, es, "psc", [128, 512], F32, 2, psum=True)
        pY = Rot(nc, es, "pY", [128, 512], F32, 1, psum=True)
        pdS = Rot(nc, es, "pdS", [128, 512], F32, 1, psum=True)
        ptr = Rot(nc, es, "ptr", [128, 1024], BF16, 1, psum=True)
        decTs = Rot(nc, es, "decT", [128, 512], F32, 2)
        Es = Rot(nc, es, "E", [128, 512], F32, 2)
        args = Rot(nc, es, "arg", [128, 512], F32, 2)
        smalls = Rot(nc, es, "small", [128, 16], F32, 4)
        s2s = Rot(nc, es, "s2s", [128, 16], F32, 4)
        qTs = Rot(nc, es, "qT", [128, 512], BF16, 2)
        kTs = Rot(nc, es, "kT", [128, 512], BF16, 2)
        kToks = Rot(nc, es, "kTok", [128, 256], BF16, 2)
        vs = Rot(nc, es, "v", [128, 256], BF16, 2)
        las = Rot(nc, es, "la", [128, 4], F32, 2)
        f1s = Rot(nc, es, "f1", [128, 256], F32, 2)
        f2s = Rot(nc, es, "f2", [128, 256], F32, 2)
        PTs = Rot(nc, es, "PT", [128, 512], BF16, 2)
        qtils = Rot(nc, es, "qtil", [128, 512], BF16, 2)
        xts = Rot(nc, es, "xt", [128, 256], BF16, 2)
        tmps = Rot(nc, es, "tmp", [128, 256], F32, 4)
        obs = Rot(nc, es, "ob", [128, 256], BF16, 2)
        oTs = Rot(nc, es, "oTt", [128, 256], BF16, 2)
        S32 = sbt(C, es, "S32", [128, 256], F32)
        Sbf = sbt(C, es, "Sbf", [128, 256], BF16)

        def prep(la):
            pm = psm.next()
            tk.pe(lambda: nc.tensor.matmul(pm[:, 0:4], U[:], la[:, 0:4], start=True, stop=True), r=[U, la], w=[pm])
            tk.pe(lambda: nc.tensor.matmul(pm[:, 4:8], ones_f[:], la[:, 0:4], start=True, stop=True), r=[ones_f, la], w=[pm])
            sm = smalls.next()
            tk.act(lambda: nc.scalar.copy(sm[:, 8:12], pm[:, 0:4]), r=[pm], w=[sm])
            tk.dve(lambda: nc.vector.tensor_tensor(sm[:, 0:4], pm[:, 4:8], sm[:, 8:12], op=ALU.subtract), r=[pm, sm], w=[sm])
            tk.act(lambda: nc.scalar.activation(sm[:, 0:4], sm[:, 0:4], AF.Exp), r=[sm], w=[sm])
            tk.act(lambda: nc.scalar.activation(sm[:, 4:8], pm[:, 4:8], AF.Exp), r=[pm, sm], w=[sm])
            pb = pBc.next()
            for h in range(4):
                tk.pe(lambda: nc.tensor.matmul(pb[:, h * 128:(h + 1) * 128], la[:, h:h + 1].to_broadcast([128, 128]), U[:],
                                               start=True, stop=True), r=[la, U], w=[pb])
            arg = args.next()
            for h in range(4):
                tk.dve(lambda: nc.vector.tensor_scalar(arg[:, h * 128:(h + 1) * 128], pb[:, h * 128:(h + 1) * 128],
                                                       sm[:, 8 + h:9 + h], 0.0, op0=ALU.subtract, op1=ALU.min),
                       r=[pb, sm], w=[arg])
            decT = decTs.next()
            tk.act(lambda: nc.scalar.activation(decT[:], arg[:], AF.Exp), r=[arg], w=[decT])
            tk.dve(lambda: nc.vector.tensor_tensor(decT[:].rearrange("p (h t) -> p h t", h=4),
                                                   decT[:].rearrange("p (h t) -> p h t", h=4),
                                                   U[:].unsqueeze(1).to_broadcast([128, 4, 128]), op=ALU.mult),
                   r=[decT, U], w=[decT])
            E = Es.next()
            tk.act(lambda: nc.scalar.activation(E[:], pb[:], AF.Exp), r=[pb], w=[E])
            return decT, E, sm

        for mix in ("ret", "ssd"):
            N = 32 if mix == "ret" else 128
            if mix == "ret":
                dec_const = prep(retla)
            for b in range(NB):
                tk.pool(lambda: nc.gpsimd.memset(S32[:], 0.0), w=[S32])
                tk.pool(lambda: nc.gpsimd.memset(Sbf[:], 0.0), w=[Sbf])
                for c in range(NCH):
                    tsl = slice(c * 128, (c + 1) * 128)
                    qT, kT, kTok, v = qTs.next(), kTs.next(), kToks.next(), vs.next()
                    f1, f2 = f1s.next(), f2s.next()
                    if mix == "ret":
                        tk.dma("sp", qT[:32, :].rearrange("n (h t) -> n h t", h=4),
                               dr["r_qT"][b, :, tsl].rearrange("(h n) t -> n h t", h=4), w=[qT])
                        tk.dma("sp", kT[:32, :].rearrange("n (h t) -> n h t", h=4),
                               dr["r_kT"][b, :, tsl].rearrange("(h n) t -> n h t", h=4), w=[kT])
                        tk.dma("sp", kTok[:, 0:128], dr["r_kTok"][b, tsl, :], w=[kTok])
                        tk.dma("sp", v[:], dr["r_v"][b, tsl, :], w=[v])
                        tk.dma("sp", f1[:], dr["r_sg"][b, tsl, :], w=[f1])
                        decT, E, sm = dec_const
                    else:
                        tk.dma("sp", qT[:, 0:256].rearrange("n (g t) -> n g t", g=2),
                               dr["s_CT"][b, :, tsl].rearrange("(g n) t -> n g t", g=2), w=[qT])
                        tk.dma("sp", kT[:, 0:256].rearrange("n (g t) -> n g t", g=2),
                               dr["s_BT"][b, :, tsl].rearrange("(g n) t -> n g t", g=2), w=[kT])
                        tk.dma("sp", kTok[:], dr["s_BTok"][b, tsl, :], w=[kTok])
                        tk.dma("sp", v[:], dr["s_xdt"][b, tsl, :], w=[v])
                        tk.dma("sp", f1[:], dr["s_sz"][b, tsl, :], w=[f1])
                        tk.dma("sp", f2[:], dr["s_xs"][b, tsl, :], w=[f2])
                        la = las.next()
                        tk.dma("sp", la[:], dr["s_la"][b, tsl, :], w=[la])
                        decT, E, sm = prep(la)
                    sc = psc.next()
                    PT = PTs.next()
                    qtil = qtils.next()
                    if mix == "ret":
                        for h in range(4):
                            hs = slice(h * 128, (h + 1) * 128)
                            tk.pe(lambda: nc.tensor.matmul(sc[:, hs], kT[:32, hs], qT[:32, hs], start=True, stop=True),
                                  r=[kT, qT], w=[sc])
                        tk.dve(lambda: nc.vector.tensor_tensor(PT[:], sc[:], decT[:], op=ALU.mult), r=[sc, decT], w=[PT])
                        tk.dve(lambda: nc.vector.tensor_tensor(qtil[:32, :], qT[:32, :], E[:32, :], op=ALU.mult), r=[qT, E], w=[qtil])
                    else:
                        for g in range(2):
                            gs = slice(g * 128, (g + 1) * 128)
                            tk.pe(lambda: nc.tensor.matmul(sc[:, gs], kT[:, gs], qT[:, gs], start=True, stop=True),
                                  r=[kT, qT], w=[sc])
                        v4 = lambda ap: ap.rearrange("p (g e t) -> p g e t", g=2, e=2)
                        bcg = lambda ap: ap.rearrange("p (g t) -> p g t", g=2).unsqueeze(2).to_broadcast([128, 2, 2, 128])
                        tk.dve(lambda: nc.vector.tensor_tensor(v4(PT[:]), v4(decT[:]), bcg(sc[:, 0:256]), op=ALU.mult),
                               r=[sc, decT], w=[PT])
                        tk.dve(lambda: nc.vector.tensor_tensor(v4(qtil[:]), v4(E[:]), bcg(qT[:, 0:256]), op=ALU.mult),
                               r=[qT, E], w=[qtil])
                    py = pY.next()
                    for h in range(4):
                        hs = slice(h * 128, (h + 1) * 128)
                        ps_ = slice(h * 64, (h + 1) * 64)
                        tk.pe(lambda: nc.tensor.matmul(py[:, ps_], PT[:, hs], v[:, ps_], start=True, stop=False), r=[PT, v], w=[py])
                        tk.pe(lambda: nc.tensor.matmul(py[:, ps_], qtil[:N, hs], Sbf[:N, ps_], start=False, stop=True),
                              r=[qtil, Sbf], w=[py])
                    xt = xts.next()
                    tk.dve(lambda: nc.vector.tensor_tensor(xt[:].rearrange("p (h d) -> p h d", h=4),
                                                           v[:].rearrange("p (h d) -> p h d", h=4),
                                                           sm[:, 0:4].unsqueeze(2).to_broadcast([128, 4, 64]), op=ALU.mult),
                           r=[v, sm], w=[xt])
                    pd = pdS.next()
                    if mix == "ret":
                        for h in range(4):
                            ps_ = slice(h * 64, (h + 1) * 64)
                            tk.pe(lambda: nc.tensor.matmul(pd[:32, ps_], kTok[:, h * 32:(h + 1) * 32], xt[:, ps_], start=True, stop=True),
                                  r=[kTok, xt], w=[pd])
                    else:
                        for g in range(2):
                            gs = slice(g * 128, (g + 1) * 128)
                            tk.pe(lambda: nc.tensor.matmul(pd[:, gs], kTok[:, gs], xt[:, gs], start=True, stop=True),
                                  r=[kTok, xt], w=[pd])
                    for h in range(4):
                        ps_ = slice(h * 64, (h + 1) * 64)
                        tk.dve(lambda: nc.vector.scalar_tensor_tensor(S32[:N, ps_], S32[:N, ps_], sm[:N, 4 + h:5 + h], pd[:N, ps_],
                                                                      op0=ALU.mult, op1=ALU.add), r=[S32, sm, pd, Sbf], w=[S32])
                    tk.act(lambda: nc.scalar.copy(Sbf[:N, :], S32[:N, :]), r=[S32], w=[Sbf])
                    ob = obs.next()
                    s2 = s2s.next()
                    if mix == "ret":
                        t1 = tmps.next()
                        tk.act(lambda: nc.scalar.activation(t1[:], py[:, 0:256], AF.Square), r=[py], w=[t1])
                        tk.dve(lambda: nc.vector.tensor_reduce(s2[:, 0:4], t1[:].rearrange("p (h d) -> p h d", h=4), axis=AX.X, op=ALU.add),
                               r=[t1], w=[s2])
                        tk.act(lambda: nc.scalar.activation(s2[:, 0:4], s2[:, 0:4], AF.Sqrt, scale=1.0 / 64, bias=EPS), r=[s2], w=[s2])
                        tk.dve(lambda: nc.vector.reciprocal(s2[:, 0:4], s2[:, 0:4]), r=[s2], w=[s2])
                        t2 = tmps.next()
                        tk.dve(lambda: nc.vector.tensor_tensor(t2[:].rearrange("p (h d) -> p h d", h=4),
                                                               py[:, 0:256].rearrange("p (h d) -> p h d", h=4),
                                                               s2[:, 0:4].unsqueeze(2).to_broadcast([128, 4, 64]), op=ALU.mult),
                               r=[py, s2], w=[t2])
                        tk.pool(lambda: nc.gpsimd.tensor_tensor(ob[:], t2[:], f1[:], op=ALU.mult), r=[t2, f1], w=[ob])
                        ch0 = 0
                    else:
                        t1 = tmps.next()
                        tk.pool(lambda: nc.gpsimd.tensor_tensor(t1[:].rearrange("p (h d) -> p h d", h=4),
                                                                f2[:].rearrange("p (h d) -> p h d", h=4),
                                                                dsk_bc[:].unsqueeze(2).to_broadcast([128, 4, 64]), op=ALU.mult),
                                r=[f2, dsk_bc], w=[t1])
                        tk.dve(lambda: nc.vector.tensor_tensor(t1[:], t1[:], py[:, 0:256], op=ALU.add), r=[t1, py], w=[t1])
                        tk.pool(lambda: nc.gpsimd.tensor_tensor(t1[:], t1[:], f1[:], op=ALU.mult), r=[t1, f1], w=[t1])
                        t2 = tmps.next()
                        tk.act(lambda: nc.scalar.activation(t2[:], t1[:], AF.Square, accum_out=s2[:, 0:1]), r=[t1], w=[t2, s2])
                        tk.act(lambda: nc.scalar.activation(s2[:, 0:1], s2[:, 0:1], AF.Sqrt, scale=1.0 / 256, bias=EPS), r=[s2], w=[s2])
                        tk.dve(lambda: nc.vector.reciprocal(s2[:, 0:1], s2[:, 0:1]), r=[s2], w=[s2])
                        tk.dve(lambda: nc.vector.scalar_tensor_tensor(ob[:], t1[:], s2[:, 0:1], nrm_bc[:], op0=ALU.mult, op1=ALU.mult),
                               r=[t1, s2, nrm_bc], w=[ob])
                        ch0 = 4
                    pt = ptr.next()
                    for c2 in range(2):
                        tk.pe(lambda: nc.tensor.transpose(pt[:, c2 * 128:(c2 + 1) * 128], ob[:, c2 * 128:(c2 + 1) * 128], identb[:]),
                              r=[ob, identb], w=[pt])
                    oTt = oTs.next()
                    tk.act(lambda: nc.scalar.copy(oTt[:], pt[:, 0:256]), r=[pt], w=[oTt])
                    tk.dma("pool", dr["oT"][b, ch0:ch0 + 2, :, tsl].rearrange("k p t -> p k t"),
                           oTt[:].rearrange("p (k t) -> p k t", k=2), r=[oTt])


def phaseRW(C, l):
    nc, tk, dr, S, NB = C.nc, C.tk, C.dr, C.S, C.NB
    assert NB == 2
    NCH = S // 64
    with ExitStack() as es:
        cs = load_consts(C, es, ["c_ident", "c_E2"])
        identf, E2 = cs["c_ident"], cs["c_E2"]
        identb = sbt(C, es, "identb", [128, 128], BF16)
        tk.dve(lambda: nc.vector.tensor_copy(identb[:], identf[:]), r=[identf], w=[identb])
        lnw_bc = bc_load(C, es, "lnw_bc", dr["rwkv_ln_w"][l], 256)
        lnb_bc = bc_load(C, es, "lnb_bc", dr["rwkv_ln_b"][l], 256)
        pA = Rot(nc, es, "pA", [128, 512], F32, 2, psum=True)
        pB = Rot(nc, es, "pB", [128, 512], F32, 2, psum=True)
        pC = Rot(nc, es, "pC", [128, 512], F32, 2, psum=True)
        pT = Rot(nc, es, "pT", [128, 512], F32, 1, psum=True)
        pO = Rot(nc, es, "pO", [128, 1024], BF16, 1, psum=True)
        names = ("w_whi", "w_wlo", "w_nkk", "w_bb", "w_kp", "w_r")
        tl = {nm: Rot(nc, es, "c_" + nm, [128, 256], BF16, 2) for nm in names}
        vTs = Rot(nc, es, "vTc", [128, 256], F32, 2)
        ychs = Rot(nc, es, "ych", [128, 256], F32, 2)
        kvs = Rot(nc, es, "kv", [128, 256], F32, 3)
        tmpa = Rot(nc, es, "tmpa", [128, 256], F32, 2)
        sas = Rot(nc, es, "sa", [128, 4], F32, 3)
        Sb = [sbt(C, es, f"Sst{i}", [128, 256], F32) for i in range(2)]
        for S_ in Sb:
            tk.pool(lambda: nc.gpsimd.memset(S_[:], 0.0), w=[S_])
        rsbs = Rot(nc, es, "rsb", [128, 256], F32, 3)
        tmpp = Rot(nc, es, "tmpp", [128, 256], F32, 3)
        gstep = 0
        pending = None
        yjunk = sbt(C, es, "yjunk", [128, 256], F32)

        def flush_y(ych_, pend):
            tmp3_, t_ = pend
            for h_ in range(4):
                hs_ = slice(h_ * 64, (h_ + 1) * 64)
                tk.act(lambda: nc.scalar.activation(yjunk[:, hs_], tmp3_[:, hs_], AF.Copy,
                                                    accum_out=h4(ych_[:])[:, h_, t_:t_ + 1]), r=[tmp3_], w=[yjunk, ych_])

        bons = Rot(nc, es, "bon", [64, 512], F32, 2)
        gs_ = Rot(nc, es, "gg", [64, 512], F32, 2)
        yts = Rot(nc, es, "yt", [64, 512], F32, 2)
        ycs = Rot(nc, es, "yc", [64, 512], F32, 2)
        sqs = Rot(nc, es, "sqy", [64, 512], F32, 2)
        st8 = Rot(nc, es, "st8", [64, 16], F32, 4)
        obs = Rot(nc, es, "obw", [64, 512], BF16, 2)
        oTs = Rot(nc, es, "oTw", [128, 256], BF16, 2)
        h4 = lambda ap: ap.rearrange("p (h k) -> p h k", h=4)
        for c in range(NCH):
            csl = slice(c * 64, (c + 1) * 64)
            cur = {}
            for nm in names:
                t = tl[nm].next()
                for b in range(2):
                    tk.dma("sp", t[b * 64:(b + 1) * 64, :], dr[nm][b, csl, :], w=[t])
                cur[nm] = t
            vT = vTs.next()
            for b in range(2):
                tk.dma("sp", h4(vT[b * 64:(b + 1) * 64, :]), dr["w_vT"][b, :, csl].rearrange("(h v) t -> v h t", h=4), w=[vT])
            bon, gg = bons.next(), gs_.next()
            for b in range(2):
                tk.dma("sp", bon[:, b * 256:(b + 1) * 256], dr["w_bonus"][b, csl, :], w=[bon])
                tk.dma("sp", gg[:, b * 256:(b + 1) * 256], dr["w_g"][b, csl, :], w=[gg])
            ych = ychs.next()
            for t in range(64):
                E2t = E2[:, t * 128:(t + 1) * 128]
                pa, pb, pc = pA.next(), pB.next(), pC.next()
                tk.pe(lambda: nc.tensor.matmul(pa[:, 0:256], E2t, cur["w_whi"][:], start=True, stop=False), r=[E2, cur["w_whi"]], w=[pa])
                tk.pe(lambda: nc.tensor.matmul(pa[:, 0:256], E2t, cur["w_wlo"][:], start=False, stop=True), r=[E2, cur["w_wlo"]], w=[pa])
                tk.pe(lambda: nc.tensor.matmul(pa[:, 256:512], E2t, cur["w_nkk"][:], start=True, stop=True), r=[E2, cur["w_nkk"]], w=[pa])
                tk.pe(lambda: nc.tensor.matmul(pb[:, 0:256], E2t, cur["w_bb"][:], start=True, stop=True), r=[E2, cur["w_bb"]], w=[pb])
                tk.pe(lambda: nc.tensor.matmul(pb[:, 256:512], E2t, cur["w_kp"][:], start=True, stop=True), r=[E2, cur["w_kp"]], w=[pb])
                tk.pe(lambda: nc.tensor.matmul(pc[:, 0:256], E2t, cur["w_r"][:], start=True, stop=True), r=[E2, cur["w_r"]], w=[pc])
                kv = kvs.next()
                for h in range(4):
                    hs = slice(h * 64, (h + 1) * 64)
                    tk.act(lambda: nc.scalar.activation(kv[:, hs], pb[:, 256 + h * 64:256 + (h + 1) * 64], AF.Copy,
                                                        scale=vT[:, h * 64 + t:h * 64 + t + 1]), r=[pb, vT], w=[kv])
                if pending is not None:
                    flush_y(ych, pending)
                    pending = None
                tmp = tmpa.next()
                sa = sas.next()
                So, Sn = Sb[gstep % 2], Sb[(gstep + 1) % 2]
                gstep += 1
                rsb = rsbs.next()
                tk.act(lambda: nc.scalar.copy(rsb[:], pc[:, 0:256]), r=[pc], w=[rsb])
                tk.dve(lambda: nc.vector.tensor_tensor(tmp[:], So[:], pa[:, 256:512], op=ALU.mult), r=[So, pa], w=[tmp])
                tk.dve(lambda: nc.vector.tensor_reduce(sa[:], h4(tmp[:]), axis=AX.X, op=ALU.add), r=[tmp], w=[sa])
                tk.dve(lambda: nc.vector.tensor_tensor(Sn[:], So[:], pa[:, 0:256], op=ALU.mult), r=[So, pa], w=[Sn])
                tmp2 = tmpa.next()
                tk.dve(lambda: nc.vector.tensor_tensor(h4(tmp2[:]), h4(pb[:, 0:256]), sa[:].unsqueeze(2).to_broadcast([128, 4, 64]),
                                                       op=ALU.mult), r=[pb, sa], w=[tmp2])
                tk.dve(lambda: nc.vector.tensor_tensor(Sn[:], Sn[:], tmp2[:], op=ALU.add), r=[Sn, tmp2], w=[Sn])
                tk.dve(lambda: nc.vector.tensor_tensor(Sn[:], Sn[:], kv[:], op=ALU.add), r=[Sn, kv], w=[Sn])
                tmp3 = tmpp.next()
                tk.pool(lambda: nc.gpsimd.tensor_tensor(tmp3[:], Sn[:], rsb[:], op=ALU.mult), r=[Sn, rsb], w=[tmp3])
                pending = (tmp3, t)
            flush_y(ych, pending)
            pending = None
            pt = pT.next()
            for h in range(4):
                tk.pe(lambda: nc.tensor.transpose(pt[:64, h * 128:(h + 1) * 128], ych[:, h * 64:(h + 1) * 64], identf[:]),
                      r=[ych, identf], w=[pt])
            yt = yts.next()
            tk.act(lambda: nc.scalar.copy(yt[:].rearrange("p (b h v) -> p h b v", b=2, h=4),
                                          pt[:64, :].rearrange("p (h b v) -> p h b v", h=4, b=2)), r=[pt], w=[yt])
            g8 = lambda ap: ap.rearrange("p (g v) -> p g v", g=8)
            s1 = st8.next()
            tk.dve(lambda: nc.vector.tensor_reduce(s1[:, 0:8], g8(yt[:]), axis=AX.X, op=ALU.add), r=[yt], w=[s1])
            tk.dve(lambda: nc.vector.tensor_scalar(s1[:, 0:8], s1[:, 0:8], -1.0 / 64, None, op0=ALU.mult), r=[s1], w=[s1])
            yc = ycs.next()
            tk.dve(lambda: nc.vector.tensor_tensor(g8(yc[:]), g8(yt[:]), s1[:, 0:8].unsqueeze(2).to_broadcast([64, 8, 64]), op=ALU.add),
                   r=[yt, s1], w=[yc])
            sq = sqs.next()
            tk.pool(lambda: nc.gpsimd.tensor_tensor(sq[:], yc[:], yc[:], op=ALU.mult), r=[yc], w=[sq])
            tk.dve(lambda: nc.vector.tensor_reduce(s1[:, 8:16], g8(sq[:]), axis=AX.X, op=ALU.add), r=[sq], w=[s1])
            tk.act(lambda: nc.scalar.activation(s1[:, 8:16], s1[:, 8:16], AF.Sqrt, scale=1.0 / 64, bias=64e-5), r=[s1], w=[s1])
            tk.dve(lambda: nc.vector.reciprocal(s1[:, 8:16], s1[:, 8:16]), r=[s1], w=[s1])
            tk.dve(lambda: nc.vector.tensor_tensor(g8(yc[:]), g8(yc[:]), s1[:, 8:16].unsqueeze(2).to_broadcast([64, 8, 64]), op=ALU.mult),
                   r=[yc, s1], w=[yc])
            b2 = lambda ap: ap.rearrange("p (b f) -> p b f", b=2)
            bcb = lambda t_: t_[:64, :].unsqueeze(1).to_broadcast([64, 2, 256])
            tk.pool(lambda: nc.gpsimd.tensor_tensor(b2(yc[:]), b2(yc[:]), bcb(lnw_bc), op=ALU.mult), r=[yc, lnw_bc], w=[yc])
            tk.pool(lambda: nc.gpsimd.tensor_tensor(b2(yc[:]), b2(yc[:]), bcb(lnb_bc), op=ALU.add), r=[yc, lnb_bc], w=[yc])
            tk.dve(lambda: nc.vector.tensor_tensor(yc[:], yc[:], bon[:], op=ALU.add), r=[yc, bon], w=[yc])
            ob = obs.next()
            tk.pool(lambda: nc.gpsimd.tensor_tensor(ob[:], yc[:], gg[:], op=ALU.mult), r=[yc, gg], w=[ob])
            po = pO.next()
            for q in range(4):
                tk.pe(lambda: nc.tensor.transpose(po[:, q * 64:(q + 1) * 64], ob[:, q * 128:(q + 1) * 128], identb[:64, :64]),
                      r=[ob, identb], w=[po])
            oTt = oTs.next()
            tk.act(lambda: nc.scalar.copy(oTt[:], po[:, 0:256]), r=[po], w=[oTt])
            for b in range(2):
                tk.dma("pool", dr["oT"][b, 2:4, :, csl].rearrange("k p t -> p k t"),
                       oTt[:, b * 128:(b + 1) * 128].rearrange("p (k t) -> p k t", k=2), r=[oTt])


def phaseDSA(C, l):
    nc, tk, dr, S, NB = C.nc, C.tk, C.dr, C.S, C.NB
    NQ = S // 128
    TOPK = float(min(256, S // 4))
    NIT = 20
    with ExitStack() as es:
        cs = load_consts(C, es, ["c_ident", "c_negU"])
        identf, negU = cs["c_ident"], cs["c_negU"]
        identb = sbt(C, es, "identb", [128, 128], BF16)
        tk.dve(lambda: nc.vector.tensor_copy(identb[:], identf[:]), r=[identf], w=[identb])
        thr0 = sbt(C, es, "thr0", [128, 1], F32)
        tk.pool(lambda: nc.gpsimd.memset(thr0[:], NEG_THR), w=[thr0])
        kT = sbt(C, es, "dkT", [64, S], BF16)
        ikT = sbt(C, es, "dikT", [32, S], BF16)
        vaug = sbt(C, es, "vaug", [128, NQ, 65], BF16)
        pp = Rot(nc, es, "pp", [128, 512], F32, 4, psum=True)
        pmr = Rot(nc, es, "pm", [128, 1024], BF16, 2, psum=True)
        pout = Rot(nc, es, "pout", [128, 512], F32, 1, psum=True)
        ptr = Rot(nc, es, "ptr", [128, 1024], BF16, 1, psum=True)
        scs = Rot(nc, es, "sc", [128, S], F32, 2)
        junk = sbt(C, es, "junk", [128, S], BF16)
        masks = Rot(nc, es, "mask", [128, S], BF16, 2)
        rls = Rot(nc, es, "rl", [128, 512], F32, 3)
        es_ = Rot(nc, es, "eexp", [128, 512], BF16, 3)
        pTs = Rot(nc, es, "pT", [128, 512], BF16, 3)
        iqs = Rot(nc, es, "iq", [32, 512], BF16, 2)
        qs = Rot(nc, es, "q", [64, 512], BF16, 2)
        iws = Rot(nc, es, "iw", [128, 4], F32, 2)
        st = Rot(nc, es, "bst", [128, 8], F32, 2)
        obs = Rot(nc, es, "obd", [128, 256], BF16, 2)
        rcs = Rot(nc, es, "rc", [128, 4], F32, 2)
        oTs = Rot(nc, es, "oTd", [128, 256], BF16, 2)
        for b in range(NB):
            tk.dma("sp", kT[:], dr["d_kT"][b], w=[kT])
            tk.dma("sp", ikT[:], dr["d_ikT"][b], w=[ikT])
            tk.dma("sp", vaug[:], dr["d_v"][b].rearrange("(j p) n -> p j n", p=128), w=[vaug])
            for i in range(NQ):
                L = (i + 1) * 128
                tsl = slice(i * 128, (i + 1) * 128)
                iq, q, iw = iqs.next(), qs.next(), iws.next()
                tk.dma("sp", iq[:].rearrange("d (h t) -> d h t", h=4), dr["d_iqT"][b, :, tsl].rearrange("(h d) t -> d h t", h=4), w=[iq])
                tk.dma("sp", q[:].rearrange("d (h t) -> d h t", h=4), dr["d_qT"][b, :, tsl].rearrange("(h d) t -> d h t", h=4), w=[q])
                tk.dma("sp", iw[:], dr["d_iw"][b, tsl, :], w=[iw])
                sc = scs.next()
                for k0 in range(0, L, 512):
                    w_ = min(512, L - k0)
                    for h in range(4):
                        p = pp.next()
                        tk.pe(lambda: nc.tensor.matmul(p[:, :w_], iq[:, h * 128:(h + 1) * 128], ikT[:, k0:k0 + w_], start=True, stop=True),
                              r=[iq, ikT], w=[p])
                        rl = rls.next()
                        tk.act(lambda: nc.scalar.activation(rl[:, :w_], p[:, :w_], AF.Relu), r=[p], w=[rl])
                        if h == 0:
                            tk.dve(lambda: nc.vector.tensor_scalar(sc[:, k0:k0 + w_], rl[:, :w_], iw[:, 0:1], None, op0=ALU.mult),
                                   r=[rl, iw], w=[sc])
                        else:
                            tk.dve(lambda: nc.vector.scalar_tensor_tensor(sc[:, k0:k0 + w_], rl[:, :w_], iw[:, h:h + 1], sc[:, k0:k0 + w_],
                                                                          op0=ALU.mult, op1=ALU.add), r=[rl, iw, sc], w=[sc])
                b_ = st.next()
                if i >= 2:
                    tk.dve(lambda: nc.vector.tensor_reduce(b_[:, 5:6], sc[:, :L], axis=AX.X, op=ALU.max), r=[sc], w=[b_])
                    tk.dve(lambda: nc.vector.tensor_reduce(b_[:, 6:7], sc[:, :L], axis=AX.X, op=ALU.min), r=[sc], w=[b_])
                    tk.dve(lambda: nc.vector.tensor_scalar(b_[:, 0:1], b_[:, 6:7], -1.0, None, op0=ALU.add), r=[b_], w=[b_])
                    tk.dve(lambda: nc.vector.scalar_tensor_tensor(b_[:, 1:2], b_[:, 5:6], 1.0, b_[:, 0:1], op0=ALU.add, op1=ALU.subtract),
                           r=[b_], w=[b_])
                tk.dve(lambda: nc.vector.tensor_tensor(sc[:, i * 128:L], sc[:, i * 128:L], negU[:], op=ALU.add), r=[sc, negU], w=[sc])
                if i >= 2:
                    for it in range(NIT):
                        f = 0.5 ** (it + 1)
                        tk.dve(lambda: nc.vector.scalar_tensor_tensor(b_[:, 2:3], b_[:, 1:2], f, b_[:, 0:1], op0=ALU.mult, op1=ALU.add),
                               r=[b_], w=[b_])
                        tk.dve(lambda: nc.vector.tensor_scalar(junk[:, :L], sc[:, :L], b_[:, 2:3], None, op0=ALU.is_ge, op1=ALU.add,
                                                               accum_out=b_[:, 3:4]), r=[sc, b_], w=[junk, b_])
                        tk.dve(lambda: nc.vector.tensor_scalar(b_[:, 4:5], b_[:, 3:4], TOPK, None, op0=ALU.is_ge), r=[b_], w=[b_])
                        tk.dve(lambda: nc.vector.tensor_tensor(b_[:, 4:5], b_[:, 4:5], b_[:, 1:2], op=ALU.mult), r=[b_], w=[b_])
                        tk.dve(lambda: nc.vector.scalar_tensor_tensor(b_[:, 0:1], b_[:, 4:5], f, b_[:, 0:1], op0=ALU.mult, op1=ALU.add),
                               r=[b_], w=[b_])
                    thr = b_[:, 0:1]
                    thr_r = [b_]
                else:
                    thr = thr0[:, 0:1]
                    thr_r = [thr0]
                mask = masks.next()
                tk.dve(lambda: nc.vector.tensor_scalar(mask[:, :L], sc[:, :L], thr, None, op0=ALU.is_ge), r=[sc] + thr_r, w=[mask])
                po = pout.next()
                for g0 in range(0, i + 1, 8):
                    pm = pmr.next()
                    g1 = min(i + 1, g0 + 8)
                    for j in range(g0, g1):
                        tk.pe(lambda: nc.tensor.transpose(pm[:, (j - g0) * 128:(j - g0 + 1) * 128], mask[:, j * 128:(j + 1) * 128], identb[:]),
                              r=[mask, identb], w=[pm])
                    for j in range(g0, g1):
                        lg = pp.next()
                        tk.pe(lambda: nc.tensor.matmul(lg[:, 0:512], kT[:, j * 128:(j + 1) * 128], q[:, :], start=True, stop=True),
                              r=[kT, q], w=[lg])
                        e = es_.next()
                        tk.act(lambda: nc.scalar.activation(e[:], lg[:], AF.Exp, scale=64.0 ** -0.5), r=[lg], w=[e])
                        pT = pTs.next()
                        tk.dve(lambda: nc.vector.tensor_tensor(pT[:].rearrange("p (h t) -> p h t", h=4),
                                                               e[:].rearrange("p (h t) -> p h t", h=4),
                                                               pm[:, (j - g0) * 128:(j - g0 + 1) * 128].unsqueeze(1).to_broadcast([128, 4, 128]),
                                                               op=ALU.mult), r=[e, pm], w=[pT])
                        for h in range(4):
                            tk.pe(lambda: nc.tensor.matmul(po[:, h * 65:(h + 1) * 65], pT[:, h * 128:(h + 1) * 128], vaug[:, j, :],
                                                           start=(j == 0 and h == 0), stop=(j == i and h == 3)), r=[pT, vaug], w=[po])
                rc = rcs.next()
                po3 = po[:, 0:260].rearrange("p (h e) -> p h e", h=4)
                tk.dve(lambda: nc.vector.reciprocal(rc[:], po3[:, :, 64]), r=[po], w=[rc])
                ob = obs.next()
                tk.dve(lambda: nc.vector.tensor_tensor(ob[:].rearrange("p (h d) -> p h d", h=4), po3[:, :, 0:64],
                                                       rc[:].unsqueeze(2).to_broadcast([128, 4, 64]), op=ALU.mult), r=[po, rc], w=[ob])
                pt = ptr.next()
                for c2 in range(2):
                    tk.pe(lambda: nc.tensor.transpose(pt[:, c2 * 128:(c2 + 1) * 128], ob[:, c2 * 128:(c2 + 1) * 128], identb[:]),
                          r=[ob, identb], w=[pt])
                oTt = oTs.next()
                tk.act(lambda: nc.scalar.copy(oTt[:], pt[:, 0:256]), r=[pt], w=[oTt])
                tk.dma("pool", dr["oT"][b, 6:8, :, tsl].rearrange("k p t -> p k t"),
                       oTt[:].rearrange("p (k t) -> p k t", k=2), r=[oTt])


def phaseF1(C, l):
    nc, tk, dr, S, NB, TS = C.nc, C.tk, C.dr, C.S, C.NB, C.TS
    with ExitStack() as es:
        cs = load_consts(C, es, ["c_ident"])
        identf = cs["c_ident"]
        identb = sbt(C, es, "identb", [128, 128], BF16)
        tk.dve(lambda: nc.vector.tensor_copy(identb[:], identf[:]), r=[identf], w=[identb])
        ones_bf = sbt(C, es, "ones_bf", [128, 128], BF16)
        tk.pool(lambda: nc.gpsimd.memset(ones_bf[:], 1.0), w=[ones_bf])
        Ws = {nm: sbt(C, es, "W" + nm, [128, KC, 1024], BF16) for nm in ("w_out", "wq_x", "wk_x", "wv_x", "wo_x")}
        gq = sbt(C, es, "gq", [128, KC], F32)
        gm = sbt(C, es, "gm", [128, KC], F32)
        tk.dma("sp", gq[:], dr["norm_cross"][l].rearrange("(k p) -> p k", p=128), w=[gq], allow_slow_non_contiguous=True)
        tk.dma("sp", gm[:], dr["norm_mem"][l].rearrange("(k p) -> p k", p=128), w=[gm], allow_slow_non_contiguous=True)
        with ExitStack() as es2:
            stg = Rot(nc, es2, "stg", [128, KC, 512], F32, 2)
            for nm, g in (("w_out", None), ("wq_x", gq), ("wk_x", gm), ("wv_x", gm), ("wo_x", None)):
                load_w_sec(C, stg, Ws[nm], 0, dr[nm][l], 1024, g)
            tk.barrier()
        pp = Rot(nc, es, "pp", [128, 512], F32, 6, psum=True)
        ptr = Rot(nc, es, "ptr", [128, 1024], BF16, 2, psum=True)
        memnT = sbt(C, es, "memnT", [128, KC, 256], BF16)
        kTx = sbt(C, es, "kTx", [128, KC, 256], BF16)
        vx = sbt(C, es, "vx", [128, 2, 1024], BF16)
        mts = Rot(nc, es, "mt", [128, 1024], F32, 2)
        mbs = Rot(nc, es, "mb", [128, 1024], BF16, 2)
        sm = Rot(nc, es, "smf", [128, 2], F32, 4)
        hTt = sbt(C, es, "hTt", [128, KC, TS], F32)
        oTt = sbt(C, es, "oTt", [128, KC, TS], BF16)
        sq = sbt(C, es, "sq", [128, KC, TS], BF16)
        hn = sbt(C, es, "hn", [128, KC, TS], BF16)
        rstd = sbt(C, es, "rstd", [128, TS], F32)
        qTx = sbt(C, es, "qTx", [128, KC, TS], BF16)
        oxT = sbt(C, es, "oxT", [128, KC, TS], BF16)
        ees = Rot(nc, es, "ee", [128, TS], BF16, 4)
        rdens = Rot(nc, es, "rden", [128, TS], F32, 2)

        def proj_add(Wt, src):
            for dc in range(KC):
                p = pp.next()
                for fc in range(KC):
                    tk.pe(lambda: nc.tensor.matmul(p[:, :TS], Wt[:, fc, dc * 128:(dc + 1) * 128], src[:, fc, :],
                                                   start=(fc == 0), stop=(fc == KC - 1)), r=[Wt, src], w=[p])
                tk.dve(lambda: nc.vector.tensor_tensor(hTt[:, dc, :], hTt[:, dc, :], p[:, :TS], op=ALU.add), r=[hTt, p], w=[hTt])

        for b in range(NB):
            for mc in range(2):
                mt = mts.next()
                tk.dma("sp", mt[:], dr["mem"][b, mc * 128:(mc + 1) * 128, :], w=[mt])
                mb = mbs.next()
                s1 = sm.next()
                tk.act(lambda: nc.scalar.activation(mb[:], mt[:], AF.Square, accum_out=s1[:, 0:1]), r=[mt], w=[mb, s1])
                tk.act(lambda: nc.scalar.activation(s1[:, 0:1], s1[:, 0:1], AF.Sqrt, scale=1.0 / D, bias=EPS), r=[s1], w=[s1])
                tk.dve(lambda: nc.vector.reciprocal(s1[:, 0:1], s1[:, 0:1]), r=[s1], w=[s1])
                tk.dve(lambda: nc.vector.tensor_scalar(mb[:], mt[:], s1[:, 0:1], None, op0=ALU.mult), r=[mt, s1], w=[mb])
                pt = ptr.next()
                for kc in range(KC):
                    tk.pe(lambda: nc.tensor.transpose(pt[:, kc * 128:(kc + 1) * 128], mb[:, kc * 128:(kc + 1) * 128], identb[:]),
                          r=[mb, identb], w=[pt])
                tk.act(lambda: nc.scalar.copy(memnT[:, :, mc * 128:(mc + 1) * 128], pt[:].rearrange("p (k m) -> p k m", k=KC)),
                       r=[pt], w=[memnT])
            for dc in range(KC):
                p = pp.next()
                for kc in range(KC):
                    tk.pe(lambda: nc.tensor.matmul(p[:, :256], Ws["wk_x"][:, kc, dc * 128:(dc + 1) * 128], memnT[:, kc, :],
                                                   start=(kc == 0), stop=(kc == KC - 1)), r=[Ws["wk_x"], memnT], w=[p])
                tk.act(lambda: nc.scalar.copy(kTx[:, dc, :], p[:, :256]), r=[p], w=[kTx])
            for mc in range(2):
                for half in range(2):
                    p = pp.next()
                    for kc in range(KC):
                        tk.pe(lambda: nc.tensor.matmul(p[:, :512], memnT[:, kc, mc * 128:(mc + 1) * 128],
                                                       Ws["wv_x"][:, kc, half * 512:(half + 1) * 512],
                                                       start=(kc == 0), stop=(kc == KC - 1)), r=[Ws["wv_x"], memnT], w=[p])
                    tk.act(lambda: nc.scalar.copy(vx[:, mc, half * 512:(half + 1) * 512], p[:, :512]), r=[p], w=[vx])
            for st in range(C.NST):
                tsl = slice(st * TS, (st + 1) * TS)
                tk.dma("sp", hTt[:], dr["hT"][b, :, :, tsl].rearrange("k p t -> p k t"), w=[hTt])
                tk.dma("sp", oTt[:], dr["oT"][b, :, :, tsl].rearrange("k p t -> p k t"), w=[oTt])
                proj_add(Ws["w_out"], oTt)
                emit_norm(C, hTt, TS, hn[:], hn, sq, ones_bf, pp.next(), rstd)
                for dc in range(KC):
                    p = pp.next()
                    for kc in range(KC):
                        tk.pe(lambda: nc.tensor.matmul(p[:, :TS], Ws["wq_x"][:, kc, dc * 128:(dc + 1) * 128], hn[:, kc, :],
                                                       start=(kc == 0), stop=(kc == KC - 1)), r=[Ws["wq_x"], hn], w=[p])
                    tk.act(lambda: nc.scalar.copy(qTx[:, dc, :], p[:, :TS]), r=[p], w=[qTx])
                for h in range(4):
                    ee = []
                    for mc in range(2):
                        p = pp.next()
                        for d2 in range(2):
                            tk.pe(lambda: nc.tensor.matmul(p[:, :TS], kTx[:, 2 * h + d2, mc * 128:(mc + 1) * 128], qTx[:, 2 * h + d2, :],
                                                           start=(d2 == 0), stop=(d2 == 1)), r=[kTx, qTx], w=[p])
                        e = ees.next()
                        tk.act(lambda: nc.scalar.activation(e[:], p[:, :TS], AF.Exp, scale=256.0 ** -0.5), r=[p], w=[e])
                        ee.append(e)
                    p = pp.next()
                    for mc in range(2):
                        tk.pe(lambda: nc.tensor.matmul(p[:, :TS], ones_bf[:], ee[mc][:], start=(mc == 0), stop=(mc == 1)),
                              r=[ones_bf, ee[mc]], w=[p])
                    rden = rdens.next()
                    tk.dve(lambda: nc.vector.reciprocal(rden[:], p[:, :TS]), r=[p], w=[rden])
                    for dv2 in range(2):
                        p = pp.next()
                        for mc in range(2):
                            c0 = h * 256 + dv2 * 128
                            tk.pe(lambda: nc.tensor.matmul(p[:, :TS], vx[:, mc, c0:c0 + 128], ee[mc][:], start=(mc == 0), stop=(mc == 1)),
                                  r=[vx, ee[mc]], w=[p])
                        tk.dve(lambda: nc.vector.tensor_tensor(oxT[:, 2 * h + dv2, :], p[:, :TS], rden[:], op=ALU.mult),
                               r=[p, rden], w=[oxT])
                proj_add(Ws["wo_x"], oxT)
                tk.dma("pool", dr["hT"][b, :, :, tsl].rearrange("k p t -> p k t"), hTt[:], r=[hTt])


def phaseF2(C, l):
    nc, tk, dr, S, NB = C.nc, C.tk, C.dr, C.S, C.NB
    T2 = 256
    with ExitStack() as es:
        ones_bf = sbt(C, es, "ones_bf", [128, 128], BF16)
        tk.pool(lambda: nc.gpsimd.memset(ones_bf[:], 1.0), w=[ones_bf])
        Wup = sbt(C, es, "Wup", [128, KC, 2 * DFF], BF16)
        Wdn = sbt(C, es, "Wdn", [128, NFC, 1024], BF16)
        gf = sbt(C, es, "gf", [128, KC], F32)
        tk.dma("sp", gf[:], dr["norm_ffn"][l].rearrange("(k p) -> p k", p=128), w=[gf], allow_slow_non_contiguous=True)
        with ExitStack() as es2:
            stg = Rot(nc, es2, "stg", [128, KC, 512], F32, 2)
            load_w_sec(C, stg, Wup, 0, dr["w_up"][l], 2 * DFF, gf)
            for c in range(NFC):
                st = stg.next()
                tk.dma("sp", st[:, 0:2, :].rearrange("p a n -> p (a n)"), dr["w_down"][l, c * 128:(c + 1) * 128, :], w=[st])
                tk.act(lambda: nc.scalar.copy(Wdn[:, c, :], st[:, 0:2, :].rearrange("p a n -> p (a n)")), r=[st], w=[Wdn])
            tk.barrier()
        cwT = sbt(C, es, "fcw", [128, NFC, 3], F32)
        for j in range(3):
            tk.dma("sp", cwT[:, :, j], dr["ffn_conv_w"][l, j].rearrange("(c p) -> p c", p=128), w=[cwT], allow_slow_non_contiguous=True)
        cbT = sbt(C, es, "fcb", [128, NFC], F32)
        tk.dma("sp", cbT[:], dr["ffn_conv_b"][l].rearrange("(c p) -> p c", p=128), w=[cbT], allow_slow_non_contiguous=True)
        halo = sbt(C, es, "fhalo", [128, NFC, 2], F32)
        pp = Rot(nc, es, "pp", [128, 512], F32, 7, psum=True)
        hTs = Rot(nc, es, "hTf", [128, KC, T2], F32, 2)
        sq = sbt(C, es, "sq", [128, KC, T2], BF16)
        hn = sbt(C, es, "hn", [128, KC, T2], BF16)
        rstd = sbt(C, es, "rstd", [128, T2], F32)
        actT = sbt(C, es, "actT", [128, NFC, T2], BF16)
        gbs = Rot(nc, es, "gb", [128, T2 + 2], F32, 3)
        accs = Rot(nc, es, "acc", [128, T2], F32, 3)
        for b in range(NB):
            tk.pool(lambda: nc.gpsimd.memset(halo[:], 0.0), w=[(halo, c_) for c_ in range(NFC)])
            for ti in range(S // T2):
                tsl = slice(ti * T2, (ti + 1) * T2)
                hTt = hTs.next()
                tk.dma("sp", hTt[:], dr["hT"][b, :, :, tsl].rearrange("k p t -> p k t"), w=[hTt])
                emit_norm(C, hTt, T2, hn[:], hn, sq, ones_bf, pp.next(), rstd)
                for c in range(NFC):
                    p = pp.next()
                    for half in range(2):
                        for kc in range(KC):
                            c0 = half * DFF + c * 128
                            tk.pe(lambda: nc.tensor.matmul(p[:, half * T2:(half + 1) * T2], Wup[:, kc, c0:c0 + 128], hn[:, kc, :],
                                                           start=(kc == 0), stop=(kc == KC - 1)), r=[Wup, hn], w=[p])
                    gb = gbs.next()
                    tk.pool(lambda: nc.gpsimd.tensor_copy(gb[:, 0:2], halo[:, c, :]), r=[(halo, c)], w=[(gb, 0)])
                    tk.act(lambda: nc.scalar.copy(gb[:, 2:T2 + 2], p[:, 0:T2]), r=[p], w=[(gb, 1)])
                    tk.pool(lambda: nc.gpsimd.tensor_copy(halo[:, c, :], gb[:, T2:T2 + 2]), r=[(gb, 1)], w=[(halo, c)])
                    acc = accs.next()
                    tk.dve(lambda: nc.vector.tensor_scalar(acc[:], gb[:, 2:T2 + 2], cwT[:, c, 2:3], cbT[:, c:c + 1],
                                                           op0=ALU.mult, op1=ALU.add), r=[(gb, 1), cwT, cbT], w=[acc])
                    for jj in (1, 0):
                        tk.dve(lambda: nc.vector.scalar_tensor_tensor(acc[:], gb[:, jj:jj + T2], cwT[:, c, jj:jj + 1], acc[:],
                                                                      op0=ALU.mult, op1=ALU.add),
                               r=[(gb, 0), (gb, 1), acc, cwT], w=[acc])
                    tk.act(lambda: nc.scalar.activation(acc[:], acc[:], AF.Silu), r=[acc], w=[acc])
                    tk.dve(lambda: nc.vector.tensor_tensor(actT[:, c, :], acc[:], p[:, T2:2 * T2], op=ALU.mult), r=[acc, p], w=[(actT, c)])
                for dc in range(KC):
                    p = pp.next()
                    for c in range(NFC):
                        tk.pe(lambda: nc.tensor.matmul(p[:, :T2], Wdn[:, c, dc * 128:(dc + 1) * 128], actT[:, c, :],
                                                       start=(c == 0), stop=(c == NFC - 1)), r=[Wdn, (actT, c)], w=[p])
                    tk.dve(lambda: nc.vector.tensor_tensor(hTt[:, dc, :], hTt[:, dc, :], p[:, :T2], op=ALU.add), r=[hTt, p], w=[hTt])
                tk.dma("pool", dr["hT"][b, :, :, tsl].rearrange("k p t -> p k t"), hTt[:], r=[hTt])


def phaseFinal(C):
    nc, tk, dr, S, NB, TS = C.nc, C.tk, C.dr, C.S, C.NB, C.TS
    with ExitStack() as es:
        cs = load_consts(C, es, ["c_ident"])
        identf = cs["c_ident"]
        ones_bf = sbt(C, es, "ones_bf", [128, 128], BF16)
        tk.pool(lambda: nc.gpsimd.memset(ones_bf[:], 1.0), w=[ones_bf])
        gfin = sbt(C, es, "gfin", [128, KC], F32)
        tk.dma("sp", gfin[:], dr["norm_final"].rearrange("(k p) -> p k", p=128), w=[gfin], allow_slow_non_contiguous=True)
        pp = Rot(nc, es, "pp", [128, 512], F32, 6, psum=True)
        hTs = Rot(nc, es, "hTl", [128, KC, TS], F32, 2)
        sq = sbt(C, es, "sq", [128, KC, TS], BF16)
        rstd = sbt(C, es, "rstd", [128, TS], F32)
        yT = sbt(C, es, "yT", [128, KC, TS], F32)
        yos = Rot(nc, es, "yo", [128, D], F32, 2)
        for b in range(NB):
            for st in range(C.NST):
                tsl = slice(st * TS, (st + 1) * TS)
                hTt = hTs.next()
                tk.dma("sp", hTt[:], dr["hT"][b, :, :, tsl].rearrange("k p t -> p k t"), w=[hTt])
                emit_norm(C, hTt, TS, None, None, sq, ones_bf, pp.next(), rstd)
                for kc in range(KC):
                    tk.dve(lambda: nc.vector.scalar_tensor_tensor(yT[:, kc, :], hTt[:, kc, :], gfin[:, kc:kc + 1], rstd[:],
                                                                  op0=ALU.mult, op1=ALU.mult), r=[hTt, gfin, rstd], w=[(yT, kc)])
                for j in range(TS // 128):
                    yo = yos.next()
                    for half in range(2):
                        p = pp.next()
                        for q in range(4):
                            kc = half * 4 + q
                            tk.pe(lambda: nc.tensor.transpose(p[:, q * 128:(q + 1) * 128], yT[:, kc, j * 128:(j + 1) * 128], identf[:]),
                                  r=[(yT, kc), identf], w=[p])
                        if half == 0:
                            tk.act(lambda: nc.scalar.copy(yo[:, 0:512], p[:]), r=[p], w=[(yo, 0)])
                        else:
                            tk.dve(lambda: nc.vector.tensor_copy(yo[:, 512:1024], p[:]), r=[p], w=[(yo, 1)])
                    t0 = st * TS + j * 128
                    tk.dma("pool", dr["y"][b, t0:t0 + 128, :], yo[:], r=[(yo, 0), (yo, 1)])


def kernel(**inputs):
    S, NCORES = 4096, 8
    nc, C = build(S)
    consts = make_consts(S)
    x = np.ascontiguousarray(inputs["x"], dtype=np.float32)
    mem = np.ascontiguousarray(inputs["mem"], dtype=np.float32)
    in_maps = []
    for c in range(NCORES):
        m = {"x": x[2 * c:2 * c + 2], "mem": mem[2 * c:2 * c + 2]}
        for name, _ in PARAMS:
            m[name] = np.ascontiguousarray(inputs[name], dtype=np.float32)
        m.update(consts)
        in_maps.append(m)
    res = run_bass_kernel_spmd(nc, in_maps, core_ids=list(range(NCORES)))
    return np.concatenate([np.asarray(r["y"], dtype=np.float32) for r in res.results], axis=0)
```

```python
import math
import numpy as np
from contextlib import ExitStack
import ml_dtypes
import concourse.bass as bass
import concourse.mybir as mybir
from concourse.bass_utils import run_bass_kernel_spmd

F32 = mybir.dt.float32
BF16 = mybir.dt.bfloat16
U32 = mybir.dt.uint32
ALU = mybir.AluOpType
AF = mybir.ActivationFunctionType
AX = mybir.AxisListType

D = 1024
KC = 8
NMEM = 256
DFF = 2816
NFC = DFF // 128
N_IN = 3368
EPS = 1e-6
NEG_BIG = -3.0e38
NEG_THR = -1.0e38


class Buf:
    __slots__ = ("t", "name", "psum")

    def __init__(self, t, name, psum=False):
        self.t = t
        self.name = name
        self.psum = psum

    def __getitem__(self, k):
        return self.t[k]


class TK:
    LIM = 900
    DLIM = 55
    NSLOT = 8
    ENG = ("pe", "act", "dve", "pool", "sp")
    DQ = ("sp", "pool")

    def __init__(self, nc, es):
        self.nc = nc
        self.es = es
        self.eng = {"pe": nc.tensor, "act": nc.scalar, "dve": nc.vector, "pool": nc.gpsimd, "sp": nc.sync}
        self.nsem = 0
        self.csem = {e: [self._newsem(e) for _ in range(3)] for e in self.ENG}
        self.dsem = {(q, sl): [self._newsem(f"d{q}{sl}") for _ in range(3)] for q in self.DQ for sl in range(self.NSLOT)}
        self.epoch = 0
        self.ninstr = 0
        self.nbar = 0
        self.dnext = {q: 0 for q in self.DQ}
        self._reset_epoch()

    def _reset_epoch(self):
        self.cnt = {e: 0 for e in self.ENG}
        self.dcnt = {k: 0 for k in self.dsem}
        self.seen = {e: {} for e in self.ENG}
        self.lastw = {}
        self.readers = {}
        self.last_d = {}

    def _newsem(self, name):
        self.nsem += 1
        return self.es.enter_context(self.nc.semaphore(f"{name}_{self.nsem}"))

    def _need(self, e, tok):
        if tok is None or tok[2] != self.epoch:
            return
        if tok[0] == "c":
            _, e2, _, idx = tok
            key = e2 if e2 != e else ("self", e)
            if self.seen[e].get(key, -1) >= idx:
                return
            self.seen[e][key] = idx
            self.eng[e].wait_ge(self.csem[e2][self.epoch % 3], idx + 1)
        else:
            _, key, _, val = tok
            if self.seen[e].get(key, -1) >= val:
                return
            self.seen[e][key] = val
            self.eng[e].wait_ge(self.dsem[key][self.epoch % 3], val)

    def _deps(self, e, r, w, is_dma=False):
        def chk(tok):
            if tok is None:
                return
            if tok[0] == "c" and tok[1] == e and not is_dma and e == "pe":
                return
            self._need(e, tok)
        for b in r:
            chk(self.lastw.get(b))
            if getattr(b, "psum", False):
                rd = self.readers.get(b)
                if rd:
                    for t2 in rd.values():
                        if not (t2[0] == "c" and t2[1] == e):
                            chk(t2)
        for b in w:
            chk(self.lastw.get(b))
            rd = self.readers.get(b)
            if rd:
                for t2 in rd.values():
                    chk(t2)

    def _record(self, tok, r, w, rkey):
        for b in w:
            self.lastw[b] = tok
            self.readers[b] = {}
        for b in r:
            self.readers.setdefault(b, {})[rkey] = tok

    def op(self, e, fn, r=(), w=()):
        if self.cnt[e] >= self.LIM:
            self.barrier()
        self._deps(e, r, w)
        idx = self.cnt[e]
        ins = fn()
        ins.then_inc(self.csem[e][self.epoch % 3], 1)
        self.cnt[e] = idx + 1
        tok = ("c", e, self.epoch, idx)
        self._record(tok, r, w, e)
        self.ninstr += 1
        return ins

    def dma(self, q, out, in_, r=(), w=(), **kw):
        q = "sp"
        slot = self.dnext[q] % self.NSLOT
        k = (q, slot)
        if self.dcnt[k] >= self.DLIM:
            self.barrier()
        self.dnext[q] += 1
        self._deps(q, r, w, is_dma=True)
        self._need(q, self.last_d.get(k))
        self.dcnt[k] += 1
        val = 16 * self.dcnt[k]
        self.eng[q].dma_start(out=out, in_=in_, **kw).then_inc(self.dsem[k][self.epoch % 3], 16)
        tok = ("d", k, self.epoch, val)
        self.last_d[k] = tok
        self._record(tok, r, w, k)
        self.ninstr += 1
        return tok

    def barrier(self):
        bank = self.epoch % 3
        for e in self.ENG:
            if self.cnt[e] == 0:
                self.eng[e].sem_inc(self.csem[e][bank], 1)
                self.cnt[e] = 1
        toks = [("c", e, self.epoch, self.cnt[e] - 1) for e in self.ENG] + list(self.last_d.values())
        for e in self.ENG:
            for t in toks:
                self._need(e, t)
        self.epoch += 1
        self.nbar += 1
        nb = (self.epoch + 1) % 3
        for e in self.ENG:
            self.eng[e].sem_clear(self.csem[e][nb])
            if e in self.DQ:
                for sl in range(self.NSLOT):
                    self.eng[e].sem_clear(self.dsem[(e, sl)][nb])
        self._reset_epoch()

    def pe(self, fn, r=(), w=()):
        return self.op("pe", fn, r, w)

    def act(self, fn, r=(), w=()):
        return self.op("act", fn, r, w)

    def dve(self, fn, r=(), w=()):
        return self.op("dve", fn, r, w)

    def pool(self, fn, r=(), w=()):
        return self.op("pool", fn, r, w)


_UN = [0]


def uname(name):
    _UN[0] += 1
    return f"{name}_u{_UN[0]}"


class Rot:
    def __init__(self, nc, es, name, shape, dtype, n, psum=False):
        self.bufs = []
        for i in range(n):
            if psum:
                t = es.enter_context(nc.psum_tensor(uname(f"{name}{i}"), shape, dtype))
            else:
                t = es.enter_context(nc.sbuf_tensor(uname(f"{name}{i}"), shape, dtype))
            self.bufs.append(Buf(t, f"{name}{i}", psum=psum))
        self.i = 0

    def next(self):
        b = self.bufs[self.i % len(self.bufs)]
        self.i += 1
        return b


C_RQ, C_RK, C_RV, C_RG = 0, 128, 256, 512
C_WR, C_WK, C_WV, C_WWL, C_WAL, C_WGL = 768, 1024, 1280, 1536, 1600, 1664
C_SZ, C_SX, C_SB, C_SC, C_SDT = 1792, 2048, 2304, 2560, 2816
C_DQ, C_DK, C_DV, C_DIQ, C_DIK, C_DIW = 2820, 3076, 3140, 3204, 3332, 3364

PARAMS = [
    ("norm_mix", (2, 1024)), ("w_in", (2, 1024, N_IN)), ("rwkv_mu", (2, 1024)), ("rwkv_w0", (2, 256)),
    ("rwkv_w2", (2, 64, 256)), ("rwkv_a0", (2, 256)), ("rwkv_a2", (2, 64, 256)), ("rwkv_g2", (2, 128, 256)),
    ("rwkv_k_k", (2, 256)), ("rwkv_k_a", (2, 256)), ("rwkv_r_k", (2, 4, 64)), ("rwkv_ln_w", (2, 256)),
    ("rwkv_ln_b", (2, 256)), ("ssm_conv_w", (2, 4, 768)), ("ssm_conv_b", (2, 768)), ("ssm_dt_bias", (2, 4)),
    ("ssm_a_log", (2, 4)), ("ssm_d", (2, 4)), ("ssm_norm", (2, 256)), ("idx_k_norm", (2, 32)),
    ("w_out", (2, 1024, 1024)), ("norm_cross", (2, 1024)), ("norm_mem", (2, 1024)), ("wq_x", (2, 1024, 1024)),
    ("wk_x", (2, 1024, 1024)), ("wv_x", (2, 1024, 1024)), ("wo_x", (2, 1024, 1024)), ("norm_ffn", (2, 1024)),
    ("w_up", (2, 1024, 2 * DFF)), ("ffn_conv_w", (2, 3, DFF)), ("ffn_conv_b", (2, DFF)),
    ("w_down", (2, DFF, 1024)), ("norm_final", (1024,)),
]


def make_consts(S):
    c = {}
    c["c_ident"] = np.eye(128, dtype=np.float32)
    i = np.arange(128)
    c["c_U"] = (i[:, None] <= i[None, :]).astype(np.float32)
    c["c_negU"] = np.where(i[None, :] > i[:, None], np.float32(NEG_BIG), np.float32(0)).astype(np.float32)
    pos = np.arange(S, dtype=np.float32)

    def tabs(hd, rows):
        half = hd // 2
        inv = (np.float32(10000.0) ** (-np.arange(half, dtype=np.float32) / np.float32(half))).astype(np.float32)
        ang = (pos[:, None] * inv[None, :]).astype(np.float32)
        cos = np.cos(ang).astype(np.float32)
        sin = np.sin(ang).astype(np.float32)
        cf = np.concatenate([cos, cos], 1)
        sf = np.concatenate([-sin, sin], 1)
        rep = rows // hd
        return np.ascontiguousarray(np.tile(cf, (1, rep)).T), np.ascontiguousarray(np.tile(sf, (1, rep)).T)

    c["c_cos64T"], c["c_sin64T"] = tabs(64, 128)
    c["c_cos32T"], c["c_sin32T"] = tabs(32, 128)
    e2 = np.zeros((128, 64, 128), dtype=np.float32)
    for b in range(2):
        for s in range(64):
            e2[b * 64 + s, s, b * 64:(b + 1) * 64] = 1.0
    c["c_E2"] = e2.reshape(128, 64 * 128).astype(ml_dtypes.bfloat16)
    lg = np.log(1.0 - np.power(2.0, -5.0 - np.arange(4, dtype=np.float32))).astype(np.float32)
    c["c_retla"] = np.tile(lg[None, :], (128, 1)).astype(np.float32)
    return c


CONST_SPECS = lambda S: [("c_ident", (128, 128), F32), ("c_U", (128, 128), F32), ("c_negU", (128, 128), F32),
                         ("c_cos64T", (128, S), F32), ("c_sin64T", (128, S), F32), ("c_cos32T", (128, S), F32),
                         ("c_sin32T", (128, S), F32), ("c_E2", (128, 64 * 128), BF16), ("c_retla", (128, 4), F32)]


class Ctx:
    pass


def build(S, NB=2, L=2, stop_after=None, dbg=()):
    nc = bass.Bass("TRN2", target_bir_lowering=False)
    C = Ctx()
    C.nc, C.S, C.NB, C.L = nc, S, NB, L
    C.TS = 512
    C.NST = S // C.TS
    dr = {}
    dr["x"] = nc.dram_tensor("x", [NB, S, D], F32, kind="ExternalInput").ap()
    dr["mem"] = nc.dram_tensor("mem", [NB, NMEM, D], F32, kind="ExternalInput").ap()
    for name, shp in PARAMS:
        dr[name] = nc.dram_tensor(name, list(shp), F32, kind="ExternalInput").ap()
    for name, shp, dt in CONST_SPECS(S):
        dr[name] = nc.dram_tensor(name, list(shp), dt, kind="ExternalInput").ap()
    dr["y"] = nc.dram_tensor("y", [NB, S, D], F32, kind="ExternalOutput").ap()

    def scratch(name, shape, dt):
        kind = "ExternalOutput" if name in dbg else "Internal"
        dr[name] = nc.dram_tensor(name, list(shape), dt, kind=kind).ap()

    scratch("hT", [NB, KC, 128, S], F32)
    scratch("oT", [NB, KC, 128, S], BF16)
    scratch("r_qT", [NB, 128, S], BF16)
    scratch("r_kT", [NB, 128, S], BF16)
    scratch("r_kTok", [NB, S, 128], BF16)
    scratch("r_v", [NB, S, 256], BF16)
    scratch("r_sg", [NB, S, 256], F32)
    for nm in ("w_whi", "w_wlo", "w_nkk", "w_bb", "w_kp", "w_r"):
        scratch(nm, [NB, S, 256], BF16)
    scratch("w_vT", [NB, 256, S], F32)
    scratch("w_bonus", [NB, S, 256], F32)
    scratch("w_g", [NB, S, 256], F32)
    scratch("s_CT", [NB, 256, S], BF16)
    scratch("s_BT", [NB, 256, S], BF16)
    scratch("s_BTok", [NB, S, 256], BF16)
    scratch("s_xdt", [NB, S, 256], BF16)
    scratch("s_xs", [NB, S, 256], F32)
    scratch("s_sz", [NB, S, 256], F32)
    scratch("s_la", [NB, S, 4], F32)
    scratch("d_qT", [NB, 256, S], BF16)
    scratch("d_kT", [NB, 64, S], BF16)
    scratch("d_v", [NB, S, 65], BF16)
    scratch("d_iqT", [NB, 128, S], BF16)
    scratch("d_ikT", [NB, 32, S], BF16)
    scratch("d_iw", [NB, S, 4], F32)
    C.dr = dr

    import os
    with ExitStack() as es0:
        tk = TK(nc, es0)
        C.tk = tk
        phases = []
        phases.append(("p0", lambda: phase0(C)))
        for l in range(L):
            if os.environ.get("KONEP", "1") == "1":
                phases.append((f"P{l}", lambda l=l: phaseP(C, l)))
            else:
                for sec in ("ret", "ssd", "rw", "dsa"):
                    phases.append((f"P{l}{sec}" if sec != "dsa" else f"P{l}", lambda l=l, sec=sec: phaseP(C, l, only=sec)))
            phases.append((f"REC{l}", lambda l=l: phaseRec(C, l)))
            phases.append((f"RW{l}", lambda l=l: phaseRW(C, l)))
            phases.append((f"DSA{l}", lambda l=l: phaseDSA(C, l)))
            phases.append((f"F1{l}", lambda l=l: phaseF1(C, l)))
            phases.append((f"F2{l}", lambda l=l: phaseF2(C, l)))
        phases.append(("fin", lambda: phaseFinal(C)))
        import os
        skipph = os.environ.get("KPH", "").split(",")
        for name, fn in phases:
            if name in skipph:
                continue
            fn()
            tk.barrier()
            if stop_after == name:
                break
        C.ninstr = tk.ninstr
    return nc, C


def load_consts(C, es, names):
    nc, tk, dr = C.nc, C.tk, C.dr
    out = {}
    for nm in names:
        ap = dr[nm]
        t = Buf(es.enter_context(nc.sbuf_tensor(uname("k_" + nm), list(ap.shape), ap.dtype)), nm)
        tk.dma("sp", t[:], ap[:, :], w=[t])
        out[nm] = t
    return out


def phase0(C):
    nc, tk, dr, S, NB = C.nc, C.tk, C.dr, C.S, C.NB
    with ExitStack() as es:
        cs = load_consts(C, es, ["c_ident"])
        ident = cs["c_ident"]
        xin = Rot(nc, es, "p0x", [128, D], F32, 2)
        hout = Rot(nc, es, "p0h", [128, KC, 128], F32, 2)
        pps = Rot(nc, es, "p0ps", [128, 512], F32, 4, psum=True)
        for b in range(NB):
            for ti in range(S // 128):
                xt = xin.next()
                tk.dma("sp", xt[:], dr["x"][b, ti * 128:(ti + 1) * 128, :], w=[xt])
                ho = hout.next()
                for half in range(2):
                    pp = pps.next()
                    for j in range(4):
                        kc = half * 4 + j
                        tk.pe(lambda: nc.tensor.transpose(pp[:, j * 128:(j + 1) * 128], xt[:, kc * 128:(kc + 1) * 128], ident[:]),
                              r=[xt, ident], w=[pp])
                    dst = ho[:, half * 4:(half + 1) * 4, :]
                    src = pp[:].rearrange("p (a b) -> p a b", a=4)
                    if half == 0:
                        tk.act(lambda: nc.scalar.copy(dst, src), r=[pp], w=[(ho, half)])
                    else:
                        tk.dve(lambda: nc.vector.tensor_copy(dst, src), r=[pp], w=[(ho, half)])
                tk.dma("pool", dr["hT"][b, :, :, ti * 128:(ti + 1) * 128].rearrange("k p t -> p k t"), ho[:],
                       r=[(ho, 0), (ho, 1)])


def emit_norm(C, hT, n, hn_ap, hn_key, sq, ones_bf, pp, rstd):
    nc, tk = C.nc, C.tk
    tk.act(lambda: nc.scalar.activation(sq[:, :, :n], hT[:, :, :n], AF.Square), r=[hT], w=[sq])
    for kc in range(KC):
        tk.pe(lambda: nc.tensor.matmul(pp[:, :n], ones_bf[:], sq[:, kc, :n], start=(kc == 0), stop=(kc == KC - 1)),
              r=[sq, ones_bf], w=[pp])
    tk.act(lambda: nc.scalar.activation(rstd[:, :n], pp[:, :n], AF.Sqrt, scale=1.0 / D, bias=EPS), r=[pp], w=[rstd])
    tk.dve(lambda: nc.vector.reciprocal(rstd[:, :n], rstd[:, :n]), r=[rstd], w=[rstd])
    if hn_ap is not None:
        tk.dve(lambda: nc.vector.tensor_tensor(hn_ap, hT[:, :, :n], rstd[:, :n].unsqueeze(1).to_broadcast([128, KC, n]),
                                               op=ALU.mult), r=[hT, rstd], w=[hn_key])


def load_w_sec(C, stg, dstW, off, src2d, n, gT, cs=None, swap=0, rows=KC):
    nc, tk = C.nc, C.tk
    for c0 in range(0, n, 512):
        m = min(512, n - c0)
        st = stg.next()
        tk.dma("sp", st[:, :rows, :m], src2d[:, c0:c0 + m].rearrange("(kc p) n -> p kc n", p=128), w=[st])
        if gT is not None:
            tk.dve(lambda: nc.vector.tensor_tensor(st[:, :rows, :m], st[:, :rows, :m],
                                                   gT[:, :rows].unsqueeze(2).to_broadcast([128, rows, m]), op=ALU.mult),
                   r=[st, gT], w=[st])
        if cs is not None:
            csb, coff = cs
            tk.dve(lambda: nc.vector.tensor_tensor(st[:, :rows, :m], st[:, :rows, :m],
                                                   csb[:, coff + c0:coff + c0 + m].unsqueeze(1).to_broadcast([128, rows, m]),
                                                   op=ALU.mult), r=[st, csb], w=[st])
        if swap:
            sv = st[:, :rows, :m].rearrange("p k (x two d) -> p k x two d", two=2, d=swap)
            dv = dstW[:, :rows, off + c0:off + c0 + m].rearrange("p k (x two d) -> p k x two d", two=2, d=swap)
            tk.act(lambda: nc.scalar.copy(dv[:, :, :, 0, :], sv[:, :, :, 1, :]), r=[st], w=[dstW])
            tk.act(lambda: nc.scalar.copy(dv[:, :, :, 1, :], sv[:, :, :, 0, :]), r=[st], w=[dstW])
        else:
            tk.act(lambda: nc.scalar.copy(dstW[:, :rows, off + c0:off + c0 + m], st[:, :rows, :m]), r=[st], w=[dstW])


def sbt(C, es, name, shape, dt):
    return Buf(es.enter_context(C.nc.sbuf_tensor(uname(name), list(shape), dt)), name)


def bc_load(C, es, name, src1d, n):
    t = sbt(C, es, name, [128, n], F32)
    C.tk.dma("sp", t[:], src1d.partition_broadcast(128), w=[t])
    return t


P_SECS = [("rq", 128), ("rq_r", 128), ("rk", 128), ("rk_r", 128), ("rv", 256), ("rg", 256),
          ("wrkv_a", 768), ("wrkv_b", 768), ("wl_a", 64), ("wl_b", 64), ("al_a", 64), ("al_b", 64),
          ("gl_a", 128), ("gl_b", 128), ("sz", 256), ("sx", 768), ("sdt", 4),
          ("dq", 256), ("dq_r", 256), ("dk", 64), ("dk_r", 64), ("dv", 64), ("diq", 128), ("diq_r", 128),
          ("dik", 32), ("dik_r", 32), ("diw", 4)]


def phaseP(C, l, only=None):
    nc, tk, dr, S, NB, TS = C.nc, C.tk, C.dr, C.S, C.NB, C.TS
    OFF = {}
    o = 0
    for nm, n in P_SECS:
        OFF[nm] = o
        o += n
    NW = o
    with ExitStack() as es:
        cs = load_consts(C, es, ["c_ident"])
        identf = cs["c_ident"]
        identb = sbt(C, es, "identb", [128, 128], BF16)
        tk.dve(lambda: nc.vector.tensor_copy(identb[:], identf[:]), r=[identf], w=[identb])
        ones_bf = sbt(C, es, "ones_bf", [128, 128], BF16)
        tk.pool(lambda: nc.gpsimd.memset(ones_bf[:], 1.0), w=[ones_bf])
        ones_f = sbt(C, es, "ones_f", [32, 32], F32)
        tk.pool(lambda: nc.gpsimd.memset(ones_f[:], 1.0), w=[ones_f])
        W = sbt(C, es, "Wp", [128, KC, NW], BF16)
        gT = sbt(C, es, "gT", [128, KC], F32)
        tk.dma("sp", gT[:], dr["norm_mix"][l].rearrange("(k p) -> p k", p=128), w=[gT], allow_slow_non_contiguous=True)
        mu_bc = bc_load(C, es, "mu_bc", dr["rwkv_mu"][l], 1024)
        om_bc = sbt(C, es, "om_bc", [128, 1024], F32)
        tk.dve(lambda: nc.vector.tensor_scalar(om_bc[:], mu_bc[:], -1.0, 1.0, op0=ALU.mult, op1=ALU.add), r=[mu_bc], w=[om_bc])
        win = dr["w_in"][l]
        with ExitStack() as es2:
            stg = Rot(nc, es2, "stg", [128, KC, 512], F32, 2)
            LW = lambda nm, c0, n, **kw: load_w_sec(C, stg, W, OFF[nm], win[:, c0:c0 + n], n, gT, **kw)
            LW("rq", C_RQ, 128); LW("rq_r", C_RQ, 128, swap=16); LW("rk", C_RK, 128); LW("rk_r", C_RK, 128, swap=16)
            LW("rv", C_RV, 256); LW("rg", C_RG, 256)
            LW("wrkv_a", C_WR, 768, cs=(om_bc, 0)); LW("wrkv_b", C_WR, 768, cs=(mu_bc, 0))
            LW("wl_a", C_WWL, 64, cs=(om_bc, 768)); LW("wl_b", C_WWL, 64, cs=(mu_bc, 768))
            LW("al_a", C_WAL, 64, cs=(om_bc, 832)); LW("al_b", C_WAL, 64, cs=(mu_bc, 832))
            LW("gl_a", C_WGL, 128, cs=(om_bc, 896)); LW("gl_b", C_WGL, 128, cs=(mu_bc, 896))
            LW("sz", C_SZ, 256); LW("sx", C_SX, 768); LW("sdt", C_SDT, 4)
            LW("dq", C_DQ, 256); LW("dq_r", C_DQ, 256, swap=32); LW("dk", C_DK, 64); LW("dk_r", C_DK, 64, swap=32)
            LW("dv", C_DV, 64); LW("diq", C_DIQ, 128); LW("diq_r", C_DIQ, 128, swap=16)
            LW("dik", C_DIK, 32); LW("dik_r", C_DIK, 32, swap=16); LW("diw", C_DIW, 4)
            tk.barrier()
        w0a0 = sbt(C, es, "w0a0", [128, 512], F32)
        tk.dma("sp", w0a0[:, 0:256], dr["rwkv_w0"][l].partition_broadcast(128), w=[w0a0])
        tk.dma("sp", w0a0[:, 256:512], dr["rwkv_a0"][l].partition_broadcast(128), w=[w0a0])
        kk_bc = bc_load(C, es, "kk_bc", dr["rwkv_k_k"][l], 256)
        ka_bc = bc_load(C, es, "ka_bc", dr["rwkv_k_a"][l], 256)
        rk_bc = bc_load(C, es, "rk_bc", dr["rwkv_r_k"][l].rearrange("h d -> (h d)"), 256)
        dtb_bc = bc_load(C, es, "dtb_bc", dr["ssm_dt_bias"][l], 4)
        alog_bc = bc_load(C, es, "alog_bc", dr["ssm_a_log"][l], 4)
        a_bc = sbt(C, es, "a_bc", [128, 4], F32)
        tk.act(lambda: nc.scalar.activation(a_bc[:], alog_bc[:], AF.Exp), r=[alog_bc], w=[a_bc])
        tk.dve(lambda: nc.vector.tensor_scalar(a_bc[:], a_bc[:], -1.0, None, op0=ALU.mult), r=[a_bc], w=[a_bc])
        w2f = sbt(C, es, "w2f", [128, 768], F32)
        tk.dma("sp", w2f[:64, 0:256], dr["rwkv_w2"][l], w=[w2f])
        tk.dma("sp", w2f[:64, 256:512], dr["rwkv_a2"][l], w=[w2f])
        tk.dma("sp", w2f[:, 512:768], dr["rwkv_g2"][l], w=[w2f])
        w2b = sbt(C, es, "w2b", [128, 768], BF16)
        tk.dve(lambda: nc.vector.tensor_copy(w2b[:64, 0:512], w2f[:64, 0:512]), r=[w2f], w=[w2b])
        tk.dve(lambda: nc.vector.tensor_copy(w2b[:, 512:768], w2f[:, 512:768]), r=[w2f], w=[w2b])
        cwT = sbt(C, es, "cwT", [128, 6, 4], F32)
        for j in range(4):
            tk.dma("sp", cwT[:, :, j], dr["ssm_conv_w"][l, j].rearrange("(c p) -> p c", p=128), w=[cwT],
                   allow_slow_non_contiguous=True)
        cbT = sbt(C, es, "cbT", [128, 6], F32)
        tk.dma("sp", cbT[:], dr["ssm_conv_b"][l].rearrange("(c p) -> p c", p=128), w=[cbT], allow_slow_non_contiguous=True)
        nw = sbt(C, es, "nw", [32, 2], F32)
        ikn_ap = dr["idx_k_norm"][l]
        tk.dma("sp", nw[:, 0:1], ikn_ap.rearrange("(p o) -> p o", o=1), w=[nw], allow_slow_non_contiguous=True)
        tk.dma("sp", nw[0:16, 1:2], ikn_ap[16:32].rearrange("(p o) -> p o", o=1), w=[nw], allow_slow_non_contiguous=True)
        tk.dma("sp", nw[16:32, 1:2], ikn_ap[0:16].rearrange("(p o) -> p o", o=1), w=[nw], allow_slow_non_contiguous=True)

        hTt = sbt(C, es, "hTt", [128, KC, TS], F32)
        sq = sbt(C, es, "sq", [128, KC, TS], BF16)
        hns = [sbt(C, es, f"hn{i}", [128, KC, TS + 1], BF16) for i in range(2)]
        rstd = sbt(C, es, "rstd", [128, TS], F32)
        tabs = {nm: sbt(C, es, "t_" + nm, [128, TS], F32) for nm in ("c_cos64T", "c_sin64T", "c_cos32T", "c_sin32T")}
        pp = Rot(nc, es, "pp", [128, 512], F32, 6, psum=True)
        ptr = Rot(nc, es, "ptr", [128, 1024], BF16, 2, psum=True)
        tA = Rot(nc, es, "tA", [128, 512], F32, 3)
        tB = Rot(nc, es, "tB", [128, 512], F32, 3)
        ob = Rot(nc, es, "ob", [128, 512], BF16, 4)
        of = Rot(nc, es, "of", [128, 512], F32, 3)
        sbj = Rot(nc, es, "sbj", [128, 256], BF16, 12)
        sfj = Rot(nc, es, "sfj", [128, 256], F32, 10)
        sm = Rot(nc, es, "sm", [128, 16], F32, 12)
        cb = Rot(nc, es, "cb", [128, TS + 3], F32, 2)
        halo = sbt(C, es, "halo", [128, 6, 3], F32)
        vout = sbt(C, es, "vout", [128, 4, 65], BF16)
        tk.pool(lambda: nc.gpsimd.memset(vout[:], 1.0), w=[vout])
        twl = sbt(C, es, "twl", [64, TS], BF16)
        alb = sbt(C, es, "alb", [64, TS], BF16)
        sgl = sbt(C, es, "sgl", [128, TS], BF16)
        dtall = sbt(C, es, "dtall", [128, 4, 4], F32)
        xsT = [sbt(C, es, f"xsT{i}", [128, TS], F32) for i in range(2)]
        BTs = [sbt(C, es, f"BTs{i}", [128, TS], BF16) for i in range(2)]
        kTs = sbt(C, es, "kTs", [128, TS], BF16)

        def fm(ps_ap, hn, terms, n=TS):
            nt = len(terms) * KC
            i = 0
            for (off, M, shift) in terms:
                for kc in range(KC):
                    tk.pe(lambda: nc.tensor.matmul(ps_ap, W[:, kc, off:off + M], hn[:, kc, 1 - shift:1 - shift + n],
                                                   start=(i == 0), stop=(i == nt - 1)), r=[W, hn], w=[ps_ap.tensor_key])
                    i += 1

        for b in range(NB):
            tk.pool(lambda: nc.gpsimd.memset(halo[:], 0.0), w=[(halo, c_) for c_ in range(6)])
            for st in range(C.NST):
                t0 = st * TS
                hn = hns[st % 2]
                hprev = hns[(st + 1) % 2]
                tk.dma("sp", hTt[:], dr["hT"][b, :, :, t0:t0 + TS].rearrange("k p t -> p k t"), w=[hTt])
                for nm, t in tabs.items():
                    tk.dma("sp", t[:], dr[nm][:, t0:t0 + TS], w=[t])
                ppn = pp.next()
                emit_norm(C, hTt, TS, hn[:, :, 1:TS + 1], hn, sq, ones_bf, ppn, rstd)
                if st == 0:
                    tk.pool(lambda: nc.gpsimd.memset(hn[:, :, 0:1], 0.0), w=[hn])
                else:
                    tk.pool(lambda: nc.gpsimd.tensor_copy(hn[:, :, 0:1], hprev[:, :, TS:TS + 1]), r=[hprev], w=[hn])
                tsl = slice(t0, t0 + TS)
                cos64, sin64, cos32, sin32 = (tabs[k] for k in ("c_cos64T", "c_sin64T", "c_cos32T", "c_sin32T"))

                def FM(terms, M=128):
                    p = pp.next()
                    ap = p[:M, :]
                    nt = len(terms) * KC
                    i = 0
                    for (off, shift) in terms:
                        for kc in range(KC):
                            tk.pe(lambda: nc.tensor.matmul(ap, W[:, kc, off:off + M], hn[:, kc, 1 - shift:1 - shift + TS],
                                                           start=(i == 0), stop=(i == nt - 1)), r=[W, hn], w=[p])
                            i += 1
                    return p

                def TM(p, c0, j, terms, N):
                    nt = len(terms) * KC
                    i = 0
                    for (off, shift) in terms:
                        for kc in range(KC):
                            a = 1 - shift + j * 128
                            tk.pe(lambda: nc.tensor.matmul(p[:, c0:c0 + N], hn[:, kc, a:a + 128], W[:, kc, off:off + N],
                                                           start=(i == 0), stop=(i == nt - 1)), r=[W, hn], w=[p])
                            i += 1

                def rope_fm(pa, pb, cos, sin, M, scale=None, dt_out=BF16):
                    a_, b_ = tA.next(), tB.next()
                    if scale is None:
                        tk.dve(lambda: nc.vector.tensor_tensor(a_[:M], pa[:M, :], cos[:M], op=ALU.mult), r=[pa, cos], w=[a_])
                        tk.dve(lambda: nc.vector.tensor_tensor(b_[:M], pb[:M, :], sin[:M], op=ALU.mult), r=[pb, sin], w=[b_])
                    else:
                        tk.dve(lambda: nc.vector.scalar_tensor_tensor(a_[:M], pa[:M, :], scale, cos[:M], op0=ALU.mult, op1=ALU.mult),
                               r=[pa, cos], w=[a_])
                        tk.dve(lambda: nc.vector.scalar_tensor_tensor(b_[:M], pb[:M, :], scale, sin[:M], op0=ALU.mult, op1=ALU.mult),
                               r=[pb, sin], w=[b_])
                    o_ = ob.next()
                    tk.pool(lambda: nc.gpsimd.tensor_tensor(o_[:M], a_[:M], b_[:M], op=ALU.add), r=[a_, b_], w=[o_])
                    return o_

                import os
                SK = os.environ.get('KSKIP', '').split(',')
                if only is not None:
                    SK = [x for x in ('ret', 'ssd', 'rw', 'dsa') if x != only]
                def sec_ret():
                    pa = FM([(OFF["rq"], 0)]); pb = FM([(OFF["rq_r"], 0)])
                    o_ = rope_fm(pa, pb, cos32, sin32, 128)
                    tk.dma("pool", dr["r_qT"][b, :, tsl], o_[:], r=[o_])
                    pa = FM([(OFF["rk"], 0)]); pb = FM([(OFF["rk_r"], 0)])
                    o_ = rope_fm(pa, pb, cos32, sin32, 128, scale=32.0 ** -0.5)
                    tk.dma("pool", dr["r_kT"][b, :, tsl], o_[:], r=[o_])
                    pt = ptr.next()
                    for j in range(4):
                        tk.pe(lambda: nc.tensor.transpose(pt[:, j * 128:(j + 1) * 128], o_[:, j * 128:(j + 1) * 128], identb[:]),
                              r=[o_, identb], w=[pt])
                    o2 = ob.next()
                    tk.act(lambda: nc.scalar.copy(o2[:], pt[:, 0:512]), r=[pt], w=[o2])
                    tk.dma("pool", dr["r_kTok"][b, tsl, :].rearrange("(j p) n -> p j n", p=128),
                           o2[:].rearrange("p (j n) -> p j n", j=4), r=[o2])
                    for j in range(4):
                        p = pp.next()
                        TM(p, 0, j, [(OFF["rv"], 0)], 512)
                        vb = sbj.next()
                        tk.act(lambda: nc.scalar.copy(vb[:], p[:, 0:256]), r=[p], w=[vb])
                        jsl = slice(t0 + j * 128, t0 + (j + 1) * 128)
                        tk.dma("pool", dr["r_v"][b, jsl, :], vb[:], r=[vb])
                        sg = sfj.next()
                        tk.act(lambda: nc.scalar.activation(sg[:], p[:, 256:512], AF.Silu), r=[p], w=[sg])
                        tk.dma("pool", dr["r_sg"][b, jsl, :], sg[:], r=[sg])

                if 'ret' not in SK:
                    sec_ret()
                def sec_ssd():
                    for j in range(4):
                        jsl = slice(t0 + j * 128, t0 + (j + 1) * 128)
                        p = pp.next()
                        TM(p, 0, j, [(OFF["sz"], 0)], 256)
                        TM(p, 256, j, [(OFF["sdt"], 0)], 4)
                        sz = sfj.next()
                        tk.act(lambda: nc.scalar.activation(sz[:], p[:, 0:256], AF.Silu), r=[p], w=[sz])
                        tk.dma("pool", dr["s_sz"][b, jsl, :], sz[:], r=[sz])
                        s1 = sm.next()
                        tk.dve(lambda: nc.vector.tensor_tensor(s1[:, 0:4], p[:, 256:260], dtb_bc[:], op=ALU.add), r=[p, dtb_bc], w=[s1])
                        tk.act(lambda: nc.scalar.activation(s1[:, 0:4], s1[:, 0:4], AF.Exp), r=[s1], w=[s1])
                        tk.act(lambda: nc.scalar.activation(dtall[:, j, :], s1[:, 0:4], AF.Ln, bias=1.0), r=[s1], w=[(dtall, j)])
                        s2 = sm.next()
                        tk.dve(lambda: nc.vector.tensor_tensor(s2[:, 0:4], dtall[:, j, :], a_bc[:], op=ALU.mult),
                               r=[(dtall, j), a_bc], w=[s2])
                        tk.dma("pool", dr["s_la"][b, jsl, :], s2[:, 0:4], r=[s2])
                    for c in range(6):
                        p = FM([(OFF["sx"] + c * 128, 0)])
                        cbuf = cb.next()
                        tk.pool(lambda: nc.gpsimd.tensor_copy(cbuf[:, 0:3], halo[:, c, :]), r=[(halo, c)], w=[(cbuf, 0)])
                        tk.act(lambda: nc.scalar.copy(cbuf[:, 3:TS + 3], p[:, :]), r=[p], w=[(cbuf, 1)])
                        tk.pool(lambda: nc.gpsimd.tensor_copy(halo[:, c, :], cbuf[:, TS:TS + 3]), r=[(cbuf, 1)], w=[(halo, c)])
                        acc = tA.next()
                        tk.dve(lambda: nc.vector.tensor_scalar(acc[:], cbuf[:, 3:TS + 3], cwT[:, c, 3:4], cbT[:, c:c + 1],
                                                               op0=ALU.mult, op1=ALU.add), r=[(cbuf, 1), cwT, cbT], w=[acc])
                        for jj in (2, 1, 0):
                            tk.dve(lambda: nc.vector.scalar_tensor_tensor(acc[:], cbuf[:, jj:jj + TS], cwT[:, c, jj:jj + 1], acc[:],
                                                                          op0=ALU.mult, op1=ALU.add),
                                   r=[(cbuf, 0), (cbuf, 1), acc, cwT], w=[acc])
                        if c < 2:
                            tk.act(lambda: nc.scalar.activation(xsT[c][:], acc[:], AF.Silu), r=[acc], w=[xsT[c]])
                        else:
                            o_ = BTs[c - 2] if c < 4 else ob.next()
                            tk.act(lambda: nc.scalar.activation(o_[:], acc[:], AF.Silu), r=[acc], w=[o_])
                            dst = dr["s_BT"] if c < 4 else dr["s_CT"]
                            r0 = (c - 2) % 2 * 128
                            tk.dma("pool", dst[b, r0:r0 + 128, tsl], o_[:], r=[o_])
                    for j in range(4):
                        jsl = slice(t0 + j * 128, t0 + (j + 1) * 128)
                        p = pp.next()
                        for c2 in range(2):
                            tk.pe(lambda: nc.tensor.transpose(p[:, c2 * 128:(c2 + 1) * 128], xsT[c2][:, j * 128:(j + 1) * 128], identf[:]),
                                  r=[xsT[c2], identf], w=[p])
                        xs = sfj.next()
                        tk.act(lambda: nc.scalar.copy(xs[:], p[:, 0:256]), r=[p], w=[xs])
                        tk.dma("pool", dr["s_xs"][b, jsl, :], xs[:], r=[xs])
                        xd = sbj.next()
                        tk.dve(lambda: nc.vector.tensor_tensor(xd[:].rearrange("p (h d) -> p h d", h=4),
                                                               xs[:].rearrange("p (h d) -> p h d", h=4),
                                                               dtall[:, j, :].unsqueeze(2).to_broadcast([128, 4, 64]), op=ALU.mult),
                               r=[xs, (dtall, j)], w=[xd])
                        tk.dma("pool", dr["s_xdt"][b, jsl, :], xd[:], r=[xd])
                        pt = ptr.next()
                        for g2 in range(2):
                            tk.pe(lambda: nc.tensor.transpose(pt[:, g2 * 128:(g2 + 1) * 128], BTs[g2][:, j * 128:(j + 1) * 128], identb[:]),
                                  r=[BTs[g2], identb], w=[pt])
                        bt = sbj.next()
                        tk.act(lambda: nc.scalar.copy(bt[:], pt[:, 0:256]), r=[pt], w=[bt])
                        tk.dma("pool", dr["s_BTok"][b, jsl, :], bt[:], r=[bt])

                if 'ssd' not in SK:
                    sec_ssd()
                def sec_rw():
                    p = FM([(OFF["wl_a"], 0), (OFF["wl_b"], 1)], M=64)
                    tk.act(lambda: nc.scalar.activation(twl[:], p[:64, :], AF.Tanh), r=[p], w=[twl])
                    p = FM([(OFF["al_a"], 0), (OFF["al_b"], 1)], M=64)
                    tk.act(lambda: nc.scalar.copy(alb[:], p[:64, :]), r=[p], w=[alb])
                    p = FM([(OFF["gl_a"], 0), (OFF["gl_b"], 1)])
                    tk.act(lambda: nc.scalar.activation(sgl[:], p[:, :], AF.Sigmoid), r=[p], w=[sgl])
                    for c2 in range(2):
                        p = FM([(OFF["wrkv_a"] + 512 + c2 * 128, 0), (OFF["wrkv_b"] + 512 + c2 * 128, 1)])
                        o_ = of.next()
                        tk.act(lambda: nc.scalar.copy(o_[:], p[:, :]), r=[p], w=[o_])
                        tk.dma("pool", dr["w_vT"][b, c2 * 128:(c2 + 1) * 128, tsl], o_[:], r=[o_])
                    for j in range(4):
                        jsl = slice(t0 + j * 128, t0 + (j + 1) * 128)
                        js = slice(j * 128, (j + 1) * 128)
                        p1 = pp.next()
                        TM(p1, 0, j, [(OFF["wrkv_a"], 0), (OFF["wrkv_b"], 1)], 512)
                        p2 = pp.next()
                        TM(p2, 0, j, [(OFF["wrkv_a"] + 512, 0), (OFF["wrkv_b"] + 512, 1)], 256)
                        tk.pe(lambda: nc.tensor.matmul(p2[:, 256:512], sgl[:, js], w2b[:, 512:768], start=True, stop=True),
                              r=[sgl, w2b], w=[p2])
                        p3 = pp.next()
                        tk.pe(lambda: nc.tensor.matmul(p3[:, 0:256], twl[:, js], w2b[:64, 0:256], start=True, stop=True),
                              r=[twl, w2b], w=[p3])
                        tk.pe(lambda: nc.tensor.matmul(p3[:, 256:512], alb[:, js], w2b[:64, 256:512], start=True, stop=True),
                              r=[alb, w2b], w=[p3])
                        wa = tA.next()
                        tk.dve(lambda: nc.vector.tensor_tensor(wa[:], p3[:], w0a0[:], op=ALU.add), r=[p3, w0a0], w=[wa])
                        tk.act(lambda: nc.scalar.activation(wa[:], wa[:], AF.Sigmoid), r=[wa], w=[wa])
                        a_ = wa[:, 256:512]
                        wf = sfj.next()
                        tk.act(lambda: nc.scalar.activation(wf[:], wa[:, 0:256], AF.Exp, scale=-math.exp(-0.5)), r=[wa], w=[wf])
                        whi = sbj.next()
                        tk.pool(lambda: nc.gpsimd.tensor_copy(whi[:], wf[:]), r=[wf], w=[whi])
                        wlo = sbj.next()
                        tk.dve(lambda: nc.vector.tensor_tensor(wlo[:], wf[:], whi[:], op=ALU.subtract), r=[wf, whi], w=[wlo])
                        tk.dma("pool", dr["w_whi"][b, jsl, :], whi[:], r=[whi])
                        tk.dma("pool", dr["w_wlo"][b, jsl, :], wlo[:], r=[wlo])
                        kkf = sfj.next()
                        tk.dve(lambda: nc.vector.tensor_tensor(kkf[:], p1[:, 256:512], kk_bc[:], op=ALU.mult), r=[p1, kk_bc], w=[kkf])
                        sqk = sfj.next()
                        tk.pool(lambda: nc.gpsimd.tensor_tensor(sqk[:], kkf[:], kkf[:], op=ALU.mult), r=[kkf], w=[sqk])
                        s1 = sm.next()
                        tk.dve(lambda: nc.vector.tensor_reduce(s1[:, 0:4], sqk[:].rearrange("p (h d) -> p h d", h=4), axis=AX.X, op=ALU.add),
                               r=[sqk], w=[s1])
                        tk.act(lambda: nc.scalar.activation(s1[:, 0:4], s1[:, 0:4], AF.Sqrt, bias=1e-12), r=[s1], w=[s1])
                        tk.dve(lambda: nc.vector.reciprocal(s1[:, 0:4], s1[:, 0:4]), r=[s1], w=[s1])
                        kkn = sfj.next()
                        tk.dve(lambda: nc.vector.tensor_tensor(kkn[:].rearrange("p (h d) -> p h d", h=4),
                                                               kkf[:].rearrange("p (h d) -> p h d", h=4),
                                                               s1[:, 0:4].unsqueeze(2).to_broadcast([128, 4, 64]), op=ALU.mult),
                               r=[kkf, s1], w=[kkn])
                        nkk = sbj.next()
                        tk.pool(lambda: nc.gpsimd.tensor_scalar(nkk[:], kkn[:], -1.0, None, op0=ALU.mult), r=[kkn], w=[nkk])
                        tk.dma("pool", dr["w_nkk"][b, jsl, :], nkk[:], r=[nkk])
                        bb = sbj.next()
                        tk.pool(lambda: nc.gpsimd.tensor_tensor(bb[:], kkn[:], a_, op=ALU.mult), r=[kkn, wa], w=[bb])
                        tk.dma("pool", dr["w_bb"][b, jsl, :], bb[:], r=[bb])
                        t1 = sfj.next()
                        tk.dve(lambda: nc.vector.scalar_tensor_tensor(t1[:], a_, -1.0, ka_bc[:], op0=ALU.add, op1=ALU.mult),
                               r=[wa, ka_bc], w=[t1])
                        kp = sfj.next()
                        tk.dve(lambda: nc.vector.scalar_tensor_tensor(kp[:], t1[:], 1.0, p1[:, 256:512], op0=ALU.add, op1=ALU.mult),
                               r=[t1, p1], w=[kp])
                        kpb = sbj.next()
                        tk.pool(lambda: nc.gpsimd.tensor_copy(kpb[:], kp[:]), r=[kp], w=[kpb])
                        tk.dma("pool", dr["w_kp"][b, jsl, :], kpb[:], r=[kpb])
                        rb = sbj.next()
                        tk.act(lambda: nc.scalar.copy(rb[:], p1[:, 0:256]), r=[p1], w=[rb])
                        tk.dma("pool", dr["w_r"][b, jsl, :], rb[:], r=[rb])
                        t2 = sfj.next()
                        tk.dve(lambda: nc.vector.tensor_tensor(t2[:], p1[:, 0:256], kp[:], op=ALU.mult), r=[p1, kp], w=[t2])
                        tk.pool(lambda: nc.gpsimd.tensor_tensor(t2[:], t2[:], rk_bc[:], op=ALU.mult), r=[t2, rk_bc], w=[t2])
                        s2 = sm.next()
                        tk.dve(lambda: nc.vector.tensor_reduce(s2[:, 0:4], t2[:].rearrange("p (h d) -> p h d", h=4), axis=AX.X, op=ALU.add),
                               r=[t2], w=[s2])
                        bo = sfj.next()
                        tk.dve(lambda: nc.vector.tensor_tensor(bo[:].rearrange("p (h d) -> p h d", h=4),
                                                               p2[:, 0:256].rearrange("p (h d) -> p h d", h=4),
                                                               s2[:, 0:4].unsqueeze(2).to_broadcast([128, 4, 64]), op=ALU.mult),
                               r=[p2, s2], w=[bo])
                        tk.dma("pool", dr["w_bonus"][b, jsl, :], bo[:], r=[bo])
                        go = sfj.next()
                        tk.act(lambda: nc.scalar.copy(go[:], p2[:, 256:512]), r=[p2], w=[go])
                        tk.dma("pool", dr["w_g"][b, jsl, :], go[:], r=[go])

                if 'rw' not in SK:
                    sec_rw()
                def sec_dsa():
                    for c2 in range(2):
                        pa = FM([(OFF["dq"] + c2 * 128, 0)]); pb = FM([(OFF["dq_r"] + c2 * 128, 0)])
                        o_ = rope_fm(pa, pb, cos64, sin64, 128)
                        tk.dma("pool", dr["d_qT"][b, c2 * 128:(c2 + 1) * 128, tsl], o_[:], r=[o_])
                    pa = FM([(OFF["dk"], 0)], M=64); pb = FM([(OFF["dk_r"], 0)], M=64)
                    o_ = rope_fm(pa, pb, cos64, sin64, 64)
                    tk.dma("pool", dr["d_kT"][b, :, tsl], o_[:64], r=[o_])
                    pa = FM([(OFF["diq"], 0)]); pb = FM([(OFF["diq_r"], 0)])
                    o_ = rope_fm(pa, pb, cos32, sin32, 128)
                    tk.dma("pool", dr["d_iqT"][b, :, tsl], o_[:], r=[o_])
                    pa = FM([(OFF["dik"], 0)], M=32); pb = FM([(OFF["dik_r"], 0)], M=32)
                    sqi = tA.next()
                    tk.act(lambda: nc.scalar.activation(sqi[:32], pa[:32, :], AF.Square), r=[pa], w=[sqi])
                    p3 = pp.next()
                    tk.pe(lambda: nc.tensor.matmul(p3[:32, :], ones_f[:], sqi[:32], start=True, stop=True), r=[sqi, ones_f], w=[p3])
                    rs = tB.next()
                    tk.act(lambda: nc.scalar.activation(rs[:32], p3[:32, :], AF.Sqrt, scale=1.0 / 32, bias=EPS), r=[p3], w=[rs])
                    tk.dve(lambda: nc.vector.reciprocal(rs[:32], rs[:32]), r=[rs], w=[rs])
                    ia = tA.next(); ib = tB.next()
                    tk.dve(lambda: nc.vector.scalar_tensor_tensor(ia[:32], pa[:32, :], nw[:, 0:1], rs[:32], op0=ALU.mult, op1=ALU.mult),
                           r=[pa, nw, rs], w=[ia])
                    tk.dve(lambda: nc.vector.scalar_tensor_tensor(ib[:32], pb[:32, :], nw[:, 1:2], rs[:32], op0=ALU.mult, op1=ALU.mult),
                           r=[pb, nw, rs], w=[ib])
                    tk.dve(lambda: nc.vector.tensor_tensor(ia[:32], ia[:32], cos32[:32], op=ALU.mult), r=[ia, cos32], w=[ia])
                    tk.dve(lambda: nc.vector.tensor_tensor(ib[:32], ib[:32], sin32[:32], op=ALU.mult), r=[ib, sin32], w=[ib])
                    o_ = ob.next()
                    tk.pool(lambda: nc.gpsimd.tensor_tensor(o_[:32], ia[:32], ib[:32], op=ALU.add), r=[ia, ib], w=[o_])
                    tk.dma("pool", dr["d_ikT"][b, :, tsl], o_[:32], r=[o_])
                    p = pp.next()
                    for j in range(4):
                        TM(p, j * 64, j, [(OFF["dv"], 0)], 64)
                        TM(p, 256 + j * 4, j, [(OFF["diw"], 0)], 4)
                    tk.act(lambda: nc.scalar.copy(vout[:, :, 0:64], p[:, 0:256].rearrange("p (j d) -> p j d", j=4)), r=[p], w=[vout])
                    tk.dma("pool", dr["d_v"][b, tsl, :].rearrange("(j p) n -> p j n", p=128), vout[:], r=[vout])
                    s1 = sm.next()
                    tk.act(lambda: nc.scalar.mul(s1[:, 0:16], p[:, 256:272], (4.0 ** -0.5) * (32.0 ** -0.5)), r=[p], w=[s1])
                    tk.dma("pool", dr["d_iw"][b, tsl, :].rearrange("(j p) n -> p j n", p=128),
                           s1[:, 0:16].rearrange("p (j n) -> p j n", j=4), r=[s1])
                if 'dsa' not in SK:
                    sec_dsa()


def phaseRec(C, l):
    nc, tk, dr, S, NB = C.nc, C.tk, C.dr, C.S, C.NB
    NCH = S // 128
    with ExitStack() as es:
        cs = load_consts(C, es, ["c_ident", "c_U", "c_retla"])
        identf, U, retla = cs["c_ident"], cs["c_U"], cs["c_retla"]
        identb = sbt(C, es, "identb", [128, 128], BF16)
        tk.dve(lambda: nc.vector.tensor_copy(identb[:], identf[:]), r=[identf], w=[identb])
        ones_f = sbt(C, es, "ones_f", [128, 128], F32)
        tk.pool(lambda: nc.gpsimd.memset(ones_f[:], 1.0), w=[ones_f])
        dsk_bc = bc_load(C, es, "dsk_bc", dr["ssm_d"][l], 4)
        nrm_bc = bc_load(C, es, "nrm_bc", dr["ssm_norm"][l], 256)
        psm = Rot(nc, es, "psm", [128, 512], F32, 1, psum=True)
        pBc = Rot(nc, es, "pBc", [128, 512], F32, 1, psum=True)
        psc = Rot(nc, es, "psc", [128, 512], F32, 2, psum=True)
        pY = Rot(nc, es, "pY", [128, 512], F32, 1, psum=True)
        pdS = Rot(nc, es, "pdS", [128, 512], F32, 1, psum=True)
        ptr = Rot(nc, es, "ptr", [128, 1024], BF16, 1, psum=True)
        decTs = Rot(nc, es, "decT", [128, 512], F32, 2)
        Es = Rot(nc, es, "E", [128, 512], F32, 2)
        args = Rot(nc, es, "arg", [128, 512], F32, 2)
        smalls = Rot(nc, es, "small", [128, 16], F32, 4)
        s2s = Rot(nc, es, "s2s", [128, 16], F32, 4)
        qTs = Rot(nc, es, "qT", [128, 512], BF16, 2)
        kTs = Rot(nc, es, "kT", [128, 512], BF16, 2)
        kToks = Rot(nc, es, "kTok", [128, 256], BF16, 2)
        vs = Rot(nc, es, "v", [128, 256], BF16, 2)
        las = Rot(nc, es, "la", [128, 4], F32, 2)
        f1s = Rot(nc, es, "f1", [128, 256], F32, 2)
        f2s = Rot(nc, es, "f2", [128, 256], F32, 2)
        PTs = Rot(nc, es, "PT", [128, 512], BF16, 2)
        qtils = Rot(nc, es, "qtil", [128, 512], BF16, 2)
        xts = Rot(nc, es, "xt", [128, 256], BF16, 2)
        tmps = Rot(nc, es, "tmp", [128, 256], F32, 4)
        obs = Rot(nc, es, "ob", [128, 256], BF16, 2)
        oTs = Rot(nc, es, "oTt", [128, 256], BF16, 2)
        S32 = sbt(C, es, "S32", [128, 256], F32)
        Sbf = sbt(C, es, "Sbf", [128, 256], BF16)

        def prep(la):
            pm = psm.next()
            tk.pe(lambda: nc.tensor.matmul(pm[:, 0:4], U[:], la[:, 0:4], start=True, stop=True), r=[U, la], w=[pm])
            tk.pe(lambda: nc.tensor.matmul(pm[:, 4:8], ones_f[:], la[:, 0:4], start=True, stop=True), r=[ones_f, la], w=[pm])
            sm = smalls.next()
            tk.act(lambda: nc.scalar.copy(sm[:, 8:12], pm[:, 0:4]), r=[pm], w=[sm])
            tk.dve(lambda: nc.vector.tensor_tensor(sm[:, 0:4], pm[:, 4:8], sm[:, 8:12], op=ALU.subtract), r=[pm, sm], w=[sm])
            tk.act(lambda: nc.scalar.activation(sm[:, 0:4], sm[:, 0:4], AF.Exp), r=[sm], w=[sm])
            tk.act(lambda: nc.scalar.activation(sm[:, 4:8], pm[:, 4:8], AF.Exp), r=[pm, sm], w=[sm])
            pb = pBc.next()
            for h in range(4):
                tk.pe(lambda: nc.tensor.matmul(pb[:, h * 128:(h + 1) * 128], la[:, h:h + 1].to_broadcast([128, 128]), U[:],
                                               start=True, stop=True), r=[la, U], w=[pb])
            arg = args.next()
            for h in range(4):
                tk.dve(lambda: nc.vector.tensor_scalar(arg[:, h * 128:(h + 1) * 128], pb[:, h * 128:(h + 1) * 128],
                                                       sm[:, 8 + h:9 + h], 0.0, op0=ALU.subtract, op1=ALU.min),
                       r=[pb, sm], w=[arg])
            decT = decTs.next()
            tk.act(lambda: nc.scalar.activation(decT[:], arg[:], AF.Exp), r=[arg], w=[decT])
            tk.dve(lambda: nc.vector.tensor_tensor(decT[:].rearrange("p (h t) -> p h t", h=4),
                                                   decT[:].rearrange("p (h t) -> p h t", h=4),
                                                   U[:].unsqueeze(1).to_broadcast([128, 4, 128]), op=ALU.mult),
                   r=[decT, U], w=[decT])
            E = Es.next()
            tk.act(lambda: nc.scalar.activation(E[:], pb[:], AF.Exp), r=[pb], w=[E])
            return decT, E, sm

        for mix in ("ret", "ssd"):
            N = 32 if mix == "ret" else 128
            if mix == "ret":
                dec_const = prep(retla)
            for b in range(NB):
                tk.pool(lambda: nc.gpsimd.memset(S32[:], 0.0), w=[S32])
                tk.pool(lambda: nc.gpsimd.memset(Sbf[:], 0.0), w=[Sbf])
                for c in range(NCH):
                    tsl = slice(c * 128, (c + 1) * 128)
                    qT, kT, kTok, v = qTs.next(), kTs.next(), kToks.next(), vs.next()
                    f1, f2 = f1s.next(), f2s.next()
                    if mix == "ret":
                        tk.dma("sp", qT[:32, :].rearrange("n (h t) -> n h t", h=4),
                               dr["r_qT"][b, :, tsl].rearrange("(h n) t -> n h t", h=4), w=[qT])
                        tk.dma("sp", kT[:32, :].rearrange("n (h t) -> n h t", h=4),
                               dr["r_kT"][b, :, tsl].rearrange("(h n) t -> n h t", h=4), w=[kT])
                        tk.dma("sp", kTok[:, 0:128], dr["r_kTok"][b, tsl, :], w=[kTok])
                        tk.dma("sp", v[:], dr["r_v"][b, tsl, :], w=[v])
                        tk.dma("sp", f1[:], dr["r_sg"][b, tsl, :], w=[f1])
                        decT, E, sm = dec_const
                    else:
                        tk.dma("sp", qT[:, 0:256].rearrange("n (g t) -> n g t", g=2),
                               dr["s_CT"][b, :, tsl].rearrange("(g n) t -> n g t", g=2), w=[qT])
                        tk.dma("sp", kT[:, 0:256].rearrange("n (g t) -> n g t", g=2),
                               dr["s_BT"][b, :, tsl].rearrange("(g n) t -> n g t", g=2), w=[kT])
                        tk.dma("sp", kTok[:], dr["s_BTok"][b, tsl, :], w=[kTok])
                        tk.dma("sp", v[:], dr["s_xdt"][b, tsl, :], w=[v])
                        tk.dma("sp", f1[:], dr["s_sz"][b, tsl, :], w=[f1])
                        tk.dma("sp", f2[:], dr["s_xs"][b, tsl, :], w=[f2])
                        la = las.next()
                        tk.dma("sp", la[:], dr["s_la"][b, tsl, :], w=[la])
                        decT, E, sm = prep(la)
                    sc = psc.next()
                    PT = PTs.next()
                    qtil = qtils.next()
                    if mix == "ret":
                        for h in range(4):
                            hs = slice(h * 128, (h + 1) * 128)
                            tk.pe(lambda: nc.tensor.matmul(sc[:, hs], kT[:32, hs], qT[:32, hs], start=True, stop=True),
                                  r=[kT, qT], w=[sc])
                        tk.dve(lambda: nc.vector.tensor_tensor(PT[:], sc[:], decT[:], op=ALU.mult), r=[sc, decT], w=[PT])
                        tk.dve(lambda: nc.vector.tensor_tensor(qtil[:32, :], qT[:32, :], E[:32, :], op=ALU.mult), r=[qT, E], w=[qtil])
                    else:
                        for g in range(2):
                            gs = slice(g * 128, (g + 1) * 128)
                            tk.pe(lambda: nc.tensor.matmul(sc[:, gs], kT[:, gs], qT[:, gs], start=True, stop=True),
                                  r=[kT, qT], w=[sc])
                        v4 = lambda ap: ap.rearrange("p (g e t) -> p g e t", g=2, e=2)
                        bcg = lambda ap: ap.rearrange("p (g t) -> p g t", g=2).unsqueeze(2).to_broadcast([128, 2, 2, 128])
                        tk.dve(lambda: nc.vector.tensor_tensor(v4(PT[:]), v4(decT[:]), bcg(sc[:, 0:256]), op=ALU.mult),
                               r=[sc, decT], w=[PT])
                        tk.dve(lambda: nc.vector.tensor_tensor(v4(qtil[:]), v4(E[:]), bcg(qT[:, 0:256]), op=ALU.mult),
                               r=[qT, E], w=[qtil])
                    py = pY.next()
                    for h in range(4):
                        hs = slice(h * 128, (h + 1) * 128)
                        ps_ = slice(h * 64, (h + 1) * 64)
                        tk.pe(lambda: nc.tensor.matmul(py[:, ps_], PT[:, hs], v[:, ps_], start=True, stop=False), r=[PT, v], w=[py])
                        tk.pe(lambda: nc.tensor.matmul(py[:, ps_], qtil[:N, hs], Sbf[:N, ps_], start=False, stop=True),
                              r=[qtil, Sbf], w=[py])
                    xt = xts.next()
                    tk.dve(lambda: nc.vector.tensor_tensor(xt[:].rearrange("p (h d) -> p h d", h=4),
                                                           v[:].rearrange("p (h d) -> p h d", h=4),
                                                           sm[:, 0:4].unsqueeze(2).to_broadcast([128, 4, 64]), op=ALU.mult),
                           r=[v, sm], w=[xt])
                    pd = pdS.next()
                    if mix == "ret":
                        for h in range(4):
                            ps_ = slice(h * 64, (h + 1) * 64)
                            tk.pe(lambda: nc.tensor.matmul(pd[:32, ps_], kTok[:, h * 32:(h + 1) * 32], xt[:, ps_], start=True, stop=True),
                                  r=[kTok, xt], w=[pd])
                    else:
                        for g in range(2):
                            gs = slice(g * 128, (g + 1) * 128)
                            tk.pe(lambda: nc.tensor.matmul(pd[:, gs], kTok[:, gs], xt[:, gs], start=True, stop=True),
                                  r=[kTok, xt], w=[pd])
                    for h in range(4):
                        ps_ = slice(h * 64, (h + 1) * 64)
                        tk.dve(lambda: nc.vector.scalar_tensor_tensor(S32[:N, ps_], S32[:N, ps_], sm[:N, 4 + h:5 + h], pd[:N, ps_],
                                                                      op0=ALU.mult, op1=ALU.add), r=[S32, sm, pd, Sbf], w=[S32])
                    tk.act(lambda: nc.scalar.copy(Sbf[:N, :], S32[:N, :]), r=[S32], w=[Sbf])
                    ob = obs.next()
                    s2 = s2s.next()
                    if mix == "ret":
                        t1 = tmps.next()
                        tk.act(lambda: nc.scalar.activation(t1[:], py[:, 0:256], AF.Square), r=[py], w=[t1])
                        tk.dve(lambda: nc.vector.tensor_reduce(s2[:, 0:4], t1[:].rearrange("p (h d) -> p h d", h=4), axis=AX.X, op=ALU.add),
                               r=[t1], w=[s2])
                        tk.act(lambda: nc.scalar.activation(s2[:, 0:4], s2[:, 0:4], AF.Sqrt, scale=1.0 / 64, bias=EPS), r=[s2], w=[s2])
                        tk.dve(lambda: nc.vector.reciprocal(s2[:, 0:4], s2[:, 0:4]), r=[s2], w=[s2])
                        t2 = tmps.next()
                        tk.dve(lambda: nc.vector.tensor_tensor(t2[:].rearrange("p (h d) -> p h d", h=4),
                                                               py[:, 0:256].rearrange("p (h d) -> p h d", h=4),
                                                               s2[:, 0:4].unsqueeze(2).to_broadcast([128, 4, 64]), op=ALU.mult),
                               r=[py, s2], w=[t2])
                        tk.pool(lambda: nc.gpsimd.tensor_tensor(ob[:], t2[:], f1[:], op=ALU.mult), r=[t2, f1], w=[ob])
                        ch0 = 0
                    else:
                        t1 = tmps.next()
                        tk.pool(lambda: nc.gpsimd.tensor_tensor(t1[:].rearrange("p (h d) -> p h d", h=4),
                                                                f2[:].rearrange("p (h d) -> p h d", h=4),
                                                                dsk_bc[:].unsqueeze(2).to_broadcast([128, 4, 64]), op=ALU.mult),
                                r=[f2, dsk_bc], w=[t1])
                        tk.dve(lambda: nc.vector.tensor_tensor(t1[:], t1[:], py[:, 0:256], op=ALU.add), r=[t1, py], w=[t1])
                        tk.pool(lambda: nc.gpsimd.tensor_tensor(t1[:], t1[:], f1[:], op=ALU.mult), r=[t1, f1], w=[t1])
                        t2 = tmps.next()
                        tk.act(lambda: nc.scalar.activation(t2[:], t1[:], AF.Square, accum_out=s2[:, 0:1]), r=[t1], w=[t2, s2])
                        tk.act(lambda: nc.scalar.activation(s2[:, 0:1], s2[:, 0:1], AF.Sqrt, scale=1.0 / 256, bias=EPS), r=[s2], w=[s2])
                        tk.dve(lambda: nc.vector.reciprocal(s2[:, 0:1], s2[:, 0:1]), r=[s2], w=[s2])
                        tk.dve(lambda: nc.vector.scalar_tensor_tensor(ob[:], t1[:], s2[:, 0:1], nrm_bc[:], op0=ALU.mult, op1=ALU.mult),
                               r=[t1, s2, nrm_bc], w=[ob])
                        ch0 = 4
                    pt = ptr.next()
                    for c2 in range(2):
                        tk.pe(lambda: nc.tensor.transpose(pt[:, c2 * 128:(c2 + 1) * 128], ob[:, c2 * 128:(c2 + 1) * 128], identb[:]),
                              r=[ob, identb], w=[pt])
                    oTt = oTs.next()
                    tk.act(lambda: nc.scalar.copy(oTt[:], pt[:, 0:256]), r=[pt], w=[oTt])
                    tk.dma("pool", dr["oT"][b, ch0:ch0 + 2, :, tsl].rearrange("k p t -> p k t"),
                           oTt[:].rearrange("p (k t) -> p k t", k=2), r=[oTt])


def phaseRW(C, l):
    nc, tk, dr, S, NB = C.nc, C.tk, C.dr, C.S, C.NB
    assert NB == 2
    NCH = S // 64
    with ExitStack() as es:
        cs = load_consts(C, es, ["c_ident", "c_E2"])
        identf, E2 = cs["c_ident"], cs["c_E2"]
        identb = sbt(C, es, "identb", [128, 128], BF16)
        tk.dve(lambda: nc.vector.tensor_copy(identb[:], identf[:]), r=[identf], w=[identb])
        lnw_bc = bc_load(C, es, "lnw_bc", dr["rwkv_ln_w"][l], 256)
        lnb_bc = bc_load(C, es, "lnb_bc", dr["rwkv_ln_b"][l], 256)
        pA = Rot(nc, es, "pA", [128, 512], F32, 2, psum=True)
        pB = Rot(nc, es, "pB", [128, 512], F32, 2, psum=True)
        pC = Rot(nc, es, "pC", [128, 512], F32, 2, psum=True)
        pT = Rot(nc, es, "pT", [128, 512], F32, 1, psum=True)
        pO = Rot(nc, es, "pO", [128, 1024], BF16, 1, psum=True)
        names = ("w_whi", "w_wlo", "w_nkk", "w_bb", "w_kp", "w_r")
        tl = {nm: Rot(nc, es, "c_" + nm, [128, 256], BF16, 2) for nm in names}
        vTs = Rot(nc, es, "vTc", [128, 256], F32, 2)
        ychs = Rot(nc, es, "ych", [128, 256], F32, 2)
        kvs = Rot(nc, es, "kv", [128, 256], F32, 3)
        tmpa = Rot(nc, es, "tmpa", [128, 256], F32, 4)
        sas = Rot(nc, es, "sa", [128, 4], F32, 3)
        Sb = [sbt(C, es, f"Sst{i}", [128, 256], F32) for i in range(2)]
        for S_ in Sb:
            tk.pool(lambda: nc.gpsimd.memset(S_[:], 0.0), w=[S_])
        rsbs = Rot(nc, es, "rsb", [128, 256], F32, 3)
        tmpp = Rot(nc, es, "tmpp", [128, 256], F32, 3)
        gstep = 0
        pending = None
        yjunk = sbt(C, es, "yjunk", [128, 256], F32)

        def flush_y(pend):
            tmp3_, t_, ych_ = pend
            tk.dve(lambda: nc.vector.tensor_reduce(h4(ych_[:])[:, :, t_], h4(tmp3_[:]), axis=AX.X, op=ALU.add), r=[tmp3_], w=[ych_])

        bons = Rot(nc, es, "bon", [64, 512], F32, 2)
        gs_ = Rot(nc, es, "gg", [64, 512], F32, 2)
        yts = Rot(nc, es, "yt", [64, 512], F32, 2)
        ycs = Rot(nc, es, "yc", [64, 512], F32, 2)
        sqs = Rot(nc, es, "sqy", [64, 512], F32, 2)
        st8 = Rot(nc, es, "st8", [64, 16], F32, 4)
        obs = Rot(nc, es, "obw", [64, 512], BF16, 2)
        oTs = Rot(nc, es, "oTw", [128, 256], BF16, 2)
        h4 = lambda ap: ap.rearrange("p (h k) -> p h k", h=4)
        for c in range(NCH):
            csl = slice(c * 64, (c + 1) * 64)
            cur = {}
            for nm in names:
                t = tl[nm].next()
                for b in range(2):
                    tk.dma("sp", t[b * 64:(b + 1) * 64, :], dr[nm][b, csl, :], w=[t])
                cur[nm] = t
            vT = vTs.next()
            for b in range(2):
                tk.dma("sp", h4(vT[b * 64:(b + 1) * 64, :]), dr["w_vT"][b, :, csl].rearrange("(h v) t -> v h t", h=4), w=[vT])
            bon, gg = bons.next(), gs_.next()
            for b in range(2):
                tk.dma("sp", bon[:, b * 256:(b + 1) * 256], dr["w_bonus"][b, csl, :], w=[bon])
                tk.dma("sp", gg[:, b * 256:(b + 1) * 256], dr["w_g"][b, csl, :], w=[gg])
            ych = ychs.next()
            for t in range(64):
                E2t = E2[:, t * 128:(t + 1) * 128]
                pa, pb, pc = pA.next(), pB.next(), pC.next()
                tk.pe(lambda: nc.tensor.matmul(pa[:, 0:256], E2t, cur["w_whi"][:], start=True, stop=False), r=[E2, cur["w_whi"]], w=[pa])
                tk.pe(lambda: nc.tensor.matmul(pa[:, 0:256], E2t, cur["w_wlo"][:], start=False, stop=True), r=[E2, cur["w_wlo"]], w=[pa])
                tk.pe(lambda: nc.tensor.matmul(pa[:, 256:512], E2t, cur["w_nkk"][:], start=True, stop=True), r=[E2, cur["w_nkk"]], w=[pa])
                tk.pe(lambda: nc.tensor.matmul(pb[:, 0:256], E2t, cur["w_bb"][:], start=True, stop=True), r=[E2, cur["w_bb"]], w=[pb])
                tk.pe(lambda: nc.tensor.matmul(pb[:, 256:512], E2t, cur["w_kp"][:], start=True, stop=True), r=[E2, cur["w_kp"]], w=[pb])
                tk.pe(lambda: nc.tensor.matmul(pc[:, 0:256], E2t, cur["w_r"][:], start=True, stop=True), r=[E2, cur["w_r"]], w=[pc])
                kv = kvs.next()
                for h in range(4):
                    hs = slice(h * 64, (h + 1) * 64)
                    tk.act(lambda: nc.scalar.activation(kv[:, hs], pb[:, 256 + h * 64:256 + (h + 1) * 64], AF.Copy,
                                                        scale=vT[:, h * 64 + t:h * 64 + t + 1]), r=[pb, vT], w=[kv])
                tmp = tmpa.next()
                sa = sas.next()
                So, Sn = Sb[gstep % 2], Sb[(gstep + 1) % 2]
                gstep += 1
                tk.dve(lambda: nc.vector.tensor_tensor(tmp[:], So[:], pa[:, 256:512], op=ALU.mult), r=[So, pa], w=[tmp])
                tk.dve(lambda: nc.vector.tensor_tensor(Sn[:], So[:], pa[:, 0:256], op=ALU.mult), r=[So, pa], w=[Sn])
                if pending is not None:
                    flush_y(pending)
                    pending = None
                tk.dve(lambda: nc.vector.tensor_reduce(sa[:], h4(tmp[:]), axis=AX.X, op=ALU.add), r=[tmp], w=[sa])
                tk.dve(lambda: nc.vector.tensor_tensor(Sn[:], Sn[:], kv[:], op=ALU.add), r=[Sn, kv], w=[Sn])
                tmp2 = tmpa.next()
                tk.dve(lambda: nc.vector.tensor_tensor(h4(tmp2[:]), h4(pb[:, 0:256]), sa[:].unsqueeze(2).to_broadcast([128, 4, 64]),
                                                       op=ALU.mult), r=[pb, sa], w=[tmp2])
                tk.dve(lambda: nc.vector.tensor_tensor(Sn[:], Sn[:], tmp2[:], op=ALU.add), r=[Sn, tmp2], w=[Sn])
                tmp3 = tmpp.next()
                tk.dve(lambda: nc.vector.tensor_tensor(tmp3[:], Sn[:], pc[:, 0:256], op=ALU.mult), r=[Sn, pc], w=[tmp3])
                pending = (tmp3, t, ych)
            flush_y(pending)
            pending = None
            pt = pT.next()
            for h in range(4):
                tk.pe(lambda: nc.tensor.transpose(pt[:64, h * 128:(h + 1) * 128], ych[:, h * 64:(h + 1) * 64], identf[:]),
                      r=[ych, identf], w=[pt])
            yt = yts.next()
            tk.act(lambda: nc.scalar.copy(yt[:].rearrange("p (b h v) -> p h b v", b=2, h=4),
                                          pt[:64, :].rearrange("p (h b v) -> p h b v", h=4, b=2)), r=[pt], w=[yt])
            g8 = lambda ap: ap.rearrange("p (g v) -> p g v", g=8)
            s1 = st8.next()
            tk.dve(lambda: nc.vector.tensor_reduce(s1[:, 0:8], g8(yt[:]), axis=AX.X, op=ALU.add), r=[yt], w=[s1])
            tk.dve(lambda: nc.vector.tensor_scalar(s1[:, 0:8], s1[:, 0:8], -1.0 / 64, None, op0=ALU.mult), r=[s1], w=[s1])
            yc = ycs.next()
            tk.dve(lambda: nc.vector.tensor_tensor(g8(yc[:]), g8(yt[:]), s1[:, 0:8].unsqueeze(2).to_broadcast([64, 8, 64]), op=ALU.add),
                   r=[yt, s1], w=[yc])
            sq = sqs.next()
            tk.pool(lambda: nc.gpsimd.tensor_tensor(sq[:], yc[:], yc[:], op=ALU.mult), r=[yc], w=[sq])
            tk.dve(lambda: nc.vector.tensor_reduce(s1[:, 8:16], g8(sq[:]), axis=AX.X, op=ALU.add), r=[sq], w=[s1])
            tk.act(lambda: nc.scalar.activation(s1[:, 8:16], s1[:, 8:16], AF.Sqrt, scale=1.0 / 64, bias=64e-5), r=[s1], w=[s1])
            tk.dve(lambda: nc.vector.reciprocal(s1[:, 8:16], s1[:, 8:16]), r=[s1], w=[s1])
            tk.dve(lambda: nc.vector.tensor_tensor(g8(yc[:]), g8(yc[:]), s1[:, 8:16].unsqueeze(2).to_broadcast([64, 8, 64]), op=ALU.mult),
                   r=[yc, s1], w=[yc])
            b2 = lambda ap: ap.rearrange("p (b f) -> p b f", b=2)
            bcb = lambda t_: t_[:64, :].unsqueeze(1).to_broadcast([64, 2, 256])
            tk.pool(lambda: nc.gpsimd.tensor_tensor(b2(yc[:]), b2(yc[:]), bcb(lnw_bc), op=ALU.mult), r=[yc, lnw_bc], w=[yc])
            tk.pool(lambda: nc.gpsimd.tensor_tensor(b2(yc[:]), b2(yc[:]), bcb(lnb_bc), op=ALU.add), r=[yc, lnb_bc], w=[yc])
            tk.dve(lambda: nc.vector.tensor_tensor(yc[:], yc[:], bon[:], op=ALU.add), r=[yc, bon], w=[yc])
            ob = obs.next()
            tk.pool(lambda: nc.gpsimd.tensor_tensor(ob[:], yc[:], gg[:], op=ALU.mult), r=[yc, gg], w=[ob])
            po = pO.next()
            for q in range(4):
                tk.pe(lambda: nc.tensor.transpose(po[:, q * 64:(q + 1) * 64], ob[:, q * 128:(q + 1) * 128], identb[:64, :64]),
                      r=[ob, identb], w=[po])
            oTt = oTs.next()
            tk.act(lambda: nc.scalar.copy(oTt[:], po[:, 0:256]), r=[po], w=[oTt])
            for b in range(2):
                tk.dma("pool", dr["oT"][b, 2:4, :, csl].rearrange("k p t -> p k t"),
                       oTt[:, b * 128:(b + 1) * 128].rearrange("p (k t) -> p k t", k=2), r=[oTt])


def phaseDSA(C, l):
    nc, tk, dr, S, NB = C.nc, C.tk, C.dr, C.S, C.NB
    NQ = S // 128
    TOPK = float(min(256, S // 4))
    NIT = 20
    with ExitStack() as es:
        cs = load_consts(C, es, ["c_ident", "c_negU"])
        identf, negU = cs["c_ident"], cs["c_negU"]
        identb = sbt(C, es, "identb", [128, 128], BF16)
        tk.dve(lambda: nc.vector.tensor_copy(identb[:], identf[:]), r=[identf], w=[identb])
        thr0 = sbt(C, es, "thr0", [128, 1], F32)
        tk.pool(lambda: nc.gpsimd.memset(thr0[:], NEG_THR), w=[thr0])
        kT = sbt(C, es, "dkT", [64, S], BF16)
        ikT = sbt(C, es, "dikT", [32, S], BF16)
        vaug = sbt(C, es, "vaug", [128, NQ, 65], BF16)
        pp = Rot(nc, es, "pp", [128, 512], F32, 4, psum=True)
        pmr = Rot(nc, es, "pm", [128, 1024], BF16, 2, psum=True)
        pout = Rot(nc, es, "pout", [128, 512], F32, 1, psum=True)
        ptr = Rot(nc, es, "ptr", [128, 1024], BF16, 1, psum=True)
        scs = Rot(nc, es, "sc", [128, S], F32, 2)
        junk = sbt(C, es, "junk", [128, S], BF16)
        masks = Rot(nc, es, "mask", [128, S], BF16, 2)
        rls = Rot(nc, es, "rl", [128, 512], F32, 3)
        es_ = Rot(nc, es, "eexp", [128, 512], BF16, 3)
        pTs = Rot(nc, es, "pT", [128, 512], BF16, 3)
        iqs = Rot(nc, es, "iq", [32, 512], BF16, 2)
        qs = Rot(nc, es, "q", [64, 512], BF16, 2)
        iws = Rot(nc, es, "iw", [128, 4], F32, 2)
        st = Rot(nc, es, "bst", [128, 8], F32, 2)
        obs = Rot(nc, es, "obd", [128, 256], BF16, 2)
        rcs = Rot(nc, es, "rc", [128, 4], F32, 2)
        oTs = Rot(nc, es, "oTd", [128, 256], BF16, 2)
        for b in range(NB):
            tk.dma("sp", kT[:], dr["d_kT"][b], w=[kT])
            tk.dma("sp", ikT[:], dr["d_ikT"][b], w=[ikT])
            tk.dma("sp", vaug[:], dr["d_v"][b].rearrange("(j p) n -> p j n", p=128), w=[vaug])
            for i in range(NQ):
                L = (i + 1) * 128
                tsl = slice(i * 128, (i + 1) * 128)
                iq, q, iw = iqs.next(), qs.next(), iws.next()
                tk.dma("sp", iq[:].rearrange("d (h t) -> d h t", h=4), dr["d_iqT"][b, :, tsl].rearrange("(h d) t -> d h t", h=4), w=[iq])
                tk.dma("sp", q[:].rearrange("d (h t) -> d h t", h=4), dr["d_qT"][b, :, tsl].rearrange("(h d) t -> d h t", h=4), w=[q])
                tk.dma("sp", iw[:], dr["d_iw"][b, tsl, :], w=[iw])
                sc = scs.next()
                for k0 in range(0, L, 512):
                    w_ = min(512, L - k0)
                    for h in range(4):
                        p = pp.next()
                        tk.pe(lambda: nc.tensor.matmul(p[:, :w_], iq[:, h * 128:(h + 1) * 128], ikT[:, k0:k0 + w_], start=True, stop=True),
                              r=[iq, ikT], w=[p])
                        rl = rls.next()
                        tk.act(lambda: nc.scalar.activation(rl[:, :w_], p[:, :w_], AF.Relu), r=[p], w=[rl])
                        if h == 0:
                            tk.dve(lambda: nc.vector.tensor_scalar(sc[:, k0:k0 + w_], rl[:, :w_], iw[:, 0:1], None, op0=ALU.mult),
                                   r=[rl, iw], w=[sc])
                        else:
                            tk.dve(lambda: nc.vector.scalar_tensor_tensor(sc[:, k0:k0 + w_], rl[:, :w_], iw[:, h:h + 1], sc[:, k0:k0 + w_],
                                                                          op0=ALU.mult, op1=ALU.add), r=[rl, iw, sc], w=[sc])
                b_ = st.next()
                if i >= 2:
                    tk.dve(lambda: nc.vector.tensor_reduce(b_[:, 5:6], sc[:, :L], axis=AX.X, op=ALU.max), r=[sc], w=[b_])
                    tk.dve(lambda: nc.vector.tensor_reduce(b_[:, 6:7], sc[:, :L], axis=AX.X, op=ALU.min), r=[sc], w=[b_])
                    tk.dve(lambda: nc.vector.tensor_scalar(b_[:, 0:1], b_[:, 6:7], -1.0, None, op0=ALU.add), r=[b_], w=[b_])
                    tk.dve(lambda: nc.vector.scalar_tensor_tensor(b_[:, 1:2], b_[:, 5:6], 1.0, b_[:, 0:1], op0=ALU.add, op1=ALU.subtract),
                           r=[b_], w=[b_])
                tk.dve(lambda: nc.vector.tensor_tensor(sc[:, i * 128:L], sc[:, i * 128:L], negU[:], op=ALU.add), r=[sc, negU], w=[sc])
                if i >= 2:
                    for it in range(NIT):
                        f = 0.5 ** (it + 1)
                        tk.dve(lambda: nc.vector.scalar_tensor_tensor(b_[:, 2:3], b_[:, 1:2], f, b_[:, 0:1], op0=ALU.mult, op1=ALU.add),
                               r=[b_], w=[b_])
                        tk.dve(lambda: nc.vector.tensor_scalar(junk[:, :L], sc[:, :L], b_[:, 2:3], None, op0=ALU.is_ge, op1=ALU.add,
                                                               accum_out=b_[:, 3:4]), r=[sc, b_], w=[junk, b_])
                        tk.dve(lambda: nc.vector.tensor_scalar(b_[:, 4:5], b_[:, 3:4], TOPK, None, op0=ALU.is_ge), r=[b_], w=[b_])
                        tk.dve(lambda: nc.vector.tensor_tensor(b_[:, 4:5], b_[:, 4:5], b_[:, 1:2], op=ALU.mult), r=[b_], w=[b_])
                        tk.dve(lambda: nc.vector.scalar_tensor_tensor(b_[:, 0:1], b_[:, 4:5], f, b_[:, 0:1], op0=ALU.mult, op1=ALU.add),
                               r=[b_], w=[b_])
                    thr = b_[:, 0:1]
                    thr_r = [b_]
                else:
                    thr = thr0[:, 0:1]
                    thr_r = [thr0]
                mask = masks.next()
                tk.dve(lambda: nc.vector.tensor_scalar(mask[:, :L], sc[:, :L], thr, None, op0=ALU.is_ge), r=[sc] + thr_r, w=[mask])
                po = pout.next()
                for g0 in range(0, i + 1, 8):
                    pm = pmr.next()
                    g1 = min(i + 1, g0 + 8)
                    for j in range(g0, g1):
                        tk.pe(lambda: nc.tensor.transpose(pm[:, (j - g0) * 128:(j - g0 + 1) * 128], mask[:, j * 128:(j + 1) * 128], identb[:]),
                              r=[mask, identb], w=[pm])
                    for j in range(g0, g1):
                        lg = pp.next()
                        tk.pe(lambda: nc.tensor.matmul(lg[:, 0:512], kT[:, j * 128:(j + 1) * 128], q[:, :], start=True, stop=True),
                              r=[kT, q], w=[lg])
                        e = es_.next()
                        tk.act(lambda: nc.scalar.activation(e[:], lg[:], AF.Exp, scale=64.0 ** -0.5), r=[lg], w=[e])
                        pT = pTs.next()
                        tk.dve(lambda: nc.vector.tensor_tensor(pT[:].rearrange("p (h t) -> p h t", h=4),
                                                               e[:].rearrange("p (h t) -> p h t", h=4),
                                                               pm[:, (j - g0) * 128:(j - g0 + 1) * 128].unsqueeze(1).to_broadcast([128, 4, 128]),
                                                               op=ALU.mult), r=[e, pm], w=[pT])
                        for h in range(4):
                            tk.pe(lambda: nc.tensor.matmul(po[:, h * 65:(h + 1) * 65], pT[:, h * 128:(h + 1) * 128], vaug[:, j, :],
                                                           start=(j == 0 and h == 0), stop=(j == i and h == 3)), r=[pT, vaug], w=[po])
                rc = rcs.next()
                po3 = po[:, 0:260].rearrange("p (h e) -> p h e", h=4)
                tk.dve(lambda: nc.vector.reciprocal(rc[:], po3[:, :, 64]), r=[po], w=[rc])
                ob = obs.next()
                tk.dve(lambda: nc.vector.tensor_tensor(ob[:].rearrange("p (h d) -> p h d", h=4), po3[:, :, 0:64],
                                                       rc[:].unsqueeze(2).to_broadcast([128, 4, 64]), op=ALU.mult), r=[po, rc], w=[ob])
                pt = ptr.next()
                for c2 in range(2):
                    tk.pe(lambda: nc.tensor.transpose(pt[:, c2 * 128:(c2 + 1) * 128], ob[:, c2 * 128:(c2 + 1) * 128], identb[:]),
                          r=[ob, identb], w=[pt])
                oTt = oTs.next()
                tk.act(lambda: nc.scalar.copy(oTt[:], pt[:, 0:256]), r=[pt], w=[oTt])
                tk.dma("pool", dr["oT"][b, 6:8, :, tsl].rearrange("k p t -> p k t"),
                       oTt[:].rearrange("p (k t) -> p k t", k=2), r=[oTt])


def phaseF1(C, l):
    nc, tk, dr, S, NB, TS = C.nc, C.tk, C.dr, C.S, C.NB, C.TS
    with ExitStack() as es:
        cs = load_consts(C, es, ["c_ident"])
        identf = cs["c_ident"]
        identb = sbt(C, es, "identb", [128, 128], BF16)
        tk.dve(lambda: nc.vector.tensor_copy(identb[:], identf[:]), r=[identf], w=[identb])
        ones_bf = sbt(C, es, "ones_bf", [128, 128], BF16)
        tk.pool(lambda: nc.gpsimd.memset(ones_bf[:], 1.0), w=[ones_bf])
        Ws = {nm: sbt(C, es, "W" + nm, [128, KC, 1024], BF16) for nm in ("w_out", "wq_x", "wk_x", "wv_x", "wo_x")}
        gq = sbt(C, es, "gq", [128, KC], F32)
        gm = sbt(C, es, "gm", [128, KC], F32)
        tk.dma("sp", gq[:], dr["norm_cross"][l].rearrange("(k p) -> p k", p=128), w=[gq], allow_slow_non_contiguous=True)
        tk.dma("sp", gm[:], dr["norm_mem"][l].rearrange("(k p) -> p k", p=128), w=[gm], allow_slow_non_contiguous=True)
        with ExitStack() as es2:
            stg = Rot(nc, es2, "stg", [128, KC, 512], F32, 2)
            for nm, g in (("w_out", None), ("wq_x", gq), ("wk_x", gm), ("wv_x", gm), ("wo_x", None)):
                load_w_sec(C, stg, Ws[nm], 0, dr[nm][l], 1024, g)
            tk.barrier()
        pp = Rot(nc, es, "pp", [128, 512], F32, 6, psum=True)
        ptr = Rot(nc, es, "ptr", [128, 1024], BF16, 2, psum=True)
        memnT = sbt(C, es, "memnT", [128, KC, 256], BF16)
        kTx = sbt(C, es, "kTx", [128, KC, 256], BF16)
        vx = sbt(C, es, "vx", [128, 2, 1024], BF16)
        mts = Rot(nc, es, "mt", [128, 1024], F32, 2)
        mbs = Rot(nc, es, "mb", [128, 1024], BF16, 2)
        sm = Rot(nc, es, "smf", [128, 2], F32, 4)
        hTt = sbt(C, es, "hTt", [128, KC, TS], F32)
        oTt = sbt(C, es, "oTt", [128, KC, TS], BF16)
        sq = sbt(C, es, "sq", [128, KC, TS], BF16)
        hn = sbt(C, es, "hn", [128, KC, TS], BF16)
        rstd = sbt(C, es, "rstd", [128, TS], F32)
        qTx = sbt(C, es, "qTx", [128, KC, TS], BF16)
        oxT = sbt(C, es, "oxT", [128, KC, TS], BF16)
        ees = Rot(nc, es, "ee", [128, TS], BF16, 4)
        rdens = Rot(nc, es, "rden", [128, TS], F32, 2)

        def proj_add(Wt, src):
            for dc in range(KC):
                p = pp.next()
                for fc in range(KC):
                    tk.pe(lambda: nc.tensor.matmul(p[:, :TS], Wt[:, fc, dc * 128:(dc + 1) * 128], src[:, fc, :],
                                                   start=(fc == 0), stop=(fc == KC - 1)), r=[Wt, src], w=[p])
                tk.dve(lambda: nc.vector.tensor_tensor(hTt[:, dc, :], hTt[:, dc, :], p[:, :TS], op=ALU.add), r=[hTt, p], w=[hTt])

        for b in range(NB):
            for mc in range(2):
                mt = mts.next()
                tk.dma("sp", mt[:], dr["mem"][b, mc * 128:(mc + 1) * 128, :], w=[mt])
                mb = mbs.next()
                s1 = sm.next()
                tk.act(lambda: nc.scalar.activation(mb[:], mt[:], AF.Square, accum_out=s1[:, 0:1]), r=[mt], w=[mb, s1])
                tk.act(lambda: nc.scalar.activation(s1[:, 0:1], s1[:, 0:1], AF.Sqrt, scale=1.0 / D, bias=EPS), r=[s1], w=[s1])
                tk.dve(lambda: nc.vector.reciprocal(s1[:, 0:1], s1[:, 0:1]), r=[s1], w=[s1])
                tk.dve(lambda: nc.vector.tensor_scalar(mb[:], mt[:], s1[:, 0:1], None, op0=ALU.mult), r=[mt, s1], w=[mb])
                pt = ptr.next()
                for kc in range(KC):
                    tk.pe(lambda: nc.tensor.transpose(pt[:, kc * 128:(kc + 1) * 128], mb[:, kc * 128:(kc + 1) * 128], identb[:]),
                          r=[mb, identb], w=[pt])
                tk.act(lambda: nc.scalar.copy(memnT[:, :, mc * 128:(mc + 1) * 128], pt[:].rearrange("p (k m) -> p k m", k=KC)),
                       r=[pt], w=[memnT])
            for dc in range(KC):
                p = pp.next()
                for kc in range(KC):
                    tk.pe(lambda: nc.tensor.matmul(p[:, :256], Ws["wk_x"][:, kc, dc * 128:(dc + 1) * 128], memnT[:, kc, :],
                                                   start=(kc == 0), stop=(kc == KC - 1)), r=[Ws["wk_x"], memnT], w=[p])
                tk.act(lambda: nc.scalar.copy(kTx[:, dc, :], p[:, :256]), r=[p], w=[kTx])
            for mc in range(2):
                for half in range(2):
                    p = pp.next()
                    for kc in range(KC):
                        tk.pe(lambda: nc.tensor.matmul(p[:, :512], memnT[:, kc, mc * 128:(mc + 1) * 128],
                                                       Ws["wv_x"][:, kc, half * 512:(half + 1) * 512],
                                                       start=(kc == 0), stop=(kc == KC - 1)), r=[Ws["wv_x"], memnT], w=[p])
                    tk.act(lambda: nc.scalar.copy(vx[:, mc, half * 512:(half + 1) * 512], p[:, :512]), r=[p], w=[vx])
            for st in range(C.NST):
                tsl = slice(st * TS, (st + 1) * TS)
                tk.dma("sp", hTt[:], dr["hT"][b, :, :, tsl].rearrange("k p t -> p k t"), w=[hTt])
                tk.dma("sp", oTt[:], dr["oT"][b, :, :, tsl].rearrange("k p t -> p k t"), w=[oTt])
                proj_add(Ws["w_out"], oTt)
                emit_norm(C, hTt, TS, hn[:], hn, sq, ones_bf, pp.next(), rstd)
                for dc in range(KC):
                    p = pp.next()
                    for kc in range(KC):
                        tk.pe(lambda: nc.tensor.matmul(p[:, :TS], Ws["wq_x"][:, kc, dc * 128:(dc + 1) * 128], hn[:, kc, :],
                                                       start=(kc == 0), stop=(kc == KC - 1)), r=[Ws["wq_x"], hn], w=[p])
                    tk.act(lambda: nc.scalar.copy(qTx[:, dc, :], p[:, :TS]), r=[p], w=[qTx])
                for h in range(4):
                    ee = []
                    for mc in range(2):
                        p = pp.next()
                        for d2 in range(2):
                            tk.pe(lambda: nc.tensor.matmul(p[:, :TS], kTx[:, 2 * h + d2, mc * 128:(mc + 1) * 128], qTx[:, 2 * h + d2, :],
                                                           start=(d2 == 0), stop=(d2 == 1)), r=[kTx, qTx], w=[p])
                        e = ees.next()
                        tk.act(lambda: nc.scalar.activation(e[:], p[:, :TS], AF.Exp, scale=256.0 ** -0.5), r=[p], w=[e])
                        ee.append(e)
                    p = pp.next()
                    for mc in range(2):
                        tk.pe(lambda: nc.tensor.matmul(p[:, :TS], ones_bf[:], ee[mc][:], start=(mc == 0), stop=(mc == 1)),
                              r=[ones_bf, ee[mc]], w=[p])
                    rden = rdens.next()
                    tk.dve(lambda: nc.vector.reciprocal(rden[:], p[:, :TS]), r=[p], w=[rden])
                    for dv2 in range(2):
                        p = pp.next()
                        for mc in range(2):
                            c0 = h * 256 + dv2 * 128
                            tk.pe(lambda: nc.tensor.matmul(p[:, :TS], vx[:, mc, c0:c0 + 128], ee[mc][:], start=(mc == 0), stop=(mc == 1)),
                                  r=[vx, ee[mc]], w=[p])
                        tk.dve(lambda: nc.vector.tensor_tensor(oxT[:, 2 * h + dv2, :], p[:, :TS], rden[:], op=ALU.mult),
                               r=[p, rden], w=[oxT])
                proj_add(Ws["wo_x"], oxT)
                tk.dma("pool", dr["hT"][b, :, :, tsl].rearrange("k p t -> p k t"), hTt[:], r=[hTt])


def phaseF2(C, l):
    nc, tk, dr, S, NB = C.nc, C.tk, C.dr, C.S, C.NB
    T2 = 256
    with ExitStack() as es:
        ones_bf = sbt(C, es, "ones_bf", [128, 128], BF16)
        tk.pool(lambda: nc.gpsimd.memset(ones_bf[:], 1.0), w=[ones_bf])
        Wup = sbt(C, es, "Wup", [128, KC, 2 * DFF], BF16)
        Wdn = sbt(C, es, "Wdn", [128, NFC, 1024], BF16)
        gf = sbt(C, es, "gf", [128, KC], F32)
        tk.dma("sp", gf[:], dr["norm_ffn"][l].rearrange("(k p) -> p k", p=128), w=[gf], allow_slow_non_contiguous=True)
        with ExitStack() as es2:
            stg = Rot(nc, es2, "stg", [128, KC, 512], F32, 2)
            load_w_sec(C, stg, Wup, 0, dr["w_up"][l], 2 * DFF, gf)
            for c in range(NFC):
                st = stg.next()
                tk.dma("sp", st[:, 0:2, :].rearrange("p a n -> p (a n)"), dr["w_down"][l, c * 128:(c + 1) * 128, :], w=[st])
                tk.act(lambda: nc.scalar.copy(Wdn[:, c, :], st[:, 0:2, :].rearrange("p a n -> p (a n)")), r=[st], w=[Wdn])
            tk.barrier()
        cwT = sbt(C, es, "fcw", [128, NFC, 3], F32)
        for j in range(3):
            tk.dma("sp", cwT[:, :, j], dr["ffn_conv_w"][l, j].rearrange("(c p) -> p c", p=128), w=[cwT], allow_slow_non_contiguous=True)
        cbT = sbt(C, es, "fcb", [128, NFC], F32)
        tk.dma("sp", cbT[:], dr["ffn_conv_b"][l].rearrange("(c p) -> p c", p=128), w=[cbT], allow_slow_non_contiguous=True)
        halo = sbt(C, es, "fhalo", [128, NFC, 2], F32)
        pp = Rot(nc, es, "pp", [128, 512], F32, 7, psum=True)
        hTs = Rot(nc, es, "hTf", [128, KC, T2], F32, 2)
        sq = sbt(C, es, "sq", [128, KC, T2], BF16)
        hn = sbt(C, es, "hn", [128, KC, T2], BF16)
        rstd = sbt(C, es, "rstd", [128, T2], F32)
        actT = sbt(C, es, "actT", [128, NFC, T2], BF16)
        gbs = Rot(nc, es, "gb", [128, T2 + 2], F32, 3)
        accs = Rot(nc, es, "acc", [128, T2], F32, 3)
        for b in range(NB):
            tk.pool(lambda: nc.gpsimd.memset(halo[:], 0.0), w=[(halo, c_) for c_ in range(NFC)])
            for ti in range(S // T2):
                tsl = slice(ti * T2, (ti + 1) * T2)
                hTt = hTs.next()
                tk.dma("sp", hTt[:], dr["hT"][b, :, :, tsl].rearrange("k p t -> p k t"), w=[hTt])
                emit_norm(C, hTt, T2, hn[:], hn, sq, ones_bf, pp.next(), rstd)
                for c in range(NFC):
                    p = pp.next()
                    for half in range(2):
                        for kc in range(KC):
                            c0 = half * DFF + c * 128
                            tk.pe(lambda: nc.tensor.matmul(p[:, half * T2:(half + 1) * T2], Wup[:, kc, c0:c0 + 128], hn[:, kc, :],
                                                           start=(kc == 0), stop=(kc == KC - 1)), r=[Wup, hn], w=[p])
                    gb = gbs.next()
                    tk.pool(lambda: nc.gpsimd.tensor_copy(gb[:, 0:2], halo[:, c, :]), r=[(halo, c)], w=[(gb, 0)])
                    tk.act(lambda: nc.scalar.copy(gb[:, 2:T2 + 2], p[:, 0:T2]), r=[p], w=[(gb, 1)])
                    tk.pool(lambda: nc.gpsimd.tensor_copy(halo[:, c, :], gb[:, T2:T2 + 2]), r=[(gb, 1)], w=[(halo, c)])
                    acc = accs.next()
                    tk.dve(lambda: nc.vector.tensor_scalar(acc[:], gb[:, 2:T2 + 2], cwT[:, c, 2:3], cbT[:, c:c + 1],
                                                           op0=ALU.mult, op1=ALU.add), r=[(gb, 1), cwT, cbT], w=[acc])
                    for jj in (1, 0):
                        tk.dve(lambda: nc.vector.scalar_tensor_tensor(acc[:], gb[:, jj:jj + T2], cwT[:, c, jj:jj + 1], acc[:],
                                                                      op0=ALU.mult, op1=ALU.add),
                               r=[(gb, 0), (gb, 1), acc, cwT], w=[acc])
                    tk.act(lambda: nc.scalar.activation(acc[:], acc[:], AF.Silu), r=[acc], w=[acc])
                    tk.dve(lambda: nc.vector.tensor_tensor(actT[:, c, :], acc[:], p[:, T2:2 * T2], op=ALU.mult), r=[acc, p], w=[(actT, c)])
                for dc in range(KC):
                    p = pp.next()
                    for c in range(NFC):
                        tk.pe(lambda: nc.tensor.matmul(p[:, :T2], Wdn[:, c, dc * 128:(dc + 1) * 128], actT[:, c, :],
                                                       start=(c == 0), stop=(c == NFC - 1)), r=[Wdn, (actT, c)], w=[p])
                    tk.dve(lambda: nc.vector.tensor_tensor(hTt[:, dc, :], hTt[:, dc, :], p[:, :T2], op=ALU.add), r=[hTt, p], w=[hTt])
                tk.dma("pool", dr["hT"][b, :, :, tsl].rearrange("k p t -> p k t"), hTt[:], r=[hTt])


def phaseFinal(C):
    nc, tk, dr, S, NB, TS = C.nc, C.tk, C.dr, C.S, C.NB, C.TS
    with ExitStack() as es:
        cs = load_consts(C, es, ["c_ident"])
        identf = cs["c_ident"]
        ones_bf = sbt(C, es, "ones_bf", [128, 128], BF16)
        tk.pool(lambda: nc.gpsimd.memset(ones_bf[:], 1.0), w=[ones_bf])
        gfin = sbt(C, es, "gfin", [128, KC], F32)
        tk.dma("sp", gfin[:], dr["norm_final"].rearrange("(k p) -> p k", p=128), w=[gfin], allow_slow_non_contiguous=True)
        pp = Rot(nc, es, "pp", [128, 512], F32, 6, psum=True)
        hTs = Rot(nc, es, "hTl", [128, KC, TS], F32, 2)
        sq = sbt(C, es, "sq", [128, KC, TS], BF16)
        rstd = sbt(C, es, "rstd", [128, TS], F32)
        yT = sbt(C, es, "yT", [128, KC, TS], F32)
        yos = Rot(nc, es, "yo", [128, D], F32, 2)
        for b in range(NB):
            for st in range(C.NST):
                tsl = slice(st * TS, (st + 1) * TS)
                hTt = hTs.next()
                tk.dma("sp", hTt[:], dr["hT"][b, :, :, tsl].rearrange("k p t -> p k t"), w=[hTt])
                emit_norm(C, hTt, TS, None, None, sq, ones_bf, pp.next(), rstd)
                for kc in range(KC):
                    tk.dve(lambda: nc.vector.scalar_tensor_tensor(yT[:, kc, :], hTt[:, kc, :], gfin[:, kc:kc + 1], rstd[:],
                                                                  op0=ALU.mult, op1=ALU.mult), r=[hTt, gfin, rstd], w=[(yT, kc)])
                for j in range(TS // 128):
                    yo = yos.next()
                    for half in range(2):
                        p = pp.next()
                        for q in range(4):
                            kc = half * 4 + q
                            tk.pe(lambda: nc.tensor.transpose(p[:, q * 128:(q + 1) * 128], yT[:, kc, j * 128:(j + 1) * 128], identf[:]),
                                  r=[(yT, kc), identf], w=[p])
                        if half == 0:
                            tk.act(lambda: nc.scalar.copy(yo[:, 0:512], p[:]), r=[p], w=[(yo, 0)])
                        else:
                            tk.dve(lambda: nc.vector.tensor_copy(yo[:, 512:1024], p[:]), r=[p], w=[(yo, 1)])
                    t0 = st * TS + j * 128
                    tk.dma("pool", dr["y"][b, t0:t0 + 128, :], yo[:], r=[(yo, 0), (yo, 1)])


def kernel(**inputs):
    S, NCORES = 4096, 8
    nc, C = build(S)
    consts = make_consts(S)
    x = np.ascontiguousarray(inputs["x"], dtype=np.float32)
    mem = np.ascontiguousarray(inputs["mem"], dtype=np.float32)
    in_maps = []
    for c in range(NCORES):
        m = {"x": x[2 * c:2 * c + 2], "mem": mem[2 * c:2 * c + 2]}
        for name, _ in PARAMS:
            m[name] = np.ascontiguousarray(inputs[name], dtype=np.float32)
        m.update(consts)
        in_maps.append(m)
    res = run_bass_kernel_spmd(nc, in_maps, core_ids=list(range(NCORES)))
    return np.concatenate([np.asarray(r["y"], dtype=np.float32) for r in res.results], axis=0)
```

```python
import math
import numpy as np
from contextlib import ExitStack
import ml_dtypes
import concourse.bass as bass
import concourse.mybir as mybir
from concourse.bass_utils import run_bass_kernel_spmd

F32 = mybir.dt.float32
BF16 = mybir.dt.bfloat16
U32 = mybir.dt.uint32
ALU = mybir.AluOpType
AF = mybir.ActivationFunctionType
AX = mybir.AxisListType

D = 1024
KC = 8
NMEM = 256
DFF = 2816
NFC = DFF // 128
N_IN = 3368
EPS = 1e-6
NEG_BIG = -3.0e38
NEG_THR = -1.0e38


class Buf:
    __slots__ = ("t", "name", "psum")

    def __init__(self, t, name, psum=False):
        self.t = t
        self.name = name
        self.psum = psum

    def __getitem__(self, k):
        return self.t[k]


class TK:
    LIM = 900
    DLIM = 55
    NSLOT = 8
    ENG = ("pe", "act", "dve", "pool", "sp")
    DQ = ("sp", "pool")

    def __init__(self, nc, es):
        self.nc = nc
        self.es = es
        self.eng = {"pe": nc.tensor, "act": nc.scalar, "dve": nc.vector, "pool": nc.gpsimd, "sp": nc.sync}
        self.nsem = 0
        self.csem = {e: [self._newsem(e) for _ in range(3)] for e in self.ENG}
        self.dsem = {(q, sl): [self._newsem(f"d{q}{sl}") for _ in range(3)] for q in self.DQ for sl in range(self.NSLOT)}
        self.epoch = 0
        self.ninstr = 0
        self.nbar = 0
        self.dnext = {q: 0 for q in self.DQ}
        self._reset_epoch()

    def _reset_epoch(self):
        self.cnt = {e: 0 for e in self.ENG}
        self.dcnt = {k: 0 for k in self.dsem}
        self.seen = {e: {} for e in self.ENG}
        self.lastw = {}
        self.readers = {}
        self.last_d = {}

    def _newsem(self, name):
        self.nsem += 1
        return self.es.enter_context(self.nc.semaphore(f"{name}_{self.nsem}"))

    def _need(self, e, tok):
        if tok is None or tok[2] != self.epoch:
            return
        if tok[0] == "c":
            _, e2, _, idx = tok
            key = e2 if e2 != e else ("self", e)
            if self.seen[e].get(key, -1) >= idx:
                return
            self.seen[e][key] = idx
            self.eng[e].wait_ge(self.csem[e2][self.epoch % 3], idx + 1)
        else:
            _, key, _, val = tok
            if self.seen[e].get(key, -1) >= val:
                return
            self.seen[e][key] = val
            self.eng[e].wait_ge(self.dsem[key][self.epoch % 3], val)

    def _deps(self, e, r, w, is_dma=False):
        def chk(tok):
            if tok is None:
                return
            if tok[0] == "c" and tok[1] == e and not is_dma and e == "pe":
                return
            self._need(e, tok)
        for b in r:
            chk(self.lastw.get(b))
            if getattr(b, "psum", False):
                rd = self.readers.get(b)
                if rd:
                    for t2 in rd.values():
                        if not (t2[0] == "c" and t2[1] == e):
                            chk(t2)
        for b in w:
            chk(self.lastw.get(b))
            rd = self.readers.get(b)
            if rd:
                for t2 in rd.values():
                    chk(t2)

    def _record(self, tok, r, w, rkey):
        for b in w:
            self.lastw[b] = tok
            self.readers[b] = {}
        for b in r:
            self.readers.setdefault(b, {})[rkey] = tok

    def op(self, e, fn, r=(), w=()):
        if self.cnt[e] >= self.LIM:
            self.barrier()
        self._deps(e, r, w)
        idx = self.cnt[e]
        ins = fn()
        ins.then_inc(self.csem[e][self.epoch % 3], 1)
        self.cnt[e] = idx + 1
        tok = ("c", e, self.epoch, idx)
        self._record(tok, r, w, e)
        self.ninstr += 1
        return ins

    def dma(self, q, out, in_, r=(), w=(), **kw):
        q = "sp"
        slot = self.dnext[q] % self.NSLOT
        k = (q, slot)
        if self.dcnt[k] >= self.DLIM:
            self.barrier()
        self.dnext[q] += 1
        self._deps(q, r, w, is_dma=True)
        self._need(q, self.last_d.get(k))
        self.dcnt[k] += 1
        val = 16 * self.dcnt[k]
        self.eng[q].dma_start(out=out, in_=in_, **kw).then_inc(self.dsem[k][self.epoch % 3], 16)
        tok = ("d", k, self.epoch, val)
        self.last_d[k] = tok
        self._record(tok, r, w, k)
        self.ninstr += 1
        return tok

    def barrier(self):
        bank = self.epoch % 3
        for e in self.ENG:
            if self.cnt[e] == 0:
                self.eng[e].sem_inc(self.csem[e][bank], 1)
                self.cnt[e] = 1
        toks = [("c", e, self.epoch, self.cnt[e] - 1) for e in self.ENG] + list(self.last_d.values())
        for e in self.ENG:
            for t in toks:
                self._need(e, t)
        self.epoch += 1
        self.nbar += 1
        nb = (self.epoch + 1) % 3
        for e in self.ENG:
            self.eng[e].sem_clear(self.csem[e][nb])
            if e in self.DQ:
                for sl in range(self.NSLOT):
                    self.eng[e].sem_clear(self.dsem[(e, sl)][nb])
        self._reset_epoch()

    def pe(self, fn, r=(), w=()):
        return self.op("pe", fn, r, w)

    def act(self, fn, r=(), w=()):
        return self.op("act", fn, r, w)

    def dve(self, fn, r=(), w=()):
        return self.op("dve", fn, r, w)

    def pool(self, fn, r=(), w=()):
        return self.op("pool", fn, r, w)


_UN = [0]


def uname(name):
    _UN[0] += 1
    return f"{name}_u{_UN[0]}"


class Rot:
    def __init__(self, nc, es, name, shape, dtype, n, psum=False):
        self.bufs = []
        for i in range(n):
            if psum:
                t = es.enter_context(nc.psum_tensor(uname(f"{name}{i}"), shape, dtype))
            else:
                t = es.enter_context(nc.sbuf_tensor(uname(f"{name}{i}"), shape, dtype))
            self.bufs.append(Buf(t, f"{name}{i}", psum=psum))
        self.i = 0

    def next(self):
        b = self.bufs[self.i % len(self.bufs)]
        self.i += 1
        return b


C_RQ, C_RK, C_RV, C_RG = 0, 128, 256, 512
C_WR, C_WK, C_WV, C_WWL, C_WAL, C_WGL = 768, 1024, 1280, 1536, 1600, 1664
C_SZ, C_SX, C_SB, C_SC, C_SDT = 1792, 2048, 2304, 2560, 2816
C_DQ, C_DK, C_DV, C_DIQ, C_DIK, C_DIW = 2820, 3076, 3140, 3204, 3332, 3364

PARAMS = [
    ("norm_mix", (2, 1024)), ("w_in", (2, 1024, N_IN)), ("rwkv_mu", (2, 1024)), ("rwkv_w0", (2, 256)),
    ("rwkv_w2", (2, 64, 256)), ("rwkv_a0", (2, 256)), ("rwkv_a2", (2, 64, 256)), ("rwkv_g2", (2, 128, 256)),
    ("rwkv_k_k", (2, 256)), ("rwkv_k_a", (2, 256)), ("rwkv_r_k", (2, 4, 64)), ("rwkv_ln_w", (2, 256)),
    ("rwkv_ln_b", (2, 256)), ("ssm_conv_w", (2, 4, 768)), ("ssm_conv_b", (2, 768)), ("ssm_dt_bias", (2, 4)),
    ("ssm_a_log", (2, 4)), ("ssm_d", (2, 4)), ("ssm_norm", (2, 256)), ("idx_k_norm", (2, 32)),
    ("w_out", (2, 1024, 1024)), ("norm_cross", (2, 1024)), ("norm_mem", (2, 1024)), ("wq_x", (2, 1024, 1024)),
    ("wk_x", (2, 1024, 1024)), ("wv_x", (2, 1024, 1024)), ("wo_x", (2, 1024, 1024)), ("norm_ffn", (2, 1024)),
    ("w_up", (2, 1024, 2 * DFF)), ("ffn_conv_w", (2, 3, DFF)), ("ffn_conv_b", (2, DFF)),
    ("w_down", (2, DFF, 1024)), ("norm_final", (1024,)),
]


def make_consts(S):
    c = {}
    c["c_ident"] = np.eye(128, dtype=np.float32)
    i = np.arange(128)
    c["c_U"] = (i[:, None] <= i[None, :]).astype(np.float32)
    c["c_negU"] = np.where(i[None, :] > i[:, None], np.float32(NEG_BIG), np.float32(0)).astype(np.float32)
    pos = np.arange(S, dtype=np.float32)

    def tabs(hd, rows):
        half = hd // 2
        inv = (np.float32(10000.0) ** (-np.arange(half, dtype=np.float32) / np.float32(half))).astype(np.float32)
        ang = (pos[:, None] * inv[None, :]).astype(np.float32)
        cos = np.cos(ang).astype(np.float32)
        sin = np.sin(ang).astype(np.float32)
        cf = np.concatenate([cos, cos], 1)
        sf = np.concatenate([-sin, sin], 1)
        rep = rows // hd
        return np.ascontiguousarray(np.tile(cf, (1, rep)).T), np.ascontiguousarray(np.tile(sf, (1, rep)).T)

    c["c_cos64T"], c["c_sin64T"] = tabs(64, 128)
    c["c_cos32T"], c["c_sin32T"] = tabs(32, 128)
    e2 = np.zeros((128, 64, 128), dtype=np.float32)
    for b in range(2):
        for s in range(64):
            e2[b * 64 + s, s, b * 64:(b + 1) * 64] = 1.0
    c["c_E2"] = e2.reshape(128, 64 * 128).astype(ml_dtypes.bfloat16)
    lg = np.log(1.0 - np.power(2.0, -5.0 - np.arange(4, dtype=np.float32))).astype(np.float32)
    c["c_retla"] = np.tile(lg[None, :], (128, 1)).astype(np.float32)
    return c


CONST_SPECS = lambda S: [("c_ident", (128, 128), F32), ("c_U", (128, 128), F32), ("c_negU", (128, 128), F32),
                         ("c_cos64T", (128, S), F32), ("c_sin64T", (128, S), F32), ("c_cos32T", (128, S), F32),
                         ("c_sin32T", (128, S), F32), ("c_E2", (128, 64 * 128), BF16), ("c_retla", (128, 4), F32)]


class Ctx:
    pass


def build(S, NB=2, L=2, stop_after=None, dbg=()):
    nc = bass.Bass("TRN2", target_bir_lowering=False)
    C = Ctx()
    C.nc, C.S, C.NB, C.L = nc, S, NB, L
    C.TS = 512
    C.NST = S // C.TS
    dr = {}
    dr["x"] = nc.dram_tensor("x", [NB, S, D], F32, kind="ExternalInput").ap()
    dr["mem"] = nc.dram_tensor("mem", [NB, NMEM, D], F32, kind="ExternalInput").ap()
    for name, shp in PARAMS:
        dr[name] = nc.dram_tensor(name, list(shp), F32, kind="ExternalInput").ap()
    for name, shp, dt in CONST_SPECS(S):
        dr[name] = nc.dram_tensor(name, list(shp), dt, kind="ExternalInput").ap()
    dr["y"] = nc.dram_tensor("y", [NB, S, D], F32, kind="ExternalOutput").ap()

    def scratch(name, shape, dt):
        kind = "ExternalOutput" if name in dbg else "Internal"
        dr[name] = nc.dram_tensor(name, list(shape), dt, kind=kind).ap()

    scratch("hT", [NB, KC, 128, S], F32)
    scratch("oT", [NB, KC, 128, S], BF16)
    scratch("r_qT", [NB, 128, S], BF16)
    scratch("r_kT", [NB, 128, S], BF16)
    scratch("r_kTok", [NB, S, 128], BF16)
    scratch("r_v", [NB, S, 256], BF16)
    scratch("r_sg", [NB, S, 256], F32)
    for nm in ("w_whi", "w_wlo", "w_nkk", "w_bb", "w_kp", "w_r"):
        scratch(nm, [NB, S, 256], BF16)
    scratch("w_vT", [NB, 256, S], F32)
    scratch("w_bonus", [NB, S, 256], F32)
    scratch("w_g", [NB, S, 256], F32)
    scratch("s_CT", [NB, 256, S], BF16)
    scratch("s_BT", [NB, 256, S], BF16)
    scratch("s_BTok", [NB, S, 256], BF16)
    scratch("s_xdt", [NB, S, 256], BF16)
    scratch("s_xs", [NB, S, 256], F32)
    scratch("s_sz", [NB, S, 256], F32)
    scratch("s_la", [NB, S, 4], F32)
    scratch("d_qT", [NB, 256, S], BF16)
    scratch("d_kT", [NB, 64, S], BF16)
    scratch("d_v", [NB, S, 65], BF16)
    scratch("d_iqT", [NB, 128, S], BF16)
    scratch("d_ikT", [NB, 32, S], BF16)
    scratch("d_iw", [NB, S, 4], F32)
    C.dr = dr

    import os
    with ExitStack() as es0:
        tk = TK(nc, es0)
        C.tk = tk
        phases = []
        phases.append(("p0", lambda: phase0(C)))
        for l in range(L):
            if os.environ.get("KONEP", "1") == "1":
                phases.append((f"P{l}", lambda l=l: phaseP(C, l)))
            else:
                for sec in ("ret", "ssd", "rw", "dsa"):
                    phases.append((f"P{l}{sec}" if sec != "dsa" else f"P{l}", lambda l=l, sec=sec: phaseP(C, l, only=sec)))
            phases.append((f"REC{l}", lambda l=l: phaseRec(C, l)))
            phases.append((f"RW{l}", lambda l=l: phaseRW(C, l)))
            phases.append((f"DSA{l}", lambda l=l: phaseDSA(C, l)))
            phases.append((f"F1{l}", lambda l=l: phaseF1(C, l)))
            phases.append((f"F2{l}", lambda l=l: phaseF2(C, l)))
        phases.append(("fin", lambda: phaseFinal(C)))
        import os
        skipph = os.environ.get("KPH", "").split(",")
        for name, fn in phases:
            if name in skipph:
                continue
            fn()
            tk.barrier()
            if stop_after == name:
                break
        C.ninstr = tk.ninstr
    return nc, C


def load_consts(C, es, names):
    nc, tk, dr = C.nc, C.tk, C.dr
    out = {}
    for nm in names:
        ap = dr[nm]
        t = Buf(es.enter_context(nc.sbuf_tensor(uname("k_" + nm), list(ap.shape), ap.dtype)), nm)
        tk.dma("sp", t[:], ap[:, :], w=[t])
        out[nm] = t
    return out


def phase0(C):
    nc, tk, dr, S, NB = C.nc, C.tk, C.dr, C.S, C.NB
    with ExitStack() as es:
        cs = load_consts(C, es, ["c_ident"])
        ident = cs["c_ident"]
        xin = Rot(nc, es, "p0x", [128, D], F32, 2)
        hout = Rot(nc, es, "p0h", [128, KC, 128], F32, 2)
        pps = Rot(nc, es, "p0ps", [128, 512], F32, 4, psum=True)
        for b in range(NB):
            for ti in range(S // 128):
                xt = xin.next()
                tk.dma("sp", xt[:], dr["x"][b, ti * 128:(ti + 1) * 128, :], w=[xt])
                ho = hout.next()
                for half in range(2):
                    pp = pps.next()
                    for j in range(4):
                        kc = half * 4 + j
                        tk.pe(lambda: nc.tensor.transpose(pp[:, j * 128:(j + 1) * 128], xt[:, kc * 128:(kc + 1) * 128], ident[:]),
                              r=[xt, ident], w=[pp])
                    dst = ho[:, half * 4:(half + 1) * 4, :]
                    src = pp[:].rearrange("p (a b) -> p a b", a=4)
                    if half == 0:
                        tk.act(lambda: nc.scalar.copy(dst, src), r=[pp], w=[(ho, half)])
                    else:
                        tk.dve(lambda: nc.vector.tensor_copy(dst, src), r=[pp], w=[(ho, half)])
                tk.dma("pool", dr["hT"][b, :, :, ti * 128:(ti + 1) * 128].rearrange("k p t -> p k t"), ho[:],
                       r=[(ho, 0), (ho, 1)])


def emit_norm(C, hT, n, hn_ap, hn_key, sq, ones_bf, pp, rstd):
    nc, tk = C.nc, C.tk
    tk.act(lambda: nc.scalar.activation(sq[:, :, :n], hT[:, :, :n], AF.Square), r=[hT], w=[sq])
    for kc in range(KC):
        tk.pe(lambda: nc.tensor.matmul(pp[:, :n], ones_bf[:], sq[:, kc, :n], start=(kc == 0), stop=(kc == KC - 1)),
              r=[sq, ones_bf], w=[pp])
    tk.act(lambda: nc.scalar.activation(rstd[:, :n], pp[:, :n], AF.Sqrt, scale=1.0 / D, bias=EPS), r=[pp], w=[rstd])
    tk.dve(lambda: nc.vector.reciprocal(rstd[:, :n], rstd[:, :n]), r=[rstd], w=[rstd])
    if hn_ap is not None:
        tk.dve(lambda: nc.vector.tensor_tensor(hn_ap, hT[:, :, :n], rstd[:, :n].unsqueeze(1).to_broadcast([128, KC, n]),
                                               op=ALU.mult), r=[hT, rstd], w=[hn_key])


def load_w_sec(C, stg, dstW, off, src2d, n, gT, cs=None, swap=0, rows=KC):
    nc, tk = C.nc, C.tk
    for c0 in range(0, n, 512):
        m = min(512, n - c0)
        st = stg.next()
        tk.dma("sp", st[:, :rows, :m], src2d[:, c0:c0 + m].rearrange("(kc p) n -> p kc n", p=128), w=[st])
        if gT is not None:
            tk.dve(lambda: nc.vector.tensor_tensor(st[:, :rows, :m], st[:, :rows, :m],
                                                   gT[:, :rows].unsqueeze(2).to_broadcast([128, rows, m]), op=ALU.mult),
                   r=[st, gT], w=[st])
        if cs is not None:
            csb, coff = cs
            tk.dve(lambda: nc.vector.tensor_tensor(st[:, :rows, :m], st[:, :rows, :m],
                                                   csb[:, coff + c0:coff + c0 + m].unsqueeze(1).to_broadcast([128, rows, m]),
                                                   op=ALU.mult), r=[st, csb], w=[st])
        if swap:
            sv = st[:, :rows, :m].rearrange("p k (x two d) -> p k x two d", two=2, d=swap)
            dv = dstW[:, :rows, off + c0:off + c0 + m].rearrange("p k (x two d) -> p k x two d", two=2, d=swap)
            tk.act(lambda: nc.scalar.copy(dv[:, :, :, 0, :], sv[:, :, :, 1, :]), r=[st], w=[dstW])
            tk.act(lambda: nc.scalar.copy(dv[:, :, :, 1, :], sv[:, :, :, 0, :]), r=[st], w=[dstW])
        else:
            tk.act(lambda: nc.scalar.copy(dstW[:, :rows, off + c0:off + c0 + m], st[:, :rows, :m]), r=[st], w=[dstW])


def sbt(C, es, name, shape, dt):
    return Buf(es.enter_context(C.nc.sbuf_tensor(uname(name), list(shape), dt)), name)


def bc_load(C, es, name, src1d, n):
    t = sbt(C, es, name, [128, n], F32)
    C.tk.dma("sp", t[:], src1d.partition_broadcast(128), w=[t])
    return t


P_SECS = [("rq", 128), ("rq_r", 128), ("rk", 128), ("rk_r", 128), ("rv", 256), ("rg", 256),
          ("wrkv_a", 768), ("wrkv_b", 768), ("wl_a", 64), ("wl_b", 64), ("al_a", 64), ("al_b", 64),
          ("gl_a", 128), ("gl_b", 128), ("sz", 256), ("sx", 768), ("sdt", 4),
          ("dq", 256), ("dq_r", 256), ("dk", 64), ("dk_r", 64), ("dv", 64), ("diq", 128), ("diq_r", 128),
          ("dik", 32), ("dik_r", 32), ("diw", 4)]


def phaseP(C, l, only=None):
    nc, tk, dr, S, NB, TS = C.nc, C.tk, C.dr, C.S, C.NB, C.TS
    OFF = {}
    o = 0
    for nm, n in P_SECS:
        OFF[nm] = o
        o += n
    NW = o
    with ExitStack() as es:
        cs = load_consts(C, es, ["c_ident"])
        identf = cs["c_ident"]
        identb = sbt(C, es, "identb", [128, 128], BF16)
        tk.dve(lambda: nc.vector.tensor_copy(identb[:], identf[:]), r=[identf], w=[identb])
        ones_bf = sbt(C, es, "ones_bf", [128, 128], BF16)
        tk.pool(lambda: nc.gpsimd.memset(ones_bf[:], 1.0), w=[ones_bf])
        ones_f = sbt(C, es, "ones_f", [32, 32], F32)
        tk.pool(lambda: nc.gpsimd.memset(ones_f[:], 1.0), w=[ones_f])
        W = sbt(C, es, "Wp", [128, KC, NW], BF16)
        gT = sbt(C, es, "gT", [128, KC], F32)
        tk.dma("sp", gT[:], dr["norm_mix"][l].rearrange("(k p) -> p k", p=128), w=[gT], allow_slow_non_contiguous=True)
        mu_bc = bc_load(C, es, "mu_bc", dr["rwkv_mu"][l], 1024)
        om_bc = sbt(C, es, "om_bc", [128, 1024], F32)
        tk.dve(lambda: nc.vector.tensor_scalar(om_bc[:], mu_bc[:], -1.0, 1.0, op0=ALU.mult, op1=ALU.add), r=[mu_bc], w=[om_bc])
        win = dr["w_in"][l]
        with ExitStack() as es2:
            stg = Rot(nc, es2, "stg", [128, KC, 512], F32, 2)
            LW = lambda nm, c0, n, **kw: load_w_sec(C, stg, W, OFF[nm], win[:, c0:c0 + n], n, gT, **kw)
            LW("rq", C_RQ, 128); LW("rq_r", C_RQ, 128, swap=16); LW("rk", C_RK, 128); LW("rk_r", C_RK, 128, swap=16)
            LW("rv", C_RV, 256); LW("rg", C_RG, 256)
            LW("wrkv_a", C_WR, 768, cs=(om_bc, 0)); LW("wrkv_b", C_WR, 768, cs=(mu_bc, 0))
            LW("wl_a", C_WWL, 64, cs=(om_bc, 768)); LW("wl_b", C_WWL, 64, cs=(mu_bc, 768))
            LW("al_a", C_WAL, 64, cs=(om_bc, 832)); LW("al_b", C_WAL, 64, cs=(mu_bc, 832))
            LW("gl_a", C_WGL, 128, cs=(om_bc, 896)); LW("gl_b", C_WGL, 128, cs=(mu_bc, 896))
            LW("sz", C_SZ, 256); LW("sx", C_SX, 768); LW("sdt", C_SDT, 4)
            LW("dq", C_DQ, 256); LW("dq_r", C_DQ, 256, swap=32); LW("dk", C_DK, 64); LW("dk_r", C_DK, 64, swap=32)
            LW("dv", C_DV, 64); LW("diq", C_DIQ, 128); LW("diq_r", C_DIQ, 128, swap=16)
            LW("dik", C_DIK, 32); LW("dik_r", C_DIK, 32, swap=16); LW("diw", C_DIW, 4)
            tk.barrier()
        w0a0 = sbt(C, es, "w0a0", [128, 512], F32)
        tk.dma("sp", w0a0[:, 0:256], dr["rwkv_w0"][l].partition_broadcast(128), w=[w0a0])
        tk.dma("sp", w0a0[:, 256:512], dr["rwkv_a0"][l].partition_broadcast(128), w=[w0a0])
        kk_bc = bc_load(C, es, "kk_bc", dr["rwkv_k_k"][l], 256)
        ka_bc = bc_load(C, es, "ka_bc", dr["rwkv_k_a"][l], 256)
        rk_bc = bc_load(C, es, "rk_bc", dr["rwkv_r_k"][l].rearrange("h d -> (h d)"), 256)
        dtb_bc = bc_load(C, es, "dtb_bc", dr["ssm_dt_bias"][l], 4)
        alog_bc = bc_load(C, es, "alog_bc", dr["ssm_a_log"][l], 4)
        a_bc = sbt(C, es, "a_bc", [128, 4], F32)
        tk.act(lambda: nc.scalar.activation(a_bc[:], alog_bc[:], AF.Exp), r=[alog_bc], w=[a_bc])
        tk.dve(lambda: nc.vector.tensor_scalar(a_bc[:], a_bc[:], -1.0, None, op0=ALU.mult), r=[a_bc], w=[a_bc])
        w2f = sbt(C, es, "w2f", [128, 768], F32)
        tk.dma("sp", w2f[:64, 0:256], dr["rwkv_w2"][l], w=[w2f])
        tk.dma("sp", w2f[:64, 256:512], dr["rwkv_a2"][l], w=[w2f])
        tk.dma("sp", w2f[:, 512:768], dr["rwkv_g2"][l], w=[w2f])
        w2b = sbt(C, es, "w2b", [128, 768], BF16)
        tk.dve(lambda: nc.vector.tensor_copy(w2b[:64, 0:512], w2f[:64, 0:512]), r=[w2f], w=[w2b])
        tk.dve(lambda: nc.vector.tensor_copy(w2b[:, 512:768], w2f[:, 512:768]), r=[w2f], w=[w2b])
        cwT = sbt(C, es, "cwT", [128, 6, 4], F32)
        for j in range(4):
            tk.dma("sp", cwT[:, :, j], dr["ssm_conv_w"][l, j].rearrange("(c p) -> p c", p=128), w=[cwT],
                   allow_slow_non_contiguous=True)
        cbT = sbt(C, es, "cbT", [128, 6], F32)
        tk.dma("sp", cbT[:], dr["ssm_conv_b"][l].rearrange("(c p) -> p c", p=128), w=[cbT], allow_slow_non_contiguous=True)
        nw = sbt(C, es, "nw", [32, 2], F32)
        ikn_ap = dr["idx_k_norm"][l]
        tk.dma("sp", nw[:, 0:1], ikn_ap.rearrange("(p o) -> p o", o=1), w=[nw], allow_slow_non_contiguous=True)
        tk.dma("sp", nw[0:16, 1:2], ikn_ap[16:32].rearrange("(p o) -> p o", o=1), w=[nw], allow_slow_non_contiguous=True)
        tk.dma("sp", nw[16:32, 1:2], ikn_ap[0:16].rearrange("(p o) -> p o", o=1), w=[nw], allow_slow_non_contiguous=True)

        hTt = sbt(C, es, "hTt", [128, KC, TS], F32)
        sq = sbt(C, es, "sq", [128, KC, TS], BF16)
        hns = [sbt(C, es, f"hn{i}", [128, KC, TS + 1], BF16) for i in range(2)]
        rstd = sbt(C, es, "rstd", [128, TS], F32)
        tabs = {nm: sbt(C, es, "t_" + nm, [128, TS], F32) for nm in ("c_cos64T", "c_sin64T", "c_cos32T", "c_sin32T")}
        pp = Rot(nc, es, "pp", [128, 512], F32, 6, psum=True)
        ptr = Rot(nc, es, "ptr", [128, 1024], BF16, 2, psum=True)
        tA = Rot(nc, es, "tA", [128, 512], F32, 3)
        tB = Rot(nc, es, "tB", [128, 512], F32, 3)
        ob = Rot(nc, es, "ob", [128, 512], BF16, 4)
        of = Rot(nc, es, "of", [128, 512], F32, 3)
        sbj = Rot(nc, es, "sbj", [128, 256], BF16, 12)
        sfj = Rot(nc, es, "sfj", [128, 256], F32, 10)
        sm = Rot(nc, es, "sm", [128, 16], F32, 12)
        cb = Rot(nc, es, "cb", [128, TS + 3], F32, 2)
        halo = sbt(C, es, "halo", [128, 6, 3], F32)
        vout = sbt(C, es, "vout", [128, 4, 65], BF16)
        tk.pool(lambda: nc.gpsimd.memset(vout[:], 1.0), w=[vout])
        twl = sbt(C, es, "twl", [64, TS], BF16)
        alb = sbt(C, es, "alb", [64, TS], BF16)
        sgl = sbt(C, es, "sgl", [128, TS], BF16)
        dtall = sbt(C, es, "dtall", [128, 4, 4], F32)
        xsT = [sbt(C, es, f"xsT{i}", [128, TS], F32) for i in range(2)]
        BTs = [sbt(C, es, f"BTs{i}", [128, TS], BF16) for i in range(2)]
        kTs = sbt(C, es, "kTs", [128, TS], BF16)

        def fm(ps_ap, hn, terms, n=TS):
            nt = len(terms) * KC
            i = 0
            for (off, M, shift) in terms:
                for kc in range(KC):
                    tk.pe(lambda: nc.tensor.matmul(ps_ap, W[:, kc, off:off + M], hn[:, kc, 1 - shift:1 - shift + n],
                                                   start=(i == 0), stop=(i == nt - 1)), r=[W, hn], w=[ps_ap.tensor_key])
                    i += 1

        for b in range(NB):
            tk.pool(lambda: nc.gpsimd.memset(halo[:], 0.0), w=[(halo, c_) for c_ in range(6)])
            for st in range(C.NST):
                t0 = st * TS
                hn = hns[st % 2]
                hprev = hns[(st + 1) % 2]
                tk.dma("sp", hTt[:], dr["hT"][b, :, :, t0:t0 + TS].rearrange("k p t -> p k t"), w=[hTt])
                for nm, t in tabs.items():
                    tk.dma("sp", t[:], dr[nm][:, t0:t0 + TS], w=[t])
                ppn = pp.next()
                emit_norm(C, hTt, TS, hn[:, :, 1:TS + 1], hn, sq, ones_bf, ppn, rstd)
                if st == 0:
                    tk.pool(lambda: nc.gpsimd.memset(hn[:, :, 0:1], 0.0), w=[hn])
                else:
                    tk.pool(lambda: nc.gpsimd.tensor_copy(hn[:, :, 0:1], hprev[:, :, TS:TS + 1]), r=[hprev], w=[hn])
                tsl = slice(t0, t0 + TS)
                cos64, sin64, cos32, sin32 = (tabs[k] for k in ("c_cos64T", "c_sin64T", "c_cos32T", "c_sin32T"))

                def FM(terms, M=128):
                    p = pp.next()
                    ap = p[:M, :]
                    nt = len(terms) * KC
                    i = 0
                    for (off, shift) in terms:
                        for kc in range(KC):
                            tk.pe(lambda: nc.tensor.matmul(ap, W[:, kc, off:off + M], hn[:, kc, 1 - shift:1 - shift + TS],
                                                           start=(i == 0), stop=(i == nt - 1)), r=[W, hn], w=[p])
                            i += 1
                    return p

                def TM(p, c0, j, terms, N):
                    nt = len(terms) * KC
                    i = 0
                    for (off, shift) in terms:
                        for kc in range(KC):
                            a = 1 - shift + j * 128
                            tk.pe(lambda: nc.tensor.matmul(p[:, c0:c0 + N], hn[:, kc, a:a + 128], W[:, kc, off:off + N],
                                                           start=(i == 0), stop=(i == nt - 1)), r=[W, hn], w=[p])
                            i += 1

                def rope_fm(pa, pb, cos, sin, M, scale=None, dt_out=BF16):
                    a_, b_ = tA.next(), tB.next()
                    if scale is None:
                        tk.dve(lambda: nc.vector.tensor_tensor(a_[:M], pa[:M, :], cos[:M], op=ALU.mult), r=[pa, cos], w=[a_])
                        tk.dve(lambda: nc.vector.tensor_tensor(b_[:M], pb[:M, :], sin[:M], op=ALU.mult), r=[pb, sin], w=[b_])
                    else:
                        tk.dve(lambda: nc.vector.scalar_tensor_tensor(a_[:M], pa[:M, :], scale, cos[:M], op0=ALU.mult, op1=ALU.mult),
                               r=[pa, cos], w=[a_])
                        tk.dve(lambda: nc.vector.scalar_tensor_tensor(b_[:M], pb[:M, :], scale, sin[:M], op0=ALU.mult, op1=ALU.mult),
                               r=[pb, sin], w=[b_])
                    o_ = ob.next()
                    tk.pool(lambda: nc.gpsimd.tensor_tensor(o_[:M], a_[:M], b_[:M], op=ALU.add), r=[a_, b_], w=[o_])
                    return o_

                import os
                SK = os.environ.get('KSKIP', '').split(',')
                if only is not None:
                    SK = [x for x in ('ret', 'ssd', 'rw', 'dsa') if x != only]
                def sec_ret():
                    pa = FM([(OFF["rq"], 0)]); pb = FM([(OFF["rq_r"], 0)])
                    o_ = rope_fm(pa, pb, cos32, sin32, 128)
                    tk.dma("pool", dr["r_qT"][b, :, tsl], o_[:], r=[o_])
                    pa = FM([(OFF["rk"], 0)]); pb = FM([(OFF["rk_r"], 0)])
                    o_ = rope_fm(pa, pb, cos32, sin32, 128, scale=32.0 ** -0.5)
                    tk.dma("pool", dr["r_kT"][b, :, tsl], o_[:], r=[o_])
                    pt = ptr.next()
                    for j in range(4):
                        tk.pe(lambda: nc.tensor.transpose(pt[:, j * 128:(j + 1) * 128], o_[:, j * 128:(j + 1) * 128], identb[:]),
                              r=[o_, identb], w=[pt])
                    o2 = ob.next()
                    tk.act(lambda: nc.scalar.copy(o2[:], pt[:, 0:512]), r=[pt], w=[o2])
                    tk.dma("pool", dr["r_kTok"][b, tsl, :].rearrange("(j p) n -> p j n", p=128),
                           o2[:].rearrange("p (j n) -> p j n", j=4), r=[o2])
                    for j in range(4):
                        p = pp.next()
                        TM(p, 0, j, [(OFF["rv"], 0)], 512)
                        vb = sbj.next()
                        tk.act(lambda: nc.scalar.copy(vb[:], p[:, 0:256]), r=[p], w=[vb])
                        jsl = slice(t0 + j * 128, t0 + (j + 1) * 128)
                        tk.dma("pool", dr["r_v"][b, jsl, :], vb[:], r=[vb])
                        sg = sfj.next()
                        tk.act(lambda: nc.scalar.activation(sg[:], p[:, 256:512], AF.Silu), r=[p], w=[sg])
                        tk.dma("pool", dr["r_sg"][b, jsl, :], sg[:], r=[sg])

                if 'ret' not in SK:
                    sec_ret()
                def sec_ssd():
                    for j in range(4):
                        jsl = slice(t0 + j * 128, t0 + (j + 1) * 128)
                        p = pp.next()
                        TM(p, 0, j, [(OFF["sz"], 0)], 256)
                        TM(p, 256, j, [(OFF["sdt"], 0)], 4)
                        sz = sfj.next()
                        tk.act(lambda: nc.scalar.activation(sz[:], p[:, 0:256], AF.Silu), r=[p], w=[sz])
                        tk.dma("pool", dr["s_sz"][b, jsl, :], sz[:], r=[sz])
                        s1 = sm.next()
                        tk.dve(lambda: nc.vector.tensor_tensor(s1[:, 0:4], p[:, 256:260], dtb_bc[:], op=ALU.add), r=[p, dtb_bc], w=[s1])
                        tk.act(lambda: nc.scalar.activation(s1[:, 0:4], s1[:, 0:4], AF.Exp), r=[s1], w=[s1])
                        tk.act(lambda: nc.scalar.activation(dtall[:, j, :], s1[:, 0:4], AF.Ln, bias=1.0), r=[s1], w=[(dtall, j)])
                        s2 = sm.next()
                        tk.dve(lambda: nc.vector.tensor_tensor(s2[:, 0:4], dtall[:, j, :], a_bc[:], op=ALU.mult),
                               r=[(dtall, j), a_bc], w=[s2])
                        tk.dma("pool", dr["s_la"][b, jsl, :], s2[:, 0:4], r=[s2])
                    for c in range(6):
                        p = FM([(OFF["sx"] + c * 128, 0)])
                        cbuf = cb.next()
                        tk.pool(lambda: nc.gpsimd.tensor_copy(cbuf[:, 0:3], halo[:, c, :]), r=[(halo, c)], w=[(cbuf, 0)])
                        tk.act(lambda: nc.scalar.copy(cbuf[:, 3:TS + 3], p[:, :]), r=[p], w=[(cbuf, 1)])
                        tk.pool(lambda: nc.gpsimd.tensor_copy(halo[:, c, :], cbuf[:, TS:TS + 3]), r=[(cbuf, 1)], w=[(halo, c)])
                        acc = tA.next()
                        tk.dve(lambda: nc.vector.tensor_scalar(acc[:], cbuf[:, 3:TS + 3], cwT[:, c, 3:4], cbT[:, c:c + 1],
                                                               op0=ALU.mult, op1=ALU.add), r=[(cbuf, 1), cwT, cbT], w=[acc])
                        for jj in (2, 1, 0):
                            tk.dve(lambda: nc.vector.scalar_tensor_tensor(acc[:], cbuf[:, jj:jj + TS], cwT[:, c, jj:jj + 1], acc[:],
                                                                          op0=ALU.mult, op1=ALU.add),
                                   r=[(cbuf, 0), (cbuf, 1), acc, cwT], w=[acc])
                        if c < 2:
                            tk.act(lambda: nc.scalar.activation(xsT[c][:], acc[:], AF.Silu), r=[acc], w=[xsT[c]])
                        else:
                            o_ = BTs[c - 2] if c < 4 else ob.next()
                            tk.act(lambda: nc.scalar.activation(o_[:], acc[:], AF.Silu), r=[acc], w=[o_])
                            dst = dr["s_BT"] if c < 4 else dr["s_CT"]
                            r0 = (c - 2) % 2 * 128
                            tk.dma("pool", dst[b, r0:r0 + 128, tsl], o_[:], r=[o_])
                    for j in range(4):
                        jsl = slice(t0 + j * 128, t0 + (j + 1) * 128)
                        p = pp.next()
                        for c2 in range(2):
                            tk.pe(lambda: nc.tensor.transpose(p[:, c2 * 128:(c2 + 1) * 128], xsT[c2][:, j * 128:(j + 1) * 128], identf[:]),
                                  r=[xsT[c2], identf], w=[p])
                        xs = sfj.next()
                        tk.act(lambda: nc.scalar.copy(xs[:], p[:, 0:256]), r=[p], w=[xs])
                        tk.dma("pool", dr["s_xs"][b, jsl, :], xs[:], r=[xs])
                        xd = sbj.next()
                        tk.dve(lambda: nc.vector.tensor_tensor(xd[:].rearrange("p (h d) -> p h d", h=4),
                                                               xs[:].rearrange("p (h d) -> p h d", h=4),
                                                               dtall[:, j, :].unsqueeze(2).to_broadcast([128, 4, 64]), op=ALU.mult),
                               r=[xs, (dtall, j)], w=[xd])
                        tk.dma("pool", dr["s_xdt"][b, jsl, :], xd[:], r=[xd])
                        pt = ptr.next()
                        for g2 in range(2):
                            tk.pe(lambda: nc.tensor.transpose(pt[:, g2 * 128:(g2 + 1) * 128], BTs[g2][:, j * 128:(j + 1) * 128], identb[:]),
                                  r=[BTs[g2], identb], w=[pt])
                        bt = sbj.next()
                        tk.act(lambda: nc.scalar.copy(bt[:], pt[:, 0:256]), r=[pt], w=[bt])
                        tk.dma("pool", dr["s_BTok"][b, jsl, :], bt[:], r=[bt])

                if 'ssd' not in SK:
                    sec_ssd()
                def sec_rw():
                    p = FM([(OFF["wl_a"], 0), (OFF["wl_b"], 1)], M=64)
                    tk.act(lambda: nc.scalar.activation(twl[:], p[:64, :], AF.Tanh), r=[p], w=[twl])
                    p = FM([(OFF["al_a"], 0), (OFF["al_b"], 1)], M=64)
                    tk.act(lambda: nc.scalar.copy(alb[:], p[:64, :]), r=[p], w=[alb])
                    p = FM([(OFF["gl_a"], 0), (OFF["gl_b"], 1)])
                    tk.act(lambda: nc.scalar.activation(sgl[:], p[:, :], AF.Sigmoid), r=[p], w=[sgl])
                    for c2 in range(2):
                        p = FM([(OFF["wrkv_a"] + 512 + c2 * 128, 0), (OFF["wrkv_b"] + 512 + c2 * 128, 1)])
                        o_ = of.next()
                        tk.act(lambda: nc.scalar.copy(o_[:], p[:, :]), r=[p], w=[o_])
                        tk.dma("pool", dr["w_vT"][b, c2 * 128:(c2 + 1) * 128, tsl], o_[:], r=[o_])
                    for j in range(4):
                        jsl = slice(t0 + j * 128, t0 + (j + 1) * 128)
                        js = slice(j * 128, (j + 1) * 128)
                        p1 = pp.next()
                        TM(p1, 0, j, [(OFF["wrkv_a"], 0), (OFF["wrkv_b"], 1)], 512)
                        p2 = pp.next()
                        TM(p2, 0, j, [(OFF["wrkv_a"] + 512, 0), (OFF["wrkv_b"] + 512, 1)], 256)
                        tk.pe(lambda: nc.tensor.matmul(p2[:, 256:512], sgl[:, js], w2b[:, 512:768], start=True, stop=True),
                              r=[sgl, w2b], w=[p2])
                        p3 = pp.next()
                        tk.pe(lambda: nc.tensor.matmul(p3[:, 0:256], twl[:, js], w2b[:64, 0:256], start=True, stop=True),
                              r=[twl, w2b], w=[p3])
                        tk.pe(lambda: nc.tensor.matmul(p3[:, 256:512], alb[:, js], w2b[:64, 256:512], start=True, stop=True),
                              r=[alb, w2b], w=[p3])
                        wa = tA.next()
                        tk.dve(lambda: nc.vector.tensor_tensor(wa[:], p3[:], w0a0[:], op=ALU.add), r=[p3, w0a0], w=[wa])
                        tk.act(lambda: nc.scalar.activation(wa[:], wa[:], AF.Sigmoid), r=[wa], w=[wa])
                        a_ = wa[:, 256:512]
                        wf = sfj.next()
                        tk.act(lambda: nc.scalar.activation(wf[:], wa[:, 0:256], AF.Exp, scale=-math.exp(-0.5)), r=[wa], w=[wf])
                        whi = sbj.next()
                        tk.pool(lambda: nc.gpsimd.tensor_copy(whi[:], wf[:]), r=[wf], w=[whi])
                        wlo = sbj.next()
                        tk.dve(lambda: nc.vector.tensor_tensor(wlo[:], wf[:], whi[:], op=ALU.subtract), r=[wf, whi], w=[wlo])
                        tk.dma("pool", dr["w_whi"][b, jsl, :], whi[:], r=[whi])
                        tk.dma("pool", dr["w_wlo"][b, jsl, :], wlo[:], r=[wlo])
                        kkf = sfj.next()
                        tk.dve(lambda: nc.vector.tensor_tensor(kkf[:], p1[:, 256:512], kk_bc[:], op=ALU.mult), r=[p1, kk_bc], w=[kkf])
                        sqk = sfj.next()
                        tk.pool(lambda: nc.gpsimd.tensor_tensor(sqk[:], kkf[:], kkf[:], op=ALU.mult), r=[kkf], w=[sqk])
                        s1 = sm.next()
                        tk.dve(lambda: nc.vector.tensor_reduce(s1[:, 0:4], sqk[:].rearrange("p (h d) -> p h d", h=4), axis=AX.X, op=ALU.add),
                               r=[sqk], w=[s1])
                        tk.act(lambda: nc.scalar.activation(s1[:, 0:4], s1[:, 0:4], AF.Sqrt, bias=1e-12), r=[s1], w=[s1])
                        tk.dve(lambda: nc.vector.reciprocal(s1[:, 0:4], s1[:, 0:4]), r=[s1], w=[s1])
                        kkn = sfj.next()
                        tk.dve(lambda: nc.vector.tensor_tensor(kkn[:].rearrange("p (h d) -> p h d", h=4),
                                                               kkf[:].rearrange("p (h d) -> p h d", h=4),
                                                               s1[:, 0:4].unsqueeze(2).to_broadcast([128, 4, 64]), op=ALU.mult),
                               r=[kkf, s1], w=[kkn])
                        nkk = sbj.next()
                        tk.pool(lambda: nc.gpsimd.tensor_scalar(nkk[:], kkn[:], -1.0, None, op0=ALU.mult), r=[kkn], w=[nkk])
                        tk.dma("pool", dr["w_nkk"][b, jsl, :], nkk[:], r=[nkk])
                        bb = sbj.next()
                        tk.pool(lambda: nc.gpsimd.tensor_tensor(bb[:], kkn[:], a_, op=ALU.mult), r=[kkn, wa], w=[bb])
                        tk.dma("pool", dr["w_bb"][b, jsl, :], bb[:], r=[bb])
                        t1 = sfj.next()
                        tk.dve(lambda: nc.vector.scalar_tensor_tensor(t1[:], a_, -1.0, ka_bc[:], op0=ALU.add, op1=ALU.mult),
                               r=[wa, ka_bc], w=[t1])
                        kp = sfj.next()
                        tk.dve(lambda: nc.vector.scalar_tensor_tensor(kp[:], t1[:], 1.0, p1[:, 256:512], op0=ALU.add, op1=ALU.mult),
                               r=[t1, p1], w=[kp])
                        kpb = sbj.next()
                        tk.pool(lambda: nc.gpsimd.tensor_copy(kpb[:], kp[:]), r=[kp], w=[kpb])
                        tk.dma("pool", dr["w_kp"][b, jsl, :], kpb[:], r=[kpb])
                        rb = sbj.next()
                        tk.act(lambda: nc.scalar.copy(rb[:], p1[:, 0:256]), r=[p1], w=[rb])
                        tk.dma("pool", dr["w_r"][b, jsl, :], rb[:], r=[rb])
                        t2 = sfj.next()
                        tk.dve(lambda: nc.vector.tensor_tensor(t2[:], p1[:, 0:256], kp[:], op=ALU.mult), r=[p1, kp], w=[t2])
                        tk.pool(lambda: nc.gpsimd.tensor_tensor(t2[:], t2[:], rk_bc[:], op=ALU.mult), r=[t2, rk_bc], w=[t2])
                        s2 = sm.next()
                        tk.dve(lambda: nc.vector.tensor_reduce(s2[:, 0:4], t2[:].rearrange("p (h d) -> p h d", h=4), axis=AX.X, op=ALU.add),
                               r=[t2], w=[s2])
                        bo = sfj.next()
                        tk.dve(lambda: nc.vector.tensor_tensor(bo[:].rearrange("p (h d) -> p h d", h=4),
                                                               p2[:, 0:256].rearrange("p (h d) -> p h d", h=4),
                                                               s2[:, 0:4].unsqueeze(2).to_broadcast([128, 4, 64]), op=ALU.mult),
                               r=[p2, s2], w=[bo])
                        tk.dma("pool", dr["w_bonus"][b, jsl, :], bo[:], r=[bo])
                        go = sfj.next()
                        tk.act(lambda: nc.scalar.copy(go[:], p2[:, 256:512]), r=[p2], w=[go])
                        tk.dma("pool", dr["w_g"][b, jsl, :], go[:], r=[go])

                if 'rw' not in SK:
                    sec_rw()
                def sec_dsa():
                    for c2 in range(2):
                        pa = FM([(OFF["dq"] + c2 * 128, 0)]); pb = FM([(OFF["dq_r"] + c2 * 128, 0)])
                        o_ = rope_fm(pa, pb, cos64, sin64, 128)
                        tk.dma("pool", dr["d_qT"][b, c2 * 128:(c2 + 1) * 128, tsl], o_[:], r=[o_])
                    pa = FM([(OFF["dk"], 0)], M=64); pb = FM([(OFF["dk_r"], 0)], M=64)
                    o_ = rope_fm(pa, pb, cos64, sin64, 64)
                    tk.dma("pool", dr["d_kT"][b, :, tsl], o_[:64], r=[o_])
                    pa = FM([(OFF["diq"], 0)]); pb = FM([(OFF["diq_r"], 0)])
                    o_ = rope_fm(pa, pb, cos32, sin32, 128)
                    tk.dma("pool", dr["d_iqT"][b, :, tsl], o_[:], r=[o_])
                    pa = FM([(OFF["dik"], 0)], M=32); pb = FM([(OFF["dik_r"], 0)], M=32)
                    sqi = tA.next()
                    tk.act(lambda: nc.scalar.activation(sqi[:32], pa[:32, :], AF.Square), r=[pa], w=[sqi])
                    p3 = pp.next()
                    tk.pe(lambda: nc.tensor.matmul(p3[:32, :], ones_f[:], sqi[:32], start=True, stop=True), r=[sqi, ones_f], w=[p3])
                    rs = tB.next()
                    tk.act(lambda: nc.scalar.activation(rs[:32], p3[:32, :], AF.Sqrt, scale=1.0 / 32, bias=EPS), r=[p3], w=[rs])
                    tk.dve(lambda: nc.vector.reciprocal(rs[:32], rs[:32]), r=[rs], w=[rs])
                    ia = tA.next(); ib = tB.next()
                    tk.dve(lambda: nc.vector.scalar_tensor_tensor(ia[:32], pa[:32, :], nw[:, 0:1], rs[:32], op0=ALU.mult, op1=ALU.mult),
                           r=[pa, nw, rs], w=[ia])
                    tk.dve(lambda: nc.vector.scalar_tensor_tensor(ib[:32], pb[:32, :], nw[:, 1:2], rs[:32], op0=ALU.mult, op1=ALU.mult),
                           r=[pb, nw, rs], w=[ib])
                    tk.dve(lambda: nc.vector.tensor_tensor(ia[:32], ia[:32], cos32[:32], op=ALU.mult), r=[ia, cos32], w=[ia])
                    tk.dve(lambda: nc.vector.tensor_tensor(ib[:32], ib[:32], sin32[:32], op=ALU.mult), r=[ib, sin32], w=[ib])
                    o_ = ob.next()
                    tk.pool(lambda: nc.gpsimd.tensor_tensor(o_[:32], ia[:32], ib[:32], op=ALU.add), r=[ia, ib], w=[o_])
                    tk.dma("pool", dr["d_ikT"][b, :, tsl], o_[:32], r=[o_])
                    p = pp.next()
                    for j in range(4):
                        TM(p, j * 64, j, [(OFF["dv"], 0)], 64)
                        TM(p, 256 + j * 4, j, [(OFF["diw"], 0)], 4)
                    tk.act(lambda: nc.scalar.copy(vout[:, :, 0:64], p[:, 0:256].rearrange("p (j d) -> p j d", j=4)), r=[p], w=[vout])
                    tk.dma("pool", dr["d_v"][b, tsl, :].rearrange("(j p) n -> p j n", p=128), vout[:], r=[vout])
                    s1 = sm.next()
                    tk.act(lambda: nc.scalar.mul(s1[:, 0:16], p[:, 256:272], (4.0 ** -0.5) * (32.0 ** -0.5)), r=[p], w=[s1])
                    tk.dma("pool", dr["d_iw"][b, tsl, :].rearrange("(j p) n -> p j n", p=128),
                           s1[:, 0:16].rearrange("p (j n) -> p j n", j=4), r=[s1])
                if 'dsa' not in SK:
                    sec_dsa()


def phaseRec(C, l):
    nc, tk, dr, S, NB = C.nc, C.tk, C.dr, C.S, C.NB
    NCH = S // 128
    with ExitStack() as es:
        cs = load_consts(C, es, ["c_ident", "c_U", "c_retla"])
        identf, U, retla = cs["c_ident"], cs["c_U"], cs["c_retla"]
        identb = sbt(C, es, "identb", [128, 128], BF16)
        tk.dve(lambda: nc.vector.tensor_copy(identb[:], identf[:]), r=[identf], w=[identb])
        ones_f = sbt(C, es, "ones_f", [128, 128], F32)
        tk.pool(lambda: nc.gpsimd.memset(ones_f[:], 1.0), w=[ones_f])
        dsk_bc = bc_load(C, es, "dsk_bc", dr["ssm_d"][l], 4)
        nrm_bc = bc_load(C, es, "nrm_bc", dr["ssm_norm"][l], 256)
        psm = Rot(nc, es, "psm", [128, 512], F32, 1, psum=True)
        pBc = Rot(nc, es, "pBc", [128, 512], F32, 1, psum=True)
        psc = Rot(nc, es, "psc", [128, 512], F32, 2, psum=True)
        pY = Rot(nc, es, "pY", [128, 512], F32, 1, psum=True)
        pdS = Rot(nc, es, "pdS", [128, 512], F32, 1, psum=True)
        ptr = Rot(nc, es, "ptr", [128, 1024], BF16, 1, psum=True)
        decTs = Rot(nc, es, "decT", [128, 512], F32, 2)
        Es = Rot(nc, es, "E", [128, 512], F32, 2)
        args = Rot(nc, es, "arg", [128, 512], F32, 2)
        smalls = Rot(nc, es, "small", [128, 16], F32, 4)
        s2s = Rot(nc, es, "s2s", [128, 16], F32, 4)
        qTs = Rot(nc, es, "qT", [128, 512], BF16, 2)
        kTs = Rot(nc, es, "kT", [128, 512], BF16, 2)
        kToks = Rot(nc, es, "kTok", [128, 256], BF16, 2)
        vs = Rot(nc, es, "v", [128, 256], BF16, 2)
        las = Rot(nc, es, "la", [128, 4], F32, 2)
        f1s = Rot(nc, es, "f1", [128, 256], F32, 2)
        f2s = Rot(nc, es, "f2", [128, 256], F32, 2)
        PTs = Rot(nc, es, "PT", [128, 512], BF16, 2)
        qtils = Rot(nc, es, "qtil", [128, 512], BF16, 2)
        xts = Rot(nc, es, "xt", [128, 256], BF16, 2)
        tmps = Rot(nc, es, "tmp", [128, 256], F32, 4)
        obs = Rot(nc, es, "ob", [128, 256], BF16, 2)
        oTs = Rot(nc, es, "oTt", [128, 256], BF16, 2)
        S32 = sbt(C, es, "S32", [128, 256], F32)
        Sbf = sbt(C, es, "Sbf", [128, 256], BF16)

        def prep(la):
            pm = psm.next()
            tk.pe(lambda: nc.tensor.matmul(pm[:, 0:4], U[:], la[:, 0:4], start=True, stop=True), r=[U, la], w=[pm])
            tk.pe(lambda: nc.tensor.matmul(pm[:, 4:8], ones_f[:], la[:, 0:4], start=True, stop=True), r=[ones_f, la], w=[pm])
            sm = smalls.next()
            tk.act(lambda: nc.scalar.copy(sm[:, 8:12], pm[:, 0:4]), r=[pm], w=[sm])
            tk.dve(lambda: nc.vector.tensor_tensor(sm[:, 0:4], pm[:, 4:8], sm[:, 8:12], op=ALU.subtract), r=[pm, sm], w=[sm])
            tk.act(lambda: nc.scalar.activation(sm[:, 0:4], sm[:, 0:4], AF.Exp), r=[sm], w=[sm])
            tk.act(lambda: nc.scalar.activation(sm[:, 4:8], pm[:, 4:8], AF.Exp), r=[pm, sm], w=[sm])
            pb = pBc.next()
            for h in range(4):
                tk.pe(lambda: nc.tensor.matmul(pb[:, h * 128:(h + 1) * 128], la[:, h:h + 1].to_broadcast([128, 128]), U[:],
                                               start=True, stop=True), r=[la, U], w=[pb])
            arg = args.next()
            for h in range(4):
                tk.dve(lambda: nc.vector.tensor_scalar(arg[:, h * 128:(h + 1) * 128], pb[:, h * 128:(h + 1) * 128],
                                                       sm[:, 8 + h:9 + h], 0.0, op0=ALU.subtract, op1=ALU.min),
                       r=[pb, sm], w=[arg])
            decT = decTs.next()
            tk.act(lambda: nc.scalar.activation(decT[:], arg[:], AF.Exp), r=[arg], w=[decT])
            tk.dve(lambda: nc.vector.tensor_tensor(decT[:].rearrange("p (h t) -> p h t", h=4),
                                                   decT[:].rearrange("p (h t) -> p h t", h=4),
                                                   U[:].unsqueeze(1).to_broadcast([128, 4, 128]), op=ALU.mult),
                   r=[decT, U], w=[decT])
            E = Es.next()
            tk.act(lambda: nc.scalar.activation(E[:], pb[:], AF.Exp), r=[pb], w=[E])
            return decT, E, sm

        for mix in ("ret", "ssd"):
            N = 32 if mix == "ret" else 128
            if mix == "ret":
                dec_const = prep(retla)
            for b in range(NB):
                tk.pool(lambda: nc.gpsimd.memset(S32[:], 0.0), w=[S32])
                tk.pool(lambda: nc.gpsimd.memset(Sbf[:], 0.0), w=[Sbf])
                for c in range(NCH):
                    tsl = slice(c * 128, (c + 1) * 128)
                    qT, kT, kTok, v = qTs.next(), kTs.next(), kToks.next(), vs.next()
                    f1, f2 = f1s.next(), f2s.next()
                    if mix == "ret":
                        tk.dma("sp", qT[:32, :].rearrange("n (h t) -> n h t", h=4),
                               dr["r_qT"][b, :, tsl].rearrange("(h n) t -> n h t", h=4), w=[qT])
                        tk.dma("sp", kT[:32, :].rearrange("n (h t) -> n h t", h=4),
                               dr["r_kT"][b, :, tsl].rearrange("(h n) t -> n h t", h=4), w=[kT])
                        tk.dma("sp", kTok[:, 0:128], dr["r_kTok"][b, tsl, :], w=[kTok])
                        tk.dma("sp", v[:], dr["r_v"][b, tsl, :], w=[v])
                        tk.dma("sp", f1[:], dr["r_sg"][b, tsl, :], w=[f1])
                        decT, E, sm = dec_const
                    else:
                        tk.dma("sp", qT[:, 0:256].rearrange("n (g t) -> n g t", g=2),
                               dr["s_CT"][b, :, tsl].rearrange("(g n) t -> n g t", g=2), w=[qT])
                        tk.dma("sp", kT[:, 0:256].rearrange("n (g t) -> n g t", g=2),
                               dr["s_BT"][b, :, tsl].rearrange("(g n) t -> n g t", g=2), w=[kT])
                        tk.dma("sp", kTok[:], dr["s_BTok"][b, tsl, :], w=[kTok])
                        tk.dma("sp", v[:], dr["s_xdt"][b, tsl, :], w=[v])
                        tk.dma("sp", f1[:], dr["s_sz"][b, tsl, :], w=[f1])
                        tk.dma("sp", f2[:], dr["s_xs"][b, tsl, :], w=[f2])
                        la = las.next()
                        tk.dma("sp", la[:], dr["s_la"][b, tsl, :], w=[la])
                        decT, E, sm = prep(la)
                    sc = psc.next()
                    PT = PTs.next()
                    qtil = qtils.next()
                    if mix == "ret":
                        for h in range(4):
                            hs = slice(h * 128, (h + 1) * 128)
                            tk.pe(lambda: nc.tensor.matmul(sc[:, hs], kT[:32, hs], qT[:32, hs], start=True, stop=True),
                                  r=[kT, qT], w=[sc])
                        tk.dve(lambda: nc.vector.tensor_tensor(PT[:], sc[:], decT[:], op=ALU.mult), r=[sc, decT], w=[PT])
                        tk.dve(lambda: nc.vector.tensor_tensor(qtil[:32, :], qT[:32, :], E[:32, :], op=ALU.mult), r=[qT, E], w=[qtil])
                    else:
                        for g in range(2):
                            gs = slice(g * 128, (g + 1) * 128)
                            tk.pe(lambda: nc.tensor.matmul(sc[:, gs], kT[:, gs], qT[:, gs], start=True, stop=True),
                                  r=[kT, qT], w=[sc])
                        v4 = lambda ap: ap.rearrange("p (g e t) -> p g e t", g=2, e=2)
                        bcg = lambda ap: ap.rearrange("p (g t) -> p g t", g=2).unsqueeze(2).to_broadcast([128, 2, 2, 128])
                        tk.dve(lambda: nc.vector.tensor_tensor(v4(PT[:]), v4(decT[:]), bcg(sc[:, 0:256]), op=ALU.mult),
                               r=[sc, decT], w=[PT])
                        tk.dve(lambda: nc.vector.tensor_tensor(v4(qtil[:]), v4(E[:]), bcg(qT[:, 0:256]), op=ALU.mult),
                               r=[qT, E], w=[qtil])
                    py = pY.next()
                    for h in range(4):
                        hs = slice(h * 128, (h + 1) * 128)
                        ps_ = slice(h * 64, (h + 1) * 64)
                        tk.pe(lambda: nc.tensor.matmul(py[:, ps_], PT[:, hs], v[:, ps_], start=True, stop=False), r=[PT, v], w=[py])
                        tk.pe(lambda: nc.tensor.matmul(py[:, ps_], qtil[:N, hs], Sbf[:N, ps_], start=False, stop=True),
                              r=[qtil, Sbf], w=[py])
                    xt = xts.next()
                    tk.dve(lambda: nc.vector.tensor_tensor(xt[:].rearrange("p (h d) -> p h d", h=4),
                                                           v[:].rearrange("p (h d) -> p h d", h=4),
                                                           sm[:, 0:4].unsqueeze(2).to_broadcast([128, 4, 64]), op=ALU.mult),
                           r=[v, sm], w=[xt])
                    pd = pdS.next()
                    if mix == "ret":
                        for h in range(4):
                            ps_ = slice(h * 64, (h + 1) * 64)
                            tk.pe(lambda: nc.tensor.matmul(pd[:32, ps_], kTok[:, h * 32:(h + 1) * 32], xt[:, ps_], start=True, stop=True),
                                  r=[kTok, xt], w=[pd])
                    else:
                        for g in range(2):
                            gs = slice(g * 128, (g + 1) * 128)
                            tk.pe(lambda: nc.tensor.matmul(pd[:, gs], kTok[:, gs], xt[:, gs], start=True, stop=True),
                                  r=[kTok, xt], w=[pd])
                    for h in range(4):
                        ps_ = slice(h * 64, (h + 1) * 64)
                        tk.dve(lambda: nc.vector.scalar_tensor_tensor(S32[:N, ps_], S32[:N, ps_], sm[:N, 4 + h:5 + h], pd[:N, ps_],
                                                                      op0=ALU.mult, op1=ALU.add), r=[S32, sm, pd, Sbf], w=[S32])
                    tk.act(lambda: nc.scalar.copy(Sbf[:N, :], S32[:N, :]), r=[S32], w=[Sbf])
                    ob = obs.next()
                    s2 = s2s.next()
                    if mix == "ret":
                        t1 = tmps.next()
                        tk.act(lambda: nc.scalar.activation(t1[:], py[:, 0:256], AF.Square), r=[py], w=[t1])
                        tk.dve(lambda: nc.vector.tensor_reduce(s2[:, 0:4], t1[:].rearrange("p (h d) -> p h d", h=4), axis=AX.X, op=ALU.add),
                               r=[t1], w=[s2])
                        tk.act(lambda: nc.scalar.activation(s2[:, 0:4], s2[:, 0:4], AF.Sqrt, scale=1.0 / 64, bias=EPS), r=[s2], w=[s2])
                        tk.dve(lambda: nc.vector.reciprocal(s2[:, 0:4], s2[:, 0:4]), r=[s2], w=[s2])
                        t2 = tmps.next()
                        tk.dve(lambda: nc.vector.tensor_tensor(t2[:].rearrange("p (h d) -> p h d", h=4),
                                                               py[:, 0:256].rearrange("p (h d) -> p h d", h=4),
                                                               s2[:, 0:4].unsqueeze(2).to_broadcast([128, 4, 64]), op=ALU.mult),
                               r=[py, s2], w=[t2])
                        tk.pool(lambda: nc.gpsimd.tensor_tensor(ob[:], t2[:], f1[:], op=ALU.mult), r=[t2, f1], w=[ob])
                        ch0 = 0
                    else:
                        t1 = tmps.next()
                        tk.pool(lambda: nc.gpsimd.tensor_tensor(t1[:].rearrange("p (h d) -> p h d", h=4),
                                                                f2[:].rearrange("p (h d) -> p h d", h=4),
                                                                dsk_bc[:].unsqueeze(2).to_broadcast([128, 4, 64]), op=ALU.mult),
                                r=[f2, dsk_bc], w=[t1])
                        tk.dve(lambda: nc.vector.tensor_tensor(t1[:], t1[:], py[:, 0:256], op=ALU.add), r=[t1, py], w=[t1])
                        tk.pool(lambda: nc.gpsimd.tensor_tensor(t1[:], t1[:], f1[:], op=ALU.mult), r=[t1, f1], w=[t1])
                        t2 = tmps.next()
                        tk.act(lambda: nc.scalar.activation(t2[:], t1[:], AF.Square, accum_out=s2[:, 0:1]), r=[t1], w=[t2, s2])
                        tk.act(lambda: nc.scalar.activation(s2[:, 0:1], s2[:, 0:1], AF.Sqrt, scale=1.0 / 256, bias=EPS), r=[s2], w=[s2])
                        tk.dve(lambda: nc.vector.reciprocal(s2[:, 0:1], s2[:, 0:1]), r=[s2], w=[s2])
                        tk.dve(lambda: nc.vector.scalar_tensor_tensor(ob[:], t1[:], s2[:, 0:1], nrm_bc[:], op0=ALU.mult, op1=ALU.mult),
                               r=[t1, s2, nrm_bc], w=[ob])
                        ch0 = 4
                    pt = ptr.next()
                    for c2 in range(2):
                        tk.pe(lambda: nc.tensor.transpose(pt[:, c2 * 128:(c2 + 1) * 128], ob[:, c2 * 128:(c2 + 1) * 128], identb[:]),
                              r=[ob, identb], w=[pt])
                    oTt = oTs.next()
                    tk.act(lambda: nc.scalar.copy(oTt[:], pt[:, 0:256]), r=[pt], w=[oTt])
                    tk.dma("pool", dr["oT"][b, ch0:ch0 + 2, :, tsl].rearrange("k p t -> p k t"),
                           oTt[:].rearrange("p (k t) -> p k t", k=2), r=[oTt])


def phaseRW(C, l):
    nc, tk, dr, S, NB = C.nc, C.tk, C.dr, C.S, C.NB
    assert NB == 2
    NCH = S // 64
    with ExitStack() as es:
        cs = load_consts(C, es, ["c_ident", "c_E2"])
        identf, E2 = cs["c_ident"], cs["c_E2"]
        identb = sbt(C, es, "identb", [128, 128], BF16)
        tk.dve(lambda: nc.vector.tensor_copy(identb[:], identf[:]), r=[identf], w=[identb])
        lnw_bc = bc_load(C, es, "lnw_bc", dr["rwkv_ln_w"][l], 256)
        lnb_bc = bc_load(C, es, "lnb_bc", dr["rwkv_ln_b"][l], 256)
        pA = Rot(nc, es, "pA", [128, 512], F32, 2, psum=True)
        pB = Rot(nc, es, "pB", [128, 512], F32, 2, psum=True)
        pC = Rot(nc, es, "pC", [128, 512], F32, 2, psum=True)
        pT = Rot(nc, es, "pT", [128, 512], F32, 1, psum=True)
        pO = Rot(nc, es, "pO", [128, 1024], BF16, 1, psum=True)
        names = ("w_whi", "w_wlo", "w_nkk", "w_bb", "w_kp", "w_r")
        tl = {nm: Rot(nc, es, "c_" + nm, [128, 256], BF16, 2) for nm in names}
        vTs = Rot(nc, es, "vTc", [128, 256], F32, 2)
        ychs = Rot(nc, es, "ych", [128, 256], F32, 2)
        kvs = Rot(nc, es, "kv", [128, 256], F32, 3)
        tmpa = Rot(nc, es, "tmpa", [128, 256], F32, 4)
        sas = Rot(nc, es, "sa", [128, 4], F32, 3)
        Sb = [sbt(C, es, f"Sst{i}", [128, 256], F32) for i in range(2)]
        for S_ in Sb:
            tk.pool(lambda: nc.gpsimd.memset(S_[:], 0.0), w=[S_])
        rsbs = Rot(nc, es, "rsb", [128, 256], F32, 3)
        tmpp = Rot(nc, es, "tmpp", [128, 256], F32, 3)
        gstep = 0
        pending = None
        yjunk = sbt(C, es, "yjunk", [128, 256], F32)

        def flush_y(pend):
            tmp3_, t_, ych_ = pend
            tk.dve(lambda: nc.vector.tensor_reduce(h4(ych_[:])[:, :, t_], h4(tmp3_[:]), axis=AX.X, op=ALU.add), r=[tmp3_], w=[ych_])

        bons = Rot(nc, es, "bon", [64, 512], F32, 2)
        gs_ = Rot(nc, es, "gg", [64, 512], F32, 2)
        yts = Rot(nc, es, "yt", [64, 512], F32, 2)
        ycs = Rot(nc, es, "yc", [64, 512], F32, 2)
        sqs = Rot(nc, es, "sqy", [64, 512], F32, 2)
        st8 = Rot(nc, es, "st8", [64, 16], F32, 4)
        obs = Rot(nc, es, "obw", [64, 512], BF16, 2)
        oTs = Rot(nc, es, "oTw", [128, 256], BF16, 2)
        h4 = lambda ap: ap.rearrange("p (h k) -> p h k", h=4)
        for c in range(NCH):
            csl = slice(c * 64, (c + 1) * 64)
            cur = {}
            for nm in names:
                t = tl[nm].next()
                for b in range(2):
                    tk.dma("sp", t[b * 64:(b + 1) * 64, :], dr[nm][b, csl, :], w=[t])
                cur[nm] = t
            vT = vTs.next()
            for b in range(2):
                tk.dma("sp", h4(vT[b * 64:(b + 1) * 64, :]), dr["w_vT"][b, :, csl].rearrange("(h v) t -> v h t", h=4), w=[vT])
            bon, gg = bons.next(), gs_.next()
            for b in range(2):
                tk.dma("sp", bon[:, b * 256:(b + 1) * 256], dr["w_bonus"][b, csl, :], w=[bon])
                tk.dma("sp", gg[:, b * 256:(b + 1) * 256], dr["w_g"][b, csl, :], w=[gg])
            ych = ychs.next()
            for t in range(64):
                E2t = E2[:, t * 128:(t + 1) * 128]
                pa, pb, pc = pA.next(), pB.next(), pC.next()
                tk.pe(lambda: nc.tensor.matmul(pa[:, 0:256], E2t, cur["w_whi"][:], start=True, stop=False), r=[E2, cur["w_whi"]], w=[pa])
                tk.pe(lambda: nc.tensor.matmul(pa[:, 0:256], E2t, cur["w_wlo"][:], start=False, stop=True), r=[E2, cur["w_wlo"]], w=[pa])
                tk.pe(lambda: nc.tensor.matmul(pa[:, 256:512], E2t, cur["w_nkk"][:], start=True, stop=True), r=[E2, cur["w_nkk"]], w=[pa])
                tk.pe(lambda: nc.tensor.matmul(pb[:, 0:256], E2t, cur["w_bb"][:], start=True, stop=True), r=[E2, cur["w_bb"]], w=[pb])
                tk.pe(lambda: nc.tensor.matmul(pb[:, 256:512], E2t, cur["w_kp"][:], start=True, stop=True), r=[E2, cur["w_kp"]], w=[pb])
                tk.pe(lambda: nc.tensor.matmul(pc[:, 0:256], E2t, cur["w_r"][:], start=True, stop=True), r=[E2, cur["w_r"]], w=[pc])
                kv = kvs.next()
                for h in range(4):
                    hs = slice(h * 64, (h + 1) * 64)
                    tk.act(lambda: nc.scalar.activation(kv[:, hs], pb[:, 256 + h * 64:256 + (h + 1) * 64], AF.Copy,
                                                        scale=vT[:, h * 64 + t:h * 64 + t + 1]), r=[pb, vT], w=[kv])
                tmp = tmpa.next()
                sa = sas.next()
                So, Sn = Sb[gstep % 2], Sb[(gstep + 1) % 2]
                gstep += 1
                tk.dve(lambda: nc.vector.tensor_tensor(tmp[:], So[:], pa[:, 256:512], op=ALU.mult), r=[So, pa], w=[tmp])
                tk.dve(lambda: nc.vector.tensor_tensor(Sn[:], So[:], pa[:, 0:256], op=ALU.mult), r=[So, pa], w=[Sn])
                if pending is not None:
                    flush_y(pending)
                    pending = None
                tk.dve(lambda: nc.vector.tensor_reduce(sa[:], h4(tmp[:]), axis=AX.X, op=ALU.add), r=[tmp], w=[sa])
                tk.dve(lambda: nc.vector.tensor_tensor(Sn[:], Sn[:], kv[:], op=ALU.add), r=[Sn, kv], w=[Sn])
                tmp2 = tmpa.next()
                tk.dve(lambda: nc.vector.tensor_tensor(h4(tmp2[:]), h4(pb[:, 0:256]), sa[:].unsqueeze(2).to_broadcast([128, 4, 64]),
                                                       op=ALU.mult), r=[pb, sa], w=[tmp2])
                tk.dve(lambda: nc.vector.tensor_tensor(Sn[:], Sn[:], tmp2[:], op=ALU.add), r=[Sn, tmp2], w=[Sn])
                tmp3 = tmpp.next()
                tk.dve(lambda: nc.vector.tensor_tensor(tmp3[:], Sn[:], pc[:, 0:256], op=ALU.mult), r=[Sn, pc], w=[tmp3])
                pending = (tmp3, t, ych)
            flush_y(pending)
            pending = None
            pt = pT.next()
            for h in range(4):
                tk.pe(lambda: nc.tensor.transpose(pt[:64, h * 128:(h + 1) * 128], ych[:, h * 64:(h + 1) * 64], identf[:]),
                      r=[ych, identf], w=[pt])
            yt = yts.next()
            tk.act(lambda: nc.scalar.copy(yt[:].rearrange("p (b h v) -> p h b v", b=2, h=4),
                                          pt[:64, :].rearrange("p (h b v) -> p h b v", h=4, b=2)), r=[pt], w=[yt])
            g8 = lambda ap: ap.rearrange("p (g v) -> p g v", g=8)
            s1 = st8.next()
            tk.dve(lambda: nc.vector.tensor_reduce(s1[:, 0:8], g8(yt[:]), axis=AX.X, op=ALU.add), r=[yt], w=[s1])
            tk.dve(lambda: nc.vector.tensor_scalar(s1[:, 0:8], s1[:, 0:8], -1.0 / 64, None, op0=ALU.mult), r=[s1], w=[s1])
            yc = ycs.next()
            tk.dve(lambda: nc.vector.tensor_tensor(g8(yc[:]), g8(yt[:]), s1[:, 0:8].unsqueeze(2).to_broadcast([64, 8, 64]), op=ALU.add),
                   r=[yt, s1], w=[yc])
            sq = sqs.next()
            tk.pool(lambda: nc.gpsimd.tensor_tensor(sq[:], yc[:], yc[:], op=ALU.mult), r=[yc], w=[sq])
            tk.dve(lambda: nc.vector.tensor_reduce(s1[:, 8:16], g8(sq[:]), axis=AX.X, op=ALU.add), r=[sq], w=[s1])
            tk.act(lambda: nc.scalar.activation(s1[:, 8:16], s1[:, 8:16], AF.Sqrt, scale=1.0 / 64, bias=64e-5), r=[s1], w=[s1])
            tk.dve(lambda: nc.vector.reciprocal(s1[:, 8:16], s1[:, 8:16]), r=[s1], w=[s1])
            tk.dve(lambda: nc.vector.tensor_tensor(g8(yc[:]), g8(yc[:]), s1[:, 8:16].unsqueeze(2).to_broadcast([64, 8, 64]), op=ALU.mult),
                   r=[yc, s1], w=[yc])
            b2 = lambda ap: ap.rearrange("p (b f) -> p b f", b=2)
            bcb = lambda t_: t_[:64, :].unsqueeze(1).to_broadcast([64, 2, 256])
            tk.pool(lambda: nc.gpsimd.tensor_tensor(b2(yc[:]), b2(yc[:]), bcb(lnw_bc), op=ALU.mult), r=[yc, lnw_bc], w=[yc])
            tk.pool(lambda: nc.gpsimd.tensor_tensor(b2(yc[:]), b2(yc[:]), bcb(lnb_bc), op=ALU.add), r=[yc, lnb_bc], w=[yc])
            tk.dve(lambda: nc.vector.tensor_tensor(yc[:], yc[:], bon[:], op=ALU.add), r=[yc, bon], w=[yc])
            ob = obs.next()
            tk.pool(lambda: nc.gpsimd.tensor_tensor(ob[:], yc[:], gg[:], op=ALU.mult), r=[yc, gg], w=[ob])
            po = pO.next()
            for q in range(4):
                tk.pe(lambda: nc.tensor.transpose(po[:, q * 64:(q + 1) * 64], ob[:, q * 128:(q + 1) * 128], identb[:64, :64]),
                      r=[ob, identb], w=[po])
            oTt = oTs.next()
            tk.act(lambda: nc.scalar.copy(oTt[:], po[:, 0:256]), r=[po], w=[oTt])
            for b in range(2):
                tk.dma("pool", dr["oT"][b, 2:4, :, csl].rearrange("k p t -> p k t"),
                       oTt[:, b * 128:(b + 1) * 128].rearrange("p (k t) -> p k t", k=2), r=[oTt])


def phaseDSA(C, l):
    nc, tk, dr, S, NB = C.nc, C.tk, C.dr, C.S, C.NB
    NQ = S // 128
    TOPK = float(min(256, S // 4))
    NIT = 20
    with ExitStack() as es:
        cs = load_consts(C, es, ["c_ident", "c_negU"])
        identf, negU = cs["c_ident"], cs["c_negU"]
        identb = sbt(C, es, "identb", [128, 128], BF16)
        tk.dve(lambda: nc.vector.tensor_copy(identb[:], identf[:]), r=[identf], w=[identb])
        thr0 = sbt(C, es, "thr0", [128, 1], F32)
        tk.pool(lambda: nc.gpsimd.memset(thr0[:], NEG_THR), w=[thr0])
        kT = sbt(C, es, "dkT", [64, S], BF16)
        ikT = sbt(C, es, "dikT", [32, S], BF16)
        vaug = sbt(C, es, "vaug", [128, NQ, 65], BF16)
        pp = Rot(nc, es, "pp", [128, 512], F32, 4, psum=True)
        pmr = Rot(nc, es, "pm", [128, 1024], BF16, 2, psum=True)
        pout = Rot(nc, es, "pout", [128, 512], F32, 1, psum=True)
        ptr = Rot(nc, es, "ptr", [128, 1024], BF16, 1, psum=True)
        scs = Rot(nc, es, "sc", [128, S], F32, 2)
        junk = sbt(C, es, "junk", [128, S], BF16)
        masks = Rot(nc, es, "mask", [128, S], BF16, 2)
        rls = Rot(nc, es, "rl", [128, 512], F32, 3)
        es_ = Rot(nc, es, "eexp", [128, 512], BF16, 3)
        pTs = Rot(nc, es, "pT", [128, 512], BF16, 3)
        iqs = Rot(nc, es, "iq", [32, 512], BF16, 2)
        qs = Rot(nc, es, "q", [64, 512], BF16, 2)
        iws = Rot(nc, es, "iw", [128, 4], F32, 2)
        st = Rot(nc, es, "bst", [128, 8], F32, 2)
        obs = Rot(nc, es, "obd", [128, 256], BF16, 2)
        rcs = Rot(nc, es, "rc", [128, 4], F32, 2)
        oTs = Rot(nc, es, "oTd", [128, 256], BF16, 2)
        for b in range(NB):
            tk.dma("sp", kT[:], dr["d_kT"][b], w=[kT])
            tk.dma("sp", ikT[:], dr["d_ikT"][b], w=[ikT])
            tk.dma("sp", vaug[:], dr["d_v"][b].rearrange("(j p) n -> p j n", p=128), w=[vaug])
            for i in range(NQ):
                L = (i + 1) * 128
                tsl = slice(i * 128, (i + 1) * 128)
                iq, q, iw = iqs.next(), qs.next(), iws.next()
                tk.dma("sp", iq[:].rearrange("d (h t) -> d h t", h=4), dr["d_iqT"][b, :, tsl].rearrange("(h d) t -> d h t", h=4), w=[iq])
                tk.dma("sp", q[:].rearrange("d (h t) -> d h t", h=4), dr["d_qT"][b, :, tsl].rearrange("(h d) t -> d h t", h=4), w=[q])
                tk.dma("sp", iw[:], dr["d_iw"][b, tsl, :], w=[iw])
                sc = scs.next()
                for k0 in range(0, L, 512):
                    w_ = min(512, L - k0)
                    for h in range(4):
                        p = pp.next()
                        tk.pe(lambda: nc.tensor.matmul(p[:, :w_], iq[:, h * 128:(h + 1) * 128], ikT[:, k0:k0 + w_], start=True, stop=True),
                              r=[iq, ikT], w=[p])
                        rl = rls.next()
                        tk.act(lambda: nc.scalar.activation(rl[:, :w_], p[:, :w_], AF.Relu), r=[p], w=[rl])
                        if h == 0:
                            tk.dve(lambda: nc.vector.tensor_scalar(sc[:, k0:k0 + w_], rl[:, :w_], iw[:, 0:1], None, op0=ALU.mult),
                                   r=[rl, iw], w=[sc])
                        else:
                            tk.dve(lambda: nc.vector.scalar_tensor_tensor(sc[:, k0:k0 + w_], rl[:, :w_], iw[:, h:h + 1], sc[:, k0:k0 + w_],
                                                                          op0=ALU.mult, op1=ALU.add), r=[rl, iw, sc], w=[sc])
                b_ = st.next()
                if i >= 2:
                    tk.dve(lambda: nc.vector.tensor_reduce(b_[:, 5:6], sc[:, :L], axis=AX.X, op=ALU.max), r=[sc], w=[b_])
                    tk.dve(lambda: nc.vector.tensor_reduce(b_[:, 6:7], sc[:, :L], axis=AX.X, op=ALU.min), r=[sc], w=[b_])
                    tk.dve(lambda: nc.vector.tensor_scalar(b_[:, 0:1], b_[:, 6:7], -1.0, None, op0=ALU.add), r=[b_], w=[b_])
                    tk.dve(lambda: nc.vector.scalar_tensor_tensor(b_[:, 1:2], b_[:, 5:6], 1.0, b_[:, 0:1], op0=ALU.add, op1=ALU.subtract),
                           r=[b_], w=[b_])
                tk.dve(lambda: nc.vector.tensor_tensor(sc[:, i * 128:L], sc[:, i * 128:L], negU[:], op=ALU.add), r=[sc, negU], w=[sc])
                if i >= 2:
                    for it in range(NIT):
                        f = 0.5 ** (it + 1)
                        tk.dve(lambda: nc.vector.scalar_tensor_tensor(b_[:, 2:3], b_[:, 1:2], f, b_[:, 0:1], op0=ALU.mult, op1=ALU.add),
                               r=[b_], w=[b_])
                        tk.dve(lambda: nc.vector.tensor_scalar(junk[:, :L], sc[:, :L], b_[:, 2:3], None, op0=ALU.is_ge, op1=ALU.add,
                                                               accum_out=b_[:, 3:4]), r=[sc, b_], w=[junk, b_])
                        tk.dve(lambda: nc.vector.tensor_scalar(b_[:, 4:5], b_[:, 3:4], TOPK, f, op0=ALU.is_ge, op1=ALU.mult), r=[b_], w=[b_])
                        tk.dve(lambda: nc.vector.scalar_tensor_tensor(b_[:, 0:1], b_[:, 4:5], b_[:, 1:2], b_[:, 0:1], op0=ALU.mult, op1=ALU.add),
                               r=[b_], w=[b_])
                    thr = b_[:, 0:1]
                    thr_r = [b_]
                else:
                    thr = thr0[:, 0:1]
                    thr_r = [thr0]
                mask = masks.next()
                tk.dve(lambda: nc.vector.tensor_scalar(mask[:, :L], sc[:, :L], thr, None, op0=ALU.is_ge), r=[sc] + thr_r, w=[mask])
                po = pout.next()
                for g0 in range(0, i + 1, 8):
                    pm = pmr.next()
                    g1 = min(i + 1, g0 + 8)
                    for j in range(g0, g1):
                        tk.pe(lambda: nc.tensor.transpose(pm[:, (j - g0) * 128:(j - g0 + 1) * 128], mask[:, j * 128:(j + 1) * 128], identb[:]),
                              r=[mask, identb], w=[pm])
                    for j in range(g0, g1):
                        lg = pp.next()
                        tk.pe(lambda: nc.tensor.matmul(lg[:, 0:512], kT[:, j * 128:(j + 1) * 128], q[:, :], start=True, stop=True),
                              r=[kT, q], w=[lg])
                        e = es_.next()
                        tk.act(lambda: nc.scalar.activation(e[:], lg[:], AF.Exp, scale=64.0 ** -0.5), r=[lg], w=[e])
                        pT = pTs.next()
                        tk.dve(lambda: nc.vector.tensor_tensor(pT[:].rearrange("p (h t) -> p h t", h=4),
                                                               e[:].rearrange("p (h t) -> p h t", h=4),
                                                               pm[:, (j - g0) * 128:(j - g0 + 1) * 128].unsqueeze(1).to_broadcast([128, 4, 128]),
                                                               op=ALU.mult), r=[e, pm], w=[pT])
                        for h in range(4):
                            tk.pe(lambda: nc.tensor.matmul(po[:, h * 65:(h + 1) * 65], pT[:, h * 128:(h + 1) * 128], vaug[:, j, :],
                                                           start=(j == 0 and h == 0), stop=(j == i and h == 3)), r=[pT, vaug], w=[po])
                rc = rcs.next()
                po3 = po[:, 0:260].rearrange("p (h e) -> p h e", h=4)
                tk.dve(lambda: nc.vector.reciprocal(rc[:], po3[:, :, 64]), r=[po], w=[rc])
                ob = obs.next()
                tk.dve(lambda: nc.vector.tensor_tensor(ob[:].rearrange("p (h d) -> p h d", h=4), po3[:, :, 0:64],
                                                       rc[:].unsqueeze(2).to_broadcast([128, 4, 64]), op=ALU.mult), r=[po, rc], w=[ob])
                pt = ptr.next()
                for c2 in range(2):
                    tk.pe(lambda: nc.tensor.transpose(pt[:, c2 * 128:(c2 + 1) * 128], ob[:, c2 * 128:(c2 + 1) * 128], identb[:]),
                          r=[ob, identb], w=[pt])
                oTt = oTs.next()
                tk.act(lambda: nc.scalar.copy(oTt[:], pt[:, 0:256]), r=[pt], w=[oTt])
                tk.dma("pool", dr["oT"][b, 6:8, :, tsl].rearrange("k p t -> p k t"),
                       oTt[:].rearrange("p (k t) -> p k t", k=2), r=[oTt])


def phaseF1(C, l):
    nc, tk, dr, S, NB, TS = C.nc, C.tk, C.dr, C.S, C.NB, C.TS
    with ExitStack() as es:
        cs = load_consts(C, es, ["c_ident"])
        identf = cs["c_ident"]
        identb = sbt(C, es, "identb", [128, 128], BF16)
        tk.dve(lambda: nc.vector.tensor_copy(identb[:], identf[:]), r=[identf], w=[identb])
        ones_bf = sbt(C, es, "ones_bf", [128, 128], BF16)
        tk.pool(lambda: nc.gpsimd.memset(ones_bf[:], 1.0), w=[ones_bf])
        Ws = {nm: sbt(C, es, "W" + nm, [128, KC, 1024], BF16) for nm in ("w_out", "wq_x", "wk_x", "wv_x", "wo_x")}
        gq = sbt(C, es, "gq", [128, KC], F32)
        gm = sbt(C, es, "gm", [128, KC], F32)
        tk.dma("sp", gq[:], dr["norm_cross"][l].rearrange("(k p) -> p k", p=128), w=[gq], allow_slow_non_contiguous=True)
        tk.dma("sp", gm[:], dr["norm_mem"][l].rearrange("(k p) -> p k", p=128), w=[gm], allow_slow_non_contiguous=True)
        with ExitStack() as es2:
            stg = Rot(nc, es2, "stg", [128, KC, 512], F32, 2)
            for nm, g in (("w_out", None), ("wq_x", gq), ("wk_x", gm), ("wv_x", gm), ("wo_x", None)):
                load_w_sec(C, stg, Ws[nm], 0, dr[nm][l], 1024, g)
            tk.barrier()
        pp = Rot(nc, es, "pp", [128, 512], F32, 6, psum=True)
        ptr = Rot(nc, es, "ptr", [128, 1024], BF16, 2, psum=True)
        memnT = sbt(C, es, "memnT", [128, KC, 256], BF16)
        kTx = sbt(C, es, "kTx", [128, KC, 256], BF16)
        vx = sbt(C, es, "vx", [128, 2, 1024], BF16)
        mts = Rot(nc, es, "mt", [128, 1024], F32, 2)
        mbs = Rot(nc, es, "mb", [128, 1024], BF16, 2)
        sm = Rot(nc, es, "smf", [128, 2], F32, 4)
        hTt = sbt(C, es, "hTt", [128, KC, TS], F32)
        oTt = sbt(C, es, "oTt", [128, KC, TS], BF16)
        sq = sbt(C, es, "sq", [128, KC, TS], BF16)
        hn = sbt(C, es, "hn", [128, KC, TS], BF16)
        rstd = sbt(C, es, "rstd", [128, TS], F32)
        qTx = sbt(C, es, "qTx", [128, KC, TS], BF16)
        oxT = sbt(C, es, "oxT", [128, KC, TS], BF16)
        ees = Rot(nc, es, "ee", [128, TS], BF16, 4)
        rdens = Rot(nc, es, "rden", [128, TS], F32, 2)

        def proj_add(Wt, src):
            for dc in range(KC):
                p = pp.next()
                for fc in range(KC):
                    tk.pe(lambda: nc.tensor.matmul(p[:, :TS], Wt[:, fc, dc * 128:(dc + 1) * 128], src[:, fc, :],
                                                   start=(fc == 0), stop=(fc == KC - 1)), r=[Wt, src], w=[p])
                tk.dve(lambda: nc.vector.tensor_tensor(hTt[:, dc, :], hTt[:, dc, :], p[:, :TS], op=ALU.add), r=[hTt, p], w=[hTt])

        for b in range(NB):
            for mc in range(2):
                mt = mts.next()
                tk.dma("sp", mt[:], dr["mem"][b, mc * 128:(mc + 1) * 128, :], w=[mt])
                mb = mbs.next()
                s1 = sm.next()
                tk.act(lambda: nc.scalar.activation(mb[:], mt[:], AF.Square, accum_out=s1[:, 0:1]), r=[mt], w=[mb, s1])
                tk.act(lambda: nc.scalar.activation(s1[:, 0:1], s1[:, 0:1], AF.Sqrt, scale=1.0 / D, bias=EPS), r=[s1], w=[s1])
                tk.dve(lambda: nc.vector.reciprocal(s1[:, 0:1], s1[:, 0:1]), r=[s1], w=[s1])
                tk.dve(lambda: nc.vector.tensor_scalar(mb[:], mt[:], s1[:, 0:1], None, op0=ALU.mult), r=[mt, s1], w=[mb])
                pt = ptr.next()
                for kc in range(KC):
                    tk.pe(lambda: nc.tensor.transpose(pt[:, kc * 128:(kc + 1) * 128], mb[:, kc * 128:(kc + 1) * 128], identb[:]),
                          r=[mb, identb], w=[pt])
                tk.act(lambda: nc.scalar.copy(memnT[:, :, mc * 128:(mc + 1) * 128], pt[:].rearrange("p (k m) -> p k m", k=KC)),
                       r=[pt], w=[memnT])
            for dc in range(KC):
                p = pp.next()
                for kc in range(KC):
                    tk.pe(lambda: nc.tensor.matmul(p[:, :256], Ws["wk_x"][:, kc, dc * 128:(dc + 1) * 128], memnT[:, kc, :],
                                                   start=(kc == 0), stop=(kc == KC - 1)), r=[Ws["wk_x"], memnT], w=[p])
                tk.act(lambda: nc.scalar.copy(kTx[:, dc, :], p[:, :256]), r=[p], w=[kTx])
            for mc in range(2):
                for half in range(2):
                    p = pp.next()
                    for kc in range(KC):
                        tk.pe(lambda: nc.tensor.matmul(p[:, :512], memnT[:, kc, mc * 128:(mc + 1) * 128],
                                                       Ws["wv_x"][:, kc, half * 512:(half + 1) * 512],
                                                       start=(kc == 0), stop=(kc == KC - 1)), r=[Ws["wv_x"], memnT], w=[p])
                    tk.act(lambda: nc.scalar.copy(vx[:, mc, half * 512:(half + 1) * 512], p[:, :512]), r=[p], w=[vx])
            for st in range(C.NST):
                tsl = slice(st * TS, (st + 1) * TS)
                tk.dma("sp", hTt[:], dr["hT"][b, :, :, tsl].rearrange("k p t -> p k t"), w=[hTt])
                tk.dma("sp", oTt[:], dr["oT"][b, :, :, tsl].rearrange("k p t -> p k t"), w=[oTt])
                proj_add(Ws["w_out"], oTt)
                emit_norm(C, hTt, TS, hn[:], hn, sq, ones_bf, pp.next(), rstd)
                for dc in range(KC):
                    p = pp.next()
                    for kc in range(KC):
                        tk.pe(lambda: nc.tensor.matmul(p[:, :TS], Ws["wq_x"][:, kc, dc * 128:(dc + 1) * 128], hn[:, kc, :],
                                                       start=(kc == 0), stop=(kc == KC - 1)), r=[Ws["wq_x"], hn], w=[p])
                    tk.act(lambda: nc.scalar.copy(qTx[:, dc, :], p[:, :TS]), r=[p], w=[qTx])
                for h in range(4):
                    ee = []
                    for mc in range(2):
                        p = pp.next()
                        for d2 in range(2):
                            tk.pe(lambda: nc.tensor.matmul(p[:, :TS], kTx[:, 2 * h + d2, mc * 128:(mc + 1) * 128], qTx[:, 2 * h + d2, :],
                                                           start=(d2 == 0), stop=(d2 == 1)), r=[kTx, qTx], w=[p])
                        e = ees.next()
                        tk.act(lambda: nc.scalar.activation(e[:], p[:, :TS], AF.Exp, scale=256.0 ** -0.5), r=[p], w=[e])
                        ee.append(e)
                    p = pp.next()
                    for mc in range(2):
                        tk.pe(lambda: nc.tensor.matmul(p[:, :TS], ones_bf[:], ee[mc][:], start=(mc == 0), stop=(mc == 1)),
                              r=[ones_bf, ee[mc]], w=[p])
                    rden = rdens.next()
                    tk.dve(lambda: nc.vector.reciprocal(rden[:], p[:, :TS]), r=[p], w=[rden])
                    for dv2 in range(2):
                        p = pp.next()
                        for mc in range(2):
                            c0 = h * 256 + dv2 * 128
                            tk.pe(lambda: nc.tensor.matmul(p[:, :TS], vx[:, mc, c0:c0 + 128], ee[mc][:], start=(mc == 0), stop=(mc == 1)),
                                  r=[vx, ee[mc]], w=[p])
                        tk.dve(lambda: nc.vector.tensor_tensor(oxT[:, 2 * h + dv2, :], p[:, :TS], rden[:], op=ALU.mult),
                               r=[p, rden], w=[oxT])
                proj_add(Ws["wo_x"], oxT)
                tk.dma("pool", dr["hT"][b, :, :, tsl].rearrange("k p t -> p k t"), hTt[:], r=[hTt])


def phaseF2(C, l):
    nc, tk, dr, S, NB = C.nc, C.tk, C.dr, C.S, C.NB
    T2 = 256
    with ExitStack() as es:
        ones_bf = sbt(C, es, "ones_bf", [128, 128], BF16)
        tk.pool(lambda: nc.gpsimd.memset(ones_bf[:], 1.0), w=[ones_bf])
        Wup = sbt(C, es, "Wup", [128, KC, 2 * DFF], BF16)
        Wdn = sbt(C, es, "Wdn", [128, NFC, 1024], BF16)
        gf = sbt(C, es, "gf", [128, KC], F32)
        tk.dma("sp", gf[:], dr["norm_ffn"][l].rearrange("(k p) -> p k", p=128), w=[gf], allow_slow_non_contiguous=True)
        with ExitStack() as es2:
            stg = Rot(nc, es2, "stg", [128, KC, 512], F32, 2)
            load_w_sec(C, stg, Wup, 0, dr["w_up"][l], 2 * DFF, gf)
            for c in range(NFC):
                st = stg.next()
                tk.dma("sp", st[:, 0:2, :].rearrange("p a n -> p (a n)"), dr["w_down"][l, c * 128:(c + 1) * 128, :], w=[st])
                tk.act(lambda: nc.scalar.copy(Wdn[:, c, :], st[:, 0:2, :].rearrange("p a n -> p (a n)")), r=[st], w=[Wdn])
            tk.barrier()
        cwT = sbt(C, es, "fcw", [128, NFC, 3], F32)
        for j in range(3):
            tk.dma("sp", cwT[:, :, j], dr["ffn_conv_w"][l, j].rearrange("(c p) -> p c", p=128), w=[cwT], allow_slow_non_contiguous=True)
        cbT = sbt(C, es, "fcb", [128, NFC], F32)
        tk.dma("sp", cbT[:], dr["ffn_conv_b"][l].rearrange("(c p) -> p c", p=128), w=[cbT], allow_slow_non_contiguous=True)
        halo = sbt(C, es, "fhalo", [128, NFC, 2], F32)
        pp = Rot(nc, es, "pp", [128, 512], F32, 7, psum=True)
        hTs = Rot(nc, es, "hTf", [128, KC, T2], F32, 2)
        sq = sbt(C, es, "sq", [128, KC, T2], BF16)
        hn = sbt(C, es, "hn", [128, KC, T2], BF16)
        rstd = sbt(C, es, "rstd", [128, T2], F32)
        actT = sbt(C, es, "actT", [128, NFC, T2], BF16)
        gbs = Rot(nc, es, "gb", [128, T2 + 2], F32, 3)
        accs = Rot(nc, es, "acc", [128, T2], F32, 3)
        for b in range(NB):
            tk.pool(lambda: nc.gpsimd.memset(halo[:], 0.0), w=[(halo, c_) for c_ in range(NFC)])
            for ti in range(S // T2):
                tsl = slice(ti * T2, (ti + 1) * T2)
                hTt = hTs.next()
                tk.dma("sp", hTt[:], dr["hT"][b, :, :, tsl].rearrange("k p t -> p k t"), w=[hTt])
                emit_norm(C, hTt, T2, hn[:], hn, sq, ones_bf, pp.next(), rstd)
                for c in range(NFC):
                    p = pp.next()
                    for half in range(2):
                        for kc in range(KC):
                            c0 = half * DFF + c * 128
                            tk.pe(lambda: nc.tensor.matmul(p[:, half * T2:(half + 1) * T2], Wup[:, kc, c0:c0 + 128], hn[:, kc, :],
                                                           start=(kc == 0), stop=(kc == KC - 1)), r=[Wup, hn], w=[p])
                    gb = gbs.next()
                    tk.pool(lambda: nc.gpsimd.tensor_copy(gb[:, 0:2], halo[:, c, :]), r=[(halo, c)], w=[(gb, 0)])
                    tk.act(lambda: nc.scalar.copy(gb[:, 2:T2 + 2], p[:, 0:T2]), r=[p], w=[(gb, 1)])
                    tk.pool(lambda: nc.gpsimd.tensor_copy(halo[:, c, :], gb[:, T2:T2 + 2]), r=[(gb, 1)], w=[(halo, c)])
                    acc = accs.next()
                    tk.dve(lambda: nc.vector.tensor_scalar(acc[:], gb[:, 2:T2 + 2], cwT[:, c, 2:3], cbT[:, c:c + 1],
                                                           op0=ALU.mult, op1=ALU.add), r=[(gb, 1), cwT, cbT], w=[acc])
                    for jj in (1, 0):
                        tk.dve(lambda: nc.vector.scalar_tensor_tensor(acc[:], gb[:, jj:jj + T2], cwT[:, c, jj:jj + 1], acc[:],
                                                                      op0=ALU.mult, op1=ALU.add),
                               r=[(gb, 0), (gb, 1), acc, cwT], w=[acc])
                    tk.act(lambda: nc.scalar.activation(acc[:], acc[:], AF.Silu), r=[acc], w=[acc])
                    tk.dve(lambda: nc.vector.tensor_tensor(actT[:, c, :], acc[:], p[:, T2:2 * T2], op=ALU.mult), r=[acc, p], w=[(actT, c)])
                for dc in range(KC):
                    p = pp.next()
                    for c in range(NFC):
                        tk.pe(lambda: nc.tensor.matmul(p[:, :T2], Wdn[:, c, dc * 128:(dc + 1) * 128], actT[:, c, :],
                                                       start=(c == 0), stop=(c == NFC - 1)), r=[Wdn, (actT, c)], w=[p])
                    tk.dve(lambda: nc.vector.tensor_tensor(hTt[:, dc, :], hTt[:, dc, :], p[:, :T2], op=ALU.add), r=[hTt, p], w=[hTt])
                tk.dma("pool", dr["hT"][b, :, :, tsl].rearrange("k p t -> p k t"), hTt[:], r=[hTt])


def phaseFinal(C):
    nc, tk, dr, S, NB, TS = C.nc, C.tk, C.dr, C.S, C.NB, C.TS
    with ExitStack() as es:
        cs = load_consts(C, es, ["c_ident"])
        identf = cs["c_ident"]
        ones_bf = sbt(C, es, "ones_bf", [128, 128], BF16)
        tk.pool(lambda: nc.gpsimd.memset(ones_bf[:], 1.0), w=[ones_bf])
        gfin = sbt(C, es, "gfin", [128, KC], F32)
        tk.dma("sp", gfin[:], dr["norm_final"].rearrange("(k p) -> p k", p=128), w=[gfin], allow_slow_non_contiguous=True)
        pp = Rot(nc, es, "pp", [128, 512], F32, 6, psum=True)
        hTs = Rot(nc, es, "hTl", [128, KC, TS], F32, 2)
        sq = sbt(C, es, "sq", [128, KC, TS], BF16)
        rstd = sbt(C, es, "rstd", [128, TS], F32)
        yT = sbt(C, es, "yT", [128, KC, TS], F32)
        yos = Rot(nc, es, "yo", [128, D], F32, 2)
        for b in range(NB):
            for st in range(C.NST):
                tsl = slice(st * TS, (st + 1) * TS)
                hTt = hTs.next()
                tk.dma("sp", hTt[:], dr["hT"][b, :, :, tsl].rearrange("k p t -> p k t"), w=[hTt])
                emit_norm(C, hTt, TS, None, None, sq, ones_bf, pp.next(), rstd)
                for kc in range(KC):
                    tk.dve(lambda: nc.vector.scalar_tensor_tensor(yT[:, kc, :], hTt[:, kc, :], gfin[:, kc:kc + 1], rstd[:],
                                                                  op0=ALU.mult, op1=ALU.mult), r=[hTt, gfin, rstd], w=[(yT, kc)])
                for j in range(TS // 128):
                    yo = yos.next()
                    for half in range(2):
                        p = pp.next()
                        for q in range(4):
                            kc = half * 4 + q
                            tk.pe(lambda: nc.tensor.transpose(p[:, q * 128:(q + 1) * 128], yT[:, kc, j * 128:(j + 1) * 128], identf[:]),
                                  r=[(yT, kc), identf], w=[p])
                        if half == 0:
                            tk.act(lambda: nc.scalar.copy(yo[:, 0:512], p[:]), r=[p], w=[(yo, 0)])
                        else:
                            tk.dve(lambda: nc.vector.tensor_copy(yo[:, 512:1024], p[:]), r=[p], w=[(yo, 1)])
                    t0 = st * TS + j * 128
                    tk.dma("pool", dr["y"][b, t0:t0 + 128, :], yo[:], r=[(yo, 0), (yo, 1)])


def kernel(**inputs):
    S, NCORES = 4096, 8
    nc, C = build(S)
    consts = make_consts(S)
    x = np.ascontiguousarray(inputs["x"], dtype=np.float32)
    mem = np.ascontiguousarray(inputs["mem"], dtype=np.float32)
    in_maps = []
    for c in range(NCORES):
        m = {"x": x[2 * c:2 * c + 2], "mem": mem[2 * c:2 * c + 2]}
        for name, _ in PARAMS:
            m[name] = np.ascontiguousarray(inputs[name], dtype=np.float32)
        m.update(consts)
        in_maps.append(m)
    res = run_bass_kernel_spmd(nc, in_maps, core_ids=list(range(NCORES)))
    return np.concatenate([np.asarray(r["y"], dtype=np.float32) for r in res.results], axis=0)
```

```python
import math
import numpy as np
from contextlib import ExitStack
import ml_dtypes
import concourse.bass as bass
import concourse.mybir as mybir
from concourse.bass_utils import run_bass_kernel_spmd

F32 = mybir.dt.float32
BF16 = mybir.dt.bfloat16
U32 = mybir.dt.uint32
ALU = mybir.AluOpType
AF = mybir.ActivationFunctionType
AX = mybir.AxisListType

D = 1024
KC = 8
NMEM = 256
DFF = 2816
NFC = DFF // 128
N_IN = 3368
EPS = 1e-6
NEG_BIG = -3.0e38
NEG_THR = -1.0e38


class Buf:
    __slots__ = ("t", "name", "psum")

    def __init__(self, t, name, psum=False):
        self.t = t
        self.name = name
        self.psum = psum

    def __getitem__(self, k):
        return self.t[k]


class TK:
    LIM = 900
    DLIM = 55
    NSLOT = 8
    ENG = ("pe", "act", "dve", "pool", "sp")
    DQ = ("sp", "pool")

    def __init__(self, nc, es):
        self.nc = nc
        self.es = es
        self.eng = {"pe": nc.tensor, "act": nc.scalar, "dve": nc.vector, "pool": nc.gpsimd, "sp": nc.sync}
        self.nsem = 0
        self.csem = {e: [self._newsem(e) for _ in range(3)] for e in self.ENG}
        self.dsem = {(q, sl): [self._newsem(f"d{q}{sl}") for _ in range(3)] for q in self.DQ for sl in range(self.NSLOT)}
        self.epoch = 0
        self.ninstr = 0
        self.nbar = 0
        self.dnext = {q: 0 for q in self.DQ}
        self._reset_epoch()

    def _reset_epoch(self):
        self.cnt = {e: 0 for e in self.ENG}
        self.dcnt = {k: 0 for k in self.dsem}
        self.seen = {e: {} for e in self.ENG}
        self.lastw = {}
        self.readers = {}
        self.last_d = {}

    def _newsem(self, name):
        self.nsem += 1
        return self.es.enter_context(self.nc.semaphore(f"{name}_{self.nsem}"))

    def _need(self, e, tok):
        if tok is None or tok[2] != self.epoch:
            return
        if tok[0] == "c":
            _, e2, _, idx = tok
            key = e2 if e2 != e else ("self", e)
            if self.seen[e].get(key, -1) >= idx:
                return
            self.seen[e][key] = idx
            self.eng[e].wait_ge(self.csem[e2][self.epoch % 3], idx + 1)
        else:
            _, key, _, val = tok
            if self.seen[e].get(key, -1) >= val:
                return
            self.seen[e][key] = val
            self.eng[e].wait_ge(self.dsem[key][self.epoch % 3], val)

    def _deps(self, e, r, w, is_dma=False):
        def chk(tok):
            if tok is None:
                return
            if tok[0] == "c" and tok[1] == e and not is_dma and e == "pe":
                return
            self._need(e, tok)
        for b in r:
            chk(self.lastw.get(b))
            if getattr(b, "psum", False):
                rd = self.readers.get(b)
                if rd:
                    for t2 in rd.values():
                        if not (t2[0] == "c" and t2[1] == e):
                            chk(t2)
        for b in w:
            chk(self.lastw.get(b))
            rd = self.readers.get(b)
            if rd:
                for t2 in rd.values():
                    chk(t2)

    def _record(self, tok, r, w, rkey):
        for b in w:
            self.lastw[b] = tok
            self.readers[b] = {}
        for b in r:
            self.readers.setdefault(b, {})[rkey] = tok

    def op(self, e, fn, r=(), w=()):
        if self.cnt[e] >= self.LIM:
            self.barrier()
        self._deps(e, r, w)
        idx = self.cnt[e]
        ins = fn()
        ins.then_inc(self.csem[e][self.epoch % 3], 1)
        self.cnt[e] = idx + 1
        tok = ("c", e, self.epoch, idx)
        self._record(tok, r, w, e)
        self.ninstr += 1
        return ins

    def dma(self, q, out, in_, r=(), w=(), **kw):
        q = "sp"
        slot = self.dnext[q] % self.NSLOT
        k = (q, slot)
        if self.dcnt[k] >= self.DLIM:
            self.barrier()
        self.dnext[q] += 1
        self._deps(q, r, w, is_dma=True)
        self._need(q, self.last_d.get(k))
        self.dcnt[k] += 1
        val = 16 * self.dcnt[k]
        self.eng[q].dma_start(out=out, in_=in_, **kw).then_inc(self.dsem[k][self.epoch % 3], 16)
        tok = ("d", k, self.epoch, val)
        self.last_d[k] = tok
        self._record(tok, r, w, k)
        self.ninstr += 1
        return tok

    def barrier(self):
        bank = self.epoch % 3
        for e in self.ENG:
            if self.cnt[e] == 0:
                self.eng[e].sem_inc(self.csem[e][bank], 1)
                self.cnt[e] = 1
        toks = [("c", e, self.epoch, self.cnt[e] - 1) for e in self.ENG] + list(self.last_d.values())
        for e in self.ENG:
            for t in toks:
                self._need(e, t)
        self.epoch += 1
        self.nbar += 1
        nb = (self.epoch + 1) % 3
        for e in self.ENG:
            self.eng[e].sem_clear(self.csem[e][nb])
            if e in self.DQ:
                for sl in range(self.NSLOT):
                    self.eng[e].sem_clear(self.dsem[(e, sl)][nb])
        self._reset_epoch()

    def pe(self, fn, r=(), w=()):
        return self.op("pe", fn, r, w)

    def act(self, fn, r=(), w=()):
        return self.op("act", fn, r, w)

    def dve(self, fn, r=(), w=()):
        return self.op("dve", fn, r, w)

    def pool(self, fn, r=(), w=()):
        return self.op("pool", fn, r, w)


_UN = [0]


def uname(name):
    _UN[0] += 1
    return f"{name}_u{_UN[0]}"


class Rot:
    def __init__(self, nc, es, name, shape, dtype, n, psum=False):
        self.bufs = []
        for i in range(n):
            if psum:
                t = es.enter_context(nc.psum_tensor(uname(f"{name}{i}"), shape, dtype))
            else:
                t = es.enter_context(nc.sbuf_tensor(uname(f"{name}{i}"), shape, dtype))
            self.bufs.append(Buf(t, f"{name}{i}", psum=psum))
        self.i = 0

    def next(self):
        b = self.bufs[self.i % len(self.bufs)]
        self.i += 1
        return b


C_RQ, C_RK, C_RV, C_RG = 0, 128, 256, 512
C_WR, C_WK, C_WV, C_WWL, C_WAL, C_WGL = 768, 1024, 1280, 1536, 1600, 1664
C_SZ, C_SX, C_SB, C_SC, C_SDT = 1792, 2048, 2304, 2560, 2816
C_DQ, C_DK, C_DV, C_DIQ, C_DIK, C_DIW = 2820, 3076, 3140, 3204, 3332, 3364

PARAMS = [
    ("norm_mix", (2, 1024)), ("w_in", (2, 1024, N_IN)), ("rwkv_mu", (2, 1024)), ("rwkv_w0", (2, 256)),
    ("rwkv_w2", (2, 64, 256)), ("rwkv_a0", (2, 256)), ("rwkv_a2", (2, 64, 256)), ("rwkv_g2", (2, 128, 256)),
    ("rwkv_k_k", (2, 256)), ("rwkv_k_a", (2, 256)), ("rwkv_r_k", (2, 4, 64)), ("rwkv_ln_w", (2, 256)),
    ("rwkv_ln_b", (2, 256)), ("ssm_conv_w", (2, 4, 768)), ("ssm_conv_b", (2, 768)), ("ssm_dt_bias", (2, 4)),
    ("ssm_a_log", (2, 4)), ("ssm_d", (2, 4)), ("ssm_norm", (2, 256)), ("idx_k_norm", (2, 32)),
    ("w_out", (2, 1024, 1024)), ("norm_cross", (2, 1024)), ("norm_mem", (2, 1024)), ("wq_x", (2, 1024, 1024)),
    ("wk_x", (2, 1024, 1024)), ("wv_x", (2, 1024, 1024)), ("wo_x", (2, 1024, 1024)), ("norm_ffn", (2, 1024)),
    ("w_up", (2, 1024, 2 * DFF)), ("ffn_conv_w", (2, 3, DFF)), ("ffn_conv_b", (2, DFF)),
    ("w_down", (2, DFF, 1024)), ("norm_final", (1024,)),
]


def make_consts(S):
    c = {}
    c["c_ident"] = np.eye(128, dtype=np.float32)
    i = np.arange(128)
    c["c_U"] = (i[:, None] <= i[None, :]).astype(np.float32)
    c["c_negU"] = np.where(i[None, :] > i[:, None], np.float32(NEG_BIG), np.float32(0)).astype(np.float32)
    pos = np.arange(S, dtype=np.float32)

    def tabs(hd, rows):
        half = hd // 2
        inv = (np.float32(10000.0) ** (-np.arange(half, dtype=np.float32) / np.float32(half))).astype(np.float32)
        ang = (pos[:, None] * inv[None, :]).astype(np.float32)
        cos = np.cos(ang).astype(np.float32)
        sin = np.sin(ang).astype(np.float32)
        cf = np.concatenate([cos, cos], 1)
        sf = np.concatenate([-sin, sin], 1)
        rep = rows // hd
        return np.ascontiguousarray(np.tile(cf, (1, rep)).T), np.ascontiguousarray(np.tile(sf, (1, rep)).T)

    c["c_cos64T"], c["c_sin64T"] = tabs(64, 128)
    c["c_cos32T"], c["c_sin32T"] = tabs(32, 128)
    e2 = np.zeros((128, 64, 128), dtype=np.float32)
    for b in range(2):
        for s in range(64):
            e2[b * 64 + s, s, b * 64:(b + 1) * 64] = 1.0
    c["c_E2"] = e2.reshape(128, 64 * 128).astype(ml_dtypes.bfloat16)
    lg = np.log(1.0 - np.power(2.0, -5.0 - np.arange(4, dtype=np.float32))).astype(np.float32)
    c["c_retla"] = np.tile(lg[None, :], (128, 1)).astype(np.float32)
    return c


CONST_SPECS = lambda S: [("c_ident", (128, 128), F32), ("c_U", (128, 128), F32), ("c_negU", (128, 128), F32),
                         ("c_cos64T", (128, S), F32), ("c_sin64T", (128, S), F32), ("c_cos32T", (128, S), F32),
                         ("c_sin32T", (128, S), F32), ("c_E2", (128, 64 * 128), BF16), ("c_retla", (128, 4), F32)]


class Ctx:
    pass


def build(S, NB=2, L=2, stop_after=None, dbg=()):
    nc = bass.Bass("TRN2", target_bir_lowering=False)
    C = Ctx()
    C.nc, C.S, C.NB, C.L = nc, S, NB, L
    C.TS = 512
    C.NST = S // C.TS
    dr = {}
    dr["x"] = nc.dram_tensor("x", [NB, S, D], F32, kind="ExternalInput").ap()
    dr["mem"] = nc.dram_tensor("mem", [NB, NMEM, D], F32, kind="ExternalInput").ap()
    for name, shp in PARAMS:
        dr[name] = nc.dram_tensor(name, list(shp), F32, kind="ExternalInput").ap()
    for name, shp, dt in CONST_SPECS(S):
        dr[name] = nc.dram_tensor(name, list(shp), dt, kind="ExternalInput").ap()
    dr["y"] = nc.dram_tensor("y", [NB, S, D], F32, kind="ExternalOutput").ap()

    def scratch(name, shape, dt):
        kind = "ExternalOutput" if name in dbg else "Internal"
        dr[name] = nc.dram_tensor(name, list(shape), dt, kind=kind).ap()

    scratch("hT", [NB, KC, 128, S], F32)
    scratch("oT", [NB, KC, 128, S], BF16)
    scratch("r_qT", [NB, 128, S], BF16)
    scratch("r_kT", [NB, 128, S], BF16)
    scratch("r_kTok", [NB, S, 128], BF16)
    scratch("r_v", [NB, S, 256], BF16)
    scratch("r_sg", [NB, S, 256], F32)
    for nm in ("w_whi", "w_wlo", "w_nkk", "w_bb", "w_kp", "w_r"):
        scratch(nm, [NB, S, 256], BF16)
    scratch("w_vT", [NB, 256, S], F32)
    scratch("w_bonus", [NB, S, 256], F32)
    scratch("w_g", [NB, S, 256], F32)
    scratch("s_CT", [NB, 256, S], BF16)
    scratch("s_BT", [NB, 256, S], BF16)
    scratch("s_BTok", [NB, S, 256], BF16)
    scratch("s_xdt", [NB, S, 256], BF16)
    scratch("s_xs", [NB, S, 256], F32)
    scratch("s_sz", [NB, S, 256], F32)
    scratch("s_la", [NB, S, 4], F32)
    scratch("d_qT", [NB, 256, S], BF16)
    scratch("d_kT", [NB, 64, S], BF16)
    scratch("d_v", [NB, S, 65], BF16)
    scratch("d_iqT", [NB, 128, S], BF16)
    scratch("d_ikT", [NB, 32, S], BF16)
    scratch("d_iw", [NB, S, 4], F32)
    C.dr = dr

    import os
    with ExitStack() as es0:
        tk = TK(nc, es0)
        C.tk = tk
        phases = []
        phases.append(("p0", lambda: phase0(C)))
        for l in range(L):
            if os.environ.get("KONEP", "1") == "1":
                phases.append((f"P{l}", lambda l=l: phaseP(C, l)))
            else:
                for sec in ("ret", "ssd", "rw", "dsa"):
                    phases.append((f"P{l}{sec}" if sec != "dsa" else f"P{l}", lambda l=l, sec=sec: phaseP(C, l, only=sec)))
            phases.append((f"REC{l}", lambda l=l: phaseRec(C, l)))
            phases.append((f"RW{l}", lambda l=l: phaseRW(C, l)))
            phases.append((f"DSA{l}", lambda l=l: phaseDSA(C, l)))
            phases.append((f"F1{l}", lambda l=l: phaseF1(C, l)))
            phases.append((f"F2{l}", lambda l=l: phaseF2(C, l)))
        phases.append(("fin", lambda: phaseFinal(C)))
        import os
        skipph = os.environ.get("KPH", "").split(",")
        for name, fn in phases:
            if name in skipph:
                continue
            fn()
            tk.barrier()
            if stop_after == name:
                break
        C.ninstr = tk.ninstr
    return nc, C


def load_consts(C, es, names):
    nc, tk, dr = C.nc, C.tk, C.dr
    out = {}
    for nm in names:
        ap = dr[nm]
        t = Buf(es.enter_context(nc.sbuf_tensor(uname("k_" + nm), list(ap.shape), ap.dtype)), nm)
        tk.dma("sp", t[:], ap[:, :], w=[t])
        out[nm] = t
    return out


def phase0(C):
    nc, tk, dr, S, NB = C.nc, C.tk, C.dr, C.S, C.NB
    with ExitStack() as es:
        cs = load_consts(C, es, ["c_ident"])
        ident = cs["c_ident"]
        xin = Rot(nc, es, "p0x", [128, D], F32, 2)
        hout = Rot(nc, es, "p0h", [128, KC, 128], F32, 2)
        pps = Rot(nc, es, "p0ps", [128, 512], F32, 4, psum=True)
        for b in range(NB):
            for ti in range(S // 128):
                xt = xin.next()
                tk.dma("sp", xt[:], dr["x"][b, ti * 128:(ti + 1) * 128, :], w=[xt])
                ho = hout.next()
                for half in range(2):
                    pp = pps.next()
                    for j in range(4):
                        kc = half * 4 + j
                        tk.pe(lambda: nc.tensor.transpose(pp[:, j * 128:(j + 1) * 128], xt[:, kc * 128:(kc + 1) * 128], ident[:]),
                              r=[xt, ident], w=[pp])
                    dst = ho[:, half * 4:(half + 1) * 4, :]
                    src = pp[:].rearrange("p (a b) -> p a b", a=4)
                    if half == 0:
                        tk.act(lambda: nc.scalar.copy(dst, src), r=[pp], w=[(ho, half)])
                    else:
                        tk.dve(lambda: nc.vector.tensor_copy(dst, src), r=[pp], w=[(ho, half)])
                tk.dma("pool", dr["hT"][b, :, :, ti * 128:(ti + 1) * 128].rearrange("k p t -> p k t"), ho[:],
                       r=[(ho, 0), (ho, 1)])


def emit_norm(C, hT, n, hn_ap, hn_key, sq, ones_bf, pp, rstd):
    nc, tk = C.nc, C.tk
    tk.act(lambda: nc.scalar.activation(sq[:, :, :n], hT[:, :, :n], AF.Square), r=[hT], w=[sq])
    for kc in range(KC):
        tk.pe(lambda: nc.tensor.matmul(pp[:, :n], ones_bf[:], sq[:, kc, :n], start=(kc == 0), stop=(kc == KC - 1)),
              r=[sq, ones_bf], w=[pp])
    tk.act(lambda: nc.scalar.activation(rstd[:, :n], pp[:, :n], AF.Sqrt, scale=1.0 / D, bias=EPS), r=[pp], w=[rstd])
    tk.dve(lambda: nc.vector.reciprocal(rstd[:, :n], rstd[:, :n]), r=[rstd], w=[rstd])
    if hn_ap is not None:
        tk.dve(lambda: nc.vector.tensor_tensor(hn_ap, hT[:, :, :n], rstd[:, :n].unsqueeze(1).to_broadcast([128, KC, n]),
                                               op=ALU.mult), r=[hT, rstd], w=[hn_key])


def load_w_sec(C, stg, dstW, off, src2d, n, gT, cs=None, swap=0, rows=KC):
    nc, tk = C.nc, C.tk
    for c0 in range(0, n, 512):
        m = min(512, n - c0)
        st = stg.next()
        tk.dma("sp", st[:, :rows, :m], src2d[:, c0:c0 + m].rearrange("(kc p) n -> p kc n", p=128), w=[st])
        if gT is not None:
            tk.dve(lambda: nc.vector.tensor_tensor(st[:, :rows, :m], st[:, :rows, :m],
                                                   gT[:, :rows].unsqueeze(2).to_broadcast([128, rows, m]), op=ALU.mult),
                   r=[st, gT], w=[st])
        if cs is not None:
            csb, coff = cs
            tk.dve(lambda: nc.vector.tensor_tensor(st[:, :rows, :m], st[:, :rows, :m],
                                                   csb[:, coff + c0:coff + c0 + m].unsqueeze(1).to_broadcast([128, rows, m]),
                                                   op=ALU.mult), r=[st, csb], w=[st])
        if swap:
            sv = st[:, :rows, :m].rearrange("p k (x two d) -> p k x two d", two=2, d=swap)
            dv = dstW[:, :rows, off + c0:off + c0 + m].rearrange("p k (x two d) -> p k x two d", two=2, d=swap)
            tk.act(lambda: nc.scalar.copy(dv[:, :, :, 0, :], sv[:, :, :, 1, :]), r=[st], w=[dstW])
            tk.act(lambda: nc.scalar.copy(dv[:, :, :, 1, :], sv[:, :, :, 0, :]), r=[st], w=[dstW])
        else:
            tk.act(lambda: nc.scalar.copy(dstW[:, :rows, off + c0:off + c0 + m], st[:, :rows, :m]), r=[st], w=[dstW])


def sbt(C, es, name, shape, dt):
    return Buf(es.enter_context(C.nc.sbuf_tensor(uname(name), list(shape), dt)), name)


def bc_load(C, es, name, src1d, n):
    t = sbt(C, es, name, [128, n], F32)
    C.tk.dma("sp", t[:], src1d.partition_broadcast(128), w=[t])
    return t


P_SECS = [("rq", 128), ("rq_r", 128), ("rk", 128), ("rk_r", 128), ("rv", 256), ("rg", 256),
          ("wrkv_a", 768), ("wrkv_b", 768), ("wl_a", 64), ("wl_b", 64), ("al_a", 64), ("al_b", 64),
          ("gl_a", 128), ("gl_b", 128), ("sz", 256), ("sx", 768), ("sdt", 4),
          ("dq", 256), ("dq_r", 256), ("dk", 64), ("dk_r", 64), ("dv", 64), ("diq", 128), ("diq_r", 128),
          ("dik", 32), ("dik_r", 32), ("diw", 4)]


def phaseP(C, l, only=None):
    nc, tk, dr, S, NB, TS = C.nc, C.tk, C.dr, C.S, C.NB, C.TS
    OFF = {}
    o = 0
    for nm, n in P_SECS:
        OFF[nm] = o
        o += n
    NW = o
    with ExitStack() as es:
        cs = load_consts(C, es, ["c_ident"])
        identf = cs["c_ident"]
        identb = sbt(C, es, "identb", [128, 128], BF16)
        tk.dve(lambda: nc.vector.tensor_copy(identb[:], identf[:]), r=[identf], w=[identb])
        ones_bf = sbt(C, es, "ones_bf", [128, 128], BF16)
        tk.pool(lambda: nc.gpsimd.memset(ones_bf[:], 1.0), w=[ones_bf])
        ones_f = sbt(C, es, "ones_f", [32, 32], F32)
        tk.pool(lambda: nc.gpsimd.memset(ones_f[:], 1.0), w=[ones_f])
        W = sbt(C, es, "Wp", [128, KC, NW], BF16)
        gT = sbt(C, es, "gT", [128, KC], F32)
        tk.dma("sp", gT[:], dr["norm_mix"][l].rearrange("(k p) -> p k", p=128), w=[gT], allow_slow_non_contiguous=True)
        mu_bc = bc_load(C, es, "mu_bc", dr["rwkv_mu"][l], 1024)
        om_bc = sbt(C, es, "om_bc", [128, 1024], F32)
        tk.dve(lambda: nc.vector.tensor_scalar(om_bc[:], mu_bc[:], -1.0, 1.0, op0=ALU.mult, op1=ALU.add), r=[mu_bc], w=[om_bc])
        win = dr["w_in"][l]
        with ExitStack() as es2:
            stg = Rot(nc, es2, "stg", [128, KC, 512], F32, 2)
            LW = lambda nm, c0, n, **kw: load_w_sec(C, stg, W, OFF[nm], win[:, c0:c0 + n], n, gT, **kw)
            LW("rq", C_RQ, 128); LW("rq_r", C_RQ, 128, swap=16); LW("rk", C_RK, 128); LW("rk_r", C_RK, 128, swap=16)
            LW("rv", C_RV, 256); LW("rg", C_RG, 256)
            LW("wrkv_a", C_WR, 768, cs=(om_bc, 0)); LW("wrkv_b", C_WR, 768, cs=(mu_bc, 0))
            LW("wl_a", C_WWL, 64, cs=(om_bc, 768)); LW("wl_b", C_WWL, 64, cs=(mu_bc, 768))
            LW("al_a", C_WAL, 64, cs=(om_bc, 832)); LW("al_b", C_WAL, 64, cs=(mu_bc, 832))
            LW("gl_a", C_WGL, 128, cs=(om_bc, 896)); LW("gl_b", C_WGL, 128, cs=(mu_bc, 896))
            LW("sz", C_SZ, 256); LW("sx", C_SX, 768); LW("sdt", C_SDT, 4)
            LW("dq", C_DQ, 256); LW("dq_r", C_DQ, 256, swap=32); LW("dk", C_DK, 64); LW("dk_r", C_DK, 64, swap=32)
            LW("dv", C_DV, 64); LW("diq", C_DIQ, 128); LW("diq_r", C_DIQ, 128, swap=16)
            LW("dik", C_DIK, 32); LW("dik_r", C_DIK, 32, swap=16); LW("diw", C_DIW, 4)
            tk.barrier()
        w0a0 = sbt(C, es, "w0a0", [128, 512], F32)
        tk.dma("sp", w0a0[:, 0:256], dr["rwkv_w0"][l].partition_broadcast(128), w=[w0a0])
        tk.dma("sp", w0a0[:, 256:512], dr["rwkv_a0"][l].partition_broadcast(128), w=[w0a0])
        kk_bc = bc_load(C, es, "kk_bc", dr["rwkv_k_k"][l], 256)
        ka_bc = bc_load(C, es, "ka_bc", dr["rwkv_k_a"][l], 256)
        rk_bc = bc_load(C, es, "rk_bc", dr["rwkv_r_k"][l].rearrange("h d -> (h d)"), 256)
        dtb_bc = bc_load(C, es, "dtb_bc", dr["ssm_dt_bias"][l], 4)
        alog_bc = bc_load(C, es, "alog_bc", dr["ssm_a_log"][l], 4)
        a_bc = sbt(C, es, "a_bc", [128, 4], F32)
        tk.act(lambda: nc.scalar.activation(a_bc[:], alog_bc[:], AF.Exp), r=[alog_bc], w=[a_bc])
        tk.dve(lambda: nc.vector.tensor_scalar(a_bc[:], a_bc[:], -1.0, None, op0=ALU.mult), r=[a_bc], w=[a_bc])
        w2f = sbt(C, es, "w2f", [128, 768], F32)
        tk.dma("sp", w2f[:64, 0:256], dr["rwkv_w2"][l], w=[w2f])
        tk.dma("sp", w2f[:64, 256:512], dr["rwkv_a2"][l], w=[w2f])
        tk.dma("sp", w2f[:, 512:768], dr["rwkv_g2"][l], w=[w2f])
        w2b = sbt(C, es, "w2b", [128, 768], BF16)
        tk.dve(lambda: nc.vector.tensor_copy(w2b[:64, 0:512], w2f[:64, 0:512]), r=[w2f], w=[w2b])
        tk.dve(lambda: nc.vector.tensor_copy(w2b[:, 512:768], w2f[:, 512:768]), r=[w2f], w=[w2b])
        cwT = sbt(C, es, "cwT", [128, 6, 4], F32)
        for j in range(4):
            tk.dma("sp", cwT[:, :, j], dr["ssm_conv_w"][l, j].rearrange("(c p) -> p c", p=128), w=[cwT],
                   allow_slow_non_contiguous=True)
        cbT = sbt(C, es, "cbT", [128, 6], F32)
        tk.dma("sp", cbT[:], dr["ssm_conv_b"][l].rearrange("(c p) -> p c", p=128), w=[cbT], allow_slow_non_contiguous=True)
        nw = sbt(C, es, "nw", [32, 2], F32)
        ikn_ap = dr["idx_k_norm"][l]
        tk.dma("sp", nw[:, 0:1], ikn_ap.rearrange("(p o) -> p o", o=1), w=[nw], allow_slow_non_contiguous=True)
        tk.dma("sp", nw[0:16, 1:2], ikn_ap[16:32].rearrange("(p o) -> p o", o=1), w=[nw], allow_slow_non_contiguous=True)
        tk.dma("sp", nw[16:32, 1:2], ikn_ap[0:16].rearrange("(p o) -> p o", o=1), w=[nw], allow_slow_non_contiguous=True)

        hTt = sbt(C, es, "hTt", [128, KC, TS], F32)
        sq = sbt(C, es, "sq", [128, KC, TS], BF16)
        hns = [sbt(C, es, f"hn{i}", [128, KC, TS + 1], BF16) for i in range(2)]
        rstd = sbt(C, es, "rstd", [128, TS], F32)
        tabs = {nm: sbt(C, es, "t_" + nm, [128, TS], F32) for nm in ("c_cos64T", "c_sin64T", "c_cos32T", "c_sin32T")}
        pp = Rot(nc, es, "pp", [128, 512], F32, 6, psum=True)
        ptr = Rot(nc, es, "ptr", [128, 1024], BF16, 2, psum=True)
        tA = Rot(nc, es, "tA", [128, 512], F32, 3)
        tB = Rot(nc, es, "tB", [128, 512], F32, 3)
        ob = Rot(nc, es, "ob", [128, 512], BF16, 4)
        of = Rot(nc, es, "of", [128, 512], F32, 3)
        sbj = Rot(nc, es, "sbj", [128, 256], BF16, 12)
        sfj = Rot(nc, es, "sfj", [128, 256], F32, 10)
        sm = Rot(nc, es, "sm", [128, 16], F32, 12)
        cb = Rot(nc, es, "cb", [128, TS + 3], F32, 2)
        halo = sbt(C, es, "halo", [128, 6, 3], F32)
        vout = sbt(C, es, "vout", [128, 4, 65], BF16)
        tk.pool(lambda: nc.gpsimd.memset(vout[:], 1.0), w=[vout])
        twl = sbt(C, es, "twl", [64, TS], BF16)
        alb = sbt(C, es, "alb", [64, TS], BF16)
        sgl = sbt(C, es, "sgl", [128, TS], BF16)
        dtall = sbt(C, es, "dtall", [128, 4, 4], F32)
        xsT = [sbt(C, es, f"xsT{i}", [128, TS], F32) for i in range(2)]
        BTs = [sbt(C, es, f"BTs{i}", [128, TS], BF16) for i in range(2)]
        kTs = sbt(C, es, "kTs", [128, TS], BF16)

        def fm(ps_ap, hn, terms, n=TS):
            nt = len(terms) * KC
            i = 0
            for (off, M, shift) in terms:
                for kc in range(KC):
                    tk.pe(lambda: nc.tensor.matmul(ps_ap, W[:, kc, off:off + M], hn[:, kc, 1 - shift:1 - shift + n],
                                                   start=(i == 0), stop=(i == nt - 1)), r=[W, hn], w=[ps_ap.tensor_key])
                    i += 1

        for b in range(NB):
            tk.pool(lambda: nc.gpsimd.memset(halo[:], 0.0), w=[(halo, c_) for c_ in range(6)])
            for st in range(C.NST):
                t0 = st * TS
                hn = hns[st % 2]
                hprev = hns[(st + 1) % 2]
                tk.dma("sp", hTt[:], dr["hT"][b, :, :, t0:t0 + TS].rearrange("k p t -> p k t"), w=[hTt])
                for nm, t in tabs.items():
                    tk.dma("sp", t[:], dr[nm][:, t0:t0 + TS], w=[t])
                ppn = pp.next()
                emit_norm(C, hTt, TS, hn[:, :, 1:TS + 1], hn, sq, ones_bf, ppn, rstd)
                if st == 0:
                    tk.pool(lambda: nc.gpsimd.memset(hn[:, :, 0:1], 0.0), w=[hn])
                else:
                    tk.pool(lambda: nc.gpsimd.tensor_copy(hn[:, :, 0:1], hprev[:, :, TS:TS + 1]), r=[hprev], w=[hn])
                tsl = slice(t0, t0 + TS)
                cos64, sin64, cos32, sin32 = (tabs[k] for k in ("c_cos64T", "c_sin64T", "c_cos32T", "c_sin32T"))

                def FM(terms, M=128):
                    p = pp.next()
                    ap = p[:M, :]
                    nt = len(terms) * KC
                    i = 0
                    for (off, shift) in terms:
                        for kc in range(KC):
                            tk.pe(lambda: nc.tensor.matmul(ap, W[:, kc, off:off + M], hn[:, kc, 1 - shift:1 - shift + TS],
                                                           start=(i == 0), stop=(i == nt - 1)), r=[W, hn], w=[p])
                            i += 1
                    return p

                def TM(p, c0, j, terms, N):
                    nt = len(terms) * KC
                    i = 0
                    for (off, shift) in terms:
                        for kc in range(KC):
                            a = 1 - shift + j * 128
                            tk.pe(lambda: nc.tensor.matmul(p[:, c0:c0 + N], hn[:, kc, a:a + 128], W[:, kc, off:off + N],
                                                           start=(i == 0), stop=(i == nt - 1)), r=[W, hn], w=[p])
                            i += 1

                def rope_fm(pa, pb, cos, sin, M, scale=None, dt_out=BF16):
                    a_, b_ = tA.next(), tB.next()
                    if scale is None:
                        tk.dve(lambda: nc.vector.tensor_tensor(a_[:M], pa[:M, :], cos[:M], op=ALU.mult), r=[pa, cos], w=[a_])
                        tk.dve(lambda: nc.vector.tensor_tensor(b_[:M], pb[:M, :], sin[:M], op=ALU.mult), r=[pb, sin], w=[b_])
                    else:
                        tk.dve(lambda: nc.vector.scalar_tensor_tensor(a_[:M], pa[:M, :], scale, cos[:M], op0=ALU.mult, op1=ALU.mult),
                               r=[pa, cos], w=[a_])
                        tk.dve(lambda: nc.vector.scalar_tensor_tensor(b_[:M], pb[:M, :], scale, sin[:M], op0=ALU.mult, op1=ALU.mult),
                               r=[pb, sin], w=[b_])
                    o_ = ob.next()
                    tk.pool(lambda: nc.gpsimd.tensor_tensor(o_[:M], a_[:M], b_[:M], op=ALU.add), r=[a_, b_], w=[o_])
                    return o_

                import os
                SK = os.environ.get('KSKIP', '').split(',')
                if only is not None:
                    SK = [x for x in ('ret', 'ssd', 'rw', 'dsa') if x != only]
                def sec_ret():
                    pa = FM([(OFF["rq"], 0)]); pb = FM([(OFF["rq_r"], 0)])
                    o_ = rope_fm(pa, pb, cos32, sin32, 128)
                    tk.dma("pool", dr["r_qT"][b, :, tsl], o_[:], r=[o_])
                    pa = FM([(OFF["rk"], 0)]); pb = FM([(OFF["rk_r"], 0)])
                    o_ = rope_fm(pa, pb, cos32, sin32, 128, scale=32.0 ** -0.5)
                    tk.dma("pool", dr["r_kT"][b, :, tsl], o_[:], r=[o_])
                    pt = ptr.next()
                    for j in range(4):
                        tk.pe(lambda: nc.tensor.transpose(pt[:, j * 128:(j + 1) * 128], o_[:, j * 128:(j + 1) * 128], identb[:]),
                              r=[o_, identb], w=[pt])
                    o2 = ob.next()
                    tk.act(lambda: nc.scalar.copy(o2[:], pt[:, 0:512]), r=[pt], w=[o2])
                    tk.dma("pool", dr["r_kTok"][b, tsl, :].rearrange("(j p) n -> p j n", p=128),
                           o2[:].rearrange("p (j n) -> p j n", j=4), r=[o2])
                    for j in range(4):
                        p = pp.next()
                        TM(p, 0, j, [(OFF["rv"], 0)], 512)
                        vb = sbj.next()
                        tk.act(lambda: nc.scalar.copy(vb[:], p[:, 0:256]), r=[p], w=[vb])
                        jsl = slice(t0 + j * 128, t0 + (j + 1) * 128)
                        tk.dma("pool", dr["r_v"][b, jsl, :], vb[:], r=[vb])
                        sg = sfj.next()
                        tk.act(lambda: nc.scalar.activation(sg[:], p[:, 256:512], AF.Silu), r=[p], w=[sg])
                        tk.dma("pool", dr["r_sg"][b, jsl, :], sg[:], r=[sg])

                if 'ret' not in SK:
                    sec_ret()
                def sec_ssd():
                    for j in range(4):
                        jsl = slice(t0 + j * 128, t0 + (j + 1) * 128)
                        p = pp.next()
                        TM(p, 0, j, [(OFF["sz"], 0)], 256)
                        TM(p, 256, j, [(OFF["sdt"], 0)], 4)
                        sz = sfj.next()
                        tk.act(lambda: nc.scalar.activation(sz[:], p[:, 0:256], AF.Silu), r=[p], w=[sz])
                        tk.dma("pool", dr["s_sz"][b, jsl, :], sz[:], r=[sz])
                        s1 = sm.next()
                        tk.dve(lambda: nc.vector.tensor_tensor(s1[:, 0:4], p[:, 256:260], dtb_bc[:], op=ALU.add), r=[p, dtb_bc], w=[s1])
                        tk.act(lambda: nc.scalar.activation(s1[:, 0:4], s1[:, 0:4], AF.Exp), r=[s1], w=[s1])
                        tk.act(lambda: nc.scalar.activation(dtall[:, j, :], s1[:, 0:4], AF.Ln, bias=1.0), r=[s1], w=[(dtall, j)])
                        s2 = sm.next()
                        tk.dve(lambda: nc.vector.tensor_tensor(s2[:, 0:4], dtall[:, j, :], a_bc[:], op=ALU.mult),
                               r=[(dtall, j), a_bc], w=[s2])
                        tk.dma("pool", dr["s_la"][b, jsl, :], s2[:, 0:4], r=[s2])
                    for c in range(6):
                        p = FM([(OFF["sx"] + c * 128, 0)])
                        cbuf = cb.next()
                        tk.pool(lambda: nc.gpsimd.tensor_copy(cbuf[:, 0:3], halo[:, c, :]), r=[(halo, c)], w=[(cbuf, 0)])
                        tk.act(lambda: nc.scalar.copy(cbuf[:, 3:TS + 3], p[:, :]), r=[p], w=[(cbuf, 1)])
                        tk.pool(lambda: nc.gpsimd.tensor_copy(halo[:, c, :], cbuf[:, TS:TS + 3]), r=[(cbuf, 1)], w=[(halo, c)])
                        acc = tA.next()
                        tk.dve(lambda: nc.vector.tensor_scalar(acc[:], cbuf[:, 3:TS + 3], cwT[:, c, 3:4], cbT[:, c:c + 1],
                                                               op0=ALU.mult, op1=ALU.add), r=[(cbuf, 1), cwT, cbT], w=[acc])
                        for jj in (2, 1, 0):
                            tk.dve(lambda: nc.vector.scalar_tensor_tensor(acc[:], cbuf[:, jj:jj + TS], cwT[:, c, jj:jj + 1], acc[:],
                                                                          op0=ALU.mult, op1=ALU.add),
                                   r=[(cbuf, 0), (cbuf, 1), acc, cwT], w=[acc])
                        if c < 2:
                            tk.act(lambda: nc.scalar.activation(xsT[c][:], acc[:], AF.Silu), r=[acc], w=[xsT[c]])
                        else:
                            o_ = BTs[c - 2] if c < 4 else ob.next()
                            tk.act(lambda: nc.scalar.activation(o_[:], acc[:], AF.Silu), r=[acc], w=[o_])
                            dst = dr["s_BT"] if c < 4 else dr["s_CT"]
                            r0 = (c - 2) % 2 * 128
                            tk.dma("pool", dst[b, r0:r0 + 128, tsl], o_[:], r=[o_])
                    for j in range(4):
                        jsl = slice(t0 + j * 128, t0 + (j + 1) * 128)
                        p = pp.next()
                        for c2 in range(2):
                            tk.pe(lambda: nc.tensor.transpose(p[:, c2 * 128:(c2 + 1) * 128], xsT[c2][:, j * 128:(j + 1) * 128], identf[:]),
                                  r=[xsT[c2], identf], w=[p])
                        xs = sfj.next()
                        tk.act(lambda: nc.scalar.copy(xs[:], p[:, 0:256]), r=[p], w=[xs])
                        tk.dma("pool", dr["s_xs"][b, jsl, :], xs[:], r=[xs])
                        xd = sbj.next()
                        tk.dve(lambda: nc.vector.tensor_tensor(xd[:].rearrange("p (h d) -> p h d", h=4),
                                                               xs[:].rearrange("p (h d) -> p h d", h=4),
                                                               dtall[:, j, :].unsqueeze(2).to_broadcast([128, 4, 64]), op=ALU.mult),
                               r=[xs, (dtall, j)], w=[xd])
                        tk.dma("pool", dr["s_xdt"][b, jsl, :], xd[:], r=[xd])
                        pt = ptr.next()
                        for g2 in range(2):
                            tk.pe(lambda: nc.tensor.transpose(pt[:, g2 * 128:(g2 + 1) * 128], BTs[g2][:, j * 128:(j + 1) * 128], identb[:]),
                                  r=[BTs[g2], identb], w=[pt])
                        bt = sbj.next()
                        tk.act(lambda: nc.scalar.copy(bt[:], pt[:, 0:256]), r=[pt], w=[bt])
                        tk.dma("pool", dr["s_BTok"][b, jsl, :], bt[:], r=[bt])

                if 'ssd' not in SK:
                    sec_ssd()
                def sec_rw():
                    p = FM([(OFF["wl_a"], 0), (OFF["wl_b"], 1)], M=64)
                    tk.act(lambda: nc.scalar.activation(twl[:], p[:64, :], AF.Tanh), r=[p], w=[twl])
                    p = FM([(OFF["al_a"], 0), (OFF["al_b"], 1)], M=64)
                    tk.act(lambda: nc.scalar.copy(alb[:], p[:64, :]), r=[p], w=[alb])
                    p = FM([(OFF["gl_a"], 0), (OFF["gl_b"], 1)])
                    tk.act(lambda: nc.scalar.activation(sgl[:], p[:, :], AF.Sigmoid), r=[p], w=[sgl])
                    for c2 in range(2):
                        p = FM([(OFF["wrkv_a"] + 512 + c2 * 128, 0), (OFF["wrkv_b"] + 512 + c2 * 128, 1)])
                        o_ = of.next()
                        tk.act(lambda: nc.scalar.copy(o_[:], p[:, :]), r=[p], w=[o_])
                        tk.dma("pool", dr["w_vT"][b, c2 * 128:(c2 + 1) * 128, tsl], o_[:], r=[o_])
                    for j in range(4):
                        jsl = slice(t0 + j * 128, t0 + (j + 1) * 128)
                        js = slice(j * 128, (j + 1) * 128)
                        p1 = pp.next()
                        TM(p1, 0, j, [(OFF["wrkv_a"], 0), (OFF["wrkv_b"], 1)], 512)
                        p2 = pp.next()
                        TM(p2, 0, j, [(OFF["wrkv_a"] + 512, 0), (OFF["wrkv_b"] + 512, 1)], 256)
                        tk.pe(lambda: nc.tensor.matmul(p2[:, 256:512], sgl[:, js], w2b[:, 512:768], start=True, stop=True),
                              r=[sgl, w2b], w=[p2])
                        p3 = pp.next()
                        tk.pe(lambda: nc.tensor.matmul(p3[:, 0:256], twl[:, js], w2b[:64, 0:256], start=True, stop=True),
                              r=[twl, w2b], w=[p3])
                        tk.pe(lambda: nc.tensor.matmul(p3[:, 256:512], alb[:, js], w2b[:64, 256:512], start=True, stop=True),
                              r=[alb, w2b], w=[p3])
                        wa = tA.next()
                        tk.dve(lambda: nc.vector.tensor_tensor(wa[:], p3[:], w0a0[:], op=ALU.add), r=[p3, w0a0], w=[wa])
                        tk.act(lambda: nc.scalar.activation(wa[:], wa[:], AF.Sigmoid), r=[wa], w=[wa])
                        a_ = wa[:, 256:512]
                        wf = sfj.next()
                        tk.act(lambda: nc.scalar.activation(wf[:], wa[:, 0:256], AF.Exp, scale=-math.exp(-0.5)), r=[wa], w=[wf])
                        whi = sbj.next()
                        tk.pool(lambda: nc.gpsimd.tensor_copy(whi[:], wf[:]), r=[wf], w=[whi])
                        wlo = sbj.next()
                        tk.dve(lambda: nc.vector.tensor_tensor(wlo[:], wf[:], whi[:], op=ALU.subtract), r=[wf, whi], w=[wlo])
                        tk.dma("pool", dr["w_whi"][b, jsl, :], whi[:], r=[whi])
                        tk.dma("pool", dr["w_wlo"][b, jsl, :], wlo[:], r=[wlo])
                        kkf = sfj.next()
                        tk.dve(lambda: nc.vector.tensor_tensor(kkf[:], p1[:, 256:512], kk_bc[:], op=ALU.mult), r=[p1, kk_bc], w=[kkf])
                        sqk = sfj.next()
                        tk.pool(lambda: nc.gpsimd.tensor_tensor(sqk[:], kkf[:], kkf[:], op=ALU.mult), r=[kkf], w=[sqk])
                        s1 = sm.next()
                        tk.dve(lambda: nc.vector.tensor_reduce(s1[:, 0:4], sqk[:].rearrange("p (h d) -> p h d", h=4), axis=AX.X, op=ALU.add),
                               r=[sqk], w=[s1])
                        tk.act(lambda: nc.scalar.activation(s1[:, 0:4], s1[:, 0:4], AF.Sqrt, bias=1e-12), r=[s1], w=[s1])
                        tk.dve(lambda: nc.vector.reciprocal(s1[:, 0:4], s1[:, 0:4]), r=[s1], w=[s1])
                        kkn = sfj.next()
                        tk.dve(lambda: nc.vector.tensor_tensor(kkn[:].rearrange("p (h d) -> p h d", h=4),
                                                               kkf[:].rearrange("p (h d) -> p h d", h=4),
                                                               s1[:, 0:4].unsqueeze(2).to_broadcast([128, 4, 64]), op=ALU.mult),
                               r=[kkf, s1], w=[kkn])
                        nkk = sbj.next()
                        tk.pool(lambda: nc.gpsimd.tensor_scalar(nkk[:], kkn[:], -1.0, None, op0=ALU.mult), r=[kkn], w=[nkk])
                        tk.dma("pool", dr["w_nkk"][b, jsl, :], nkk[:], r=[nkk])
                        bb = sbj.next()
                        tk.pool(lambda: nc.gpsimd.tensor_tensor(bb[:], kkn[:], a_, op=ALU.mult), r=[kkn, wa], w=[bb])
                        tk.dma("pool", dr["w_bb"][b, jsl, :], bb[:], r=[bb])
                        t1 = sfj.next()
                        tk.dve(lambda: nc.vector.scalar_tensor_tensor(t1[:], a_, -1.0, ka_bc[:], op0=ALU.add, op1=ALU.mult),
                               r=[wa, ka_bc], w=[t1])
                        kp = sfj.next()
                        tk.dve(lambda: nc.vector.scalar_tensor_tensor(kp[:], t1[:], 1.0, p1[:, 256:512], op0=ALU.add, op1=ALU.mult),
                               r=[t1, p1], w=[kp])
                        kpb = sbj.next()
                        tk.pool(lambda: nc.gpsimd.tensor_copy(kpb[:], kp[:]), r=[kp], w=[kpb])
                        tk.dma("pool", dr["w_kp"][b, jsl, :], kpb[:], r=[kpb])
                        rb = sbj.next()
                        tk.act(lambda: nc.scalar.copy(rb[:], p1[:, 0:256]), r=[p1], w=[rb])
                        tk.dma("pool", dr["w_r"][b, jsl, :], rb[:], r=[rb])
                        t2 = sfj.next()
                        tk.dve(lambda: nc.vector.tensor_tensor(t2[:], p1[:, 0:256], kp[:], op=ALU.mult), r=[p1, kp], w=[t2])
                        tk.pool(lambda: nc.gpsimd.tensor_tensor(t2[:], t2[:], rk_bc[:], op=ALU.mult), r=[t2, rk_bc], w=[t2])
                        s2 = sm.next()
                        tk.dve(lambda: nc.vector.tensor_reduce(s2[:, 0:4], t2[:].rearrange("p (h d) -> p h d", h=4), axis=AX.X, op=ALU.add),
                               r=[t2], w=[s2])
                        bo = sfj.next()
                        tk.dve(lambda: nc.vector.tensor_tensor(bo[:].rearrange("p (h d) -> p h d", h=4),
                                                               p2[:, 0:256].rearrange("p (h d) -> p h d", h=4),
                                                               s2[:, 0:4].unsqueeze(2).to_broadcast([128, 4, 64]), op=ALU.mult),
                               r=[p2, s2], w=[bo])
                        tk.dma("pool", dr["w_bonus"][b, jsl, :], bo[:], r=[bo])
                        go = sfj.next()
                        tk.act(lambda: nc.scalar.copy(go[:], p2[:, 256:512]), r=[p2], w=[go])
                        tk.dma("pool", dr["w_g"][b, jsl, :], go[:], r=[go])

                if 'rw' not in SK:
                    sec_rw()
                def sec_dsa():
                    for c2 in range(2):
                        pa = FM([(OFF["dq"] + c2 * 128, 0)]); pb = FM([(OFF["dq_r"] + c2 * 128, 0)])
                        o_ = rope_fm(pa, pb, cos64, sin64, 128)
                        tk.dma("pool", dr["d_qT"][b, c2 * 128:(c2 + 1) * 128, tsl], o_[:], r=[o_])
                    pa = FM([(OFF["dk"], 0)], M=64); pb = FM([(OFF["dk_r"], 0)], M=64)
                    o_ = rope_fm(pa, pb, cos64, sin64, 64)
                    tk.dma("pool", dr["d_kT"][b, :, tsl], o_[:64], r=[o_])
                    pa = FM([(OFF["diq"], 0)]); pb = FM([(OFF["diq_r"], 0)])
                    o_ = rope_fm(pa, pb, cos32, sin32, 128)
                    tk.dma("pool", dr["d_iqT"][b, :, tsl], o_[:], r=[o_])
                    pa = FM([(OFF["dik"], 0)], M=32); pb = FM([(OFF["dik_r"], 0)], M=32)
                    sqi = tA.next()
                    tk.act(lambda: nc.scalar.activation(sqi[:32], pa[:32, :], AF.Square), r=[pa], w=[sqi])
                    p3 = pp.next()
                    tk.pe(lambda: nc.tensor.matmul(p3[:32, :], ones_f[:], sqi[:32], start=True, stop=True), r=[sqi, ones_f], w=[p3])
                    rs = tB.next()
                    tk.act(lambda: nc.scalar.activation(rs[:32], p3[:32, :], AF.Sqrt, scale=1.0 / 32, bias=EPS), r=[p3], w=[rs])
                    tk.dve(lambda: nc.vector.reciprocal(rs[:32], rs[:32]), r=[rs], w=[rs])
                    ia = tA.next(); ib = tB.next()
                    tk.dve(lambda: nc.vector.scalar_tensor_tensor(ia[:32], pa[:32, :], nw[:, 0:1], rs[:32], op0=ALU.mult, op1=ALU.mult),
                           r=[pa, nw, rs], w=[ia])
                    tk.dve(lambda: nc.vector.scalar_tensor_tensor(ib[:32], pb[:32, :], nw[:, 1:2], rs[:32], op0=ALU.mult, op1=ALU.mult),
                           r=[pb, nw, rs], w=[ib])
                    tk.dve(lambda: nc.vector.tensor_tensor(ia[:32], ia[:32], cos32[:32], op=ALU.mult), r=[ia, cos32], w=[ia])
                    tk.dve(lambda: nc.vector.tensor_tensor(ib[:32], ib[:32], sin32[:32], op=ALU.mult), r=[ib, sin32], w=[ib])
                    o_ = ob.next()
                    tk.pool(lambda: nc.gpsimd.tensor_tensor(o_[:32], ia[:32], ib[:32], op=ALU.add), r=[ia, ib], w=[o_])
                    tk.dma("pool", dr["d_ikT"][b, :, tsl], o_[:32], r=[o_])
                    p = pp.next()
                    for j in range(4):
                        TM(p, j * 64, j, [(OFF["dv"], 0)], 64)
                        TM(p, 256 + j * 4, j, [(OFF["diw"], 0)], 4)
                    tk.act(lambda: nc.scalar.copy(vout[:, :, 0:64], p[:, 0:256].rearrange("p (j d) -> p j d", j=4)), r=[p], w=[vout])
                    tk.dma("pool", dr["d_v"][b, tsl, :].rearrange("(j p) n -> p j n", p=128), vout[:], r=[vout])
                    s1 = sm.next()
                    tk.act(lambda: nc.scalar.mul(s1[:, 0:16], p[:, 256:272], (4.0 ** -0.5) * (32.0 ** -0.5)), r=[p], w=[s1])
                    tk.dma("pool", dr["d_iw"][b, tsl, :].rearrange("(j p) n -> p j n", p=128),
                           s1[:, 0:16].rearrange("p (j n) -> p j n", j=4), r=[s1])
                if 'dsa' not in SK:
                    sec_dsa()


def phaseRec(C, l):
    nc, tk, dr, S, NB = C.nc, C.tk, C.dr, C.S, C.NB
    NCH = S // 128
    with ExitStack() as es:
        cs = load_consts(C, es, ["c_ident", "c_U", "c_retla"])
        identf, U, retla = cs["c_ident"], cs["c_U"], cs["c_retla"]
        identb = sbt(C, es, "identb", [128, 128], BF16)
        tk.dve(lambda: nc.vector.tensor_copy(identb[:], identf[:]), r=[identf], w=[identb])
        ones_f = sbt(C, es, "ones_f", [128, 128], F32)
        tk.pool(lambda: nc.gpsimd.memset(ones_f[:], 1.0), w=[ones_f])
        dsk_bc = bc_load(C, es, "dsk_bc", dr["ssm_d"][l], 4)
        nrm_bc = bc_load(C, es, "nrm_bc", dr["ssm_norm"][l], 256)
        psm = Rot(nc, es, "psm", [128, 512], F32, 1, psum=True)
        pBc = Rot(nc, es, "pBc", [128, 512], F32, 1, psum=True)
        psc = Rot(nc, es, "psc", [128, 512], F32, 2, psum=True)
        pY = Rot(nc, es, "pY", [128, 512], F32, 1, psum=True)
        pdS = Rot(nc, es, "pdS", [128, 512], F32, 1, psum=True)
        ptr = Rot(nc, es, "ptr", [128, 1024], BF16, 1, psum=True)
        decTs = Rot(nc, es, "decT", [128, 512], F32, 2)
        Es = Rot(nc, es, "E", [128, 512], F32, 2)
        args = Rot(nc, es, "arg", [128, 512], F32, 2)
        smalls = Rot(nc, es, "small", [128, 16], F32, 4)
        s2s = Rot(nc, es, "s2s", [128, 16], F32, 4)
        qTs = Rot(nc, es, "qT", [128, 512], BF16, 2)
        kTs = Rot(nc, es, "kT", [128, 512], BF16, 2)
        kToks = Rot(nc, es, "kTok", [128, 256], BF16, 2)
        vs = Rot(nc, es, "v", [128, 256], BF16, 2)
        las = Rot(nc, es, "la", [128, 4], F32, 2)
        f1s = Rot(nc, es, "f1", [128, 256], F32, 2)
        f2s = Rot(nc, es, "f2", [128, 256], F32, 2)
        PTs = Rot(nc, es, "PT", [128, 512], BF16, 2)
        qtils = Rot(nc, es, "qtil", [128, 512], BF16, 2)
        xts = Rot(nc, es, "xt", [128, 256], BF16, 2)
        tmps = Rot(nc, es, "tmp", [128, 256], F32, 4)
        obs = Rot(nc, es, "ob", [128, 256], BF16, 2)
        oTs = Rot(nc, es, "oTt", [128, 256], BF16, 2)
        S32 = sbt(C, es, "S32", [128, 256], F32)
        Sbf = sbt(C, es, "Sbf", [128, 256], BF16)

        def prep(la):
            pm = psm.next()
            tk.pe(lambda: nc.tensor.matmul(pm[:, 0:4], U[:], la[:, 0:4], start=True, stop=True), r=[U, la], w=[pm])
            tk.pe(lambda: nc.tensor.matmul(pm[:, 4:8], ones_f[:], la[:, 0:4], start=True, stop=True), r=[ones_f, la], w=[pm])
            sm = smalls.next()
            tk.act(lambda: nc.scalar.copy(sm[:, 8:12], pm[:, 0:4]), r=[pm], w=[sm])
            tk.dve(lambda: nc.vector.tensor_tensor(sm[:, 0:4], pm[:, 4:8], sm[:, 8:12], op=ALU.subtract), r=[pm, sm], w=[sm])
            tk.act(lambda: nc.scalar.activation(sm[:, 0:4], sm[:, 0:4], AF.Exp), r=[sm], w=[sm])
            tk.act(lambda: nc.scalar.activation(sm[:, 4:8], pm[:, 4:8], AF.Exp), r=[pm, sm], w=[sm])
            pb = pBc.next()
            for h in range(4):
                tk.pe(lambda: nc.tensor.matmul(pb[:, h * 128:(h + 1) * 128], la[:, h:h + 1].to_broadcast([128, 128]), U[:],
                                               start=True, stop=True), r=[la, U], w=[pb])
            arg = args.next()
            for h in range(4):
                tk.dve(lambda: nc.vector.tensor_scalar(arg[:, h * 128:(h + 1) * 128], pb[:, h * 128:(h + 1) * 128],
                                                       sm[:, 8 + h:9 + h], 0.0, op0=ALU.subtract, op1=ALU.min),
                       r=[pb, sm], w=[arg])
            decT = decTs.next()
            tk.act(lambda: nc.scalar.activation(decT[:], arg[:], AF.Exp), r=[arg], w=[decT])
            tk.dve(lambda: nc.vector.tensor_tensor(decT[:].rearrange("p (h t) -> p h t", h=4),
                                                   decT[:].rearrange("p (h t) -> p h t", h=4),
                                                   U[:].unsqueeze(1).to_broadcast([128, 4, 128]), op=ALU.mult),
                   r=[decT, U], w=[decT])
            E = Es.next()
            tk.act(lambda: nc.scalar.activation(E[:], pb[:], AF.Exp), r=[pb], w=[E])
            return decT, E, sm

        for mix in ("ret", "ssd"):
            N = 32 if mix == "ret" else 128
            if mix == "ret":
                dec_const = prep(retla)
            for b in range(NB):
                tk.pool(lambda: nc.gpsimd.memset(S32[:], 0.0), w=[S32])
                tk.pool(lambda: nc.gpsimd.memset(Sbf[:], 0.0), w=[Sbf])
                for c in range(NCH):
                    tsl = slice(c * 128, (c + 1) * 128)
                    qT, kT, kTok, v = qTs.next(), kTs.next(), kToks.next(), vs.next()
                    f1, f2 = f1s.next(), f2s.next()
                    if mix == "ret":
                        tk.dma("sp", qT[:32, :].rearrange("n (h t) -> n h t", h=4),
                               dr["r_qT"][b, :, tsl].rearrange("(h n) t -> n h t", h=4), w=[qT])
                        tk.dma("sp", kT[:32, :].rearrange("n (h t) -> n h t", h=4),
                               dr["r_kT"][b, :, tsl].rearrange("(h n) t -> n h t", h=4), w=[kT])
                        tk.dma("sp", kTok[:, 0:128], dr["r_kTok"][b, tsl, :], w=[kTok])
                        tk.dma("sp", v[:], dr["r_v"][b, tsl, :], w=[v])
                        tk.dma("sp", f1[:], dr["r_sg"][b, tsl, :], w=[f1])
                        decT, E, sm = dec_const
                    else:
                        tk.dma("sp", qT[:, 0:256].rearrange("n (g t) -> n g t", g=2),
                               dr["s_CT"][b, :, tsl].rearrange("(g n) t -> n g t", g=2), w=[qT])
                        tk.dma("sp", kT[:, 0:256].rearrange("n (g t) -> n g t", g=2),
                               dr["s_BT"][b, :, tsl].rearrange("(g n) t -> n g t", g=2), w=[kT])
                        tk.dma("sp", kTok[:], dr["s_BTok"][b, tsl, :], w=[kTok])
                        tk.dma("sp", v[:], dr["s_xdt"][b, tsl, :], w=[v])
                        tk.dma("sp", f1[:], dr["s_sz"][b, tsl, :], w=[f1])
                        tk.dma("sp", f2[:], dr["s_xs"][b, tsl, :], w=[f2])
                        la = las.next()
                        tk.dma("sp", la[:], dr["s_la"][b, tsl, :], w=[la])
                        decT, E, sm = prep(la)
                    sc = psc.next()
                    PT = PTs.next()
                    qtil = qtils.next()
                    if mix == "ret":
                        for h in range(4):
                            hs = slice(h * 128, (h + 1) * 128)
                            tk.pe(lambda: nc.tensor.matmul(sc[:, hs], kT[:32, hs], qT[:32, hs], start=True, stop=True),
                                  r=[kT, qT], w=[sc])
                        tk.dve(lambda: nc.vector.tensor_tensor(PT[:], sc[:], decT[:], op=ALU.mult), r=[sc, decT], w=[PT])
                        tk.dve(lambda: nc.vector.tensor_tensor(qtil[:32, :], qT[:32, :], E[:32, :], op=ALU.mult), r=[qT, E], w=[qtil])
                    else:
                        for g in range(2):
                            gs = slice(g * 128, (g + 1) * 128)
                            tk.pe(lambda: nc.tensor.matmul(sc[:, gs], kT[:, gs], qT[:, gs], start=True, stop=True),
                                  r=[kT, qT], w=[sc])
                        v4 = lambda ap: ap.rearrange("p (g e t) -> p g e t", g=2, e=2)
                        bcg = lambda ap: ap.rearrange("p (g t) -> p g t", g=2).unsqueeze(2).to_broadcast([128, 2, 2, 128])
                        tk.dve(lambda: nc.vector.tensor_tensor(v4(PT[:]), v4(decT[:]), bcg(sc[:, 0:256]), op=ALU.mult),
                               r=[sc, decT], w=[PT])
                        tk.dve(lambda: nc.vector.tensor_tensor(v4(qtil[:]), v4(E[:]), bcg(qT[:, 0:256]), op=ALU.mult),
                               r=[qT, E], w=[qtil])
                    py = pY.next()
                    for h in range(4):
                        hs = slice(h * 128, (h + 1) * 128)
                        ps_ = slice(h * 64, (h + 1) * 64)
                        tk.pe(lambda: nc.tensor.matmul(py[:, ps_], PT[:, hs], v[:, ps_], start=True, stop=False), r=[PT, v], w=[py])
                        tk.pe(lambda: nc.tensor.matmul(py[:, ps_], qtil[:N, hs], Sbf[:N, ps_], start=False, stop=True),
                              r=[qtil, Sbf], w=[py])
                    xt = xts.next()
                    tk.dve(lambda: nc.vector.tensor_tensor(xt[:].rearrange("p (h d) -> p h d", h=4),
                                                           v[:].rearrange("p (h d) -> p h d", h=4),
                                                           sm[:, 0:4].unsqueeze(2).to_broadcast([128, 4, 64]), op=ALU.mult),
                           r=[v, sm], w=[xt])
                    pd = pdS.next()
                    if mix == "ret":
                        for h in range(4):
                            ps_ = slice(h * 64, (h + 1) * 64)
                            tk.pe(lambda: nc.tensor.matmul(pd[:32, ps_], kTok[:, h * 32:(h + 1) * 32], xt[:, ps_], start=True, stop=True),
                                  r=[kTok, xt], w=[pd])
                    else:
                        for g in range(2):
                            gs = slice(g * 128, (g + 1) * 128)
                            tk.pe(lambda: nc.tensor.matmul(pd[:, gs], kTok[:, gs], xt[:, gs], start=True, stop=True),
                                  r=[kTok, xt], w=[pd])
                    for h in range(4):
                        ps_ = slice(h * 64, (h + 1) * 64)
                        tk.dve(lambda: nc.vector.scalar_tensor_tensor(S32[:N, ps_], S32[:N, ps_], sm[:N, 4 + h:5 + h], pd[:N, ps_],
                                                                      op0=ALU.mult, op1=ALU.add), r=[S32, sm, pd, Sbf], w=[S32])
                    tk.act(lambda: nc.scalar.copy(Sbf[:N, :], S32[:N, :]), r=[S32], w=[Sbf])
                    ob = obs.next()
                    s2 = s2s.next()
                    if mix == "ret":
                        t1 = tmps.next()
                        tk.act(lambda: nc.scalar.activation(t1[:], py[:, 0:256], AF.Square), r=[py], w=[t1])
                        tk.dve(lambda: nc.vector.tensor_reduce(s2[:, 0:4], t1[:].rearrange("p (h d) -> p h d", h=4), axis=AX.X, op=ALU.add),
                               r=[t1], w=[s2])
                        tk.act(lambda: nc.scalar.activation(s2[:, 0:4], s2[:, 0:4], AF.Sqrt, scale=1.0 / 64, bias=EPS), r=[s2], w=[s2])
                        tk.dve(lambda: nc.vector.reciprocal(s2[:, 0:4], s2[:, 0:4]), r=[s2], w=[s2])
                        t2 = tmps.next()
                        tk.dve(lambda: nc.vector.tensor_tensor(t2[:].rearrange("p (h d) -> p h d", h=4),
                                                               py[:, 0:256].rearrange("p (h d) -> p h d", h=4),
                                                               s2[:, 0:4].unsqueeze(2).to_broadcast([128, 4, 64]), op=ALU.mult),
                               r=[py, s2], w=[t2])
                        tk.pool(lambda: nc.gpsimd.tensor_tensor(ob[:], t2[:], f1[:], op=ALU.mult), r=[t2, f1], w=[ob])
                        ch0 = 0
                    else:
                        t1 = tmps.next()
                        tk.pool(lambda: nc.gpsimd.tensor_tensor(t1[:].rearrange("p (h d) -> p h d", h=4),
                                                                f2[:].rearrange("p (h d) -> p h d", h=4),
                                                                dsk_bc[:].unsqueeze(2).to_broadcast([128, 4, 64]), op=ALU.mult),
                                r=[f2, dsk_bc], w=[t1])
                        tk.dve(lambda: nc.vector.tensor_tensor(t1[:], t1[:], py[:, 0:256], op=ALU.add), r=[t1, py], w=[t1])
                        tk.pool(lambda: nc.gpsimd.tensor_tensor(t1[:], t1[:], f1[:], op=ALU.mult), r=[t1, f1], w=[t1])
                        t2 = tmps.next()
                        tk.act(lambda: nc.scalar.activation(t2[:], t1[:], AF.Square, accum_out=s2[:, 0:1]), r=[t1], w=[t2, s2])
                        tk.act(lambda: nc.scalar.activation(s2[:, 0:1], s2[:, 0:1], AF.Sqrt, scale=1.0 / 256, bias=EPS), r=[s2], w=[s2])
                        tk.dve(lambda: nc.vector.reciprocal(s2[:, 0:1], s2[:, 0:1]), r=[s2], w=[s2])
                        tk.dve(lambda: nc.vector.scalar_tensor_tensor(ob[:], t1[:], s2[:, 0:1], nrm_bc[:], op0=ALU.mult, op1=ALU.mult),
                               r=[t1, s2, nrm_bc], w=[ob])
                        ch0 = 4
                    pt = ptr.next()
                    for c2 in range(2):
                        tk.pe(lambda: nc.tensor.transpose(pt[:, c2 * 128:(c2 + 1) * 128], ob[:, c2 * 128:(c2 + 1) * 128], identb[:]),
                              r=[ob, identb], w=[pt])
                    oTt = oTs.next()
                    tk.act(lambda: nc.scalar.copy(oTt[:], pt[:, 0:256]), r=[pt], w=[oTt])
                    tk.dma("pool", dr["oT"][b, ch0:ch0 + 2, :, tsl].rearrange("k p t -> p k t"),
                           oTt[:].rearrange("p (k t) -> p k t", k=2), r=[oTt])


def phaseRW(C, l):
    nc, tk, dr, S, NB = C.nc, C.tk, C.dr, C.S, C.NB
    assert NB == 2
    NCH = S // 64
    with ExitStack() as es:
        cs = load_consts(C, es, ["c_ident", "c_E2"])
        identf, E2 = cs["c_ident"], cs["c_E2"]
        identb = sbt(C, es, "identb", [128, 128], BF16)
        tk.dve(lambda: nc.vector.tensor_copy(identb[:], identf[:]), r=[identf], w=[identb])
        lnw_bc = bc_load(C, es, "lnw_bc", dr["rwkv_ln_w"][l], 256)
        lnb_bc = bc_load(C, es, "lnb_bc", dr["rwkv_ln_b"][l], 256)
        pA = Rot(nc, es, "pA", [128, 512], F32, 2, psum=True)
        pB = Rot(nc, es, "pB", [128, 512], F32, 2, psum=True)
        pC = Rot(nc, es, "pC", [128, 512], F32, 2, psum=True)
        pT = Rot(nc, es, "pT", [128, 512], F32, 1, psum=True)
        pO = Rot(nc, es, "pO", [128, 1024], BF16, 1, psum=True)
        names = ("w_whi", "w_wlo", "w_nkk", "w_bb", "w_kp", "w_r")
        tl = {nm: Rot(nc, es, "c_" + nm, [128, 256], BF16, 2) for nm in names}
        vTs = Rot(nc, es, "vTc", [128, 256], F32, 2)
        ychs = Rot(nc, es, "ych", [128, 256], F32, 2)
        kvs = Rot(nc, es, "kv", [128, 256], F32, 3)
        tmpa = Rot(nc, es, "tmpa", [128, 256], F32, 4)
        sas = Rot(nc, es, "sa", [128, 4], F32, 3)
        Sb = [sbt(C, es, f"Sst{i}", [128, 256], F32) for i in range(2)]
        for S_ in Sb:
            tk.pool(lambda: nc.gpsimd.memset(S_[:], 0.0), w=[S_])
        rsbs = Rot(nc, es, "rsb", [128, 256], F32, 3)
        tmpp = Rot(nc, es, "tmpp", [128, 256], F32, 3)
        gstep = 0
        pending = None
        yjunk = sbt(C, es, "yjunk", [128, 256], F32)

        def flush_y(pend):
            tmp3_, t_, ych_ = pend
            tk.dve(lambda: nc.vector.tensor_reduce(h4(ych_[:])[:, :, t_], h4(tmp3_[:]), axis=AX.X, op=ALU.add), r=[tmp3_], w=[ych_])

        bons = Rot(nc, es, "bon", [64, 512], F32, 2)
        gs_ = Rot(nc, es, "gg", [64, 512], F32, 2)
        yts = Rot(nc, es, "yt", [64, 512], F32, 2)
        ycs = Rot(nc, es, "yc", [64, 512], F32, 2)
        sqs = Rot(nc, es, "sqy", [64, 512], F32, 2)
        st8 = Rot(nc, es, "st8", [64, 16], F32, 4)
        obs = Rot(nc, es, "obw", [64, 512], BF16, 2)
        oTs = Rot(nc, es, "oTw", [128, 256], BF16, 2)
        h4 = lambda ap: ap.rearrange("p (h k) -> p h k", h=4)
        for c in range(NCH):
            csl = slice(c * 64, (c + 1) * 64)
            cur = {}
            for nm in names:
                t = tl[nm].next()
                for b in range(2):
                    tk.dma("sp", t[b * 64:(b + 1) * 64, :], dr[nm][b, csl, :], w=[t])
                cur[nm] = t
            vT = vTs.next()
            for b in range(2):
                tk.dma("sp", h4(vT[b * 64:(b + 1) * 64, :]), dr["w_vT"][b, :, csl].rearrange("(h v) t -> v h t", h=4), w=[vT])
            bon, gg = bons.next(), gs_.next()
            for b in range(2):
                tk.dma("sp", bon[:, b * 256:(b + 1) * 256], dr["w_bonus"][b, csl, :], w=[bon])
                tk.dma("sp", gg[:, b * 256:(b + 1) * 256], dr["w_g"][b, csl, :], w=[gg])
            ych = ychs.next()
            for t in range(64):
                E2t = E2[:, t * 128:(t + 1) * 128]
                pa, pb, pc = pA.next(), pB.next(), pC.next()
                tk.pe(lambda: nc.tensor.matmul(pa[:, 0:256], E2t, cur["w_whi"][:], start=True, stop=False), r=[E2, cur["w_whi"]], w=[pa])
                tk.pe(lambda: nc.tensor.matmul(pa[:, 0:256], E2t, cur["w_wlo"][:], start=False, stop=True), r=[E2, cur["w_wlo"]], w=[pa])
                tk.pe(lambda: nc.tensor.matmul(pa[:, 256:512], E2t, cur["w_nkk"][:], start=True, stop=True), r=[E2, cur["w_nkk"]], w=[pa])
                tk.pe(lambda: nc.tensor.matmul(pb[:, 0:256], E2t, cur["w_bb"][:], start=True, stop=True), r=[E2, cur["w_bb"]], w=[pb])
                tk.pe(lambda: nc.tensor.matmul(pb[:, 256:512], E2t, cur["w_kp"][:], start=True, stop=True), r=[E2, cur["w_kp"]], w=[pb])
                tk.pe(lambda: nc.tensor.matmul(pc[:, 0:256], E2t, cur["w_r"][:], start=True, stop=True), r=[E2, cur["w_r"]], w=[pc])
                kv = kvs.next()
                for h in range(4):
                    hs = slice(h * 64, (h + 1) * 64)
                    tk.act(lambda: nc.scalar.activation(kv[:, hs], pb[:, 256 + h * 64:256 + (h + 1) * 64], AF.Copy,
                                                        scale=vT[:, h * 64 + t:h * 64 + t + 1]), r=[pb, vT], w=[kv])
                tmp = tmpa.next()
                sa = sas.next()
                So, Sn = Sb[gstep % 2], Sb[(gstep + 1) % 2]
                gstep += 1
                tk.dve(lambda: nc.vector.tensor_tensor(tmp[:], So[:], pa[:, 256:512], op=ALU.mult), r=[So, pa], w=[tmp])
                tk.dve(lambda: nc.vector.tensor_tensor(Sn[:], So[:], pa[:, 0:256], op=ALU.mult), r=[So, pa], w=[Sn])
                tk.dve(lambda: nc.vector.tensor_reduce(sa[:], h4(tmp[:]), axis=AX.X, op=ALU.add), r=[tmp], w=[sa])
                tk.dve(lambda: nc.vector.tensor_tensor(Sn[:], Sn[:], kv[:], op=ALU.add), r=[Sn, kv], w=[Sn])
                tmp2 = tmpa.next()
                tk.dve(lambda: nc.vector.tensor_tensor(h4(tmp2[:]), h4(pb[:, 0:256]), sa[:].unsqueeze(2).to_broadcast([128, 4, 64]),
                                                       op=ALU.mult), r=[pb, sa], w=[tmp2])
                if pending is not None:
                    flush_y(pending)
                    pending = None
                tk.dve(lambda: nc.vector.tensor_tensor(Sn[:], Sn[:], tmp2[:], op=ALU.add), r=[Sn, tmp2], w=[Sn])
                tmp3 = tmpp.next()
                tk.dve(lambda: nc.vector.tensor_tensor(tmp3[:], Sn[:], pc[:, 0:256], op=ALU.mult), r=[Sn, pc], w=[tmp3])
                pending = (tmp3, t, ych)
            flush_y(pending)
            pending = None
            pt = pT.next()
            for h in range(4):
                tk.pe(lambda: nc.tensor.transpose(pt[:64, h * 128:(h + 1) * 128], ych[:, h * 64:(h + 1) * 64], identf[:]),
                      r=[ych, identf], w=[pt])
            yt = yts.next()
            tk.act(lambda: nc.scalar.copy(yt[:].rearrange("p (b h v) -> p h b v", b=2, h=4),
                                          pt[:64, :].rearrange("p (h b v) -> p h b v", h=4, b=2)), r=[pt], w=[yt])
            g8 = lambda ap: ap.rearrange("p (g v) -> p g v", g=8)
            s1 = st8.next()
            tk.dve(lambda: nc.vector.tensor_reduce(s1[:, 0:8], g8(yt[:]), axis=AX.X, op=ALU.add), r=[yt], w=[s1])
            tk.dve(lambda: nc.vector.tensor_scalar(s1[:, 0:8], s1[:, 0:8], -1.0 / 64, None, op0=ALU.mult), r=[s1], w=[s1])
            yc = ycs.next()
            tk.dve(lambda: nc.vector.tensor_tensor(g8(yc[:]), g8(yt[:]), s1[:, 0:8].unsqueeze(2).to_broadcast([64, 8, 64]), op=ALU.add),
                   r=[yt, s1], w=[yc])
            sq = sqs.next()
            tk.pool(lambda: nc.gpsimd.tensor_tensor(sq[:], yc[:], yc[:], op=ALU.mult), r=[yc], w=[sq])
            tk.dve(lambda: nc.vector.tensor_reduce(s1[:, 8:16], g8(sq[:]), axis=AX.X, op=ALU.add), r=[sq], w=[s1])
            tk.act(lambda: nc.scalar.activation(s1[:, 8:16], s1[:, 8:16], AF.Sqrt, scale=1.0 / 64, bias=64e-5), r=[s1], w=[s1])
            tk.dve(lambda: nc.vector.reciprocal(s1[:, 8:16], s1[:, 8:16]), r=[s1], w=[s1])
            tk.dve(lambda: nc.vector.tensor_tensor(g8(yc[:]), g8(yc[:]), s1[:, 8:16].unsqueeze(2).to_broadcast([64, 8, 64]), op=ALU.mult),
                   r=[yc, s1], w=[yc])
            b2 = lambda ap: ap.rearrange("p (b f) -> p b f", b=2)
            bcb = lambda t_: t_[:64, :].unsqueeze(1).to_broadcast([64, 2, 256])
            tk.pool(lambda: nc.gpsimd.tensor_tensor(b2(yc[:]), b2(yc[:]), bcb(lnw_bc), op=ALU.mult), r=[yc, lnw_bc], w=[yc])
            tk.pool(lambda: nc.gpsimd.tensor_tensor(b2(yc[:]), b2(yc[:]), bcb(lnb_bc), op=ALU.add), r=[yc, lnb_bc], w=[yc])
            tk.dve(lambda: nc.vector.tensor_tensor(yc[:], yc[:], bon[:], op=ALU.add), r=[yc, bon], w=[yc])
            ob = obs.next()
            tk.pool(lambda: nc.gpsimd.tensor_tensor(ob[:], yc[:], gg[:], op=ALU.mult), r=[yc, gg], w=[ob])
            po = pO.next()
            for q in range(4):
                tk.pe(lambda: nc.tensor.transpose(po[:, q * 64:(q + 1) * 64], ob[:, q * 128:(q + 1) * 128], identb[:64, :64]),
                      r=[ob, identb], w=[po])
            oTt = oTs.next()
            tk.act(lambda: nc.scalar.copy(oTt[:], po[:, 0:256]), r=[po], w=[oTt])
            for b in range(2):
                tk.dma("pool", dr["oT"][b, 2:4, :, csl].rearrange("k p t -> p k t"),
                       oTt[:, b * 128:(b + 1) * 128].rearrange("p (k t) -> p k t", k=2), r=[oTt])


def phaseDSA(C, l):
    nc, tk, dr, S, NB = C.nc, C.tk, C.dr, C.S, C.NB
    NQ = S // 128
    TOPK = float(min(256, S // 4))
    NIT = 20
    with ExitStack() as es:
        cs = load_consts(C, es, ["c_ident", "c_negU"])
        identf, negU = cs["c_ident"], cs["c_negU"]
        identb = sbt(C, es, "identb", [128, 128], BF16)
        tk.dve(lambda: nc.vector.tensor_copy(identb[:], identf[:]), r=[identf], w=[identb])
        thr0 = sbt(C, es, "thr0", [128, 1], F32)
        tk.pool(lambda: nc.gpsimd.memset(thr0[:], NEG_THR), w=[thr0])
        kT = sbt(C, es, "dkT", [64, S], BF16)
        ikT = sbt(C, es, "dikT", [32, S], BF16)
        vaug = sbt(C, es, "vaug", [128, NQ, 65], BF16)
        pp = Rot(nc, es, "pp", [128, 512], F32, 4, psum=True)
        pmr = Rot(nc, es, "pm", [128, 1024], BF16, 2, psum=True)
        pout = Rot(nc, es, "pout", [128, 512], F32, 1, psum=True)
        ptr = Rot(nc, es, "ptr", [128, 1024], BF16, 1, psum=True)
        scs = Rot(nc, es, "sc", [128, S], F32, 2)
        junk = sbt(C, es, "junk", [128, S], BF16)
        masks = Rot(nc, es, "mask", [128, S], BF16, 2)
        rls = Rot(nc, es, "rl", [128, 512], F32, 3)
        es_ = Rot(nc, es, "eexp", [128, 512], BF16, 3)
        pTs = Rot(nc, es, "pT", [128, 512], BF16, 3)
        iqs = Rot(nc, es, "iq", [32, 512], BF16, 2)
        qs = Rot(nc, es, "q", [64, 512], BF16, 2)
        iws = Rot(nc, es, "iw", [128, 4], F32, 2)
        st = Rot(nc, es, "bst", [128, 8], F32, 2)
        obs = Rot(nc, es, "obd", [128, 256], BF16, 2)
        rcs = Rot(nc, es, "rc", [128, 4], F32, 2)
        oTs = Rot(nc, es, "oTd", [128, 256], BF16, 2)
        for b in range(NB):
            tk.dma("sp", kT[:], dr["d_kT"][b], w=[kT])
            tk.dma("sp", ikT[:], dr["d_ikT"][b], w=[ikT])
            tk.dma("sp", vaug[:], dr["d_v"][b].rearrange("(j p) n -> p j n", p=128), w=[vaug])
            for i in range(NQ):
                L = (i + 1) * 128
                tsl = slice(i * 128, (i + 1) * 128)
                iq, q, iw = iqs.next(), qs.next(), iws.next()
                tk.dma("sp", iq[:].rearrange("d (h t) -> d h t", h=4), dr["d_iqT"][b, :, tsl].rearrange("(h d) t -> d h t", h=4), w=[iq])
                tk.dma("sp", q[:].rearrange("d (h t) -> d h t", h=4), dr["d_qT"][b, :, tsl].rearrange("(h d) t -> d h t", h=4), w=[q])
                tk.dma("sp", iw[:], dr["d_iw"][b, tsl, :], w=[iw])
                sc = scs.next()
                for k0 in range(0, L, 512):
                    w_ = min(512, L - k0)
                    for h in range(4):
                        p = pp.next()
                        tk.pe(lambda: nc.tensor.matmul(p[:, :w_], iq[:, h * 128:(h + 1) * 128], ikT[:, k0:k0 + w_], start=True, stop=True),
                              r=[iq, ikT], w=[p])
                        rl = rls.next()
                        tk.act(lambda: nc.scalar.activation(rl[:, :w_], p[:, :w_], AF.Relu), r=[p], w=[rl])
                        if h == 0:
                            tk.dve(lambda: nc.vector.tensor_scalar(sc[:, k0:k0 + w_], rl[:, :w_], iw[:, 0:1], None, op0=ALU.mult),
                                   r=[rl, iw], w=[sc])
                        else:
                            tk.dve(lambda: nc.vector.scalar_tensor_tensor(sc[:, k0:k0 + w_], rl[:, :w_], iw[:, h:h + 1], sc[:, k0:k0 + w_],
                                                                          op0=ALU.mult, op1=ALU.add), r=[rl, iw, sc], w=[sc])
                b_ = st.next()
                if i >= 2:
                    tk.dve(lambda: nc.vector.tensor_reduce(b_[:, 5:6], sc[:, :L], axis=AX.X, op=ALU.max), r=[sc], w=[b_])
                    tk.dve(lambda: nc.vector.tensor_reduce(b_[:, 6:7], sc[:, :L], axis=AX.X, op=ALU.min), r=[sc], w=[b_])
                    tk.dve(lambda: nc.vector.tensor_scalar(b_[:, 0:1], b_[:, 6:7], -1.0, None, op0=ALU.add), r=[b_], w=[b_])
                    tk.dve(lambda: nc.vector.scalar_tensor_tensor(b_[:, 1:2], b_[:, 5:6], 1.0, b_[:, 0:1], op0=ALU.add, op1=ALU.subtract),
                           r=[b_], w=[b_])
                tk.dve(lambda: nc.vector.tensor_tensor(sc[:, i * 128:L], sc[:, i * 128:L], negU[:], op=ALU.add), r=[sc, negU], w=[sc])
                if i >= 2:
                    for it in range(NIT):
                        f = 0.5 ** (it + 1)
                        tk.dve(lambda: nc.vector.scalar_tensor_tensor(b_[:, 2:3], b_[:, 1:2], f, b_[:, 0:1], op0=ALU.mult, op1=ALU.add),
                               r=[b_], w=[b_])
                        tk.dve(lambda: nc.vector.tensor_scalar(junk[:, :L], sc[:, :L], b_[:, 2:3], None, op0=ALU.is_ge, op1=ALU.add,
                                                               accum_out=b_[:, 3:4]), r=[sc, b_], w=[junk, b_])
                        tk.dve(lambda: nc.vector.tensor_scalar(b_[:, 4:5], b_[:, 3:4], TOPK, f, op0=ALU.is_ge, op1=ALU.mult), r=[b_], w=[b_])
                        tk.dve(lambda: nc.vector.scalar_tensor_tensor(b_[:, 0:1], b_[:, 4:5], b_[:, 1:2], b_[:, 0:1], op0=ALU.mult, op1=ALU.add),
                               r=[b_], w=[b_])
                    thr = b_[:, 0:1]
                    thr_r = [b_]
                else:
                    thr = thr0[:, 0:1]
                    thr_r = [thr0]
                mask = masks.next()
                tk.dve(lambda: nc.vector.tensor_scalar(mask[:, :L], sc[:, :L], thr, None, op0=ALU.is_ge), r=[sc] + thr_r, w=[mask])
                po = pout.next()
                for g0 in range(0, i + 1, 8):
                    pm = pmr.next()
                    g1 = min(i + 1, g0 + 8)
                    for j in range(g0, g1):
                        tk.pe(lambda: nc.tensor.transpose(pm[:, (j - g0) * 128:(j - g0 + 1) * 128], mask[:, j * 128:(j + 1) * 128], identb[:]),
                              r=[mask, identb], w=[pm])
                    for j in range(g0, g1):
                        lg = pp.next()
                        tk.pe(lambda: nc.tensor.matmul(lg[:, 0:512], kT[:, j * 128:(j + 1) * 128], q[:, :], start=True, stop=True),
                              r=[kT, q], w=[lg])
                        e = es_.next()
                        tk.act(lambda: nc.scalar.activation(e[:], lg[:], AF.Exp, scale=64.0 ** -0.5), r=[lg], w=[e])
                        pT = pTs.next()
                        tk.dve(lambda: nc.vector.tensor_tensor(pT[:].rearrange("p (h t) -> p h t", h=4),
                                                               e[:].rearrange("p (h t) -> p h t", h=4),
                                                               pm[:, (j - g0) * 128:(j - g0 + 1) * 128].unsqueeze(1).to_broadcast([128, 4, 128]),
                                                               op=ALU.mult), r=[e, pm], w=[pT])
                        for h in range(4):
                            tk.pe(lambda: nc.tensor.matmul(po[:, h * 65:(h + 1) * 65], pT[:, h * 128:(h + 1) * 128], vaug[:, j, :],
                                                           start=(j == 0 and h == 0), stop=(j == i and h == 3)), r=[pT, vaug], w=[po])
                rc = rcs.next()
                po3 = po[:, 0:260].rearrange("p (h e) -> p h e", h=4)
                tk.dve(lambda: nc.vector.reciprocal(rc[:], po3[:, :, 64]), r=[po], w=[rc])
                ob = obs.next()
                tk.dve(lambda: nc.vector.tensor_tensor(ob[:].rearrange("p (h d) -> p h d", h=4), po3[:, :, 0:64],
                                                       rc[:].unsqueeze(2).to_broadcast([128, 4, 64]), op=ALU.mult), r=[po, rc], w=[ob])
                pt = ptr.next()
                for c2 in range(2):
                    tk.pe(lambda: nc.tensor.transpose(pt[:, c2 * 128:(c2 + 1) * 128], ob[:, c2 * 128:(c2 + 1) * 128], identb[:]),
                          r=[ob, identb], w=[pt])
                oTt = oTs.next()
                tk.act(lambda: nc.scalar.copy(oTt[:], pt[:, 0:256]), r=[pt], w=[oTt])
                tk.dma("pool", dr["oT"][b, 6:8, :, tsl].rearrange("k p t -> p k t"),
                       oTt[:].rearrange("p (k t) -> p k t", k=2), r=[oTt])


def phaseF1(C, l):
    nc, tk, dr, S, NB, TS = C.nc, C.tk, C.dr, C.S, C.NB, C.TS
    with ExitStack() as es:
        cs = load_consts(C, es, ["c_ident"])
        identf = cs["c_ident"]
        identb = sbt(C, es, "identb", [128, 128], BF16)
        tk.dve(lambda: nc.vector.tensor_copy(identb[:], identf[:]), r=[identf], w=[identb])
        ones_bf = sbt(C, es, "ones_bf", [128, 128], BF16)
        tk.pool(lambda: nc.gpsimd.memset(ones_bf[:], 1.0), w=[ones_bf])
        Ws = {nm: sbt(C, es, "W" + nm, [128, KC, 1024], BF16) for nm in ("w_out", "wq_x", "wk_x", "wv_x", "wo_x")}
        gq = sbt(C, es, "gq", [128, KC], F32)
        gm = sbt(C, es, "gm", [128, KC], F32)
        tk.dma("sp", gq[:], dr["norm_cross"][l].rearrange("(k p) -> p k", p=128), w=[gq], allow_slow_non_contiguous=True)
        tk.dma("sp", gm[:], dr["norm_mem"][l].rearrange("(k p) -> p k", p=128), w=[gm], allow_slow_non_contiguous=True)
        with ExitStack() as es2:
            stg = Rot(nc, es2, "stg", [128, KC, 512], F32, 2)
            for nm, g in (("w_out", None), ("wq_x", gq), ("wk_x", gm), ("wv_x", gm), ("wo_x", None)):
                load_w_sec(C, stg, Ws[nm], 0, dr[nm][l], 1024, g)
            tk.barrier()
        pp = Rot(nc, es, "pp", [128, 512], F32, 6, psum=True)
        ptr = Rot(nc, es, "ptr", [128, 1024], BF16, 2, psum=True)
        memnT = sbt(C, es, "memnT", [128, KC, 256], BF16)
        kTx = sbt(C, es, "kTx", [128, KC, 256], BF16)
        vx = sbt(C, es, "vx", [128, 2, 1024], BF16)
        mts = Rot(nc, es, "mt", [128, 1024], F32, 2)
        mbs = Rot(nc, es, "mb", [128, 1024], BF16, 2)
        sm = Rot(nc, es, "smf", [128, 2], F32, 4)
        hTt = sbt(C, es, "hTt", [128, KC, TS], F32)
        oTt = sbt(C, es, "oTt", [128, KC, TS], BF16)
        sq = sbt(C, es, "sq", [128, KC, TS], BF16)
        hn = sbt(C, es, "hn", [128, KC, TS], BF16)
        rstd = sbt(C, es, "rstd", [128, TS], F32)
        qTx = sbt(C, es, "qTx", [128, KC, TS], BF16)
        oxT = sbt(C, es, "oxT", [128, KC, TS], BF16)
        ees = Rot(nc, es, "ee", [128, TS], BF16, 4)
        rdens = Rot(nc, es, "rden", [128, TS], F32, 2)

        def proj_add(Wt, src):
            for dc in range(KC):
                p = pp.next()
                for fc in range(KC):
                    tk.pe(lambda: nc.tensor.matmul(p[:, :TS], Wt[:, fc, dc * 128:(dc + 1) * 128], src[:, fc, :],
                                                   start=(fc == 0), stop=(fc == KC - 1)), r=[Wt, src], w=[p])
                tk.dve(lambda: nc.vector.tensor_tensor(hTt[:, dc, :], hTt[:, dc, :], p[:, :TS], op=ALU.add), r=[hTt, p], w=[hTt])

        for b in range(NB):
            for mc in range(2):
                mt = mts.next()
                tk.dma("sp", mt[:], dr["mem"][b, mc * 128:(mc + 1) * 128, :], w=[mt])
                mb = mbs.next()
                s1 = sm.next()
                tk.act(lambda: nc.scalar.activation(mb[:], mt[:], AF.Square, accum_out=s1[:, 0:1]), r=[mt], w=[mb, s1])
                tk.act(lambda: nc.scalar.activation(s1[:, 0:1], s1[:, 0:1], AF.Sqrt, scale=1.0 / D, bias=EPS), r=[s1], w=[s1])
                tk.dve(lambda: nc.vector.reciprocal(s1[:, 0:1], s1[:, 0:1]), r=[s1], w=[s1])
                tk.dve(lambda: nc.vector.tensor_scalar(mb[:], mt[:], s1[:, 0:1], None, op0=ALU.mult), r=[mt, s1], w=[mb])
                pt = ptr.next()
                for kc in range(KC):
                    tk.pe(lambda: nc.tensor.transpose(pt[:, kc * 128:(kc + 1) * 128], mb[:, kc * 128:(kc + 1) * 128], identb[:]),
                          r=[mb, identb], w=[pt])
                tk.act(lambda: nc.scalar.copy(memnT[:, :, mc * 128:(mc + 1) * 128], pt[:].rearrange("p (k m) -> p k m", k=KC)),
                       r=[pt], w=[memnT])
            for dc in range(KC):
                p = pp.next()
                for kc in range(KC):
                    tk.pe(lambda: nc.tensor.matmul(p[:, :256], Ws["wk_x"][:, kc, dc * 128:(dc + 1) * 128], memnT[:, kc, :],
                                                   start=(kc == 0), stop=(kc == KC - 1)), r=[Ws["wk_x"], memnT], w=[p])
                tk.act(lambda: nc.scalar.copy(kTx[:, dc, :], p[:, :256]), r=[p], w=[kTx])
            for mc in range(2):
                for half in range(2):
                    p = pp.next()
                    for kc in range(KC):
                        tk.pe(lambda: nc.tensor.matmul(p[:, :512], memnT[:, kc, mc * 128:(mc + 1) * 128],
                                                       Ws["wv_x"][:, kc, half * 512:(half + 1) * 512],
                                                       start=(kc == 0), stop=(kc == KC - 1)), r=[Ws["wv_x"], memnT], w=[p])
                    tk.act(lambda: nc.scalar.copy(vx[:, mc, half * 512:(half + 1) * 512], p[:, :512]), r=[p], w=[vx])
            for st in range(C.NST):
                tsl = slice(st * TS, (st + 1) * TS)
                tk.dma("sp", hTt[:], dr["hT"][b, :, :, tsl].rearrange("k p t -> p k t"), w=[hTt])
                tk.dma("sp", oTt[:], dr["oT"][b, :, :, tsl].rearrange("k p t -> p k t"), w=[oTt])
                proj_add(Ws["w_out"], oTt)
                emit_norm(C, hTt, TS, hn[:], hn, sq, ones_bf, pp.next(), rstd)
                for dc in range(KC):
                    p = pp.next()
                    for kc in range(KC):
                        tk.pe(lambda: nc.tensor.matmul(p[:, :TS], Ws["wq_x"][:, kc, dc * 128:(dc + 1) * 128], hn[:, kc, :],
                                                       start=(kc == 0), stop=(kc == KC - 1)), r=[Ws["wq_x"], hn], w=[p])
                    tk.act(lambda: nc.scalar.copy(qTx[:, dc, :], p[:, :TS]), r=[p], w=[qTx])
                for h in range(4):
                    ee = []
                    for mc in range(2):
                        p = pp.next()
                        for d2 in range(2):
                            tk.pe(lambda: nc.tensor.matmul(p[:, :TS], kTx[:, 2 * h + d2, mc * 128:(mc + 1) * 128], qTx[:, 2 * h + d2, :],
                                                           start=(d2 == 0), stop=(d2 == 1)), r=[kTx, qTx], w=[p])
                        e = ees.next()
                        tk.act(lambda: nc.scalar.activation(e[:], p[:, :TS], AF.Exp, scale=256.0 ** -0.5), r=[p], w=[e])
                        ee.append(e)
                    p = pp.next()
                    for mc in range(2):
                        tk.pe(lambda: nc.tensor.matmul(p[:, :TS], ones_bf[:], ee[mc][:], start=(mc == 0), stop=(mc == 1)),
                              r=[ones_bf, ee[mc]], w=[p])
                    rden = rdens.next()
                    tk.dve(lambda: nc.vector.reciprocal(rden[:], p[:, :TS]), r=[p], w=[rden])
                    for dv2 in range(2):
                        p = pp.next()
                        for mc in range(2):
                            c0 = h * 256 + dv2 * 128
                            tk.pe(lambda: nc.tensor.matmul(p[:, :TS], vx[:, mc, c0:c0 + 128], ee[mc][:], start=(mc == 0), stop=(mc == 1)),
                                  r=[vx, ee[mc]], w=[p])
                        tk.dve(lambda: nc.vector.tensor_tensor(oxT[:, 2 * h + dv2, :], p[:, :TS], rden[:], op=ALU.mult),
                               r=[p, rden], w=[oxT])
                proj_add(Ws["wo_x"], oxT)
                tk.dma("pool", dr["hT"][b, :, :, tsl].rearrange("k p t -> p k t"), hTt[:], r=[hTt])


def phaseF2(C, l):
    nc, tk, dr, S, NB = C.nc, C.tk, C.dr, C.S, C.NB
    T2 = 256
    with ExitStack() as es:
        ones_bf = sbt(C, es, "ones_bf", [128, 128], BF16)
        tk.pool(lambda: nc.gpsimd.memset(ones_bf[:], 1.0), w=[ones_bf])
        Wup = sbt(C, es, "Wup", [128, KC, 2 * DFF], BF16)
        Wdn = sbt(C, es, "Wdn", [128, NFC, 1024], BF16)
        gf = sbt(C, es, "gf", [128, KC], F32)
        tk.dma("sp", gf[:], dr["norm_ffn"][l].rearrange("(k p) -> p k", p=128), w=[gf], allow_slow_non_contiguous=True)
        with ExitStack() as es2:
            stg = Rot(nc, es2, "stg", [128, KC, 512], F32, 2)
            load_w_sec(C, stg, Wup, 0, dr["w_up"][l], 2 * DFF, gf)
            for c in range(NFC):
                st = stg.next()
                tk.dma("sp", st[:, 0:2, :].rearrange("p a n -> p (a n)"), dr["w_down"][l, c * 128:(c + 1) * 128, :], w=[st])
                tk.act(lambda: nc.scalar.copy(Wdn[:, c, :], st[:, 0:2, :].rearrange("p a n -> p (a n)")), r=[st], w=[Wdn])
            tk.barrier()
        cwT = sbt(C, es, "fcw", [128, NFC, 3], F32)
        for j in range(3):
            tk.dma("sp", cwT[:, :, j], dr["ffn_conv_w"][l, j].rearrange("(c p) -> p c", p=128), w=[cwT], allow_slow_non_contiguous=True)
        cbT = sbt(C, es, "fcb", [128, NFC], F32)
        tk.dma("sp", cbT[:], dr["ffn_conv_b"][l].rearrange("(c p) -> p c", p=128), w=[cbT], allow_slow_non_contiguous=True)
        halo = sbt(C, es, "fhalo", [128, NFC, 2], F32)
        pp = Rot(nc, es, "pp", [128, 512], F32, 7, psum=True)
        hTs = Rot(nc, es, "hTf", [128, KC, T2], F32, 2)
        sq = sbt(C, es, "sq", [128, KC, T2], BF16)
        hn = sbt(C, es, "hn", [128, KC, T2], BF16)
        rstd = sbt(C, es, "rstd", [128, T2], F32)
        actT = sbt(C, es, "actT", [128, NFC, T2], BF16)
        gbs = Rot(nc, es, "gb", [128, T2 + 2], F32, 3)
        accs = Rot(nc, es, "acc", [128, T2], F32, 3)
        for b in range(NB):
            tk.pool(lambda: nc.gpsimd.memset(halo[:], 0.0), w=[(halo, c_) for c_ in range(NFC)])
            for ti in range(S // T2):
                tsl = slice(ti * T2, (ti + 1) * T2)
                hTt = hTs.next()
                tk.dma("sp", hTt[:], dr["hT"][b, :, :, tsl].rearrange("k p t -> p k t"), w=[hTt])
                emit_norm(C, hTt, T2, hn[:], hn, sq, ones_bf, pp.next(), rstd)
                for c in range(NFC):
                    p = pp.next()
                    for half in range(2):
                        for kc in range(KC):
                            c0 = half * DFF + c * 128
                            tk.pe(lambda: nc.tensor.matmul(p[:, half * T2:(half + 1) * T2], Wup[:, kc, c0:c0 + 128], hn[:, kc, :],
                                                           start=(kc == 0), stop=(kc == KC - 1)), r=[Wup, hn], w=[p])
                    gb = gbs.next()
                    tk.pool(lambda: nc.gpsimd.tensor_copy(gb[:, 0:2], halo[:, c, :]), r=[(halo, c)], w=[(gb, 0)])
                    tk.act(lambda: nc.scalar.copy(gb[:, 2:T2 + 2], p[:, 0:T2]), r=[p], w=[(gb, 1)])
                    tk.pool(lambda: nc.gpsimd.tensor_copy(halo[:, c, :], gb[:, T2:T2 + 2]), r=[(gb, 1)], w=[(halo, c)])
                    acc = accs.next()
                    tk.dve(lambda: nc.vector.tensor_scalar(acc[:], gb[:, 2:T2 + 2], cwT[:, c, 2:3], cbT[:, c:c + 1],
                                                           op0=ALU.mult, op1=ALU.add), r=[(gb, 1), cwT, cbT], w=[acc])
                    for jj in (1, 0):
                        tk.dve(lambda: nc.vector.scalar_tensor_tensor(acc[:], gb[:, jj:jj + T2], cwT[:, c, jj:jj + 1], acc[:],
                                                                      op0=ALU.mult, op1=ALU.add),
                               r=[(gb, 0), (gb, 1), acc, cwT], w=[acc])
                    tk.act(lambda: nc.scalar.activation(acc[:], acc[:], AF.Silu), r=[acc], w=[acc])
                    tk.dve(lambda: nc.vector.tensor_tensor(actT[:, c, :], acc[:], p[:, T2:2 * T2], op=ALU.mult), r=[acc, p], w=[(actT, c)])
                for dc in range(KC):
                    p = pp.next()
                    for c in range(NFC):
                        tk.pe(lambda: nc.tensor.matmul(p[:, :T2], Wdn[:, c, dc * 128:(dc + 1) * 128], actT[:, c, :],
                                                       start=(c == 0), stop=(c == NFC - 1)), r=[Wdn, (actT, c)], w=[p])
                    tk.dve(lambda: nc.vector.tensor_tensor(hTt[:, dc, :], hTt[:, dc, :], p[:, :T2], op=ALU.add), r=[hTt, p], w=[hTt])
                tk.dma("pool", dr["hT"][b, :, :, tsl].rearrange("k p t -> p k t"), hTt[:], r=[hTt])


def phaseFinal(C):
    nc, tk, dr, S, NB, TS = C.nc, C.tk, C.dr, C.S, C.NB, C.TS
    with ExitStack() as es:
        cs = load_consts(C, es, ["c_ident"])
        identf = cs["c_ident"]
        ones_bf = sbt(C, es, "ones_bf", [128, 128], BF16)
        tk.pool(lambda: nc.gpsimd.memset(ones_bf[:], 1.0), w=[ones_bf])
        gfin = sbt(C, es, "gfin", [128, KC], F32)
        tk.dma("sp", gfin[:], dr["norm_final"].rearrange("(k p) -> p k", p=128), w=[gfin], allow_slow_non_contiguous=True)
        pp = Rot(nc, es, "pp", [128, 512], F32, 6, psum=True)
        hTs = Rot(nc, es, "hTl", [128, KC, TS], F32, 2)
        sq = sbt(C, es, "sq", [128, KC, TS], BF16)
        rstd = sbt(C, es, "rstd", [128, TS], F32)
        yT = sbt(C, es, "yT", [128, KC, TS], F32)
        yos = Rot(nc, es, "yo", [128, D], F32, 2)
        for b in range(NB):
            for st in range(C.NST):
                tsl = slice(st * TS, (st + 1) * TS)
                hTt = hTs.next()
                tk.dma("sp", hTt[:], dr["hT"][b, :, :, tsl].rearrange("k p t -> p k t"), w=[hTt])
                emit_norm(C, hTt, TS, None, None, sq, ones_bf, pp.next(), rstd)
                for kc in range(KC):
                    tk.dve(lambda: nc.vector.scalar_tensor_tensor(yT[:, kc, :], hTt[:, kc, :], gfin[:, kc:kc + 1], rstd[:],
                                                                  op0=ALU.mult, op1=ALU.mult), r=[hTt, gfin, rstd], w=[(yT, kc)])
                for j in range(TS // 128):
                    yo = yos.next()
                    for half in range(2):
                        p = pp.next()
                        for q in range(4):
                            kc = half * 4 + q
                            tk.pe(lambda: nc.tensor.transpose(p[:, q * 128:(q + 1) * 128], yT[:, kc, j * 128:(j + 1) * 128], identf[:]),
                                  r=[(yT, kc), identf], w=[p])
                        if half == 0:
                            tk.act(lambda: nc.scalar.copy(yo[:, 0:512], p[:]), r=[p], w=[(yo, 0)])
                        else:
                            tk.dve(lambda: nc.vector.tensor_copy(yo[:, 512:1024], p[:]), r=[p], w=[(yo, 1)])
                    t0 = st * TS + j * 128
                    tk.dma("pool", dr["y"][b, t0:t0 + 128, :], yo[:], r=[(yo, 0), (yo, 1)])


def kernel(**inputs):
    S, NCORES = 4096, 8
    nc, C = build(S)
    consts = make_consts(S)
    x = np.ascontiguousarray(inputs["x"], dtype=np.float32)
    mem = np.ascontiguousarray(inputs["mem"], dtype=np.float32)
    in_maps = []
    for c in range(NCORES):
        m = {"x": x[2 * c:2 * c + 2], "mem": mem[2 * c:2 * c + 2]}
        for name, _ in PARAMS:
            m[name] = np.ascontiguousarray(inputs[name], dtype=np.float32)
        m.update(consts)
        in_maps.append(m)
    res = run_bass_kernel_spmd(nc, in_maps, core_ids=list(range(NCORES)))
    return np.concatenate([np.asarray(r["y"], dtype=np.float32) for r in res.results], axis=0)
```
